# Optimizing a Trainium2 kernel written in Bass

```python
import math
import jax, jax.numpy as jnp
from jax import lax
import numpy as np

D_MODEL = 1024
BATCH = 16
SEQ = 4096
DEPTH = 1

CHUNK = 64
Q_BLOCK = 128
SSD_INNER = D_MODEL // 2
SSD_HEAD_DIM = 64
SSD_HEADS = SSD_INNER // SSD_HEAD_DIM
SSD_GROUPS = 2
SSD_STATE = 128
SSD_CONV = 4
SSD_BC = SSD_GROUPS * SSD_STATE
XBC_DIM = SSD_INNER + 2 * SSD_BC
MLA_V = 64
MLA_HEADS = (D_MODEL - SSD_INNER) // MLA_V
MLA_NOPE = 64
MLA_ROPE = 32
Q_LORA = 384
KV_LORA = 256
ROPE_BASE = 10000.0
MIX_WIDTH = SSD_INNER + MLA_HEADS * MLA_V
OFF_Z = 0
OFF_XBC = OFF_Z + SSD_INNER
OFF_DT = OFF_XBC + XBC_DIM
OFF_QA = OFF_DT + SSD_HEADS
OFF_KVA = OFF_QA + Q_LORA
OFF_KR = OFF_KVA + KV_LORA
IN_COLS = OFF_KR + MLA_ROPE
N_EXPERTS = 32
TOP_K = 4
D_FF_EXPERT = 1024
SWIGLU_LIMIT = 7.0
SWIGLU_ALPHA = 1.702
MOE_BLOCK = 256
NORM_EPS = 1e-6
MAX_STREAM_OFFSET = 16384

kernel_name = 'hybrid_ssd_mla_moe_stream_block'


def rms_norm(x, gain):
    xf = x.astype(jnp.float32)
    y = xf * lax.rsqrt(jnp.mean(xf * xf, axis=-1, keepdims=True) + NORM_EPS)
    return (y * gain.astype(jnp.float32)).astype(x.dtype)


def causal_depthwise_conv(x, w, b):
    k = w.shape[0]
    y = lax.conv_general_dilated(x, w[:, None, :], window_strides=(1,), padding=[(k - 1, 0)],
                                 dimension_numbers=('NWC', 'WIO', 'NWC'),
                                 feature_group_count=x.shape[-1])
    return y + b


def apply_rope(x, cos, sin):
    half = x.shape[-1] // 2
    x1, x2 = x[..., :half], x[..., half:]
    return jnp.concatenate([x1 * cos - x2 * sin, x2 * cos + x1 * sin], axis=-1)


def ssd_mixer(z, xbc, dt_raw, conv_w, conv_b, dt_bias, a_log, d_skip, norm_gain):
    f32 = jnp.float32
    bsz, seq, _ = xbc.shape
    nc = seq // CHUNK
    G, R, P, N = SSD_GROUPS, SSD_HEADS // SSD_GROUPS, SSD_HEAD_DIM, SSD_STATE
    xbc = jax.nn.silu(causal_depthwise_conv(xbc, conv_w, conv_b)).astype(f32)
    xs = xbc[..., :SSD_INNER].reshape(bsz, nc, CHUNK, G, R, P)
    bm = xbc[..., SSD_INNER:SSD_INNER + SSD_BC].reshape(bsz, nc, CHUNK, G, N)
    cm = xbc[..., SSD_INNER + SSD_BC:].reshape(bsz, nc, CHUNK, G, N)
    dt = jax.nn.softplus(dt_raw.astype(f32) + dt_bias.astype(f32)).reshape(bsz, nc, CHUNK, G, R)
    a = -jnp.exp(a_log.astype(f32)).reshape(G, R)
    cs = jnp.cumsum(jnp.moveaxis(dt * a, 2, -1), axis=-1)
    xdt = xs * dt[..., None]
    causal = jnp.tril(jnp.ones((CHUNK, CHUNK), dtype=bool))
    seg = cs[..., :, None] - cs[..., None, :]
    decay_in = jnp.exp(jnp.where(causal, seg, -jnp.inf))
    cb = jnp.einsum('bclgn,bcsgn->bcgls', cm, bm)
    y_diag = jnp.einsum('bcgls,bcgrls,bcsgrp->bclgrp', cb, decay_in, xdt)
    decay_to_end = jnp.exp(cs[..., -1:] - cs)
    chunk_states = jnp.einsum('bclgn,bcgrl,bclgrp->bcgrpn', bm, decay_to_end, xdt)
    chunk_decay = jnp.exp(cs[..., -1])

    def carry_state(state, inp):
        new_s, dec = inp
        return dec[..., None, None] * state + new_s, state

    init = jnp.zeros((bsz, G, R, P, N), f32)
    _, prev_states = lax.scan(carry_state, init,
                              (jnp.moveaxis(chunk_states, 1, 0), jnp.moveaxis(chunk_decay, 1, 0)))
    prev_states = jnp.moveaxis(prev_states, 0, 1)
    y_off = jnp.einsum('bclgn,bcgrpn,bcgrl->bclgrp', cm, prev_states, jnp.exp(cs))
    y = y_diag + y_off + xs * d_skip.astype(f32).reshape(G, R, 1)
    y = y.reshape(bsz, seq, SSD_INNER) * jax.nn.silu(z.astype(f32))
    yg = y.reshape(bsz, seq, G, SSD_INNER // G)
    yg = yg * lax.rsqrt(jnp.mean(yg * yg, axis=-1, keepdims=True) + NORM_EPS)
    return (yg.reshape(bsz, seq, SSD_INNER) * norm_gain.astype(f32)).astype(z.dtype)


def mla_mixer(q_a, kv_a, k_rope_raw, cos, sin, q_a_norm, w_q_up, kv_a_norm, w_kv_up, out_norm):
    bsz, seq, _ = q_a.shape
    H = MLA_HEADS
    q = (rms_norm(q_a, q_a_norm) @ w_q_up).reshape(bsz, seq, H, MLA_NOPE + MLA_ROPE)
    q_nope = q[..., :MLA_NOPE]
    q_rope = apply_rope(q[..., MLA_NOPE:], cos[:, :, None, :], sin[:, :, None, :])
    kv = (rms_norm(kv_a, kv_a_norm) @ w_kv_up).reshape(bsz, seq, H, MLA_NOPE + MLA_V)
    k_nope, v = kv[..., :MLA_NOPE], kv[..., MLA_NOPE:]
    k_rope = apply_rope(k_rope_raw, cos, sin)
    scale = 1.0 / math.sqrt(MLA_NOPE + MLA_ROPE)
    outs = []
    for qb in range(seq // Q_BLOCK):
        q0 = qb * Q_BLOCK
        k_end = q0 + Q_BLOCK
        s = (jnp.einsum('bqhd,bkhd->bhqk', q_nope[:, q0:k_end], k_nope[:, :k_end])
             + jnp.einsum('bqhd,bkd->bhqk', q_rope[:, q0:k_end], k_rope[:, :k_end]))
        s = s.astype(jnp.float32) * scale
        q_chunk = (q0 + jnp.arange(Q_BLOCK)) // CHUNK
        k_chunk = jnp.arange(k_end) // CHUNK
        s = jnp.where(k_chunk[None, :] <= q_chunk[:, None], s, -jnp.inf)
        p = jax.nn.softmax(s, axis=-1).astype(v.dtype)
        outs.append(jnp.einsum('bhqk,bkhd->bqhd', p, v[:, :k_end]))
    o = jnp.concatenate(outs, axis=1).reshape(bsz, seq, H * MLA_V)
    return rms_norm(o, out_norm)


def moe_ffn(h, w_router, b_router, w_gate_up, b_gate_up, w_down, b_down):
    n_tok, d = h.shape
    logits = (h @ w_router).astype(jnp.float32) + b_router.astype(jnp.float32)
    top_vals, top_idx = lax.top_k(logits, TOP_K)
    gates = jax.nn.softmax(top_vals, axis=-1)
    n_assign = n_tok * TOP_K
    flat_e = top_idx.reshape(-1).astype(jnp.int32)
    flat_tok = jnp.repeat(jnp.arange(n_tok, dtype=jnp.int32), TOP_K)
    flat_g = gates.reshape(-1)
    order = jnp.argsort(flat_e)
    sorted_e = flat_e[order]
    counts = jnp.bincount(flat_e, length=N_EXPERTS).astype(jnp.int32)
    padded = ((counts + MOE_BLOCK - 1) // MOE_BLOCK) * MOE_BLOCK
    starts = jnp.cumsum(counts) - counts
    padded_end = jnp.cumsum(padded)
    padded_start = padded_end - padded
    rank = jnp.arange(n_assign, dtype=jnp.int32) - starts[sorted_e]
    dest = padded_start[sorted_e] + rank
    n_slots = ((n_assign + MOE_BLOCK - 1) // MOE_BLOCK + N_EXPERTS) * MOE_BLOCK
    n_blocks = n_slots // MOE_BLOCK
    slot_tok = jnp.zeros((n_slots,), jnp.int32).at[dest].set(flat_tok[order])
    slot_gate = jnp.zeros((n_slots,), jnp.float32).at[dest].set(flat_g[order])
    block_start = jnp.arange(n_blocks, dtype=jnp.int32) * MOE_BLOCK
    block_expert = jnp.minimum(jnp.searchsorted(padded_end, block_start, side='right'),
                               N_EXPERTS - 1).astype(jnp.int32)

    def expert_block(args):
        tok, g, e = args
        xb = h[tok]
        gu = xb @ w_gate_up[e] + b_gate_up[e]
        glu = jnp.minimum(gu[:, :D_FF_EXPERT], SWIGLU_LIMIT)
        lin = jnp.clip(gu[:, D_FF_EXPERT:], -SWIGLU_LIMIT, SWIGLU_LIMIT)
        act = glu * jax.nn.sigmoid(SWIGLU_ALPHA * glu) * (lin + 1.0)
        y = act @ w_down[e] + b_down[e]
        return y * g[:, None].astype(y.dtype)

    y = lax.map(expert_block, (slot_tok.reshape(n_blocks, MOE_BLOCK),
                               slot_gate.reshape(n_blocks, MOE_BLOCK), block_expert))
    return jax.ops.segment_sum(y.reshape(n_slots, d), slot_tok, num_segments=n_tok)


def setup_inputs(seed: int = 0) -> dict:
    key = jax.random.key(seed)
    ks = jax.random.split(key, 40)
    f32 = jnp.float32
    nrm = lambda k, shape, s: jax.random.normal(k, shape, f32) * s
    gain = lambda k, shape: 1.0 + 0.05 * jax.random.normal(k, shape, f32)
    L, D, E, F = DEPTH, D_MODEL, N_EXPERTS, D_FF_EXPERT
    x = jax.random.normal(ks[0], (BATCH, SEQ, D), f32)
    c = jax.random.normal(ks[1], (BATCH, D), f32)
    offset = jax.random.randint(ks[2], (BATCH, 1), 0, MAX_STREAM_OFFSET, dtype=jnp.int32)
    positions = offset + jnp.arange(SEQ, dtype=jnp.int32)[None, :]
    dt0 = jnp.exp(jax.random.uniform(ks[9], (L, SSD_HEADS), f32, math.log(1e-3), math.log(1e-1)))
    return {
        'x': x,
        'c': c,
        'positions': positions,
        'w_ada': nrm(ks[3], (L, D, 6 * D), 0.5 * D ** -0.5),
        'b_ada': nrm(ks[4], (L, 6 * D), 0.02),
        'pre_mix_norm': gain(ks[5], (L, D)),
        'w_in': nrm(ks[6], (L, D, IN_COLS), D ** -0.5),
        'conv_w': nrm(ks[7], (L, SSD_CONV, XBC_DIM), SSD_CONV ** -0.5),
        'conv_b': nrm(ks[8], (L, XBC_DIM), 0.02),
        'dt_bias': dt0 + jnp.log(-jnp.expm1(-dt0)),
        'a_log': jnp.log(jax.random.uniform(ks[10], (L, SSD_HEADS), f32, 1.0, 16.0)),
        'd_skip': gain(ks[11], (L, SSD_HEADS)),
        'ssd_norm': gain(ks[12], (L, SSD_INNER)),
        'q_a_norm': gain(ks[13], (L, Q_LORA)),
        'w_q_up': nrm(ks[14], (L, Q_LORA, MLA_HEADS * (MLA_NOPE + MLA_ROPE)), Q_LORA ** -0.5),
        'kv_a_norm': gain(ks[15], (L, KV_LORA)),
        'w_kv_up': nrm(ks[16], (L, KV_LORA, MLA_HEADS * (MLA_NOPE + MLA_V)), KV_LORA ** -0.5),
        'mla_norm': gain(ks[17], (L, MLA_HEADS * MLA_V)),
        'w_out': nrm(ks[18], (L, MIX_WIDTH, D), MIX_WIDTH ** -0.5),
        'post_mix_norm': gain(ks[19], (L, D)),
        'pre_ffn_norm': gain(ks[20], (L, D)),
        'w_router': nrm(ks[21], (L, D, E), D ** -0.5),
        'b_router': nrm(ks[22], (L, E), 0.01),
        'w_gate_up': nrm(ks[23], (L, E, D, 2 * F), D ** -0.5),
        'b_gate_up': nrm(ks[24], (L, E, 2 * F), 0.02),
        'w_down': nrm(ks[25], (L, E, F, D), F ** -0.5),
        'b_down': nrm(ks[26], (L, E, D), 0.02),
        'post_ffn_norm': gain(ks[27], (L, D)),
    }


def reference(x, c, positions, w_ada, b_ada, pre_mix_norm, w_in, conv_w, conv_b, dt_bias, a_log,
              d_skip, ssd_norm, q_a_norm, w_q_up, kv_a_norm, w_kv_up, mla_norm, w_out,
              post_mix_norm, pre_ffn_norm, w_router, b_router, w_gate_up, b_gate_up, w_down,
              b_down, post_ffn_norm):
    bsz, seq, d = x.shape
    inv_freq = ROPE_BASE ** (-(jnp.arange(MLA_ROPE // 2, dtype=jnp.float32) * 2.0 / MLA_ROPE))
    angles = positions.astype(jnp.float32)[..., None] * inv_freq
    cos = jnp.cos(angles).astype(x.dtype)
    sin = jnp.sin(angles).astype(x.dtype)
    c_act = jax.nn.silu(c)
    for l in range(DEPTH):
        mod = c_act @ w_ada[l] + b_ada[l]
        sh1, sc1, g1, sh2, sc2, g2 = jnp.split(mod[:, None, :], 6, axis=-1)
        h = rms_norm(x, pre_mix_norm[l]) * (1.0 + sc1) + sh1
        proj = h @ w_in[l]
        y_ssd = ssd_mixer(proj[..., OFF_Z:OFF_XBC], proj[..., OFF_XBC:OFF_DT], proj[..., OFF_DT:OFF_QA],
                          conv_w[l], conv_b[l], dt_bias[l], a_log[l], d_skip[l], ssd_norm[l])
        y_mla = mla_mixer(proj[..., OFF_QA:OFF_KVA], proj[..., OFF_KVA:OFF_KR], proj[..., OFF_KR:IN_COLS],
                          cos, sin, q_a_norm[l], w_q_up[l], kv_a_norm[l], w_kv_up[l], mla_norm[l])
        mix = jnp.concatenate([y_ssd, y_mla], axis=-1) @ w_out[l]
        x = x + g1 * rms_norm(mix, post_mix_norm[l])
        h = rms_norm(x, pre_ffn_norm[l]) * (1.0 + sc2) + sh2
        f = moe_ffn(h.reshape(bsz * seq, d), w_router[l], b_router[l], w_gate_up[l], b_gate_up[l],
                    w_down[l], b_down[l]).reshape(bsz, seq, d)
        x = x + g2 * rms_norm(f, post_ffn_norm[l])
    return x
```

```python
import math
from contextlib import ExitStack
import numpy as np
import concourse.bass as bass
import concourse.mybir as mybir
from concourse.bass_utils import run_bass_kernel_spmd

F32 = mybir.dt.float32
BF16 = mybir.dt.bfloat16
I32 = mybir.dt.int32
AF = mybir.ActivationFunctionType
ALU = mybir.AluOpType
AX = mybir.AxisListType

import os
_PH = float(os.environ.get("KPH", "9"))
DM = 1024
NE = 32
BLK = 512
EPS = 1e-6
MAGIC = 12582912.0
C1 = 6.28125
C2 = 2.0 * math.pi - 6.28125


class Res:
    __slots__ = ("w", "rs")

    def __init__(self):
        self.w = None
        self.rs = {}


class Eng:
    def __init__(self, h, sid, is_pe=False):
        self.h = h
        self.sid = sid
        self.n = 0
        self.known = {}
        self.is_pe = is_pe
        self.ring = []
        self.rpos = 0


class Kern:
    def __init__(self, nc, es):
        self.nc = nc
        self.sems = []
        self.semvals = []
        self.es = es
        mk = lambda nm: self._newsem(nm)
        self.pe = Eng(nc.tensor, mk("s_pe"), True)
        self.dve = Eng(nc.vector, mk("s_dve"))
        self.act = Eng(nc.scalar, mk("s_act"))
        self.pool = Eng(nc.gpsimd, mk("s_pool"))
        self.sp = Eng(nc.sync, mk("s_sp"))
        for e, n in ((self.sp, 44), (self.act, 12), (self.pool, 36)):
            e.ring = [mk("d%d_%d" % (e.sid, i)) for i in range(n)]
        self.out_evs = []

    def _newsem(self, nm):
        s = self.es.enter_context(self.nc.semaphore(nm))
        self.sems.append(s)
        self.semvals.append(0)
        return len(self.sems) - 1

    def _wait(self, eng, evs):
        best = {}
        for sid, val in evs:
            if val > best.get(sid, 0):
                best[sid] = val
        for sid, val in best.items():
            if eng.is_pe and sid == eng.sid:
                continue
            if eng.known.get(sid, 0) >= val:
                continue
            eng.h.wait_ge(self.sems[sid], val)
            eng.known[sid] = val

    @staticmethod
    def _deps(reads, writes):
        evs = []
        for r in reads:
            if r.w is not None:
                evs.append(r.w)
        for r in writes:
            if r.w is not None:
                evs.append(r.w)
            evs.extend(r.rs.items())
        return evs

    @staticmethod
    def _mark(ev, reads, writes):
        for r in reads:
            if ev[1] > r.rs.get(ev[0], 0):
                r.rs[ev[0]] = ev[1]
        for r in writes:
            r.w = ev
            r.rs = {}

    def I(self, eng, fn, reads=(), writes=()):
        evs = self._deps(reads, writes)
        own = eng.sid
        evs2 = []
        raw = set()
        for r in reads:
            if r.w is not None:
                raw.add(r.w)
        for ev in evs:
            if ev[0] == own and ev not in raw:
                continue
            evs2.append(ev)
        self._wait(eng, evs2)
        ins = fn()
        eng.n += 1
        ins.then_inc(self.sems[eng.sid], 1)
        self._mark((eng.sid, eng.n), reads, writes)

    def dma(self, q, fn, reads=(), writes=(), is_out=False):
        evs = self._deps(reads, writes)
        sid = q.ring[q.rpos % len(q.ring)]
        q.rpos += 1
        prev = self.semvals[sid]
        if prev > 0:
            evs.append((sid, prev))
        self._wait(q, evs)
        ins = fn()
        self.semvals[sid] = prev + 16
        ins.then_inc(self.sems[sid], 16)
        ev = (sid, prev + 16)
        self._mark(ev, reads, writes)
        if is_out:
            self.out_evs.append(ev)
        return ev

    def barrier(self):
        evs = []
        for e in (self.sp, self.act, self.pool):
            evs += [(sid, self.semvals[sid]) for sid in e.ring if self.semvals[sid] > 0]
        for e in (self.pe, self.dve, self.act, self.pool):
            if e.n > 0:
                evs.append((e.sid, e.n))
        for e in (self.pe, self.dve, self.act, self.pool, self.sp):
            self._wait(e, [ev for ev in evs if ev[0] != e.sid])

    def finish(self):
        evs = list(self.out_evs)
        for e in (self.sp, self.act, self.pool):
            evs += [(sid, self.semvals[sid]) for sid in e.ring if self.semvals[sid] > 0]
        for e in (self.pe, self.dve, self.act, self.pool):
            if e.n > 0:
                evs.append((e.sid, e.n))
        self._wait(self.sp, evs)


class _Cut(Exception):
    pass


def build(S, dbg=()):
    ctx = {}
    try:
        return _build_inner(S, dbg, ctx)
    except _Cut:
        ctx["K"].finish()
        return ctx["nc"], ctx["dbg_d"]


def _build_inner(S, dbg, ctx):
    NT = S // 128
    NG = S // 512
    T = 2 * S
    NTT = T // 128
    NB = (T * 4) // BLK + NE
    nc = bass.Bass("TRN2", target_bir_lowering=False)
    es = ExitStack()
    K = Kern(nc, es)
    pe, dve, act, pool, sp = K.pe, K.dve, K.act, K.pool, K.sp
    dbg_d = {}

    def DI(name, shape, dt=F32):
        return nc.dram_tensor(name, list(shape), dt, kind="ExternalInput").ap()

    def DS(name, shape, dt=F32):
        if name in dbg:
            d = nc.dram_tensor("dbg_" + name, list(shape), dt, kind="ExternalOutput").ap()
            dbg_d[name] = d
            return d
        return nc.dram_tensor(name, list(shape), dt, kind="Internal").ap()

    x_d = DI("x", [T, DM])
    cT_d = DI("cT", [128, 8, 2])
    pos_d = DI("pos", [2, 128, NT], I32)
    wada_d = DI("w_ada", [DM, 6 * DM]).rearrange("(k p) f -> p k f", p=128)
    badaT_d = DI("b_adaT", [128, 48])
    gpre1_d = DI("g_pre1", [128, 8])
    gpost1_d = DI("g_post1", [128, 8])
    gpre2_d = DI("g_pre2", [128, 8])
    gpost2_d = DI("g_post2", [128, 8])
    wtm_d = DI("w_tm", [DM, 552]).rearrange("(k p) f -> p k f", p=128)
    wfm_d = DI("w_fm", [DM, 1664]).rearrange("(k p) f -> p k f", p=128)
    convw_d = DI("convw", [128, 8, 4])
    convb_d = DI("convb", [128, 8])
    dtb_d = DI("dtb", [128, 8])
    alog_d = DI("alog", [128, 8])
    dsk_d = DI("dsk", [128, 8])
    ssdn_d = DI("ssdn", [128, 512])
    mlan_d = DI("mlan", [128, 512])
    qan_d = DI("qan", [128, 3])
    kvan_d = DI("kvan", [128, 2])
    wq_d = DI("wq", [384, 768]).rearrange("(k p) f -> p k f", p=128)
    wkv_d = DI("wkv", [256, 1024]).rearrange("(k p) f -> p k f", p=128)
    wout_d = DI("wout", [DM, DM]).rearrange("(k p) f -> p k f", p=128)
    wr_d = DI("wr", [DM, NE]).rearrange("(k p) f -> p k f", p=128)
    br_d = DI("br", [128, NE])
    wgu_d = DI("wgu", [NE * DM, 2 * DM])
    wd_d = DI("wd", [NE * DM, DM])
    bgu_d = DI("bgu", [NE * 128, 16])
    bd_d = DI("bd", [NE * 128, DM])
    identf_d = DI("identf", [128, 128])
    U1_d = DI("U1", [128, 128])
    U2_d = DI("U2", [128, 128])
    Lst_d = DI("Lst", [128, 128])
    m01_d = DI("m01", [128, 128])
    invf_d = DI("invf", [128, 16])
    iotab_d = DI("iotab", [128, NB])
    pk_d = DI("pk", [128, 8])
    rm_d = DI("rm", [128, 2])
    x1s_d = DS("x1s", [T, DM])
    h2s_d = DS("h2s", [T, DM], BF16)
    xg_d = DS("xg", [NB * BLK, DM], BF16)
    yb_d = DS("yb", [NB * BLK, DM])
    out_d = nc.dram_tensor("out", [T, DM], F32, kind="ExternalOutput").ap()
    ctx.update(K=K, es=es, nc=nc, dbg_d=dbg_d)

    def cut(level):
        if _PH < level:
            raise _Cut()

    def SB(name, shape, dt=F32, stack=None):
        t = (stack or es).enter_context(nc.sbuf_tensor("sb_" + name, list(shape), dt))
        return t, Res()

    def PS(name, shape, dt=F32):
        return es.enter_context(nc.psum_tensor("ps_" + name, list(shape), dt))

    P2 = PS("P2", [128, 1024])
    rP2a, rP2b = Res(), Res()
    PBs = [PS("PB%d" % i, [128, 512]) for i in range(5)]
    rPB = [Res() for _ in range(5)]
    PT = PS("PT", [128, 1024], BF16)
    rPT = Res()

    def mm(out, lhsT, rhs, start, stop, rd, wr):
        K.I(pe, lambda: nc.tensor.matmul(out, lhsT, rhs, start=start, stop=stop), reads=rd, writes=wr)

    def tr(out, in_, ident, rd, wr):
        K.I(pe, lambda: nc.tensor.transpose(out, in_, ident), reads=rd, writes=wr)

    def actf(out, in_, func, rd, wr, bias=None, scale=None, accum_out=None):
        kw = {}
        if bias is not None:
            kw["bias"] = bias
        if scale is not None:
            kw["scale"] = scale
        if accum_out is not None:
            kw["accum_out"] = accum_out
        K.I(act, lambda: nc.scalar.activation(out, in_, func, **kw), reads=rd, writes=wr)

    def ts(eng, out, in0, s1, s2, op0, op1, rd, wr):
        h = eng.h
        if op1 is None:
            K.I(eng, lambda: h.tensor_scalar(out, in0, s1, None, op0), reads=rd, writes=wr)
        else:
            K.I(eng, lambda: h.tensor_scalar(out, in0, s1, s2, op0, op1), reads=rd, writes=wr)

    def tt(eng, out, in0, in1, op, rd, wr):
        h = eng.h
        K.I(eng, lambda: h.tensor_tensor(out, in0, in1, op), reads=rd, writes=wr)

    def stt(eng, out, in0, scalar, in1, op0, op1, rd, wr):
        h = eng.h
        K.I(eng, lambda: h.scalar_tensor_tensor(out, in0, scalar, in1, op0, op1), reads=rd, writes=wr)

    def cp(eng, out, in_, rd, wr):
        h = eng.h
        if eng is act:
            K.I(eng, lambda: nc.scalar.activation(out, in_, AF.Copy), reads=rd, writes=wr)
        else:
            K.I(eng, lambda: h.tensor_copy(out, in_), reads=rd, writes=wr)

    def ld(q, out, in_, wr, rd=()):
        K.dma(q, lambda: q.h.dma_start(out=out, in_=in_), reads=rd, writes=wr)

    def dump(name, ap, res, shape, dt=F32):
        if name not in dbg:
            return
        d = nc.dram_tensor("dbg_" + name, list(shape), dt, kind="ExternalOutput").ap()
        dbg_d[name] = d
        K.dma(sp, lambda: nc.sync.dma_start(out=d, in_=ap), reads=[res], writes=[Res()], is_out=True)

    identf, r_identf = SB("identf", [128, 128])
    identb, r_identb = SB("identb", [128, 128], BF16)
    onesf, r_onesf = SB("onesf", [128, 128])
    onesb, r_onesb = SB("onesb", [128, 128], BF16)
    U1, r_U1 = SB("U1", [128, 128])
    U2, r_U2 = SB("U2", [128, 128])
    Lst, r_Lst = SB("Lst", [128, 128])
    m01, r_m01 = SB("m01", [128, 128])
    invf, r_invf = SB("invf", [128, 16])
    ld(sp, identf[:], identf_d, [r_identf])
    ld(sp, U1[:], U1_d, [r_U1])
    ld(sp, U2[:], U2_d, [r_U2])
    ld(sp, Lst[:], Lst_d, [r_Lst])
    ld(sp, m01[:], m01_d, [r_m01])
    ld(sp, invf[:], invf_d, [r_invf])
    rm, r_rm = SB("rm", [128, 2])
    Mc, r_Mc = SB("Mc", [128, 2, 128])
    ld(sp, rm[:], rm_d, [r_rm])
    cp(dve, identb[:], identf[:], [r_identf], [r_identb])
    K.I(dve, lambda: nc.vector.memset(onesf[:], 1.0), writes=[r_onesf])
    K.I(dve, lambda: nc.vector.memset(onesb[:], 1.0), writes=[r_onesb])
    for c in range(2):
        ts(dve, Mc[:, c, :], onesf[:], rm[:, c:c + 1], None, ALU.mult, None, [r_onesf, r_rm], [r_Mc])
    epsb, r_epsb = SB("epsb", [128, 1])
    oneb, r_oneb = SB("oneb", [128, 1])
    K.I(dve, lambda: nc.vector.memset(epsb[:], EPS), writes=[r_epsb])
    K.I(dve, lambda: nc.vector.memset(oneb[:], 1.0), writes=[r_oneb])

    small = {}
    for nm, d, shp in (("gpre1", gpre1_d, [128, 8]), ("gpost1", gpost1_d, [128, 8]), ("gpre2", gpre2_d, [128, 8]),
                       ("gpost2", gpost2_d, [128, 8]), ("convw", convw_d, [128, 8, 4]), ("convb", convb_d, [128, 8]),
                       ("dtb", dtb_d, [128, 8]), ("alog", alog_d, [128, 8]), ("dsk", dsk_d, [128, 8]),
                       ("ssdn", ssdn_d, [128, 512]), ("mlan", mlan_d, [128, 512]), ("qan", qan_d, [128, 3]),
                       ("kvan", kvan_d, [128, 2]), ("br", br_d, [128, NE]), ("badaT", badaT_d, [128, 48]),
                       ("cT", cT_d, [128, 8, 2]), ("wr", wr_d, [128, 8, NE])):
        t, r = SB("c_" + nm, shp)
        ld(sp, t[:], d, [r])
        small[nm] = (t, r)

    aneg, r_aneg = SB("aneg", [128, 8])
    actf(aneg[:], small["alog"][0][:], AF.Exp, [small["alog"][1]], [r_aneg])
    ts(dve, aneg[:], aneg[:], -1.0, None, ALU.mult, None, [r_aneg], [r_aneg])

    modT, r_modT = SB("modT", [128, 48, 2])
    cact, r_cact = SB("cact", [128, 8, 2])
    actf(cact[:], small["cT"][0][:], AF.Silu, [small["cT"][1]], [r_cact])
    with ExitStack() as st0:
        wst = [SB("wadast%d" % i, [128, 8, 512], F32, st0) for i in range(2)]
        for blk in range(12):
            w_t, w_r = wst[blk % 2]
            ld(sp, w_t[:], wada_d[:, :, blk * 512:(blk + 1) * 512], [w_r])
            for j in range(4):
                fc = blk * 4 + j
                pb, rpb = PBs[fc % 2], rPB[fc % 2]
                for k in range(8):
                    mm(pb[:, 0:2], w_t[:, k, j * 128:(j + 1) * 128], cact[:, k, :], k == 0, k == 7,
                       [w_r, r_cact], [rpb])
                ts(dve, modT[:, fc, :], pb[:, 0:2], small["badaT"][0][:, fc:fc + 1], None, ALU.add, None,
                   [rpb, small["badaT"][1]], [r_modT])
    K.barrier()
    a1T, r_a1T = SB("a1T", [128, 2, 8])
    sh1T, r_sh1T = SB("sh1T", [128, 2, 8])
    vtmp, r_vtmp = SB("vtmp", [128, 8])
    btmp = [SB("btmp%d" % i, [128, 128]) for i in range(2)]
    bcn = [0]

    def bcast_rows(vT_ap, vres, dst_ap, dres):
        for half in range(2):
            pb, rpb = PBs[2 + half], rPB[2 + half]
            for kk in range(4):
                k = half * 4 + kk
                bt, rbt = btmp[bcn[0] % 2]
                bcn[0] += 1
                ts(dve, bt[:], onesf[:], vT_ap[:, k:k + 1], None, ALU.mult, None, [r_onesf, vres], [rbt])
                mm(pb[:, kk * 128:(kk + 1) * 128], bt[:], identf[:], True, True, [rbt, r_identf], [rpb])
            cp(act, dst_ap[:, half * 512:(half + 1) * 512], pb[:], [rpb], [dres])

    for b in range(2):
        ts(dve, vtmp[:], modT[:, 8:16, b], 1.0, None, ALU.add, None, [r_modT], [r_vtmp])
        tt(dve, a1T[:, b, :], vtmp[:], small["gpre1"][0][:], ALU.mult, [r_vtmp, small["gpre1"][1]], [r_a1T])
        cp(dve, sh1T[:, b, :], modT[:, 0:8, b], [r_modT], [r_sh1T])

    lg_all, r_lg = SB("lg_all", [128, NTT, NE])
    v8_all, r_v8 = SB("v8_all", [128, NTT, 8])
    gate_all, r_gate = SB("gate_all", [128, NTT, 4])

    castn = [0]

    def cast_any(out, in_, rd, wr):
        e = (dve, pool, act)[castn[0] % 3]
        castn[0] += 1
        cp(e, out, in_, rd, wr)

    def rstd_from(ss_ap, ssres, out_ap, ores, n, eng=dve):
        actf(out_ap, ss_ap, AF.Sqrt, [ssres], [ores], bias=epsb[:, 0:1], scale=1.0 / n)
        K.I(eng, lambda: eng.h.reciprocal(out_ap, out_ap), reads=[ores], writes=[ores])

    class Bag:
        def __init__(self):
            self.d = {}

        def add(self, ev):
            if ev[1] > self.d.get(ev[0], 0):
                self.d[ev[0]] = ev[1]

        def evs(self):
            return list(self.d.items())

    kn_s = DS("kn_s", [2, 128, 4 * S], BF16)
    kr_s = DS("kr_s", [2, 128, S], BF16)
    v_s = DS("v_s", [2, 128, NT * 8 * 65], BF16)
    q_s = DS("q_s", [2 * NT, 128, 2048], BF16)
    mix_s = DS("mix_s", [T, DM], BF16)
    bagA = [Bag(), Bag()]
    bagT = [Bag(), Bag()]

    def st(q, out, in_, rd, bag):
        ev = K.dma(q, lambda: q.h.dma_start(out=out, in_=in_), reads=rd, writes=[])
        bag.add(ev)

    dump("modT", modT[:], r_modT, [128, 48, 2])
    cut(0.2)
    with ExitStack() as p1:
        wtm, r_wtm = SB("wtm", [128, 8, 552], BF16, p1)
        wfm, r_wfm = SB("wfm", [128, 8, 1664], BF16, p1)
        wq, r_wq = SB("wq", [128, 3, 768], BF16, p1)
        wkv, r_wkv = SB("wkv", [128, 2, 1024], BF16, p1)
        with ExitStack() as pst:
            stg = [SB("stg%d" % i, [128, 1664], F32, pst) for i in range(2)]
            sn = 0
            for (dst, rdst, src, nk, ncol) in ((wtm, r_wtm, wtm_d, 8, 552), (wfm, r_wfm, wfm_d, 8, 1664),
                                               (wq, r_wq, wq_d, 3, 768), (wkv, r_wkv, wkv_d, 2, 1024)):
                for k in range(nk):
                    s_t, s_r = stg[sn % 2]
                    sn += 1
                    ld(sp, s_t[:, 0:ncol], src[:, k, :], [s_r])
                    cast_any(dst[:, k, :], s_t[:, 0:ncol], [s_r], [rdst])
        K.barrier()
        cut(0.25)
        Sst, r_Sst = SB("Sst", [128, 8, 64], F32, p1)
        Sbf, r_Sbf = SB("Sbf", [128, 8, 64], BF16, p1)
        cosT, r_cos = SB("cosT", [128, NT, 16], F32, p1)
        sinT, r_sin = SB("sinT", [128, NT, 16], F32, p1)
        posi, r_posi = SB("posi", [128, NT], I32, p1)
        posf, r_posf = SB("posf", [128, NT], F32, p1)
        ang, r_ang = SB("ang", [128, NT, 16], F32, p1)
        ang2, r_ang2 = SB("ang2", [128, NT, 16], F32, p1)
        xt = [SB("xt%d" % i, [128, DM], F32, p1) for i in range(2)]
        junk, r_junk = SB("junk", [128, DM], BF16, p1)
        xn, r_xn = SB("xn", [128, DM], BF16, p1)
        st1 = [SB("st1_%d" % i, [128, 8], F32, p1) for i in range(4)]
        hT, r_hT = SB("hT", [128, 8, 512], BF16, p1)
        rw, r_rw = SB("raw", [128, 8, 515], F32, p1)
        halo, r_halo = SB("halo", [128, 8, 3], F32, p1)
        cacc, r_cacc = SB("cacc", [128, 512], F32, p1)
        xact, r_xact = SB("xact", [128, 8, 512], BF16, p1)
        qag, r_qag = SB("qag", [128, 3, 512], F32, p1)
        kvag, r_kvag = SB("kvag", [128, 2, 512], F32, p1)
        sq5, r_sq5 = SB("sq5", [128, 5, 512], BF16, p1)
        rsb, r_rsb = SB("rsb", [128, 2, 512], F32, p1)
        qaTn, r_qaTn = SB("qaTn", [128, 3, 512], BF16, p1)
        kvaTn, r_kvaTn = SB("kvaTn", [128, 2, 512], BF16, p1)
        kng, r_kng = SB("kng", [128, 4, 512], BF16, p1)
        zs4 = [SB("zs%d" % i, [128, 512], F32, p1) for i in range(4)]
        dtt4 = [SB("dtt%d" % i, [128, 8], F32, p1) for i in range(4)]
        dta4 = [SB("dta%d" % i, [128, 8], F32, p1) for i in range(4)]
        krp, r_krp = SB("krp", [128, 4, 32], BF16, p1)
        K.I(pool, lambda: nc.gpsimd.memset(krp[:], 0.0), writes=[r_krp])
        krT, r_krT = SB("krT", [128, 128], BF16, p1)
        rtmp = [SB("rtmp%d" % i, [128, 8, 16], F32, p1) for i in range(4)]
        qn_tm, r_qn = SB("qn_tm", [128, 512], BF16, p1)
        qr_pad, r_qrp = SB("qr_pad", [128, 4, 2, 64], BF16, p1)
        K.I(pool, lambda: nc.gpsimd.memset(qr_pad[:], 0.0), writes=[r_qrp])
        QT, r_QT = SB("QT", [128, 2048], BF16, p1)
        K.I(pool, lambda: nc.gpsimd.memset(QT[:], 0.0), writes=[r_QT])
        Vt, r_Vt = SB("Vt", [128, 8, 65], BF16, p1)
        K.I(pool, lambda: nc.gpsimd.memset(Vt[:], 1.0), writes=[r_Vt])
        xs_tm2 = [SB("xs_tm%d" % i, [128, 8, 64], BF16, p1) for i in range(2)]
        B_tm2 = [SB("B_tm%d" % i, [128, 256], BF16, p1) for i in range(2)]
        xdt, r_xdt = SB("xdt", [128, 8, 64], BF16, p1)
        xdtd2 = [SB("xdtd%d" % i, [128, 2, 8, 64], BF16, p1) for i in range(2)]
        ecsm2 = [SB("ecsm%d" % i, [128, 2, 8], F32, p1) for i in range(2)]
        dtem, r_dtem = SB("dtem", [128, 2, 8], F32, p1)
        lseg, r_lseg = SB("lseg", [128, 8, 128], F32, p1)
        dec, r_dec = SB("dec", [128, 8, 128], F32, p1)
        cbm, r_cbm = SB("cbm", [128, 2, 128], F32, p1)
        MT, r_MT = SB("MT", [128, 8, 128], BF16, p1)
        ecs, r_ecs = SB("ecs", [128, 8], F32, p1)
        dte, r_dte = SB("dte", [128, 8], F32, p1)
        cdB2 = [SB("cdB%d" % i, [128, 2, 8], F32, p1) for i in range(2)]
        yd2 = [SB("yd%d" % i, [128, 8, 64], F32, p1) for i in range(2)]
        yt, r_yt = SB("yt", [128, 8, 64], F32, p1)
        yt2, r_yt2 = SB("yt2", [128, 8, 64], F32, p1)
        mixs, r_mixs = SB("mixs", [128, 512], BF16, p1)

        pbn = [0]

        def next_pb():
            i = pbn[0] % 2
            pbn[0] += 1
            return PBs[i], rPB[i]

        for b in range(2):
            bag = bagA[b]
            ld(sp, posi[:], pos_d[b], [r_posi])
            cp(dve, posf[:], posi[:], [r_posi], [r_posf])
            tt(dve, ang[:], posf[:].unsqueeze(2).broadcast_to((128, NT, 16)),
               invf[:].unsqueeze(1).broadcast_to((128, NT, 16)), ALU.mult, [r_posf, r_invf], [r_ang])
            ts(dve, ang2[:], ang[:], 1.0 / (2.0 * math.pi), MAGIC, ALU.mult, ALU.add, [r_ang], [r_ang2])
            ts(dve, ang2[:], ang2[:], -MAGIC, None, ALU.add, None, [r_ang2], [r_ang2])
            stt(dve, ang[:], ang2[:], -C1, ang[:], ALU.mult, ALU.add, [r_ang2, r_ang], [r_ang])
            stt(dve, ang[:], ang2[:], -C2, ang[:], ALU.mult, ALU.add, [r_ang2, r_ang], [r_ang])
            ts(dve, ang[:], ang[:], 3.14159, -3.14159, ALU.min, ALU.max, [r_ang], [r_ang])
            actf(sinT[:], ang[:], AF.Sin, [r_ang], [r_sin])
            ts(dve, ang2[:], ang[:], -1.0, None, ALU.mult, None, [r_ang], [r_ang2])
            tt(dve, ang2[:], ang2[:], ang[:], ALU.max, [r_ang2, r_ang], [r_ang2])
            ts(dve, ang2[:], ang2[:], -1.0, math.pi / 2.0, ALU.mult, ALU.add, [r_ang2], [r_ang2])
            actf(cosT[:], ang2[:], AF.Sin, [r_ang2], [r_cos])
            cut(0.30)
            K.I(dve, lambda: nc.vector.memset(Sst[:], 0.0), writes=[r_Sst])
            K.I(pool, lambda: nc.gpsimd.memset(Sbf[:], 0.0), writes=[r_Sbf])
            K.I(pool, lambda: nc.gpsimd.memset(halo[:], 0.0), writes=[r_halo])

            for gi in range(NG):
                for i in range(4):
                    ti = gi * 4 + i
                    gt = b * NT + ti
                    x_t, x_r = xt[gt % 2]
                    s1, r_s1 = st1[i]
                    ld(sp, x_t[:], x_d[gt * 128:(gt + 1) * 128, :], [x_r])
                    actf(junk[:], x_t[:], AF.Square, [x_r], [r_junk, r_s1], accum_out=s1[:, 0:1])
                    rstd_from(s1[:, 0:1], r_s1, s1[:, 1:2], r_s1, DM)
                    ts(dve, xn[:], x_t[:], s1[:, 1:2], None, ALU.mult, None, [x_r, r_s1], [r_xn])
                    for k in range(8):
                        tr(PT[:, k * 128:(k + 1) * 128], xn[:, k * 128:(k + 1) * 128], identb[:], [r_xn, r_identb], [rPT])
                    for k in range(8):
                        actf(hT[:, k, i * 128:(i + 1) * 128], PT[:, k * 128:(k + 1) * 128], AF.Identity,
                             [rPT, r_a1T, r_sh1T], [r_hT], bias=sh1T[:, b, k:k + 1], scale=a1T[:, b, k:k + 1])
                cut(0.32)
                cp(pool, rw[:, :, 0:3], halo[:], [r_halo], [r_rw])
                for mc in range(13):
                    pb, rpb = next_pb()
                    for k in range(8):
                        mm(pb[:], wfm[:, k, mc * 128:(mc + 1) * 128], hT[:, k, :], k == 0, k == 7, [r_wfm, r_hT], [rpb])
                    if mc < 8:
                        cp(act, rw[:, mc, 3:515], pb[:], [rpb], [r_rw])
                    elif mc < 11:
                        c = mc - 8
                        actf(qag[:, c, :], pb[:], AF.Copy, [rpb, small["qan"][1]], [r_qag], scale=small["qan"][0][:, c:c + 1])
                        actf(sq5[:, c, :], pb[:], AF.Square, [rpb], [r_sq5])
                    else:
                        c = mc - 11
                        actf(kvag[:, c, :], pb[:], AF.Copy, [rpb, small["kvan"][1]], [r_kvag], scale=small["kvan"][0][:, c:c + 1])
                        actf(sq5[:, 3 + c, :], pb[:], AF.Square, [rpb], [r_sq5])
                cp(pool, halo[:], rw[:, :, 512:515], [r_rw], [r_halo])
                cut(0.34)
                cw, r_cw = small["convw"]
                cb_, r_cb = small["convb"]
                for mc in range(8):
                    ts(dve, cacc[:], rw[:, mc, 0:512], cw[:, mc, 0:1], cb_[:, mc:mc + 1], ALU.mult, ALU.add,
                       [r_rw, r_cw, r_cb], [r_cacc])
                    for kk in range(1, 4):
                        stt(dve, cacc[:], rw[:, mc, kk:kk + 512], cw[:, mc, kk:kk + 1], cacc[:], ALU.mult, ALU.add,
                            [r_rw, r_cw, r_cacc], [r_cacc])
                    actf(xact[:, mc, :], cacc[:], AF.Silu, [r_cacc], [r_xact])
                cut(0.36)
                for (c0, ncn, n, slot) in ((0, 3, 384, 0), (3, 2, 256, 1)):
                    pb, rpb = PBs[2], rPB[2]
                    for c in range(ncn):
                        mm(pb[:], onesb[:], sq5[:, c0 + c, :], c == 0, c == ncn - 1, [r_onesb, r_sq5], [rpb])
                    rstd_from(pb[:], rpb, rsb[:, slot, :], r_rsb, n)
                for c in range(3):
                    tt(dve, qaTn[:, c, :], qag[:, c, :], rsb[:, 0, :], ALU.mult, [r_qag, r_rsb], [r_qaTn])
                for c in range(2):
                    tt(pool, kvaTn[:, c, :], kvag[:, c, :], rsb[:, 1, :], ALU.mult, [r_kvag, r_rsb], [r_kvaTn])
                cut(0.38)
                for j in range(4):
                    pb, rpb = next_pb()
                    for c in range(2):
                        mm(pb[:], wkv[:, c, j * 128:(j + 1) * 128], kvaTn[:, c, :], c == 0, c == 1, [r_wkv, r_kvaTn], [rpb])
                    cp(act, kng[:, j, :], pb[:], [rpb], [r_kng])
                st(sp, kn_s[b].rearrange("p (j s) -> p j s", j=4)[:, :, gi * 512:(gi + 1) * 512], kng[:], [r_kng], bag)
                cut(0.40)

                def stX(i):
                    ti = gi * 4 + i
                    gt = b * NT + ti
                    cs_ = slice(i * 128, (i + 1) * 128)
                    s1, r_s1 = st1[i]
                    zsQ, r_zsQ = zs4[i]
                    dttQ, r_dttQ = dtt4[i]
                    dtaQ, r_dtaQ = dta4[i]
                    xs_tmW, r_xsW = xs_tm2[i % 2]
                    B_tmW, r_BtmW = B_tm2[i % 2]
                    xdtdW, r_xdtdW = xdtd2[i % 2]
                    ecsmW, r_ecsmW = ecsm2[i % 2]
                    cdBW, r_cdBW = cdB2[i % 2]
                    ydW, r_ydW = yd2[i % 2]
                    for k in range(8):
                        mm(P2[:, 0:512], hT[:, k, cs_], wtm[:, k, 0:512], k == 0, k == 7, [r_hT, r_wtm], [rP2a])
                    for k in range(8):
                        mm(P2[:, 512:552], hT[:, k, cs_], wtm[:, k, 512:552], k == 0, k == 7, [r_hT, r_wtm], [rP2b])
                    actf(zsQ[:], P2[:, 0:512], AF.Silu, [rP2a], [r_zsQ])
                    tt(dve, dttQ[:], P2[:, 512:520], small["dtb"][0][:], ALU.add, [rP2b, small["dtb"][1]], [r_dttQ])
                    actf(dttQ[:], dttQ[:], AF.Exp, [r_dttQ], [r_dttQ])
                    actf(dttQ[:], dttQ[:], AF.Ln, [r_dttQ], [r_dttQ], bias=oneb[:, 0:1])
                    tt(dve, dtaQ[:], dttQ[:], aneg[:], ALU.mult, [r_dttQ, r_aneg], [r_dtaQ])
                    cs16 = cosT[:, ti, :]
                    sn16 = sinT[:, ti, :]
                    k1 = P2[:, 520:536]
                    k2 = P2[:, 536:552]
                    t0_, t1_, t2_, t3_ = [rtmp[q][0][:, 0, :] for q in range(4)]
                    rr = [rtmp[q][1] for q in range(4)]
                    tt(dve, t0_, k1, cs16, ALU.mult, [rP2b, r_cos], [rr[0]])
                    tt(dve, t1_, k2, sn16, ALU.mult, [rP2b, r_sin], [rr[1]])
                    tt(dve, t2_, k2, cs16, ALU.mult, [rP2b, r_cos], [rr[2]])
                    tt(dve, t3_, k1, sn16, ALU.mult, [rP2b, r_sin], [rr[3]])
                    tt(dve, krp[:, 0, 0:16], t0_, t1_, ALU.subtract, [rr[0], rr[1]], [r_krp])
                    tt(dve, krp[:, 0, 16:32], t2_, t3_, ALU.add, [rr[2], rr[3]], [r_krp])
                    cp(pool, krp[:, 2, :], krp[:, 0, :], [r_krp], [r_krp])
                    tr(PT[:, 0:128], krp[:].rearrange("p a c -> p (a c)"), identb[:], [r_krp, r_identb], [rPT])
                    cp(act, krT[:], PT[:, 0:128], [rPT], [r_krT])
                    st(sp, kr_s[b][:, ti * 128:(ti + 1) * 128], krT[:], [r_krT], bag)
                    for c in range(3):
                        mm(P2[:, 0:512], qaTn[:, c, cs_], wq[:, c, 0:512], c == 0, c == 2, [r_qaTn, r_wq], [rP2a])
                    for c in range(3):
                        mm(P2[:, 512:768], qaTn[:, c, cs_], wq[:, c, 512:768], c == 0, c == 2, [r_qaTn, r_wq], [rP2b])
                    cp(act, qn_tm[:], P2[:, 0:512], [rP2a], [r_qn])
                    qr = P2[:, 512:768].rearrange("p (h c) -> p h c", c=32)
                    q1 = qr[:, :, 0:16]
                    q2 = qr[:, :, 16:32]
                    cb8 = cs16.unsqueeze(1).broadcast_to((128, 8, 16))
                    sb8 = sn16.unsqueeze(1).broadcast_to((128, 8, 16))
                    T0, T1, T2, T3 = [rtmp[q][0][:] for q in range(4)]
                    tt(dve, T0, q1, cb8, ALU.mult, [rP2b, r_cos], [rr[0]])
                    tt(dve, T1, q2, sb8, ALU.mult, [rP2b, r_sin], [rr[1]])
                    tt(dve, T2, q2, cb8, ALU.mult, [rP2b, r_cos], [rr[2]])
                    tt(dve, T3, q1, sb8, ALU.mult, [rP2b, r_sin], [rr[3]])
                    qrv = qr_pad[:].rearrange("p j s c -> p s j c")
                    for s_ in range(2):
                        tt(dve, qrv[:, s_, :, 0:16], rtmp[0][0][:, s_ * 4:(s_ + 1) * 4, :], rtmp[1][0][:, s_ * 4:(s_ + 1) * 4, :],
                           ALU.subtract, [rr[0], rr[1]], [r_qrp])
                        tt(dve, qrv[:, s_, :, 16:32], rtmp[2][0][:, s_ * 4:(s_ + 1) * 4, :], rtmp[3][0][:, s_ * 4:(s_ + 1) * 4, :],
                           ALU.add, [rr[2], rr[3]], [r_qrp])
                    for j in range(4):
                        tr(PT[:, j * 128:(j + 1) * 128], qn_tm[:, j * 128:(j + 1) * 128], identb[:], [r_qn, r_identb], [rPT])
                    for j in range(4):
                        tr(PT[:, 512 + j * 128:512 + (j + 1) * 128], qr_pad[:, j, :, :].rearrange("p s c -> p (s c)"),
                           identb[:], [r_qrp, r_identb], [rPT])
                    cp(act, QT[0:64, 0:512], PT[0:64, 0:512], [rPT], [r_QT])
                    cp(act, QT[64:128, 512:1024], PT[64:128, 0:512], [rPT], [r_QT])
                    cp(act, QT[0:64, 1024:1536], PT[0:64, 512:1024], [rPT], [r_QT])
                    cp(act, QT[64:128, 1536:2048], PT[64:128, 512:1024], [rPT], [r_QT])
                    st(sp, q_s[gt], QT[:], [r_QT], bag)
                    pb, rpb = PBs[2], rPB[2]
                    for c in range(2):
                        mm(pb[:], kvaTn[:, c, cs_], wkv[:, c, 512:1024], c == 0, c == 1, [r_kvaTn, r_wkv], [rpb])
                    cp(act, Vt[:, :, 0:64], pb[:].rearrange("p (h c) -> p h c", c=64), [rpb], [r_Vt])
                    st(sp, v_s[b][:, ti * 520:(ti + 1) * 520], Vt[:].rearrange("p h c -> p (h c)"), [r_Vt], bag)


                def stYa(i):
                    ti = gi * 4 + i
                    gt = b * NT + ti
                    cs_ = slice(i * 128, (i + 1) * 128)
                    s1, r_s1 = st1[i]
                    zsQ, r_zsQ = zs4[i]
                    dttQ, r_dttQ = dtt4[i]
                    dtaQ, r_dtaQ = dta4[i]
                    xs_tmW, r_xsW = xs_tm2[i % 2]
                    B_tmW, r_BtmW = B_tm2[i % 2]
                    xdtdW, r_xdtdW = xdtd2[i % 2]
                    ecsmW, r_ecsmW = ecsm2[i % 2]
                    cdBW, r_cdBW = cdB2[i % 2]
                    ydW, r_ydW = yd2[i % 2]
                    for j in range(4):
                        tr(PT[:, j * 128:(j + 1) * 128], xact[:, j, cs_], identb[:], [r_xact, r_identb], [rPT])
                    for j in range(2):
                        tr(PT[:, 512 + j * 128:512 + (j + 1) * 128], xact[:, 4 + j, cs_], identb[:], [r_xact, r_identb], [rPT])
                    cp(act, xs_tmW[:].rearrange("p h c -> p (h c)"), PT[:, 0:512], [rPT], [r_xsW])
                    cp(act, B_tmW[:], PT[:, 512:768], [rPT], [r_BtmW])
                    dt_b = dttQ[:].unsqueeze(2).broadcast_to((128, 8, 64))
                    tt(dve, xdt[:], xs_tmW[:], dt_b, ALU.mult, [r_xsW, r_dttQ], [r_xdt])
                    tt(dve, lseg[:], U1[:].unsqueeze(1).broadcast_to((128, 8, 128)),
                       dtaQ[:].unsqueeze(2).broadcast_to((128, 8, 128)), ALU.mult, [r_U1, r_dtaQ], [r_lseg])
                    for hh in range(2):
                        for h4 in range(4):
                            mm(PBs[0][:, h4 * 128:(h4 + 1) * 128], lseg[:, hh * 4 + h4, :], U2[:], True, True, [r_lseg, r_U2], [rPB[0]])
                        actf(dec[:, hh * 4:(hh + 1) * 4, :].rearrange("p h l -> p (h l)"), PBs[0][:], AF.Exp, [rPB[0]], [r_dec])
                    pb3, rpb3 = PBs[3], rPB[3]
                    for g in range(2):
                        mm(pb3[:, g * 128:(g + 1) * 128], xact[:, 4 + g, cs_], xact[:, 6 + g, cs_], True, True, [r_xact], [rpb3])
                    tt(dve, cbm[:], pb3[:, 0:256].rearrange("p (g l) -> p g l", g=2),
                       m01[:].unsqueeze(1).broadcast_to((128, 2, 128)), ALU.mult, [rpb3, r_m01], [r_cbm])
                    for h in range(8):
                        tt(dve, MT[:, h, :], dec[:, h, :], cbm[:, h // 4, :], ALU.mult, [r_dec, r_cbm], [r_MT])
                    pb4, rpb4 = PBs[4], rPB[4]
                    mm(pb4[:, 0:8], U2[:], dtaQ[:], True, True, [r_U2, r_dtaQ], [rpb4])
                    mm(pb4[:, 8:16], U1[:], dtaQ[:], True, True, [r_U1, r_dtaQ], [rpb4])
                    mm(pb4[:, 16:24], Mc[:, 0, :], dtaQ[:], True, True, [r_Mc, r_dtaQ], [rpb4])
                    mm(pb4[:, 24:32], Mc[:, 1, :], dtaQ[:], True, True, [r_Mc, r_dtaQ], [rpb4])
                    actf(ecs[:], pb4[:, 0:8], AF.Exp, [rpb4], [r_ecs])
                    actf(dte[:], pb4[:, 8:16], AF.Exp, [rpb4], [r_dte])
                    actf(cdBW[:].rearrange("p c h -> p (c h)"), pb4[:, 16:32], AF.Exp, [rpb4], [r_cdBW])
                    for c in range(2):
                        ts(dve, ecsmW[:, c, :], ecs[:], rm[:, c:c + 1], None, ALU.mult, None, [r_ecs, r_rm], [r_ecsmW])
                        ts(dve, dtem[:, c, :], dte[:], rm[:, c:c + 1], None, ALU.mult, None, [r_dte, r_rm], [r_dtem])
                        tt(dve, xdtdW[:, c, :, :], xdt[:], dtem[:, c, :].unsqueeze(2).broadcast_to((128, 8, 64)), ALU.mult,
                           [r_xdt, r_dtem], [r_xdtdW])
                    pb0, rpb0 = PBs[0], rPB[0]
                    for h in range(8):
                        mm(pb0[:, h * 64:(h + 1) * 64], MT[:, h, :], xdt[:, h, :], True, True, [r_MT, r_xdt], [rpb0])
                    cp(act, ydW[:].rearrange("p h c -> p (h c)"), pb0[:], [rpb0], [r_ydW])

                def stYb(i):
                    ti = gi * 4 + i
                    gt = b * NT + ti
                    cs_ = slice(i * 128, (i + 1) * 128)
                    s1, r_s1 = st1[i]
                    zsQ, r_zsQ = zs4[i]
                    dttQ, r_dttQ = dtt4[i]
                    dtaQ, r_dtaQ = dta4[i]
                    xs_tmW, r_xsW = xs_tm2[i % 2]
                    B_tmW, r_BtmW = B_tm2[i % 2]
                    xdtdW, r_xdtdW = xdtd2[i % 2]
                    ecsmW, r_ecsmW = ecsm2[i % 2]
                    cdBW, r_cdBW = cdB2[i % 2]
                    ydW, r_ydW = yd2[i % 2]
                    pb2, rpb2 = P2[:, 0:512], rP2a
                    pbY = (PBs[1], PBs[2])
                    rpbY = (rPB[1], rPB[2])
                    for c in range(2):
                        for g in range(2):
                            mm(pbY[c][:, g * 256:(g + 1) * 256], xact[:, 6 + g, cs_],
                               Sbf[:, g * 4:(g + 1) * 4, :].rearrange("p h c -> p (h c)"), True, True, [r_xact, r_Sbf], [rpbY[c]])
                        for g in range(2):
                            mm(pb2[:, g * 256:(g + 1) * 256], B_tmW[:, g * 128:(g + 1) * 128],
                               xdtdW[:, c, g * 4:(g + 1) * 4, :].rearrange("p h c -> p (h c)"), True, True, [r_BtmW, r_xdtdW], [rpb2])
                        tt(dve, Sst[:], Sst[:], cdBW[:, c, :].unsqueeze(2).broadcast_to((128, 8, 64)), ALU.mult, [r_Sst, r_cdBW], [r_Sst])
                        tt(dve, Sst[:], Sst[:], pb2[:].rearrange("p (h c) -> p h c", c=64), ALU.add, [r_Sst, rpb2], [r_Sst])
                        cp(act, Sbf[:], Sst[:], [r_Sst], [r_Sbf])
                    tt(dve, yt[:], pbY[0][:].rearrange("p (h c) -> p h c", c=64), ecsmW[:, 0, :].unsqueeze(2).broadcast_to((128, 8, 64)),
                       ALU.mult, [rpbY[0], r_ecsmW], [r_yt])
                    tt(dve, yt2[:], pbY[1][:].rearrange("p (h c) -> p h c", c=64), ecsmW[:, 1, :].unsqueeze(2).broadcast_to((128, 8, 64)),
                       ALU.mult, [rpbY[1], r_ecsmW], [r_yt2])
                    tt(pool, yt[:], yt[:], yt2[:], ALU.add, [r_yt, r_yt2], [r_yt])
                    tt(pool, yt[:], yt[:], ydW[:], ALU.add, [r_yt, r_ydW], [r_yt])
                    tt(dve, yt2[:], xs_tmW[:], small["dsk"][0][:].unsqueeze(2).broadcast_to((128, 8, 64)), ALU.mult,
                       [r_xsW, small["dsk"][1]], [r_yt2])
                    tt(pool, yt[:], yt[:], yt2[:], ALU.add, [r_yt, r_yt2], [r_yt])
                    tt(dve, yt[:].rearrange("p h c -> p (h c)"), yt[:].rearrange("p h c -> p (h c)"), zsQ[:], ALU.mult,
                       [r_yt, r_zsQ], [r_yt])
                    for g in range(2):
                        actf(junk[:, g * 256:(g + 1) * 256], yt[:, g * 4:(g + 1) * 4, :].rearrange("p h c -> p (h c)"), AF.Square,
                             [r_yt], [r_junk, r_s1], accum_out=s1[:, 2 + g:3 + g])
                    rstd_from(s1[:, 2:4], r_s1, s1[:, 2:4], r_s1, 256)
                    for g in range(2):
                        stt(dve, mixs[:, g * 256:(g + 1) * 256], yt[:, g * 4:(g + 1) * 4, :].rearrange("p h c -> p (h c)"),
                            s1[:, 2 + g:3 + g], small["ssdn"][0][:, g * 256:(g + 1) * 256], ALU.mult, ALU.mult,
                            [r_yt, r_s1, small["ssdn"][1]], [r_mixs])
                    st(sp, mix_s[gt * 128:(gt + 1) * 128, 0:512], mixs[:], [r_mixs], bag)

                for i in range(4):
                    stX(i)
                stYa(0)
                for i in range(4):
                    if i + 1 < 4:
                        stYa(i + 1)
                    stYb(i)

    K.barrier()
    cut(0.6)
    with ExitStack() as pt_:
        KnT, r_KnT = SB("KnT", [128, 4, S], BF16, pt_)
        KrT, r_KrT = SB("KrT", [128, S], BF16, pt_)
        Vst, r_Vst = SB("Vst", [128, NT, 8, 65], BF16, pt_)
        Qb = [SB("Qb%d" % i, [128, 16, 128], BF16, pt_) for i in range(2)]
        PTs = [SB("PTs%d" % i, [128, 4, 128], BF16, pt_) for i in range(4)]
        rec, r_rec = SB("rec", [128, 8], F32, pt_)
        osb, r_osb = SB("osb", [128, 8, 64], F32, pt_)
        junk2, r_junk2 = SB("junk2", [128, 512], BF16, pt_)
        sA, r_sA = SB("sA", [128, 2], F32, pt_)
        mixm = [SB("mixm%d" % i, [128, 512], BF16, pt_) for i in range(2)]
        sc = 1.0 / math.sqrt(96.0)
        for b in range(2):
            K._wait(sp, bagA[b].evs())
            ld(sp, KnT[:].rearrange("p j s -> p (j s)"), kn_s[b], [r_KnT])
            ld(sp, KrT[:], kr_s[b], [r_KrT])
            ld(sp, Vst[:].rearrange("p t h c -> p (t h c)"), v_s[b], [r_Vst])
            for ti in range(NT):
                gt = b * NT + ti
                q_t, q_r = Qb[ti % 2]
                ld(sp, q_t[:].rearrange("p a q -> p (a q)"), q_s[gt], [q_r])
                nkt = ti + 1
                pbO = (P2[:, 0:512], P2[:, 512:1024])
                rpbO = (rP2a, rP2b)
                groups = [(h, k0, min(4, nkt - k0)) for h in range(8) for k0 in range(0, nkt, 4)]
                DEP = 2

                def emit_qk(gi):
                    h, k0, nk = groups[gi]
                    j = h % 4
                    hs = h // 4
                    pbs, rpbs = PBs[gi % 3], rPB[gi % 3]
                    for kk in range(nk):
                        kt = k0 + kk
                        kc = slice(kt * 128, (kt + 1) * 128)
                        mm(pbs[:, kk * 128:(kk + 1) * 128], KnT[:, j, kc], q_t[:, hs * 4 + j, :], True, False, [r_KnT, q_r], [rpbs])
                        mm(pbs[:, kk * 128:(kk + 1) * 128], KrT[:, kc], q_t[:, 8 + hs * 4 + j, :], False, True, [r_KrT, q_r], [rpbs])

                def emit_pv(gi):
                    h, k0, nk = groups[gi]
                    po, rpo = pbO[h // 4], rpbO[h // 4]
                    ocol = (h % 4) * 65
                    pbs, rpbs = PBs[gi % 3], rPB[gi % 3]
                    pts, rpts = PTs[gi % 4]
                    actf(pts[:, 0:nk, :].rearrange("p a q -> p (a q)"), pbs[:, 0:nk * 128], AF.Exp, [rpbs], [rpts], scale=sc)
                    if k0 + nk == nkt:
                        K.I(pool, lambda: nc.gpsimd.memset(pts[64:128, nk - 1, 0:64], 0.0), writes=[rpts])
                    for kk in range(nk):
                        kt = k0 + kk
                        mm(po[:, ocol:ocol + 65], pts[:, kk, :], Vst[:, kt, h, :], kt == 0, kt == nkt - 1, [rpts, r_Vst], [rpo])

                for gi in range(min(DEP, len(groups))):
                    emit_qk(gi)
                for gi in range(len(groups)):
                    if gi + DEP < len(groups):
                        emit_qk(gi + DEP)
                    emit_pv(gi)
                for hh in range(2):
                    ov = pbO[hh][:, 0:260].rearrange("p (h c) -> p h c", c=65)
                    K.I(dve, lambda: nc.vector.reciprocal(rec[:, hh * 4:(hh + 1) * 4], ov[:, :, 64]), reads=[rpbO[hh]], writes=[r_rec])
                    tt(dve, osb[:, hh * 4:(hh + 1) * 4, :], ov[:, :, 0:64],
                       rec[:, hh * 4:(hh + 1) * 4].unsqueeze(2).broadcast_to((128, 4, 64)), ALU.mult, [rpbO[hh], r_rec], [r_osb])
                if gt == 1:
                    dump("osb", osb[:], r_osb, [128, 8, 64])
                    dump("rec", rec[:], r_rec, [128, 8])
                    dump("pts", PTs[0][0][:], PTs[0][1], [128, 4, 128], BF16)
                    dump("qb", q_t[:], q_r, [128, 16, 128], BF16)
                    dump("KrT", KrT[:], r_KrT, [128, S], BF16)
                    dump("KnT", KnT[:], r_KnT, [128, 4, S], BF16)
                    dump("Vst", Vst[:], r_Vst, [128, NT, 8, 65], BF16)
                actf(junk2[:], osb[:].rearrange("p h c -> p (h c)"), AF.Square, [r_osb], [r_junk2, r_sA], accum_out=sA[:, 0:1])
                rstd_from(sA[:, 0:1], r_sA, sA[:, 1:2], r_sA, 512)
                m_t, m_r = mixm[ti % 2]
                stt(dve, m_t[:], osb[:].rearrange("p h c -> p (h c)"), sA[:, 1:2], small["mlan"][0][:],
                    ALU.mult, ALU.mult, [r_osb, r_sA, small["mlan"][1]], [m_r])
                st(sp, mix_s[gt * 128:(gt + 1) * 128, 512:1024], m_t[:], [m_r], bagT[b])

    K.barrier()
    cut(0.8)
    with ExitStack() as pb_:
        wout, r_wout = SB("wout", [128, 8, 1024], BF16, pb_)
        with ExitStack() as pst:
            stg = [SB("stgo%d" % i, [128, 1024], F32, pst) for i in range(2)]
            for k in range(8):
                s_t, s_r = stg[k % 2]
                ld(sp, s_t[:], wout_d[:, k, :], [s_r])
                cast_any(wout[:, k, :], s_t[:], [s_r], [r_wout])
        K.barrier()
        G1, r_G1 = SB("G1", [128, DM], F32, pb_)
        A2, r_A2 = SB("A2", [128, DM], F32, pb_)
        SH2, r_SH2 = SB("SH2", [128, DM], F32, pb_)
        xt = [SB("xtb%d" % i, [128, DM], F32, pb_) for i in range(2)]
        mixin = [SB("mixin%d" % i, [128, DM], BF16, pb_) for i in range(2)]
        mixT, r_mixT = SB("mixT", [128, 8, 128], BF16, pb_)
        junk, r_junk = SB("junkb", [128, DM], BF16, pb_)
        x1, r_x1 = SB("x1", [128, DM], F32, pb_)
        h2s = [SB("h2_%d" % i, [128, DM], F32, pb_) for i in range(2)]
        h2b, r_h2b = SB("h2b", [128, DM], BF16, pb_)
        h2T, r_h2T = SB("h2T", [128, 8, 128], F32, pb_)
        s1s = [SB("s1b%d" % i, [128, 8], F32, pb_) for i in range(2)]
        nv0, r_nv0 = SB("nv0", [128, 1], F32, pb_)
        e4, r_e4 = SB("e4", [128, 4], F32, pb_)
        pb4, rpb4 = PBs[4], rPB[4]
        bag1 = Bag()
        for b in range(2):
            K._wait(sp, bagT[b].evs() + bagA[b].evs())
            tt(dve, vtmp[:], modT[:, 16:24, b], small["gpost1"][0][:], ALU.mult, [r_modT, small["gpost1"][1]], [r_vtmp])
            bcast_rows(vtmp, r_vtmp, G1, r_G1)
            ts(dve, vtmp[:], modT[:, 32:40, b], 1.0, None, ALU.add, None, [r_modT], [r_vtmp])
            tt(dve, vtmp[:], vtmp[:], small["gpre2"][0][:], ALU.mult, [r_vtmp, small["gpre2"][1]], [r_vtmp])
            bcast_rows(vtmp, r_vtmp, A2, r_A2)
            cp(dve, vtmp[:], modT[:, 24:32, b], [r_modT], [r_vtmp])
            bcast_rows(vtmp, r_vtmp, SH2, r_SH2)
            def stageA(ti):
                gt = b * NT + ti
                x_t, x_r = xt[gt % 2]
                mi, r_mi = mixin[gt % 2]
                h2, r_h2 = h2s[gt % 2]
                s1, r_s1 = s1s[gt % 2]
                ld(sp, x_t[:], x_d[gt * 128:(gt + 1) * 128, :], [x_r])
                ld(sp, mi[:], mix_s[gt * 128:(gt + 1) * 128, :], [r_mi])
                for k in range(8):
                    tr(PT[:, k * 128:(k + 1) * 128], mi[:, k * 128:(k + 1) * 128], identb[:], [r_mi, r_identb], [rPT])
                cp(act, mixT[:].rearrange("p k t -> p (k t)"), PT[:], [rPT], [r_mixT])
                for hf in range(2):
                    rp = rP2a if hf == 0 else rP2b
                    for k in range(8):
                        mm(P2[:, hf * 512:(hf + 1) * 512], mixT[:, k, :], wout[:, k, hf * 512:(hf + 1) * 512], k == 0, k == 7,
                           [r_mixT, r_wout], [rp])
                actf(junk[:], P2[:], AF.Square, [rP2a, rP2b], [r_junk, r_s1], accum_out=s1[:, 0:1])
                rstd_from(s1[:, 0:1], r_s1, s1[:, 1:2], r_s1, DM)
                stt(dve, x1[:], P2[:], s1[:, 1:2], G1[:], ALU.mult, ALU.mult, [rP2a, rP2b, r_s1, r_G1], [r_x1])
                tt(pool, x1[:], x1[:], x_t[:], ALU.add, [r_x1, x_r], [r_x1])
                st(sp, x1s_d[gt * 128:(gt + 1) * 128, :], x1[:], [r_x1], bag1)
                actf(junk[:], x1[:], AF.Square, [r_x1], [r_junk, r_s1], accum_out=s1[:, 2:3])
                rstd_from(s1[:, 2:3], r_s1, s1[:, 3:4], r_s1, DM)
                stt(dve, h2[:], x1[:], s1[:, 3:4], A2[:], ALU.mult, ALU.mult, [r_x1, r_s1, r_A2], [r_h2])
                tt(pool, h2[:], h2[:], SH2[:], ALU.add, [r_h2, r_SH2], [r_h2])
                cp(act, h2b[:], h2[:], [r_h2], [r_h2b])
                st(sp, h2s_d[gt * 128:(gt + 1) * 128, :], h2b[:], [r_h2b], bag1)

            def stageB(ti):
                gt = b * NT + ti
                h2, r_h2 = h2s[gt % 2]
                s1, r_s1 = s1s[gt % 2]
                for k in range(8):
                    pbx, rpx = PBs[k // 4], rPB[k // 4]
                    tr(pbx[:, (k % 4) * 128:(k % 4 + 1) * 128], h2[:, k * 128:(k + 1) * 128], identf[:], [r_h2, r_identf], [rpx])
                for hf in range(2):
                    cp(act, h2T[:, hf * 4:(hf + 1) * 4, :].rearrange("p k t -> p (k t)"), PBs[hf][:], [rPB[hf]], [r_h2T])
                wr_t, r_wr = small["wr"]
                for k in range(8):
                    mm(pb4[:, 0:NE], h2T[:, k, :], wr_t[:, k, :], k == 0, k == 7, [r_h2T, r_wr], [rpb4])
                lgt = lg_all[:, gt, :]
                tt(dve, lgt, pb4[:, 0:NE], small["br"][0][:], ALU.add, [rpb4, small["br"][1]], [r_lg])
                K.I(dve, lambda: nc.vector.max(v8_all[:, gt, :], lgt), reads=[r_lg], writes=[r_v8])
                ts(dve, nv0[:], v8_all[:, gt, 0:1], -1.0, None, ALU.mult, None, [r_v8], [r_nv0])
                actf(e4[:], v8_all[:, gt, 0:4], AF.Exp, [r_v8, r_nv0], [r_e4, r_s1], bias=nv0[:, 0:1], accum_out=s1[:, 4:5])
                K.I(dve, lambda: nc.vector.reciprocal(s1[:, 5:6], s1[:, 4:5]), reads=[r_s1], writes=[r_s1])
                ts(dve, gate_all[:, gt, :], e4[:], s1[:, 5:6], None, ALU.mult, None, [r_e4, r_s1], [r_gate])

            stageA(0)
            for ti in range(NT):
                if ti + 1 < NT:
                    stageA(ti + 1)
                stageB(ti)
    K.barrier()
    dump("lg", lg_all[:], r_lg, [128, NTT, NE])
    dump("v8", v8_all[:], r_v8, [128, NTT, 8])
    cut(2)

    base, r_base = SB("base", [128, NE])
    msk, r_msk = SB("msk", [128, NE])
    pos_all, r_pos = SB("pos_all", [128, NTT, NE])
    K.I(dve, lambda: nc.vector.memset(base[:], 0.0), writes=[r_base])
    pb4, rpb4 = PBs[4], rPB[4]
    for gt in range(NTT):
        ts(dve, msk[:], lg_all[:, gt, :], v8_all[:, gt, 3:4], None, ALU.is_ge, None, [r_lg, r_v8], [r_msk])
        mm(pb4[:, 32:64], Lst[:], msk[:], True, True, [r_Lst, r_msk], [rpb4])
        mm(pb4[:, 64:96], onesf[:], msk[:], True, True, [r_onesf, r_msk], [rpb4])
        tt(dve, pos_all[:, gt, :], pb4[:, 32:64], base[:], ALU.add, [rpb4, r_base], [r_pos])
        tt(dve, base[:], pb4[:, 64:96], base[:], ALU.add, [rpb4, r_base], [r_base])
    nblk, r_nblk = SB("nblk", [128, NE])
    incl, r_incl = SB("incl", [128, NE])
    pstart, r_pstart = SB("pstart", [128, NE])
    iotab, r_iotab = SB("iotab", [128, NB])
    bexp_f, r_bexpf = SB("bexp_f", [128, NB])
    bexp_i, r_bexpi = SB("bexp_i", [128, NB], I32)
    dest_i, r_desti = SB("dest_i", [128, NTT, 4], I32)
    widx_i, r_widx = SB("widx_i", [128, NB, 8], I32)
    bidx_i, r_bidx = SB("bidx_i", [128, NB], I32)
    pk, r_pk = SB("pk", [128, 8])
    c1024, r_c1024 = SB("c1024", [128, 8])
    ld(sp, pk[:], pk_d, [r_pk])
    K.I(dve, lambda: nc.vector.memset(c1024[:], 1024.0), writes=[r_c1024])
    ld(sp, iotab[:], iotab_d, [r_iotab])
    off = -0.5 + 1.0 / 1024.0
    ts(dve, nblk[:], base[:], float(BLK - 1), 1.0 / BLK, ALU.add, ALU.mult, [r_base], [r_nblk])
    ts(dve, nblk[:], nblk[:], off, MAGIC, ALU.add, ALU.add, [r_nblk], [r_nblk])
    ts(dve, nblk[:], nblk[:], -MAGIC, None, ALU.add, None, [r_nblk], [r_nblk])
    K.I(dve, lambda: nc.vector.tensor_tensor_scan(incl[:], onesf[:, 0:NE], nblk[:], 0.0, ALU.mult, ALU.add),
        reads=[r_onesf, r_nblk], writes=[r_incl])
    tt(dve, pstart[:], incl[:], nblk[:], ALU.subtract, [r_incl, r_nblk], [r_pstart])
    ts(dve, pstart[:], pstart[:], float(BLK), None, ALU.mult, None, [r_pstart], [r_pstart])
    with ExitStack() as p2a:
        cmp3, r_cmp3 = SB("cmp3", [128, NB, NE], F32, p2a)
        tt(dve, cmp3[:], incl[:].unsqueeze(1).broadcast_to((128, NB, NE)), iotab[:].unsqueeze(2).broadcast_to((128, NB, NE)),
           ALU.is_le, [r_incl, r_iotab], [r_cmp3])
        K.I(dve, lambda: nc.vector.tensor_reduce(bexp_f[:], cmp3[:], AX.X, ALU.add), reads=[r_cmp3], writes=[r_bexpf])
        ts(dve, bexp_f[:], bexp_f[:], float(NE - 1), None, ALU.min, None, [r_bexpf], [r_bexpf])
        cp(dve, bexp_i[:], bexp_f[:], [r_bexpf], [r_bexpi])
        widx_f, r_widxf = SB("widx_f", [128, NB, 8], F32, p2a)
        tt(dve, widx_f[:], bexp_f[:].unsqueeze(2).broadcast_to((128, NB, 8)), c1024[:].unsqueeze(1).broadcast_to((128, NB, 8)),
           ALU.mult, [r_bexpf, r_c1024], [r_widxf])
        tt(dve, widx_f[:], widx_f[:], pk[:].unsqueeze(1).broadcast_to((128, NB, 8)), ALU.add, [r_widxf, r_pk], [r_widxf])
        cp(dve, widx_i[:], widx_f[:], [r_widxf], [r_widx])
        ts(dve, bexp_f[:], bexp_f[:], 128.0, pk[:, 0:1], ALU.mult, ALU.add, [r_bexpf, r_pk], [r_bexpf])
        cp(dve, bidx_i[:], bexp_f[:], [r_bexpf], [r_bidx])
        destf, r_destf = SB("destf", [128, NE], F32, p2a)
        oh, r_oh = SB("oh", [128, 4, NE], F32, p2a)
        dk, r_dk = SB("dk", [128, 4], F32, p2a)
        hb = [SB("hb%d" % i, [128, DM], BF16, p2a) for i in range(2)]
        r_xg = Res()
        for gt in range(NTT):
            tt(dve, destf[:], pos_all[:, gt, :], pstart[:], ALU.add, [r_pos, r_pstart], [r_destf])
            tt(dve, oh[:], lg_all[:, gt, :].unsqueeze(1).broadcast_to((128, 4, NE)),
               v8_all[:, gt, 0:4].unsqueeze(2).broadcast_to((128, 4, NE)), ALU.is_equal, [r_lg, r_v8], [r_oh])
            tt(dve, oh[:], oh[:], destf[:].unsqueeze(1).broadcast_to((128, 4, NE)), ALU.mult, [r_oh, r_destf], [r_oh])
            K.I(dve, lambda: nc.vector.tensor_reduce(dk[:], oh[:], AX.X, ALU.add), reads=[r_oh], writes=[r_dk])
            cp(dve, dest_i[:, gt, :], dk[:], [r_dk], [r_desti])
            h_t, h_r = hb[gt % 2]
            ld(sp, h_t[:], h2s_d[gt * 128:(gt + 1) * 128, :], [h_r])
            if gt == 0:
                K._wait(sp, bag1.evs())
            for k in range(4):
                K.dma(pool, lambda: nc.gpsimd.indirect_dma_start(
                    out=xg_d, out_offset=bass.IndirectOffsetOnAxis(ap=dest_i[:, gt, k:k + 1], axis=0),
                    in_=h_t[:], in_offset=None), reads=[h_r, r_desti], writes=[])
    K.barrier()
    dump("desti", dest_i[:], r_desti, [128, NTT, 4], I32)
    dump("bexp", bexp_i[:], r_bexpi, [128, NB], I32)

    cut(3)
    scat_evs = [(sid, K.semvals[sid]) for sid in pool.ring if K.semvals[sid] > 0]

    with ExitStack() as p2:
        wgu = [SB("wgu%d" % i, [128, 8, 2 * DM], BF16, p2) for i in range(2)]
        wdn = [SB("wdn%d" % i, [128, 8, DM], BF16, p2) for i in range(2)]
        bgu = [SB("bgus%d" % i, [128, 16], F32, p2) for i in range(2)]
        bdn = [SB("bdns%d" % i, [128, DM], F32, p2) for i in range(2)]
        xgt = [SB("xgt%d" % i, [128, DM], BF16, p2) for i in range(2)]
        xgTs = [SB("xgT%d" % i, [128, 8, BLK], BF16, p2) for i in range(2)]
        actT, r_actT = SB("actT", [128, 8, BLK], BF16, p2)
        gg, r_gg = SB("gg", [128, BLK], F32, p2)
        sg, r_sg = SB("sg", [128, BLK], F32, p2)
        ll, r_ll = SB("ll", [128, BLK], F32, p2)
        yo = [SB("yo%d" % i, [128, DM], F32, p2) for i in range(2)]
        K._wait(sp, scat_evs)
        wgr = [[Res() for _ in range(8)] for _ in range(2)]
        wdr = [[Res() for _ in range(8)] for _ in range(2)]

        def load_weights(blk):
            wg_t = wgu[blk % 2][0]
            wd_t = wdn[blk % 2][0]
            bg_t, bg_r = bgu[blk % 2]
            bd_t, bd_r = bdn[blk % 2]
            for k in range(8):
                K.dma(pool, lambda: nc.gpsimd.indirect_dma_start(
                    out=wg_t[:, k, :], out_offset=None, in_=wgu_d,
                    in_offset=bass.IndirectOffsetOnAxis(ap=widx_i[:, blk, k:k + 1], axis=0)), reads=[r_widx], writes=[wgr[blk % 2][k]])
            for k in range(8):
                K.dma(pool, lambda: nc.gpsimd.indirect_dma_start(
                    out=wd_t[:, k, :], out_offset=None, in_=wd_d,
                    in_offset=bass.IndirectOffsetOnAxis(ap=widx_i[:, blk, k:k + 1], axis=0)), reads=[r_widx], writes=[wdr[blk % 2][k]])
            K.dma(pool, lambda: nc.gpsimd.indirect_dma_start(
                out=bg_t[:], out_offset=None, in_=bgu_d,
                in_offset=bass.IndirectOffsetOnAxis(ap=bidx_i[:, blk:blk + 1], axis=0)), reads=[r_bidx], writes=[bg_r])
            K.dma(pool, lambda: nc.gpsimd.indirect_dma_start(
                out=bd_t[:], out_offset=None, in_=bd_d,
                in_offset=bass.IndirectOffsetOnAxis(ap=bidx_i[:, blk:blk + 1], axis=0)), reads=[r_bidx], writes=[bd_r])

        PT2 = PBs[4][:].bitcast(BF16)
        ptn = [0]

        def prep_tokens(blk):
            xgT_, r_xgT_ = xgTs[blk % 2]
            for st in range(4):
                g_t, g_r = xgt[st % 2]
                r0 = blk * BLK + st * 128
                ld(sp, g_t[:], xg_d[r0:r0 + 128, :], [g_r])
                if ptn[0] % 2 == 0:
                    pt_ap, pt_r = PT[:], rPT
                else:
                    pt_ap, pt_r = PT2, rPB[4]
                ptn[0] += 1
                for k in range(8):
                    tr(pt_ap[:, k * 128:(k + 1) * 128], g_t[:, k * 128:(k + 1) * 128], identb[:], [g_r, r_identb], [pt_r])
                cp(act, xgT_[:, :, st * 128:(st + 1) * 128],
                   pt_ap.rearrange("p (k t) -> p k t", t=128), [pt_r], [r_xgT_])

        load_weights(0)
        prep_tokens(0)
        for blk in range(NB):
            if blk + 1 < NB:
                load_weights(blk + 1)
                prep_tokens(blk + 1)
            wg_t = wgu[blk % 2][0]
            wd_t = wdn[blk % 2][0]
            wg_rs = wgr[blk % 2]
            wd_rs = wdr[blk % 2]
            bg_t, bg_r = bgu[blk % 2]
            bd_t, bd_r = bdn[blk % 2]
            xgT, r_xgT = xgTs[blk % 2]
            for fc in range(8):
                pg, rpg = PBs[(fc % 2) * 2], rPB[(fc % 2) * 2]
                pl, rpl = PBs[(fc % 2) * 2 + 1], rPB[(fc % 2) * 2 + 1]
                for k in range(8):
                    mm(pg[:], wg_t[:, k, fc * 128:(fc + 1) * 128], xgT[:, k, :], k == 0, k == 7, [wg_rs[k], r_xgT], [rpg])
                for k in range(8):
                    mm(pl[:], wg_t[:, k, DM + fc * 128:DM + (fc + 1) * 128], xgT[:, k, :], k == 0, k == 7, [wg_rs[k], r_xgT], [rpl])
                ts(dve, gg[:], pg[:], bg_t[:, fc:fc + 1], 7.0, ALU.add, ALU.min, [rpg, bg_r], [r_gg])
                actf(sg[:], gg[:], AF.Sigmoid, [r_gg], [r_sg], scale=1.702)
                ts(dve, ll[:], pl[:], bg_t[:, 8 + fc:9 + fc], 7.0, ALU.add, ALU.min, [rpl, bg_r], [r_ll])
                ts(dve, ll[:], ll[:], -7.0, 1.0, ALU.max, ALU.add, [r_ll], [r_ll])
                tt(dve, gg[:], gg[:], sg[:], ALU.mult, [r_gg, r_sg], [r_gg])
                tt(dve, actT[:, fc, :], gg[:], ll[:], ALU.mult, [r_gg, r_ll], [r_actT])
            dbanks = ((P2[:, 0:512], rP2a), (P2[:, 512:1024], rP2b), (PBs[2][:], rPB[2]), (PBs[3][:], rPB[3]))
            for st in range(4):
                y_t, y_r = yo[st % 2]
                for hf in range(2):
                    pd, rpd = dbanks[(st % 2) * 2 + hf]
                    for k in range(8):
                        mm(pd, actT[:, k, st * 128:(st + 1) * 128], wd_t[:, k, hf * 512:(hf + 1) * 512], k == 0, k == 7,
                           [r_actT, wd_rs[k]], [rpd])
                    tt(dve, y_t[:, hf * 512:(hf + 1) * 512], pd, bd_t[:, hf * 512:(hf + 1) * 512], ALU.add, [rpd, bd_r], [y_r])
                r0 = blk * BLK + st * 128
                K.dma(act, lambda: nc.scalar.dma_start(out=yb_d[r0:r0 + 128, :], in_=y_t[:]), reads=[y_r], writes=[])
    K.barrier()
    yb_evs = [(sid, K.semvals[sid]) for sid in act.ring if K.semvals[sid] > 0]

    with ExitStack() as p3:
        yg = [SB("yg%d" % i, [128, DM], F32, p3) for i in range(8)]
        fa, r_fa = SB("fa", [128, DM], F32, p3)
        x1r = [SB("x1r%d" % i, [128, DM], F32, p3) for i in range(2)]
        ob = [SB("ob%d" % i, [128, DM], F32, p3) for i in range(2)]
        junk3, r_junk3 = SB("junk3", [128, DM], BF16, p3)
        s3, r_s3 = SB("s3", [128, 2], F32, p3)
        G2, r_G2 = SB("G2", [128, 2, DM], F32, p3)
        for b in range(2):
            tt(dve, vtmp[:], modT[:, 40:48, b], small["gpost2"][0][:], ALU.mult, [r_modT, small["gpost2"][1]], [r_vtmp])
            bcast_rows(vtmp, r_vtmp, G2[:, b, :], r_G2)
        K._wait(pool, yb_evs)
        K._wait(sp, bag1.evs())
        def p3_loads(gt):
            x_t, x_r = x1r[gt % 2]
            ld(sp, x_t[:], x1s_d[gt * 128:(gt + 1) * 128, :], [x_r])
            for k in range(4):
                y_t, y_r = yg[(gt % 2) * 4 + k]
                K.dma(pool, lambda: nc.gpsimd.indirect_dma_start(
                    out=y_t[:], out_offset=None, in_=yb_d,
                    in_offset=bass.IndirectOffsetOnAxis(ap=dest_i[:, gt, k:k + 1], axis=0)), reads=[r_desti], writes=[y_r])

        def p3_compute(gt):
            b = gt // NT
            x_t, x_r = x1r[gt % 2]
            o_t, o_r = ob[gt % 2]
            ygs = [yg[(gt % 2) * 4 + k] for k in range(4)]
            actf(fa[:], ygs[0][0][:], AF.Copy, [ygs[0][1], r_gate], [r_fa], scale=gate_all[:, gt, 0:1])
            for k in range(1, 4):
                stt(dve, fa[:], ygs[k][0][:], gate_all[:, gt, k:k + 1], fa[:], ALU.mult, ALU.add, [ygs[k][1], r_gate, r_fa], [r_fa])
            actf(junk3[:], fa[:], AF.Square, [r_fa], [r_junk3, r_s3], accum_out=s3[:, 0:1])
            actf(s3[:, 1:2], s3[:, 0:1], AF.Sqrt, [r_s3], [r_s3], scale=1.0 / DM, bias=epsb[:, 0:1])
            K.I(dve, lambda: nc.vector.reciprocal(s3[:, 1:2], s3[:, 1:2]), reads=[r_s3], writes=[r_s3])
            stt(dve, o_t[:], fa[:], s3[:, 1:2], G2[:, b, :], ALU.mult, ALU.mult, [r_fa, r_s3, r_G2], [o_r])
            tt(pool, o_t[:], o_t[:], x_t[:], ALU.add, [o_r, x_r], [o_r])
            K.dma(sp, lambda: nc.sync.dma_start(out=out_d[gt * 128:(gt + 1) * 128, :], in_=o_t[:]), reads=[o_r], writes=[], is_out=True)

        p3_loads(0)
        for gt in range(NTT):
            if gt + 1 < NTT:
                p3_loads(gt + 1)
            p3_compute(gt)
    K.barrier()
    K.finish()
    es.close()
    return nc, dbg_d


def _consts(NB):
    idx = np.arange(128)
    ch = idx // 64
    same = ch[:, None] == ch[None, :]
    U1 = (same & (idx[:, None] > idx[None, :])).astype(np.float32)
    U2 = (same & (idx[:, None] <= idx[None, :])).astype(np.float32)
    m01 = U2.copy()
    Lst = (idx[:, None] < idx[None, :]).astype(np.float32)
    invf = (np.float32(10000.0) ** (-(np.arange(16, dtype=np.float32) * np.float32(2.0) / np.float32(32.0)))).astype(np.float32)
    rm = np.stack([(ch == 0), (ch == 1)], axis=1).astype(np.float32)
    return dict(identf=np.eye(128, dtype=np.float32), U1=U1, U2=U2, m01=m01, Lst=Lst, rm=rm,
                invf=np.tile(invf[None, :], (128, 1)).astype(np.float32),
                iotab=np.tile(np.arange(NB, dtype=np.float32)[None, :], (128, 1)),
                pk=(np.arange(8, dtype=np.float32)[None, :] * 128 + np.arange(128, dtype=np.float32)[:, None]).astype(np.float32))


def _rep(v, n=128):
    return np.ascontiguousarray(np.tile(np.asarray(v, np.float32).reshape(1, -1), (n, 1)))


def _pk(v, k):
    return np.ascontiguousarray(np.asarray(v, np.float32).reshape(k, 128).T)


_CACHE = {}


def kernel(x, c, positions, w_ada, b_ada, pre_mix_norm, w_in, conv_w, conv_b, dt_bias, a_log, d_skip, ssd_norm,
           q_a_norm, w_q_up, kv_a_norm, w_kv_up, mla_norm, w_out, post_mix_norm, pre_ffn_norm, w_router, b_router,
           w_gate_up, b_gate_up, w_down, b_down, post_ffn_norm, _dbg=()):
    x = np.asarray(x, np.float32)
    Bsz, S, _ = x.shape
    ncores = Bsz // 2
    NT = S // 128
    NB = (2 * S * 4) // BLK + NE
    key = (S, tuple(_dbg))
    if key not in _CACHE:
        _CACHE[key] = build(S, _dbg)
    nc, dbg_d = _CACHE[key]
    f = lambda a: np.ascontiguousarray(np.asarray(a, np.float32))
    w_in = f(w_in)[0]
    w_tm = np.ascontiguousarray(np.concatenate([w_in[:, 0:512], w_in[:, 1536:1544], w_in[:, 2184:2216]], axis=1))
    w_fm = np.ascontiguousarray(np.concatenate([w_in[:, 512:1536], w_in[:, 1544:1928], w_in[:, 1928:2184]], axis=1))
    wq = f(w_q_up)[0].reshape(384, 8, 96)
    qn = wq[:, :, 0:64]
    qr = wq[:, :, 64:96]
    qn_pairs = np.stack([np.concatenate([qn[:, j], qn[:, j + 4]], axis=1) for j in range(4)], axis=1)
    wq2 = np.ascontiguousarray(np.concatenate([qn_pairs.reshape(384, 512), qr.reshape(384, 256)], axis=1))
    wkv = f(w_kv_up)[0].reshape(256, 8, 128)
    kn = wkv[:, :, 0:64]
    vv = wkv[:, :, 64:128]
    kn_pairs = np.stack([np.concatenate([kn[:, j], kn[:, j + 4]], axis=1) for j in range(4)], axis=1)
    wkv2 = np.ascontiguousarray(np.concatenate([kn_pairs.reshape(256, 512), vv.reshape(256, 512)], axis=1))
    cw = f(conv_w)[0]
    convw = np.ascontiguousarray(cw.reshape(4, 8, 128).transpose(2, 1, 0))
    shared = dict(
        w_ada=f(w_ada)[0], b_adaT=_pk(f(b_ada)[0], 48), g_pre1=_pk(f(pre_mix_norm)[0], 8), g_post1=_pk(f(post_mix_norm)[0], 8),
        g_pre2=_pk(f(pre_ffn_norm)[0], 8), g_post2=_pk(f(post_ffn_norm)[0], 8), w_tm=w_tm, w_fm=w_fm, convw=convw,
        convb=_pk(f(conv_b)[0], 8), dtb=_rep(f(dt_bias)[0]), alog=_rep(f(a_log)[0]), dsk=_rep(f(d_skip)[0]),
        ssdn=_rep(f(ssd_norm)[0]), mlan=_rep(f(mla_norm)[0]), qan=_pk(f(q_a_norm)[0], 3), kvan=_pk(f(kv_a_norm)[0], 2),
        wq=wq2, wkv=wkv2, wout=f(w_out)[0], wr=f(w_router)[0], br=_rep(f(b_router)[0]),
        wgu=f(w_gate_up)[0].reshape(NE * DM, 2 * DM), wd=f(w_down)[0].reshape(NE * DM, DM),
        bgu=np.ascontiguousarray(f(b_gate_up)[0].reshape(NE, 16, 128).transpose(0, 2, 1)).reshape(NE * 128, 16),
        bd=np.ascontiguousarray(np.broadcast_to(f(b_down)[0][:, None, :], (NE, 128, DM))).reshape(NE * 128, DM),
    )
    shared.update(_consts(NB))
    cc = f(c)
    pp = np.asarray(positions, np.int32)
    in_maps = []
    for ci in range(ncores):
        m = dict(shared)
        m["x"] = np.ascontiguousarray(x[2 * ci:2 * ci + 2].reshape(2 * S, DM))
        m["cT"] = np.ascontiguousarray(cc[2 * ci:2 * ci + 2].reshape(2, 8, 128).transpose(2, 1, 0))
        m["pos"] = np.ascontiguousarray(pp[2 * ci:2 * ci + 2].reshape(2, NT, 128).transpose(0, 2, 1))
        in_maps.append(m)
    res = run_bass_kernel_spmd(nc, in_maps, core_ids=list(range(ncores)))
    out = np.stack([np.asarray(r["out"], np.float32).reshape(2, S, DM) for r in res.results], axis=0).reshape(Bsz, S, DM)
    if _dbg:
        return out, [{k: np.asarray(r["dbg_" + k]) for k in dbg_d} for r in res.results]
    return out
```

```python
import math
from contextlib import ExitStack
import numpy as np
import concourse.bass as bass
import concourse.mybir as mybir
from concourse.bass_utils import run_bass_kernel_spmd

F32 = mybir.dt.float32
BF16 = mybir.dt.bfloat16
I32 = mybir.dt.int32
AF = mybir.ActivationFunctionType
ALU = mybir.AluOpType
AX = mybir.AxisListType

import os
_PH = float(os.environ.get("KPH", "9"))
DM = 1024
NE = 32
BLK = 512
EPS = 1e-6
MAGIC = 12582912.0
C1 = 6.28125
C2 = 2.0 * math.pi - 6.28125


class Res:
    __slots__ = ("w", "rs")

    def __init__(self):
        self.w = None
        self.rs = {}


class Eng:
    def __init__(self, h, sid, is_pe=False):
        self.h = h
        self.sid = sid
        self.n = 0
        self.known = {}
        self.is_pe = is_pe
        self.ring = []
        self.rpos = 0


class Kern:
    def __init__(self, nc, es):
        self.nc = nc
        self.sems = []
        self.semvals = []
        self.es = es
        mk = lambda nm: self._newsem(nm)
        self.pe = Eng(nc.tensor, mk("s_pe"), True)
        self.dve = Eng(nc.vector, mk("s_dve"))
        self.act = Eng(nc.scalar, mk("s_act"))
        self.pool = Eng(nc.gpsimd, mk("s_pool"))
        self.sp = Eng(nc.sync, mk("s_sp"))
        for e, n in ((self.sp, 44), (self.act, 12), (self.pool, 36)):
            e.ring = [mk("d%d_%d" % (e.sid, i)) for i in range(n)]
        self.out_evs = []

    def _newsem(self, nm):
        s = self.es.enter_context(self.nc.semaphore(nm))
        self.sems.append(s)
        self.semvals.append(0)
        return len(self.sems) - 1

    def _wait(self, eng, evs):
        best = {}
        for sid, val in evs:
            if val > best.get(sid, 0):
                best[sid] = val
        for sid, val in best.items():
            if eng.is_pe and sid == eng.sid:
                continue
            if eng.known.get(sid, 0) >= val:
                continue
            eng.h.wait_ge(self.sems[sid], val)
            eng.known[sid] = val

    @staticmethod
    def _deps(reads, writes):
        evs = []
        for r in reads:
            if r.w is not None:
                evs.append(r.w)
        for r in writes:
            if r.w is not None:
                evs.append(r.w)
            evs.extend(r.rs.items())
        return evs

    @staticmethod
    def _mark(ev, reads, writes):
        for r in reads:
            if ev[1] > r.rs.get(ev[0], 0):
                r.rs[ev[0]] = ev[1]
        for r in writes:
            r.w = ev
            r.rs = {}

    def I(self, eng, fn, reads=(), writes=()):
        evs = self._deps(reads, writes)
        own = eng.sid
        evs2 = []
        raw = set()
        for r in reads:
            if r.w is not None:
                raw.add(r.w)
        for ev in evs:
            if ev[0] == own and ev not in raw:
                continue
            evs2.append(ev)
        self._wait(eng, evs2)
        ins = fn()
        eng.n += 1
        ins.then_inc(self.sems[eng.sid], 1)
        self._mark((eng.sid, eng.n), reads, writes)

    def dma(self, q, fn, reads=(), writes=(), is_out=False):
        evs = self._deps(reads, writes)
        sid = q.ring[q.rpos % len(q.ring)]
        q.rpos += 1
        prev = self.semvals[sid]
        if prev > 0:
            evs.append((sid, prev))
        self._wait(q, evs)
        ins = fn()
        self.semvals[sid] = prev + 16
        ins.then_inc(self.sems[sid], 16)
        ev = (sid, prev + 16)
        self._mark(ev, reads, writes)
        if is_out:
            self.out_evs.append(ev)
        return ev

    def barrier(self):
        evs = []
        for e in (self.sp, self.act, self.pool):
            evs += [(sid, self.semvals[sid]) for sid in e.ring if self.semvals[sid] > 0]
        for e in (self.pe, self.dve, self.act, self.pool):
            if e.n > 0:
                evs.append((e.sid, e.n))
        for e in (self.pe, self.dve, self.act, self.pool, self.sp):
            self._wait(e, [ev for ev in evs if ev[0] != e.sid])

    def finish(self):
        evs = list(self.out_evs)
        for e in (self.sp, self.act, self.pool):
            evs += [(sid, self.semvals[sid]) for sid in e.ring if self.semvals[sid] > 0]
        for e in (self.pe, self.dve, self.act, self.pool):
            if e.n > 0:
                evs.append((e.sid, e.n))
        self._wait(self.sp, evs)


class _Cut(Exception):
    pass


def build(S, dbg=()):
    ctx = {}
    try:
        return _build_inner(S, dbg, ctx)
    except _Cut:
        ctx["K"].finish()
        return ctx["nc"], ctx["dbg_d"]


def _build_inner(S, dbg, ctx):
    NT = S // 128
    NG = S // 512
    T = 2 * S
    NTT = T // 128
    NB = (T * 4) // BLK + NE
    nc = bass.Bass("TRN2", target_bir_lowering=False)
    es = ExitStack()
    K = Kern(nc, es)
    pe, dve, act, pool, sp = K.pe, K.dve, K.act, K.pool, K.sp
    dbg_d = {}

    def DI(name, shape, dt=F32):
        return nc.dram_tensor(name, list(shape), dt, kind="ExternalInput").ap()

    def DS(name, shape, dt=F32):
        if name in dbg:
            d = nc.dram_tensor("dbg_" + name, list(shape), dt, kind="ExternalOutput").ap()
            dbg_d[name] = d
            return d
        return nc.dram_tensor(name, list(shape), dt, kind="Internal").ap()

    x_d = DI("x", [T, DM])
    cT_d = DI("cT", [128, 8, 2])
    pos_d = DI("pos", [2, 128, NT], I32)
    wada_d = DI("w_ada", [DM, 6 * DM]).rearrange("(k p) f -> p k f", p=128)
    badaT_d = DI("b_adaT", [128, 48])
    gpre1_d = DI("g_pre1", [128, 8])
    gpost1_d = DI("g_post1", [128, 8])
    gpre2_d = DI("g_pre2", [128, 8])
    gpost2_d = DI("g_post2", [128, 8])
    wtm_d = DI("w_tm", [DM, 552]).rearrange("(k p) f -> p k f", p=128)
    wfm_d = DI("w_fm", [DM, 1664]).rearrange("(k p) f -> p k f", p=128)
    convw_d = DI("convw", [128, 8, 4])
    convb_d = DI("convb", [128, 8])
    dtb_d = DI("dtb", [128, 8])
    alog_d = DI("alog", [128, 8])
    dsk_d = DI("dsk", [128, 8])
    ssdn_d = DI("ssdn", [128, 512])
    mlan_d = DI("mlan", [128, 512])
    qan_d = DI("qan", [128, 3])
    kvan_d = DI("kvan", [128, 2])
    wq_d = DI("wq", [384, 768]).rearrange("(k p) f -> p k f", p=128)
    wkv_d = DI("wkv", [256, 1024]).rearrange("(k p) f -> p k f", p=128)
    wout_d = DI("wout", [DM, DM]).rearrange("(k p) f -> p k f", p=128)
    wr_d = DI("wr", [DM, NE]).rearrange("(k p) f -> p k f", p=128)
    br_d = DI("br", [128, NE])
    wgu_d = DI("wgu", [NE * DM, 2 * DM])
    wd_d = DI("wd", [NE * DM, DM])
    bgu_d = DI("bgu", [NE * 128, 16])
    bd_d = DI("bd", [NE * 128, DM])
    identf_d = DI("identf", [128, 128])
    U1_d = DI("U1", [128, 128])
    U2_d = DI("U2", [128, 128])
    Lst_d = DI("Lst", [128, 128])
    m01_d = DI("m01", [128, 128])
    invf_d = DI("invf", [128, 16])
    iotab_d = DI("iotab", [128, NB])
    pk_d = DI("pk", [128, 8])
    rm_d = DI("rm", [128, 2])
    x1s_d = DS("x1s", [T, DM])
    h2s_d = DS("h2s", [T, DM], BF16)
    xg_d = DS("xg", [NB * BLK, DM], BF16)
    yb_d = DS("yb", [NB * BLK, DM])
    out_d = nc.dram_tensor("out", [T, DM], F32, kind="ExternalOutput").ap()
    ctx.update(K=K, es=es, nc=nc, dbg_d=dbg_d)

    def cut(level):
        if _PH < level:
            raise _Cut()

    def SB(name, shape, dt=F32, stack=None):
        t = (stack or es).enter_context(nc.sbuf_tensor("sb_" + name, list(shape), dt))
        return t, Res()

    def PS(name, shape, dt=F32):
        return es.enter_context(nc.psum_tensor("ps_" + name, list(shape), dt))

    P2 = PS("P2", [128, 1024])
    rP2a, rP2b = Res(), Res()
    PBs = [PS("PB%d" % i, [128, 512]) for i in range(5)]
    rPB = [Res() for _ in range(5)]
    PT = PS("PT", [128, 1024], BF16)
    rPT = Res()

    def mm(out, lhsT, rhs, start, stop, rd, wr):
        K.I(pe, lambda: nc.tensor.matmul(out, lhsT, rhs, start=start, stop=stop), reads=rd, writes=wr)

    def tr(out, in_, ident, rd, wr):
        K.I(pe, lambda: nc.tensor.transpose(out, in_, ident), reads=rd, writes=wr)

    def actf(out, in_, func, rd, wr, bias=None, scale=None, accum_out=None):
        kw = {}
        if bias is not None:
            kw["bias"] = bias
        if scale is not None:
            kw["scale"] = scale
        if accum_out is not None:
            kw["accum_out"] = accum_out
        K.I(act, lambda: nc.scalar.activation(out, in_, func, **kw), reads=rd, writes=wr)

    def ts(eng, out, in0, s1, s2, op0, op1, rd, wr):
        h = eng.h
        if op1 is None:
            K.I(eng, lambda: h.tensor_scalar(out, in0, s1, None, op0), reads=rd, writes=wr)
        else:
            K.I(eng, lambda: h.tensor_scalar(out, in0, s1, s2, op0, op1), reads=rd, writes=wr)

    def tt(eng, out, in0, in1, op, rd, wr):
        h = eng.h
        K.I(eng, lambda: h.tensor_tensor(out, in0, in1, op), reads=rd, writes=wr)

    def stt(eng, out, in0, scalar, in1, op0, op1, rd, wr):
        h = eng.h
        K.I(eng, lambda: h.scalar_tensor_tensor(out, in0, scalar, in1, op0, op1), reads=rd, writes=wr)

    def cp(eng, out, in_, rd, wr):
        h = eng.h
        if eng is act:
            K.I(eng, lambda: nc.scalar.activation(out, in_, AF.Copy), reads=rd, writes=wr)
        else:
            K.I(eng, lambda: h.tensor_copy(out, in_), reads=rd, writes=wr)

    def ld(q, out, in_, wr, rd=()):
        K.dma(q, lambda: q.h.dma_start(out=out, in_=in_), reads=rd, writes=wr)

    def dump(name, ap, res, shape, dt=F32):
        if name not in dbg:
            return
        d = nc.dram_tensor("dbg_" + name, list(shape), dt, kind="ExternalOutput").ap()
        dbg_d[name] = d
        K.dma(sp, lambda: nc.sync.dma_start(out=d, in_=ap), reads=[res], writes=[Res()], is_out=True)

    identf, r_identf = SB("identf", [128, 128])
    identb, r_identb = SB("identb", [128, 128], BF16)
    onesf, r_onesf = SB("onesf", [128, 128])
    onesb, r_onesb = SB("onesb", [128, 128], BF16)
    U1, r_U1 = SB("U1", [128, 128])
    U2, r_U2 = SB("U2", [128, 128])
    Lst, r_Lst = SB("Lst", [128, 128])
    m01, r_m01 = SB("m01", [128, 128])
    invf, r_invf = SB("invf", [128, 16])
    ld(sp, identf[:], identf_d, [r_identf])
    ld(sp, U1[:], U1_d, [r_U1])
    ld(sp, U2[:], U2_d, [r_U2])
    ld(sp, Lst[:], Lst_d, [r_Lst])
    ld(sp, m01[:], m01_d, [r_m01])
    ld(sp, invf[:], invf_d, [r_invf])
    rm, r_rm = SB("rm", [128, 2])
    Mc, r_Mc = SB("Mc", [128, 2, 128])
    ld(sp, rm[:], rm_d, [r_rm])
    cp(dve, identb[:], identf[:], [r_identf], [r_identb])
    K.I(dve, lambda: nc.vector.memset(onesf[:], 1.0), writes=[r_onesf])
    K.I(dve, lambda: nc.vector.memset(onesb[:], 1.0), writes=[r_onesb])
    for c in range(2):
        ts(dve, Mc[:, c, :], onesf[:], rm[:, c:c + 1], None, ALU.mult, None, [r_onesf, r_rm], [r_Mc])
    epsb, r_epsb = SB("epsb", [128, 1])
    oneb, r_oneb = SB("oneb", [128, 1])
    K.I(dve, lambda: nc.vector.memset(epsb[:], EPS), writes=[r_epsb])
    K.I(dve, lambda: nc.vector.memset(oneb[:], 1.0), writes=[r_oneb])

    small = {}
    for nm, d, shp in (("gpre1", gpre1_d, [128, 8]), ("gpost1", gpost1_d, [128, 8]), ("gpre2", gpre2_d, [128, 8]),
                       ("gpost2", gpost2_d, [128, 8]), ("convw", convw_d, [128, 8, 4]), ("convb", convb_d, [128, 8]),
                       ("dtb", dtb_d, [128, 8]), ("alog", alog_d, [128, 8]), ("dsk", dsk_d, [128, 8]),
                       ("ssdn", ssdn_d, [128, 512]), ("mlan", mlan_d, [128, 512]), ("qan", qan_d, [128, 3]),
                       ("kvan", kvan_d, [128, 2]), ("br", br_d, [128, NE]), ("badaT", badaT_d, [128, 48]),
                       ("cT", cT_d, [128, 8, 2]), ("wr", wr_d, [128, 8, NE])):
        t, r = SB("c_" + nm, shp)
        ld(sp, t[:], d, [r])
        small[nm] = (t, r)

    aneg, r_aneg = SB("aneg", [128, 8])
    actf(aneg[:], small["alog"][0][:], AF.Exp, [small["alog"][1]], [r_aneg])
    ts(dve, aneg[:], aneg[:], -1.0, None, ALU.mult, None, [r_aneg], [r_aneg])

    modT, r_modT = SB("modT", [128, 48, 2])
    cact, r_cact = SB("cact", [128, 8, 2])
    actf(cact[:], small["cT"][0][:], AF.Silu, [small["cT"][1]], [r_cact])
    with ExitStack() as st0:
        wst = [SB("wadast%d" % i, [128, 8, 512], F32, st0) for i in range(2)]
        for blk in range(12):
            w_t, w_r = wst[blk % 2]
            ld(sp, w_t[:], wada_d[:, :, blk * 512:(blk + 1) * 512], [w_r])
            for j in range(4):
                fc = blk * 4 + j
                pb, rpb = PBs[fc % 2], rPB[fc % 2]
                for k in range(8):
                    mm(pb[:, 0:2], w_t[:, k, j * 128:(j + 1) * 128], cact[:, k, :], k == 0, k == 7,
                       [w_r, r_cact], [rpb])
                ts(dve, modT[:, fc, :], pb[:, 0:2], small["badaT"][0][:, fc:fc + 1], None, ALU.add, None,
                   [rpb, small["badaT"][1]], [r_modT])
    K.barrier()
    a1T, r_a1T = SB("a1T", [128, 2, 8])
    sh1T, r_sh1T = SB("sh1T", [128, 2, 8])
    vtmp, r_vtmp = SB("vtmp", [128, 8])
    btmp = [SB("btmp%d" % i, [128, 128]) for i in range(2)]
    bcn = [0]

    def bcast_rows(vT_ap, vres, dst_ap, dres):
        for half in range(2):
            pb, rpb = PBs[2 + half], rPB[2 + half]
            for kk in range(4):
                k = half * 4 + kk
                bt, rbt = btmp[bcn[0] % 2]
                bcn[0] += 1
                ts(dve, bt[:], onesf[:], vT_ap[:, k:k + 1], None, ALU.mult, None, [r_onesf, vres], [rbt])
                mm(pb[:, kk * 128:(kk + 1) * 128], bt[:], identf[:], True, True, [rbt, r_identf], [rpb])
            cp(act, dst_ap[:, half * 512:(half + 1) * 512], pb[:], [rpb], [dres])

    for b in range(2):
        ts(dve, vtmp[:], modT[:, 8:16, b], 1.0, None, ALU.add, None, [r_modT], [r_vtmp])
        tt(dve, a1T[:, b, :], vtmp[:], small["gpre1"][0][:], ALU.mult, [r_vtmp, small["gpre1"][1]], [r_a1T])
        cp(dve, sh1T[:, b, :], modT[:, 0:8, b], [r_modT], [r_sh1T])

    lg_all, r_lg = SB("lg_all", [128, NTT, NE])
    v8_all, r_v8 = SB("v8_all", [128, NTT, 8])
    gate_all, r_gate = SB("gate_all", [128, NTT, 4])

    castn = [0]

    def cast_any(out, in_, rd, wr):
        e = (dve, pool, act)[castn[0] % 3]
        castn[0] += 1
        cp(e, out, in_, rd, wr)

    def rstd_from(ss_ap, ssres, out_ap, ores, n, eng=dve):
        actf(out_ap, ss_ap, AF.Sqrt, [ssres], [ores], bias=epsb[:, 0:1], scale=1.0 / n)
        K.I(eng, lambda: eng.h.reciprocal(out_ap, out_ap), reads=[ores], writes=[ores])

    class Bag:
        def __init__(self):
            self.d = {}

        def add(self, ev):
            if ev[1] > self.d.get(ev[0], 0):
                self.d[ev[0]] = ev[1]

        def evs(self):
            return list(self.d.items())

    kn_s = DS("kn_s", [2, 128, 4 * S], BF16)
    kr_s = DS("kr_s", [2, 128, S], BF16)
    v_s = DS("v_s", [2, 128, NT * 8 * 65], BF16)
    q_s = DS("q_s", [2 * NT, 128, 2048], BF16)
    mix_s = DS("mix_s", [T, DM], BF16)
    bagA = [Bag(), Bag()]
    bagT = [Bag(), Bag()]

    def st(q, out, in_, rd, bag):
        ev = K.dma(q, lambda: q.h.dma_start(out=out, in_=in_), reads=rd, writes=[])
        bag.add(ev)

    dump("modT", modT[:], r_modT, [128, 48, 2])
    cut(0.2)
    with ExitStack() as p1:
        wtm, r_wtm = SB("wtm", [128, 8, 552], BF16, p1)
        wfm, r_wfm = SB("wfm", [128, 8, 1664], BF16, p1)
        wq, r_wq = SB("wq", [128, 3, 768], BF16, p1)
        wkv, r_wkv = SB("wkv", [128, 2, 1024], BF16, p1)
        with ExitStack() as pst:
            stg = [SB("stg%d" % i, [128, 1664], F32, pst) for i in range(2)]
            sn = 0
            for (dst, rdst, src, nk, ncol) in ((wtm, r_wtm, wtm_d, 8, 552), (wfm, r_wfm, wfm_d, 8, 1664),
                                               (wq, r_wq, wq_d, 3, 768), (wkv, r_wkv, wkv_d, 2, 1024)):
                for k in range(nk):
                    s_t, s_r = stg[sn % 2]
                    sn += 1
                    ld(sp, s_t[:, 0:ncol], src[:, k, :], [s_r])
                    cast_any(dst[:, k, :], s_t[:, 0:ncol], [s_r], [rdst])
        K.barrier()
        cut(0.25)
        Sst, r_Sst = SB("Sst", [128, 8, 64], F32, p1)
        Sbf, r_Sbf = SB("Sbf", [128, 8, 64], BF16, p1)
        cosT, r_cos = SB("cosT", [128, NT, 16], F32, p1)
        sinT, r_sin = SB("sinT", [128, NT, 16], F32, p1)
        posi, r_posi = SB("posi", [128, NT], I32, p1)
        posf, r_posf = SB("posf", [128, NT], F32, p1)
        ang, r_ang = SB("ang", [128, NT, 16], F32, p1)
        ang2, r_ang2 = SB("ang2", [128, NT, 16], F32, p1)
        xt = [SB("xt%d" % i, [128, DM], F32, p1) for i in range(2)]
        junk, r_junk = SB("junk", [128, DM], BF16, p1)
        xn, r_xn = SB("xn", [128, DM], BF16, p1)
        st1 = [SB("st1_%d" % i, [128, 8], F32, p1) for i in range(4)]
        hT, r_hT = SB("hT", [128, 8, 512], BF16, p1)
        rw, r_rw = SB("raw", [128, 8, 515], F32, p1)
        halo, r_halo = SB("halo", [128, 8, 3], F32, p1)
        cacc, r_cacc = SB("cacc", [128, 512], F32, p1)
        xact, r_xact = SB("xact", [128, 8, 512], BF16, p1)
        qag, r_qag = SB("qag", [128, 3, 512], F32, p1)
        kvag, r_kvag = SB("kvag", [128, 2, 512], F32, p1)
        sq5, r_sq5 = SB("sq5", [128, 5, 512], BF16, p1)
        rsb, r_rsb = SB("rsb", [128, 2, 512], F32, p1)
        qaTn, r_qaTn = SB("qaTn", [128, 3, 512], BF16, p1)
        kvaTn, r_kvaTn = SB("kvaTn", [128, 2, 512], BF16, p1)
        kng, r_kng = SB("kng", [128, 4, 512], BF16, p1)
        zs, r_zs = SB("zs", [128, 512], F32, p1)
        dtt, r_dtt = SB("dtt", [128, 8], F32, p1)
        dta, r_dta = SB("dta", [128, 8], F32, p1)
        krp, r_krp = SB("krp", [128, 4, 32], BF16, p1)
        K.I(pool, lambda: nc.gpsimd.memset(krp[:], 0.0), writes=[r_krp])
        krT, r_krT = SB("krT", [128, 128], BF16, p1)
        rtmp = [SB("rtmp%d" % i, [128, 8, 16], F32, p1) for i in range(4)]
        qn_tm, r_qn = SB("qn_tm", [128, 512], BF16, p1)
        qr_pad, r_qrp = SB("qr_pad", [128, 4, 2, 64], BF16, p1)
        K.I(pool, lambda: nc.gpsimd.memset(qr_pad[:], 0.0), writes=[r_qrp])
        QT, r_QT = SB("QT", [128, 2048], BF16, p1)
        K.I(pool, lambda: nc.gpsimd.memset(QT[:], 0.0), writes=[r_QT])
        Vt, r_Vt = SB("Vt", [128, 8, 65], BF16, p1)
        K.I(pool, lambda: nc.gpsimd.memset(Vt[:], 1.0), writes=[r_Vt])
        xs_tm, r_xs = SB("xs_tm", [128, 8, 64], BF16, p1)
        B_tm, r_Btm = SB("B_tm", [128, 256], BF16, p1)
        xdt, r_xdt = SB("xdt", [128, 8, 64], BF16, p1)
        xdtd, r_xdtd = SB("xdtd", [128, 2, 8, 64], BF16, p1)
        ecsm, r_ecsm = SB("ecsm", [128, 2, 8], F32, p1)
        dtem, r_dtem = SB("dtem", [128, 2, 8], F32, p1)
        lseg, r_lseg = SB("lseg", [128, 8, 128], F32, p1)
        dec, r_dec = SB("dec", [128, 8, 128], F32, p1)
        cbm, r_cbm = SB("cbm", [128, 2, 128], F32, p1)
        MT, r_MT = SB("MT", [128, 8, 128], BF16, p1)
        ecs, r_ecs = SB("ecs", [128, 8], F32, p1)
        dte, r_dte = SB("dte", [128, 8], F32, p1)
        cdB, r_cdB = SB("cdB", [128, 2, 8], F32, p1)
        yd, r_yd = SB("yd", [128, 8, 64], F32, p1)
        yt, r_yt = SB("yt", [128, 8, 64], F32, p1)
        yt2, r_yt2 = SB("yt2", [128, 8, 64], F32, p1)
        mixs, r_mixs = SB("mixs", [128, 512], BF16, p1)

        pbn = [0]

        def next_pb():
            i = pbn[0] % 2
            pbn[0] += 1
            return PBs[i], rPB[i]

        for b in range(2):
            bag = bagA[b]
            ld(sp, posi[:], pos_d[b], [r_posi])
            cp(dve, posf[:], posi[:], [r_posi], [r_posf])
            tt(dve, ang[:], posf[:].unsqueeze(2).broadcast_to((128, NT, 16)),
               invf[:].unsqueeze(1).broadcast_to((128, NT, 16)), ALU.mult, [r_posf, r_invf], [r_ang])
            ts(dve, ang2[:], ang[:], 1.0 / (2.0 * math.pi), MAGIC, ALU.mult, ALU.add, [r_ang], [r_ang2])
            ts(dve, ang2[:], ang2[:], -MAGIC, None, ALU.add, None, [r_ang2], [r_ang2])
            stt(dve, ang[:], ang2[:], -C1, ang[:], ALU.mult, ALU.add, [r_ang2, r_ang], [r_ang])
            stt(dve, ang[:], ang2[:], -C2, ang[:], ALU.mult, ALU.add, [r_ang2, r_ang], [r_ang])
            ts(dve, ang[:], ang[:], 3.14159, -3.14159, ALU.min, ALU.max, [r_ang], [r_ang])
            actf(sinT[:], ang[:], AF.Sin, [r_ang], [r_sin])
            ts(dve, ang2[:], ang[:], -1.0, None, ALU.mult, None, [r_ang], [r_ang2])
            tt(dve, ang2[:], ang2[:], ang[:], ALU.max, [r_ang2, r_ang], [r_ang2])
            ts(dve, ang2[:], ang2[:], -1.0, math.pi / 2.0, ALU.mult, ALU.add, [r_ang2], [r_ang2])
            actf(cosT[:], ang2[:], AF.Sin, [r_ang2], [r_cos])
            cut(0.30)
            K.I(dve, lambda: nc.vector.memset(Sst[:], 0.0), writes=[r_Sst])
            K.I(pool, lambda: nc.gpsimd.memset(Sbf[:], 0.0), writes=[r_Sbf])
            K.I(pool, lambda: nc.gpsimd.memset(halo[:], 0.0), writes=[r_halo])

            for gi in range(NG):
                for i in range(4):
                    ti = gi * 4 + i
                    gt = b * NT + ti
                    x_t, x_r = xt[gt % 2]
                    s1, r_s1 = st1[i]
                    ld(sp, x_t[:], x_d[gt * 128:(gt + 1) * 128, :], [x_r])
                    actf(junk[:], x_t[:], AF.Square, [x_r], [r_junk, r_s1], accum_out=s1[:, 0:1])
                    rstd_from(s1[:, 0:1], r_s1, s1[:, 1:2], r_s1, DM)
                    ts(dve, xn[:], x_t[:], s1[:, 1:2], None, ALU.mult, None, [x_r, r_s1], [r_xn])
                    for k in range(8):
                        tr(PT[:, k * 128:(k + 1) * 128], xn[:, k * 128:(k + 1) * 128], identb[:], [r_xn, r_identb], [rPT])
                    for k in range(8):
                        actf(hT[:, k, i * 128:(i + 1) * 128], PT[:, k * 128:(k + 1) * 128], AF.Identity,
                             [rPT, r_a1T, r_sh1T], [r_hT], bias=sh1T[:, b, k:k + 1], scale=a1T[:, b, k:k + 1])
                cut(0.32)
                cp(pool, rw[:, :, 0:3], halo[:], [r_halo], [r_rw])
                for mc in range(13):
                    pb, rpb = next_pb()
                    for k in range(8):
                        mm(pb[:], wfm[:, k, mc * 128:(mc + 1) * 128], hT[:, k, :], k == 0, k == 7, [r_wfm, r_hT], [rpb])
                    if mc < 8:
                        cp(act, rw[:, mc, 3:515], pb[:], [rpb], [r_rw])
                    elif mc < 11:
                        c = mc - 8
                        actf(qag[:, c, :], pb[:], AF.Copy, [rpb, small["qan"][1]], [r_qag], scale=small["qan"][0][:, c:c + 1])
                        actf(sq5[:, c, :], pb[:], AF.Square, [rpb], [r_sq5])
                    else:
                        c = mc - 11
                        actf(kvag[:, c, :], pb[:], AF.Copy, [rpb, small["kvan"][1]], [r_kvag], scale=small["kvan"][0][:, c:c + 1])
                        actf(sq5[:, 3 + c, :], pb[:], AF.Square, [rpb], [r_sq5])
                cp(pool, halo[:], rw[:, :, 512:515], [r_rw], [r_halo])
                cut(0.34)
                cw, r_cw = small["convw"]
                cb_, r_cb = small["convb"]
                for mc in range(8):
                    ts(dve, cacc[:], rw[:, mc, 0:512], cw[:, mc, 0:1], cb_[:, mc:mc + 1], ALU.mult, ALU.add,
                       [r_rw, r_cw, r_cb], [r_cacc])
                    for kk in range(1, 4):
                        stt(dve, cacc[:], rw[:, mc, kk:kk + 512], cw[:, mc, kk:kk + 1], cacc[:], ALU.mult, ALU.add,
                            [r_rw, r_cw, r_cacc], [r_cacc])
                    actf(xact[:, mc, :], cacc[:], AF.Silu, [r_cacc], [r_xact])
                cut(0.36)
                for (c0, ncn, n, slot) in ((0, 3, 384, 0), (3, 2, 256, 1)):
                    pb, rpb = PBs[2], rPB[2]
                    for c in range(ncn):
                        mm(pb[:], onesb[:], sq5[:, c0 + c, :], c == 0, c == ncn - 1, [r_onesb, r_sq5], [rpb])
                    rstd_from(pb[:], rpb, rsb[:, slot, :], r_rsb, n)
                for c in range(3):
                    tt(dve, qaTn[:, c, :], qag[:, c, :], rsb[:, 0, :], ALU.mult, [r_qag, r_rsb], [r_qaTn])
                for c in range(2):
                    tt(pool, kvaTn[:, c, :], kvag[:, c, :], rsb[:, 1, :], ALU.mult, [r_kvag, r_rsb], [r_kvaTn])
                cut(0.38)
                for j in range(4):
                    pb, rpb = next_pb()
                    for c in range(2):
                        mm(pb[:], wkv[:, c, j * 128:(j + 1) * 128], kvaTn[:, c, :], c == 0, c == 1, [r_wkv, r_kvaTn], [rpb])
                    cp(act, kng[:, j, :], pb[:], [rpb], [r_kng])
                st(sp, kn_s[b].rearrange("p (j s) -> p j s", j=4)[:, :, gi * 512:(gi + 1) * 512], kng[:], [r_kng], bag)
                cut(0.40)

                for i in range(4):
                    ti = gi * 4 + i
                    gt = b * NT + ti
                    cs_ = slice(i * 128, (i + 1) * 128)
                    s1, r_s1 = st1[i]
                    for k in range(8):
                        mm(P2[:, 0:512], hT[:, k, cs_], wtm[:, k, 0:512], k == 0, k == 7, [r_hT, r_wtm], [rP2a])
                    for k in range(8):
                        mm(P2[:, 512:552], hT[:, k, cs_], wtm[:, k, 512:552], k == 0, k == 7, [r_hT, r_wtm], [rP2b])
                    actf(zs[:], P2[:, 0:512], AF.Silu, [rP2a], [r_zs])
                    tt(dve, dtt[:], P2[:, 512:520], small["dtb"][0][:], ALU.add, [rP2b, small["dtb"][1]], [r_dtt])
                    actf(dtt[:], dtt[:], AF.Exp, [r_dtt], [r_dtt])
                    actf(dtt[:], dtt[:], AF.Ln, [r_dtt], [r_dtt], bias=oneb[:, 0:1])
                    tt(dve, dta[:], dtt[:], aneg[:], ALU.mult, [r_dtt, r_aneg], [r_dta])
                    cut(0.42)
                    cs16 = cosT[:, ti, :]
                    sn16 = sinT[:, ti, :]
                    k1 = P2[:, 520:536]
                    k2 = P2[:, 536:552]
                    t0_, t1_, t2_, t3_ = [rtmp[q][0][:, 0, :] for q in range(4)]
                    rr = [rtmp[q][1] for q in range(4)]
                    tt(dve, t0_, k1, cs16, ALU.mult, [rP2b, r_cos], [rr[0]])
                    tt(dve, t1_, k2, sn16, ALU.mult, [rP2b, r_sin], [rr[1]])
                    tt(dve, t2_, k2, cs16, ALU.mult, [rP2b, r_cos], [rr[2]])
                    tt(dve, t3_, k1, sn16, ALU.mult, [rP2b, r_sin], [rr[3]])
                    tt(dve, krp[:, 0, 0:16], t0_, t1_, ALU.subtract, [rr[0], rr[1]], [r_krp])
                    tt(dve, krp[:, 0, 16:32], t2_, t3_, ALU.add, [rr[2], rr[3]], [r_krp])
                    cp(pool, krp[:, 2, :], krp[:, 0, :], [r_krp], [r_krp])
                    tr(PT[:, 0:128], krp[:].rearrange("p a c -> p (a c)"), identb[:], [r_krp, r_identb], [rPT])
                    cp(act, krT[:], PT[:, 0:128], [rPT], [r_krT])
                    st(sp, kr_s[b][:, ti * 128:(ti + 1) * 128], krT[:], [r_krT], bag)
                    cut(0.44)
                    for c in range(3):
                        mm(P2[:, 0:512], qaTn[:, c, cs_], wq[:, c, 0:512], c == 0, c == 2, [r_qaTn, r_wq], [rP2a])
                    for c in range(3):
                        mm(P2[:, 512:768], qaTn[:, c, cs_], wq[:, c, 512:768], c == 0, c == 2, [r_qaTn, r_wq], [rP2b])
                    cp(act, qn_tm[:], P2[:, 0:512], [rP2a], [r_qn])
                    qr = P2[:, 512:768].rearrange("p (h c) -> p h c", c=32)
                    q1 = qr[:, :, 0:16]
                    q2 = qr[:, :, 16:32]
                    cb8 = cs16.unsqueeze(1).broadcast_to((128, 8, 16))
                    sb8 = sn16.unsqueeze(1).broadcast_to((128, 8, 16))
                    T0, T1, T2, T3 = [rtmp[q][0][:] for q in range(4)]
                    tt(dve, T0, q1, cb8, ALU.mult, [rP2b, r_cos], [rr[0]])
                    tt(dve, T1, q2, sb8, ALU.mult, [rP2b, r_sin], [rr[1]])
                    tt(dve, T2, q2, cb8, ALU.mult, [rP2b, r_cos], [rr[2]])
                    tt(dve, T3, q1, sb8, ALU.mult, [rP2b, r_sin], [rr[3]])
                    qrv = qr_pad[:].rearrange("p j s c -> p s j c")
                    for s_ in range(2):
                        tt(dve, qrv[:, s_, :, 0:16], rtmp[0][0][:, s_ * 4:(s_ + 1) * 4, :], rtmp[1][0][:, s_ * 4:(s_ + 1) * 4, :],
                           ALU.subtract, [rr[0], rr[1]], [r_qrp])
                        tt(dve, qrv[:, s_, :, 16:32], rtmp[2][0][:, s_ * 4:(s_ + 1) * 4, :], rtmp[3][0][:, s_ * 4:(s_ + 1) * 4, :],
                           ALU.add, [rr[2], rr[3]], [r_qrp])
                    for j in range(4):
                        tr(PT[:, j * 128:(j + 1) * 128], qn_tm[:, j * 128:(j + 1) * 128], identb[:], [r_qn, r_identb], [rPT])
                    for j in range(4):
                        tr(PT[:, 512 + j * 128:512 + (j + 1) * 128], qr_pad[:, j, :, :].rearrange("p s c -> p (s c)"),
                           identb[:], [r_qrp, r_identb], [rPT])
                    cp(act, QT[0:64, 0:512], PT[0:64, 0:512], [rPT], [r_QT])
                    cp(act, QT[64:128, 512:1024], PT[64:128, 0:512], [rPT], [r_QT])
                    cp(act, QT[0:64, 1024:1536], PT[0:64, 512:1024], [rPT], [r_QT])
                    cp(act, QT[64:128, 1536:2048], PT[64:128, 512:1024], [rPT], [r_QT])
                    st(sp, q_s[gt], QT[:], [r_QT], bag)
                    cut(0.46)
                    pb, rpb = PBs[2], rPB[2]
                    for c in range(2):
                        mm(pb[:], kvaTn[:, c, cs_], wkv[:, c, 512:1024], c == 0, c == 1, [r_kvaTn, r_wkv], [rpb])
                    cp(act, Vt[:, :, 0:64], pb[:].rearrange("p (h c) -> p h c", c=64), [rpb], [r_Vt])
                    cut(0.475)
                    st(sp, v_s[b][:, ti * 520:(ti + 1) * 520], Vt[:].rearrange("p h c -> p (h c)"), [r_Vt], bag)
                    cut(0.48)

                    for j in range(4):
                        tr(PT[:, j * 128:(j + 1) * 128], xact[:, j, cs_], identb[:], [r_xact, r_identb], [rPT])
                    for j in range(2):
                        tr(PT[:, 512 + j * 128:512 + (j + 1) * 128], xact[:, 4 + j, cs_], identb[:], [r_xact, r_identb], [rPT])
                    cp(act, xs_tm[:].rearrange("p h c -> p (h c)"), PT[:, 0:512], [rPT], [r_xs])
                    cp(act, B_tm[:], PT[:, 512:768], [rPT], [r_Btm])
                    cut(0.495)
                    dt_b = dtt[:].unsqueeze(2).broadcast_to((128, 8, 64))
                    tt(dve, xdt[:], xs_tm[:], dt_b, ALU.mult, [r_xs, r_dtt], [r_xdt])
                    cut(0.50)
                    tt(dve, lseg[:], U1[:].unsqueeze(1).broadcast_to((128, 8, 128)),
                       dta[:].unsqueeze(2).broadcast_to((128, 8, 128)), ALU.mult, [r_U1, r_dta], [r_lseg])
                    for h in range(8):
                        rp = rP2a if h < 4 else rP2b
                        mm(P2[:, h * 128:(h + 1) * 128], lseg[:, h, :], U2[:], True, True, [r_lseg, r_U2], [rp])
                    actf(dec[:].rearrange("p h l -> p (h l)"), P2[:], AF.Exp, [rP2a, rP2b], [r_dec])
                    cut(0.51)
                    pb3, rpb3 = PBs[3], rPB[3]
                    for g in range(2):
                        mm(pb3[:, g * 128:(g + 1) * 128], xact[:, 4 + g, cs_], xact[:, 6 + g, cs_], True, True, [r_xact], [rpb3])
                    cut(0.516)
                    tt(dve, cbm[:], pb3[:, 0:256].rearrange("p (g l) -> p g l", g=2),
                       m01[:].unsqueeze(1).broadcast_to((128, 2, 128)), ALU.mult, [rpb3, r_m01], [r_cbm])
                    cut(0.518)
                    for h in range(8):
                        tt(dve, MT[:, h, :], dec[:, h, :], cbm[:, h // 4, :], ALU.mult, [r_dec, r_cbm], [r_MT])
                    cut(0.52)
                    pb4, rpb4 = PBs[4], rPB[4]
                    mm(pb4[:, 0:8], U2[:], dta[:], True, True, [r_U2, r_dta], [rpb4])
                    mm(pb4[:, 8:16], U1[:], dta[:], True, True, [r_U1, r_dta], [rpb4])
                    mm(pb4[:, 16:24], Mc[:, 0, :], dta[:], True, True, [r_Mc, r_dta], [rpb4])
                    mm(pb4[:, 24:32], Mc[:, 1, :], dta[:], True, True, [r_Mc, r_dta], [rpb4])
                    actf(ecs[:], pb4[:, 0:8], AF.Exp, [rpb4], [r_ecs])
                    actf(dte[:], pb4[:, 8:16], AF.Exp, [rpb4], [r_dte])
                    actf(cdB[:].rearrange("p c h -> p (c h)"), pb4[:, 16:32], AF.Exp, [rpb4], [r_cdB])
                    for c in range(2):
                        ts(dve, ecsm[:, c, :], ecs[:], rm[:, c:c + 1], None, ALU.mult, None, [r_ecs, r_rm], [r_ecsm])
                        ts(dve, dtem[:, c, :], dte[:], rm[:, c:c + 1], None, ALU.mult, None, [r_dte, r_rm], [r_dtem])
                        tt(dve, xdtd[:, c, :, :], xdt[:], dtem[:, c, :].unsqueeze(2).broadcast_to((128, 8, 64)), ALU.mult,
                           [r_xdt, r_dtem], [r_xdtd])
                    cut(0.53)
                    pb0, rpb0 = PBs[0], rPB[0]
                    for h in range(8):
                        mm(pb0[:, h * 64:(h + 1) * 64], MT[:, h, :], xdt[:, h, :], True, True, [r_MT, r_xdt], [rpb0])
                    cp(act, yd[:].rearrange("p h c -> p (h c)"), pb0[:], [rpb0], [r_yd])
                    cut(0.54)
                    pb2, rpb2 = PBs[2], rPB[2]
                    pbY = (PBs[1], PBs[3])
                    rpbY = (rPB[1], rPB[3])
                    for c in range(2):
                        for g in range(2):
                            mm(pbY[c][:, g * 256:(g + 1) * 256], xact[:, 6 + g, cs_],
                               Sbf[:, g * 4:(g + 1) * 4, :].rearrange("p h c -> p (h c)"), True, True, [r_xact, r_Sbf], [rpbY[c]])
                        for g in range(2):
                            mm(pb2[:, g * 256:(g + 1) * 256], B_tm[:, g * 128:(g + 1) * 128],
                               xdtd[:, c, g * 4:(g + 1) * 4, :].rearrange("p h c -> p (h c)"), True, True, [r_Btm, r_xdtd], [rpb2])
                        tt(dve, Sst[:], Sst[:], cdB[:, c, :].unsqueeze(2).broadcast_to((128, 8, 64)), ALU.mult, [r_Sst, r_cdB], [r_Sst])
                        tt(dve, Sst[:], Sst[:], pb2[:].rearrange("p (h c) -> p h c", c=64), ALU.add, [r_Sst, rpb2], [r_Sst])
                        cp(act, Sbf[:], Sst[:], [r_Sst], [r_Sbf])
                    cut(0.55)
                    tt(dve, yt[:], pbY[0][:].rearrange("p (h c) -> p h c", c=64), ecsm[:, 0, :].unsqueeze(2).broadcast_to((128, 8, 64)),
                       ALU.mult, [rpbY[0], r_ecsm], [r_yt])
                    tt(dve, yt2[:], pbY[1][:].rearrange("p (h c) -> p h c", c=64), ecsm[:, 1, :].unsqueeze(2).broadcast_to((128, 8, 64)),
                       ALU.mult, [rpbY[1], r_ecsm], [r_yt2])
                    tt(pool, yt[:], yt[:], yt2[:], ALU.add, [r_yt, r_yt2], [r_yt])
                    tt(pool, yt[:], yt[:], yd[:], ALU.add, [r_yt, r_yd], [r_yt])
                    tt(dve, yt2[:], xs_tm[:], small["dsk"][0][:].unsqueeze(2).broadcast_to((128, 8, 64)), ALU.mult,
                       [r_xs, small["dsk"][1]], [r_yt2])
                    tt(pool, yt[:], yt[:], yt2[:], ALU.add, [r_yt, r_yt2], [r_yt])
                    tt(dve, yt[:].rearrange("p h c -> p (h c)"), yt[:].rearrange("p h c -> p (h c)"), zs[:], ALU.mult,
                       [r_yt, r_zs], [r_yt])
                    for g in range(2):
                        actf(junk[:, g * 256:(g + 1) * 256], yt[:, g * 4:(g + 1) * 4, :].rearrange("p h c -> p (h c)"), AF.Square,
                             [r_yt], [r_junk, r_s1], accum_out=s1[:, 2 + g:3 + g])
                    rstd_from(s1[:, 2:4], r_s1, s1[:, 2:4], r_s1, 256)
                    for g in range(2):
                        stt(dve, mixs[:, g * 256:(g + 1) * 256], yt[:, g * 4:(g + 1) * 4, :].rearrange("p h c -> p (h c)"),
                            s1[:, 2 + g:3 + g], small["ssdn"][0][:, g * 256:(g + 1) * 256], ALU.mult, ALU.mult,
                            [r_yt, r_s1, small["ssdn"][1]], [r_mixs])
                    st(sp, mix_s[gt * 128:(gt + 1) * 128, 0:512], mixs[:], [r_mixs], bag)
                    cut(0.56)

    K.barrier()
    cut(0.6)
    with ExitStack() as pt_:
        KnT, r_KnT = SB("KnT", [128, 4, S], BF16, pt_)
        KrT, r_KrT = SB("KrT", [128, S], BF16, pt_)
        Vst, r_Vst = SB("Vst", [128, NT, 8, 65], BF16, pt_)
        Qb = [SB("Qb%d" % i, [128, 16, 128], BF16, pt_) for i in range(2)]
        PTs = [SB("PTs%d" % i, [128, 4, 128], BF16, pt_) for i in range(5)]
        rec, r_rec = SB("rec", [128, 8], F32, pt_)
        osb, r_osb = SB("osb", [128, 8, 64], F32, pt_)
        junk2, r_junk2 = SB("junk2", [128, 512], BF16, pt_)
        sA, r_sA = SB("sA", [128, 2], F32, pt_)
        mixm = [SB("mixm%d" % i, [128, 512], BF16, pt_) for i in range(2)]
        sc = 1.0 / math.sqrt(96.0)
        for b in range(2):
            K._wait(sp, bagA[b].evs())
            ld(sp, KnT[:].rearrange("p j s -> p (j s)"), kn_s[b], [r_KnT])
            ld(sp, KrT[:], kr_s[b], [r_KrT])
            ld(sp, Vst[:].rearrange("p t h c -> p (t h c)"), v_s[b], [r_Vst])
            for ti in range(NT):
                gt = b * NT + ti
                q_t, q_r = Qb[ti % 2]
                ld(sp, q_t[:].rearrange("p a q -> p (a q)"), q_s[gt], [q_r])
                nkt = ti + 1
                pbO = (P2[:, 0:512], P2[:, 512:1024])
                rpbO = (rP2a, rP2b)
                groups = [(h, k0, min(4, nkt - k0)) for h in range(8) for k0 in range(0, nkt, 4)]
                DEP = 3

                def emit_qk(gi):
                    h, k0, nk = groups[gi]
                    j = h % 4
                    hs = h // 4
                    pbs, rpbs = PBs[gi % 4], rPB[gi % 4]
                    for kk in range(nk):
                        kt = k0 + kk
                        kc = slice(kt * 128, (kt + 1) * 128)
                        mm(pbs[:, kk * 128:(kk + 1) * 128], KnT[:, j, kc], q_t[:, hs * 4 + j, :], True, False, [r_KnT, q_r], [rpbs])
                        mm(pbs[:, kk * 128:(kk + 1) * 128], KrT[:, kc], q_t[:, 8 + hs * 4 + j, :], False, True, [r_KrT, q_r], [rpbs])

                def emit_pv(gi):
                    h, k0, nk = groups[gi]
                    po, rpo = pbO[h // 4], rpbO[h // 4]
                    ocol = (h % 4) * 65
                    pbs, rpbs = PBs[gi % 4], rPB[gi % 4]
                    pts, rpts = PTs[gi % 5]
                    actf(pts[:, 0:nk, :].rearrange("p a q -> p (a q)"), pbs[:, 0:nk * 128], AF.Exp, [rpbs], [rpts], scale=sc)
                    if k0 + nk == nkt:
                        K.I(dve, lambda: nc.vector.memset(pts[64:128, nk - 1, 0:64], 0.0), writes=[rpts])
                    for kk in range(nk):
                        kt = k0 + kk
                        mm(po[:, ocol:ocol + 65], pts[:, kk, :], Vst[:, kt, h, :], kt == 0, kt == nkt - 1, [rpts, r_Vst], [rpo])

                for gi in range(min(DEP, len(groups))):
                    emit_qk(gi)
                for gi in range(len(groups)):
                    if gi + DEP < len(groups):
                        emit_qk(gi + DEP)
                    emit_pv(gi)
                for hh in range(2):
                    ov = pbO[hh][:, 0:260].rearrange("p (h c) -> p h c", c=65)
                    K.I(dve, lambda: nc.vector.reciprocal(rec[:, hh * 4:(hh + 1) * 4], ov[:, :, 64]), reads=[rpbO[hh]], writes=[r_rec])
                    tt(dve, osb[:, hh * 4:(hh + 1) * 4, :], ov[:, :, 0:64],
                       rec[:, hh * 4:(hh + 1) * 4].unsqueeze(2).broadcast_to((128, 4, 64)), ALU.mult, [rpbO[hh], r_rec], [r_osb])
                if gt == 1:
                    dump("osb", osb[:], r_osb, [128, 8, 64])
                    dump("rec", rec[:], r_rec, [128, 8])
                    dump("pts", PTs[0][0][:], PTs[0][1], [128, 4, 128], BF16)
                    dump("qb", q_t[:], q_r, [128, 16, 128], BF16)
                    dump("KrT", KrT[:], r_KrT, [128, S], BF16)
                    dump("KnT", KnT[:], r_KnT, [128, 4, S], BF16)
                    dump("Vst", Vst[:], r_Vst, [128, NT, 8, 65], BF16)
                actf(junk2[:], osb[:].rearrange("p h c -> p (h c)"), AF.Square, [r_osb], [r_junk2, r_sA], accum_out=sA[:, 0:1])
                rstd_from(sA[:, 0:1], r_sA, sA[:, 1:2], r_sA, 512)
                m_t, m_r = mixm[ti % 2]
                stt(dve, m_t[:], osb[:].rearrange("p h c -> p (h c)"), sA[:, 1:2], small["mlan"][0][:],
                    ALU.mult, ALU.mult, [r_osb, r_sA, small["mlan"][1]], [m_r])
                st(sp, mix_s[gt * 128:(gt + 1) * 128, 512:1024], m_t[:], [m_r], bagT[b])

    K.barrier()
    cut(0.8)
    with ExitStack() as pb_:
        wout, r_wout = SB("wout", [128, 8, 1024], BF16, pb_)
        with ExitStack() as pst:
            stg = [SB("stgo%d" % i, [128, 1024], F32, pst) for i in range(2)]
            for k in range(8):
                s_t, s_r = stg[k % 2]
                ld(sp, s_t[:], wout_d[:, k, :], [s_r])
                cast_any(wout[:, k, :], s_t[:], [s_r], [r_wout])
        K.barrier()
        G1, r_G1 = SB("G1", [128, DM], F32, pb_)
        A2, r_A2 = SB("A2", [128, DM], F32, pb_)
        SH2, r_SH2 = SB("SH2", [128, DM], F32, pb_)
        xt = [SB("xtb%d" % i, [128, DM], F32, pb_) for i in range(2)]
        mixin = [SB("mixin%d" % i, [128, DM], BF16, pb_) for i in range(2)]
        mixT, r_mixT = SB("mixT", [128, 8, 128], BF16, pb_)
        junk, r_junk = SB("junkb", [128, DM], BF16, pb_)
        x1, r_x1 = SB("x1", [128, DM], F32, pb_)
        h2s = [SB("h2_%d" % i, [128, DM], F32, pb_) for i in range(2)]
        h2b, r_h2b = SB("h2b", [128, DM], BF16, pb_)
        h2T, r_h2T = SB("h2T", [128, 8, 128], F32, pb_)
        s1s = [SB("s1b%d" % i, [128, 8], F32, pb_) for i in range(2)]
        nv0, r_nv0 = SB("nv0", [128, 1], F32, pb_)
        e4, r_e4 = SB("e4", [128, 4], F32, pb_)
        pb4, rpb4 = PBs[4], rPB[4]
        bag1 = Bag()
        for b in range(2):
            K._wait(sp, bagT[b].evs() + bagA[b].evs())
            tt(dve, vtmp[:], modT[:, 16:24, b], small["gpost1"][0][:], ALU.mult, [r_modT, small["gpost1"][1]], [r_vtmp])
            bcast_rows(vtmp, r_vtmp, G1, r_G1)
            ts(dve, vtmp[:], modT[:, 32:40, b], 1.0, None, ALU.add, None, [r_modT], [r_vtmp])
            tt(dve, vtmp[:], vtmp[:], small["gpre2"][0][:], ALU.mult, [r_vtmp, small["gpre2"][1]], [r_vtmp])
            bcast_rows(vtmp, r_vtmp, A2, r_A2)
            cp(dve, vtmp[:], modT[:, 24:32, b], [r_modT], [r_vtmp])
            bcast_rows(vtmp, r_vtmp, SH2, r_SH2)
            def stageA(ti):
                gt = b * NT + ti
                x_t, x_r = xt[gt % 2]
                mi, r_mi = mixin[gt % 2]
                h2, r_h2 = h2s[gt % 2]
                s1, r_s1 = s1s[gt % 2]
                ld(sp, x_t[:], x_d[gt * 128:(gt + 1) * 128, :], [x_r])
                ld(sp, mi[:], mix_s[gt * 128:(gt + 1) * 128, :], [r_mi])
                for k in range(8):
                    tr(PT[:, k * 128:(k + 1) * 128], mi[:, k * 128:(k + 1) * 128], identb[:], [r_mi, r_identb], [rPT])
                cp(act, mixT[:].rearrange("p k t -> p (k t)"), PT[:], [rPT], [r_mixT])
                for hf in range(2):
                    rp = rP2a if hf == 0 else rP2b
                    for k in range(8):
                        mm(P2[:, hf * 512:(hf + 1) * 512], mixT[:, k, :], wout[:, k, hf * 512:(hf + 1) * 512], k == 0, k == 7,
                           [r_mixT, r_wout], [rp])
                actf(junk[:], P2[:], AF.Square, [rP2a, rP2b], [r_junk, r_s1], accum_out=s1[:, 0:1])
                rstd_from(s1[:, 0:1], r_s1, s1[:, 1:2], r_s1, DM)
                stt(dve, x1[:], P2[:], s1[:, 1:2], G1[:], ALU.mult, ALU.mult, [rP2a, rP2b, r_s1, r_G1], [r_x1])
                tt(pool, x1[:], x1[:], x_t[:], ALU.add, [r_x1, x_r], [r_x1])
                st(sp, x1s_d[gt * 128:(gt + 1) * 128, :], x1[:], [r_x1], bag1)
                actf(junk[:], x1[:], AF.Square, [r_x1], [r_junk, r_s1], accum_out=s1[:, 2:3])
                rstd_from(s1[:, 2:3], r_s1, s1[:, 3:4], r_s1, DM)
                stt(dve, h2[:], x1[:], s1[:, 3:4], A2[:], ALU.mult, ALU.mult, [r_x1, r_s1, r_A2], [r_h2])
                tt(pool, h2[:], h2[:], SH2[:], ALU.add, [r_h2, r_SH2], [r_h2])
                cp(act, h2b[:], h2[:], [r_h2], [r_h2b])
                st(sp, h2s_d[gt * 128:(gt + 1) * 128, :], h2b[:], [r_h2b], bag1)

            def stageB(ti):
                gt = b * NT + ti
                h2, r_h2 = h2s[gt % 2]
                s1, r_s1 = s1s[gt % 2]
                for k in range(8):
                    pbx, rpx = PBs[k // 4], rPB[k // 4]
                    tr(pbx[:, (k % 4) * 128:(k % 4 + 1) * 128], h2[:, k * 128:(k + 1) * 128], identf[:], [r_h2, r_identf], [rpx])
                for hf in range(2):
                    cp(act, h2T[:, hf * 4:(hf + 1) * 4, :].rearrange("p k t -> p (k t)"), PBs[hf][:], [rPB[hf]], [r_h2T])
                wr_t, r_wr = small["wr"]
                for k in range(8):
                    mm(pb4[:, 0:NE], h2T[:, k, :], wr_t[:, k, :], k == 0, k == 7, [r_h2T, r_wr], [rpb4])
                lgt = lg_all[:, gt, :]
                tt(dve, lgt, pb4[:, 0:NE], small["br"][0][:], ALU.add, [rpb4, small["br"][1]], [r_lg])
                K.I(dve, lambda: nc.vector.max(v8_all[:, gt, :], lgt), reads=[r_lg], writes=[r_v8])
                ts(dve, nv0[:], v8_all[:, gt, 0:1], -1.0, None, ALU.mult, None, [r_v8], [r_nv0])
                actf(e4[:], v8_all[:, gt, 0:4], AF.Exp, [r_v8, r_nv0], [r_e4, r_s1], bias=nv0[:, 0:1], accum_out=s1[:, 4:5])
                K.I(dve, lambda: nc.vector.reciprocal(s1[:, 5:6], s1[:, 4:5]), reads=[r_s1], writes=[r_s1])
                ts(dve, gate_all[:, gt, :], e4[:], s1[:, 5:6], None, ALU.mult, None, [r_e4, r_s1], [r_gate])

            stageA(0)
            for ti in range(NT):
                if ti + 1 < NT:
                    stageA(ti + 1)
                stageB(ti)
    K.barrier()
    dump("lg", lg_all[:], r_lg, [128, NTT, NE])
    dump("v8", v8_all[:], r_v8, [128, NTT, 8])
    cut(2)

    base, r_base = SB("base", [128, NE])
    msk, r_msk = SB("msk", [128, NE])
    pos_all, r_pos = SB("pos_all", [128, NTT, NE])
    K.I(dve, lambda: nc.vector.memset(base[:], 0.0), writes=[r_base])
    pb4, rpb4 = PBs[4], rPB[4]
    for gt in range(NTT):
        ts(dve, msk[:], lg_all[:, gt, :], v8_all[:, gt, 3:4], None, ALU.is_ge, None, [r_lg, r_v8], [r_msk])
        mm(pb4[:, 32:64], Lst[:], msk[:], True, True, [r_Lst, r_msk], [rpb4])
        mm(pb4[:, 64:96], onesf[:], msk[:], True, True, [r_onesf, r_msk], [rpb4])
        tt(dve, pos_all[:, gt, :], pb4[:, 32:64], base[:], ALU.add, [rpb4, r_base], [r_pos])
        tt(dve, base[:], pb4[:, 64:96], base[:], ALU.add, [rpb4, r_base], [r_base])
    nblk, r_nblk = SB("nblk", [128, NE])
    incl, r_incl = SB("incl", [128, NE])
    pstart, r_pstart = SB("pstart", [128, NE])
    iotab, r_iotab = SB("iotab", [128, NB])
    bexp_f, r_bexpf = SB("bexp_f", [128, NB])
    bexp_i, r_bexpi = SB("bexp_i", [128, NB], I32)
    dest_i, r_desti = SB("dest_i", [128, NTT, 4], I32)
    widx_i, r_widx = SB("widx_i", [128, NB, 8], I32)
    bidx_i, r_bidx = SB("bidx_i", [128, NB], I32)
    pk, r_pk = SB("pk", [128, 8])
    c1024, r_c1024 = SB("c1024", [128, 8])
    ld(sp, pk[:], pk_d, [r_pk])
    K.I(dve, lambda: nc.vector.memset(c1024[:], 1024.0), writes=[r_c1024])
    ld(sp, iotab[:], iotab_d, [r_iotab])
    off = -0.5 + 1.0 / 1024.0
    ts(dve, nblk[:], base[:], float(BLK - 1), 1.0 / BLK, ALU.add, ALU.mult, [r_base], [r_nblk])
    ts(dve, nblk[:], nblk[:], off, MAGIC, ALU.add, ALU.add, [r_nblk], [r_nblk])
    ts(dve, nblk[:], nblk[:], -MAGIC, None, ALU.add, None, [r_nblk], [r_nblk])
    K.I(dve, lambda: nc.vector.tensor_tensor_scan(incl[:], onesf[:, 0:NE], nblk[:], 0.0, ALU.mult, ALU.add),
        reads=[r_onesf, r_nblk], writes=[r_incl])
    tt(dve, pstart[:], incl[:], nblk[:], ALU.subtract, [r_incl, r_nblk], [r_pstart])
    ts(dve, pstart[:], pstart[:], float(BLK), None, ALU.mult, None, [r_pstart], [r_pstart])
    with ExitStack() as p2a:
        cmp3, r_cmp3 = SB("cmp3", [128, NB, NE], F32, p2a)
        tt(dve, cmp3[:], incl[:].unsqueeze(1).broadcast_to((128, NB, NE)), iotab[:].unsqueeze(2).broadcast_to((128, NB, NE)),
           ALU.is_le, [r_incl, r_iotab], [r_cmp3])
        K.I(dve, lambda: nc.vector.tensor_reduce(bexp_f[:], cmp3[:], AX.X, ALU.add), reads=[r_cmp3], writes=[r_bexpf])
        ts(dve, bexp_f[:], bexp_f[:], float(NE - 1), None, ALU.min, None, [r_bexpf], [r_bexpf])
        cp(dve, bexp_i[:], bexp_f[:], [r_bexpf], [r_bexpi])
        widx_f, r_widxf = SB("widx_f", [128, NB, 8], F32, p2a)
        tt(dve, widx_f[:], bexp_f[:].unsqueeze(2).broadcast_to((128, NB, 8)), c1024[:].unsqueeze(1).broadcast_to((128, NB, 8)),
           ALU.mult, [r_bexpf, r_c1024], [r_widxf])
        tt(dve, widx_f[:], widx_f[:], pk[:].unsqueeze(1).broadcast_to((128, NB, 8)), ALU.add, [r_widxf, r_pk], [r_widxf])
        cp(dve, widx_i[:], widx_f[:], [r_widxf], [r_widx])
        ts(dve, bexp_f[:], bexp_f[:], 128.0, pk[:, 0:1], ALU.mult, ALU.add, [r_bexpf, r_pk], [r_bexpf])
        cp(dve, bidx_i[:], bexp_f[:], [r_bexpf], [r_bidx])
        destf, r_destf = SB("destf", [128, NE], F32, p2a)
        oh, r_oh = SB("oh", [128, 4, NE], F32, p2a)
        dk, r_dk = SB("dk", [128, 4], F32, p2a)
        hb = [SB("hb%d" % i, [128, DM], BF16, p2a) for i in range(2)]
        r_xg = Res()
        for gt in range(NTT):
            tt(dve, destf[:], pos_all[:, gt, :], pstart[:], ALU.add, [r_pos, r_pstart], [r_destf])
            tt(dve, oh[:], lg_all[:, gt, :].unsqueeze(1).broadcast_to((128, 4, NE)),
               v8_all[:, gt, 0:4].unsqueeze(2).broadcast_to((128, 4, NE)), ALU.is_equal, [r_lg, r_v8], [r_oh])
            tt(dve, oh[:], oh[:], destf[:].unsqueeze(1).broadcast_to((128, 4, NE)), ALU.mult, [r_oh, r_destf], [r_oh])
            K.I(dve, lambda: nc.vector.tensor_reduce(dk[:], oh[:], AX.X, ALU.add), reads=[r_oh], writes=[r_dk])
            cp(dve, dest_i[:, gt, :], dk[:], [r_dk], [r_desti])
            h_t, h_r = hb[gt % 2]
            ld(sp, h_t[:], h2s_d[gt * 128:(gt + 1) * 128, :], [h_r])
            if gt == 0:
                K._wait(sp, bag1.evs())
            for k in range(4):
                K.dma(pool, lambda: nc.gpsimd.indirect_dma_start(
                    out=xg_d, out_offset=bass.IndirectOffsetOnAxis(ap=dest_i[:, gt, k:k + 1], axis=0),
                    in_=h_t[:], in_offset=None), reads=[h_r, r_desti], writes=[])
    K.barrier()
    dump("desti", dest_i[:], r_desti, [128, NTT, 4], I32)
    dump("bexp", bexp_i[:], r_bexpi, [128, NB], I32)

    cut(3)
    scat_evs = [(sid, K.semvals[sid]) for sid in pool.ring if K.semvals[sid] > 0]

    with ExitStack() as p2:
        wgu = [SB("wgu%d" % i, [128, 8, 2 * DM], BF16, p2) for i in range(2)]
        wdn = [SB("wdn%d" % i, [128, 8, DM], BF16, p2) for i in range(2)]
        bgu = [SB("bgus%d" % i, [128, 16], F32, p2) for i in range(2)]
        bdn = [SB("bdns%d" % i, [128, DM], F32, p2) for i in range(2)]
        xgt = [SB("xgt%d" % i, [128, DM], BF16, p2) for i in range(2)]
        xgTs = [SB("xgT%d" % i, [128, 8, BLK], BF16, p2) for i in range(2)]
        actT, r_actT = SB("actT", [128, 8, BLK], BF16, p2)
        gg, r_gg = SB("gg", [128, BLK], F32, p2)
        sg, r_sg = SB("sg", [128, BLK], F32, p2)
        ll, r_ll = SB("ll", [128, BLK], F32, p2)
        yo = [SB("yo%d" % i, [128, DM], F32, p2) for i in range(2)]
        K._wait(sp, scat_evs)
        wgr = [[Res() for _ in range(8)] for _ in range(2)]
        wdr = [[Res() for _ in range(8)] for _ in range(2)]

        def load_weights(blk):
            wg_t = wgu[blk % 2][0]
            wd_t = wdn[blk % 2][0]
            bg_t, bg_r = bgu[blk % 2]
            bd_t, bd_r = bdn[blk % 2]
            for k in range(8):
                K.dma(pool, lambda: nc.gpsimd.indirect_dma_start(
                    out=wg_t[:, k, :], out_offset=None, in_=wgu_d,
                    in_offset=bass.IndirectOffsetOnAxis(ap=widx_i[:, blk, k:k + 1], axis=0)), reads=[r_widx], writes=[wgr[blk % 2][k]])
            for k in range(8):
                K.dma(pool, lambda: nc.gpsimd.indirect_dma_start(
                    out=wd_t[:, k, :], out_offset=None, in_=wd_d,
                    in_offset=bass.IndirectOffsetOnAxis(ap=widx_i[:, blk, k:k + 1], axis=0)), reads=[r_widx], writes=[wdr[blk % 2][k]])
            K.dma(pool, lambda: nc.gpsimd.indirect_dma_start(
                out=bg_t[:], out_offset=None, in_=bgu_d,
                in_offset=bass.IndirectOffsetOnAxis(ap=bidx_i[:, blk:blk + 1], axis=0)), reads=[r_bidx], writes=[bg_r])
            K.dma(pool, lambda: nc.gpsimd.indirect_dma_start(
                out=bd_t[:], out_offset=None, in_=bd_d,
                in_offset=bass.IndirectOffsetOnAxis(ap=bidx_i[:, blk:blk + 1], axis=0)), reads=[r_bidx], writes=[bd_r])

        PT2 = PBs[4][:].bitcast(BF16)
        ptn = [0]

        def prep_tokens(blk):
            xgT_, r_xgT_ = xgTs[blk % 2]
            for st in range(4):
                g_t, g_r = xgt[st % 2]
                r0 = blk * BLK + st * 128
                ld(sp, g_t[:], xg_d[r0:r0 + 128, :], [g_r])
                if ptn[0] % 2 == 0:
                    pt_ap, pt_r = PT[:], rPT
                else:
                    pt_ap, pt_r = PT2, rPB[4]
                ptn[0] += 1
                for k in range(8):
                    tr(pt_ap[:, k * 128:(k + 1) * 128], g_t[:, k * 128:(k + 1) * 128], identb[:], [g_r, r_identb], [pt_r])
                cp(act, xgT_[:, :, st * 128:(st + 1) * 128],
                   pt_ap.rearrange("p (k t) -> p k t", t=128), [pt_r], [r_xgT_])

        load_weights(0)
        prep_tokens(0)
        for blk in range(NB):
            if blk + 1 < NB:
                load_weights(blk + 1)
                prep_tokens(blk + 1)
            wg_t = wgu[blk % 2][0]
            wd_t = wdn[blk % 2][0]
            wg_rs = wgr[blk % 2]
            wd_rs = wdr[blk % 2]
            bg_t, bg_r = bgu[blk % 2]
            bd_t, bd_r = bdn[blk % 2]
            xgT, r_xgT = xgTs[blk % 2]
            for fc in range(8):
                pg, rpg = PBs[(fc % 2) * 2], rPB[(fc % 2) * 2]
                pl, rpl = PBs[(fc % 2) * 2 + 1], rPB[(fc % 2) * 2 + 1]
                for k in range(8):
                    mm(pg[:], wg_t[:, k, fc * 128:(fc + 1) * 128], xgT[:, k, :], k == 0, k == 7, [wg_rs[k], r_xgT], [rpg])
                for k in range(8):
                    mm(pl[:], wg_t[:, k, DM + fc * 128:DM + (fc + 1) * 128], xgT[:, k, :], k == 0, k == 7, [wg_rs[k], r_xgT], [rpl])
                ts(dve, gg[:], pg[:], bg_t[:, fc:fc + 1], 7.0, ALU.add, ALU.min, [rpg, bg_r], [r_gg])
                actf(sg[:], gg[:], AF.Sigmoid, [r_gg], [r_sg], scale=1.702)
                ts(dve, ll[:], pl[:], bg_t[:, 8 + fc:9 + fc], 7.0, ALU.add, ALU.min, [rpl, bg_r], [r_ll])
                ts(dve, ll[:], ll[:], -7.0, 1.0, ALU.max, ALU.add, [r_ll], [r_ll])
                tt(dve, gg[:], gg[:], sg[:], ALU.mult, [r_gg, r_sg], [r_gg])
                tt(dve, actT[:, fc, :], gg[:], ll[:], ALU.mult, [r_gg, r_ll], [r_actT])
            dbanks = ((P2[:, 0:512], rP2a), (P2[:, 512:1024], rP2b), (PBs[2][:], rPB[2]), (PBs[3][:], rPB[3]))
            for st in range(4):
                y_t, y_r = yo[st % 2]
                for hf in range(2):
                    pd, rpd = dbanks[(st % 2) * 2 + hf]
                    for k in range(8):
                        mm(pd, actT[:, k, st * 128:(st + 1) * 128], wd_t[:, k, hf * 512:(hf + 1) * 512], k == 0, k == 7,
                           [r_actT, wd_rs[k]], [rpd])
                    tt(dve, y_t[:, hf * 512:(hf + 1) * 512], pd, bd_t[:, hf * 512:(hf + 1) * 512], ALU.add, [rpd, bd_r], [y_r])
                r0 = blk * BLK + st * 128
                K.dma(act, lambda: nc.scalar.dma_start(out=yb_d[r0:r0 + 128, :], in_=y_t[:]), reads=[y_r], writes=[])
    K.barrier()
    yb_evs = [(sid, K.semvals[sid]) for sid in act.ring if K.semvals[sid] > 0]

    with ExitStack() as p3:
        yg = [SB("yg%d" % i, [128, DM], F32, p3) for i in range(8)]
        fa, r_fa = SB("fa", [128, DM], F32, p3)
        x1r = [SB("x1r%d" % i, [128, DM], F32, p3) for i in range(2)]
        ob = [SB("ob%d" % i, [128, DM], F32, p3) for i in range(2)]
        junk3, r_junk3 = SB("junk3", [128, DM], BF16, p3)
        s3, r_s3 = SB("s3", [128, 2], F32, p3)
        G2, r_G2 = SB("G2", [128, 2, DM], F32, p3)
        for b in range(2):
            tt(dve, vtmp[:], modT[:, 40:48, b], small["gpost2"][0][:], ALU.mult, [r_modT, small["gpost2"][1]], [r_vtmp])
            bcast_rows(vtmp, r_vtmp, G2[:, b, :], r_G2)
        K._wait(pool, yb_evs)
        K._wait(sp, bag1.evs())
        def p3_loads(gt):
            x_t, x_r = x1r[gt % 2]
            ld(sp, x_t[:], x1s_d[gt * 128:(gt + 1) * 128, :], [x_r])
            for k in range(4):
                y_t, y_r = yg[(gt % 2) * 4 + k]
                K.dma(pool, lambda: nc.gpsimd.indirect_dma_start(
                    out=y_t[:], out_offset=None, in_=yb_d,
                    in_offset=bass.IndirectOffsetOnAxis(ap=dest_i[:, gt, k:k + 1], axis=0)), reads=[r_desti], writes=[y_r])

        def p3_compute(gt):
            b = gt // NT
            x_t, x_r = x1r[gt % 2]
            o_t, o_r = ob[gt % 2]
            ygs = [yg[(gt % 2) * 4 + k] for k in range(4)]
            actf(fa[:], ygs[0][0][:], AF.Copy, [ygs[0][1], r_gate], [r_fa], scale=gate_all[:, gt, 0:1])
            for k in range(1, 4):
                stt(dve, fa[:], ygs[k][0][:], gate_all[:, gt, k:k + 1], fa[:], ALU.mult, ALU.add, [ygs[k][1], r_gate, r_fa], [r_fa])
            actf(junk3[:], fa[:], AF.Square, [r_fa], [r_junk3, r_s3], accum_out=s3[:, 0:1])
            actf(s3[:, 1:2], s3[:, 0:1], AF.Sqrt, [r_s3], [r_s3], scale=1.0 / DM, bias=epsb[:, 0:1])
            K.I(dve, lambda: nc.vector.reciprocal(s3[:, 1:2], s3[:, 1:2]), reads=[r_s3], writes=[r_s3])
            stt(dve, o_t[:], fa[:], s3[:, 1:2], G2[:, b, :], ALU.mult, ALU.mult, [r_fa, r_s3, r_G2], [o_r])
            tt(pool, o_t[:], o_t[:], x_t[:], ALU.add, [o_r, x_r], [o_r])
            K.dma(sp, lambda: nc.sync.dma_start(out=out_d[gt * 128:(gt + 1) * 128, :], in_=o_t[:]), reads=[o_r], writes=[], is_out=True)

        p3_loads(0)
        for gt in range(NTT):
            if gt + 1 < NTT:
                p3_loads(gt + 1)
            p3_compute(gt)
    K.barrier()
    K.finish()
    es.close()
    return nc, dbg_d


def _consts(NB):
    idx = np.arange(128)
    ch = idx // 64
    same = ch[:, None] == ch[None, :]
    U1 = (same & (idx[:, None] > idx[None, :])).astype(np.float32)
    U2 = (same & (idx[:, None] <= idx[None, :])).astype(np.float32)
    m01 = U2.copy()
    Lst = (idx[:, None] < idx[None, :]).astype(np.float32)
    invf = (np.float32(10000.0) ** (-(np.arange(16, dtype=np.float32) * np.float32(2.0) / np.float32(32.0)))).astype(np.float32)
    rm = np.stack([(ch == 0), (ch == 1)], axis=1).astype(np.float32)
    return dict(identf=np.eye(128, dtype=np.float32), U1=U1, U2=U2, m01=m01, Lst=Lst, rm=rm,
                invf=np.tile(invf[None, :], (128, 1)).astype(np.float32),
                iotab=np.tile(np.arange(NB, dtype=np.float32)[None, :], (128, 1)),
                pk=(np.arange(8, dtype=np.float32)[None, :] * 128 + np.arange(128, dtype=np.float32)[:, None]).astype(np.float32))


def _rep(v, n=128):
    return np.ascontiguousarray(np.tile(np.asarray(v, np.float32).reshape(1, -1), (n, 1)))


def _pk(v, k):
    return np.ascontiguousarray(np.asarray(v, np.float32).reshape(k, 128).T)


_CACHE = {}


def kernel(x, c, positions, w_ada, b_ada, pre_mix_norm, w_in, conv_w, conv_b, dt_bias, a_log, d_skip, ssd_norm,
           q_a_norm, w_q_up, kv_a_norm, w_kv_up, mla_norm, w_out, post_mix_norm, pre_ffn_norm, w_router, b_router,
           w_gate_up, b_gate_up, w_down, b_down, post_ffn_norm, _dbg=()):
    x = np.asarray(x, np.float32)
    Bsz, S, _ = x.shape
    ncores = Bsz // 2
    NT = S // 128
    NB = (2 * S * 4) // BLK + NE
    key = (S, tuple(_dbg))
    if key not in _CACHE:
        _CACHE[key] = build(S, _dbg)
    nc, dbg_d = _CACHE[key]
    f = lambda a: np.ascontiguousarray(np.asarray(a, np.float32))
    w_in = f(w_in)[0]
    w_tm = np.ascontiguousarray(np.concatenate([w_in[:, 0:512], w_in[:, 1536:1544], w_in[:, 2184:2216]], axis=1))
    w_fm = np.ascontiguousarray(np.concatenate([w_in[:, 512:1536], w_in[:, 1544:1928], w_in[:, 1928:2184]], axis=1))
    wq = f(w_q_up)[0].reshape(384, 8, 96)
    qn = wq[:, :, 0:64]
    qr = wq[:, :, 64:96]
    qn_pairs = np.stack([np.concatenate([qn[:, j], qn[:, j + 4]], axis=1) for j in range(4)], axis=1)
    wq2 = np.ascontiguousarray(np.concatenate([qn_pairs.reshape(384, 512), qr.reshape(384, 256)], axis=1))
    wkv = f(w_kv_up)[0].reshape(256, 8, 128)
    kn = wkv[:, :, 0:64]
    vv = wkv[:, :, 64:128]
    kn_pairs = np.stack([np.concatenate([kn[:, j], kn[:, j + 4]], axis=1) for j in range(4)], axis=1)
    wkv2 = np.ascontiguousarray(np.concatenate([kn_pairs.reshape(256, 512), vv.reshape(256, 512)], axis=1))
    cw = f(conv_w)[0]
    convw = np.ascontiguousarray(cw.reshape(4, 8, 128).transpose(2, 1, 0))
    shared = dict(
        w_ada=f(w_ada)[0], b_adaT=_pk(f(b_ada)[0], 48), g_pre1=_pk(f(pre_mix_norm)[0], 8), g_post1=_pk(f(post_mix_norm)[0], 8),
        g_pre2=_pk(f(pre_ffn_norm)[0], 8), g_post2=_pk(f(post_ffn_norm)[0], 8), w_tm=w_tm, w_fm=w_fm, convw=convw,
        convb=_pk(f(conv_b)[0], 8), dtb=_rep(f(dt_bias)[0]), alog=_rep(f(a_log)[0]), dsk=_rep(f(d_skip)[0]),
        ssdn=_rep(f(ssd_norm)[0]), mlan=_rep(f(mla_norm)[0]), qan=_pk(f(q_a_norm)[0], 3), kvan=_pk(f(kv_a_norm)[0], 2),
        wq=wq2, wkv=wkv2, wout=f(w_out)[0], wr=f(w_router)[0], br=_rep(f(b_router)[0]),
        wgu=f(w_gate_up)[0].reshape(NE * DM, 2 * DM), wd=f(w_down)[0].reshape(NE * DM, DM),
        bgu=np.ascontiguousarray(f(b_gate_up)[0].reshape(NE, 16, 128).transpose(0, 2, 1)).reshape(NE * 128, 16),
        bd=np.ascontiguousarray(np.broadcast_to(f(b_down)[0][:, None, :], (NE, 128, DM))).reshape(NE * 128, DM),
    )
    shared.update(_consts(NB))
    cc = f(c)
    pp = np.asarray(positions, np.int32)
    in_maps = []
    for ci in range(ncores):
        m = dict(shared)
        m["x"] = np.ascontiguousarray(x[2 * ci:2 * ci + 2].reshape(2 * S, DM))
        m["cT"] = np.ascontiguousarray(cc[2 * ci:2 * ci + 2].reshape(2, 8, 128).transpose(2, 1, 0))
        m["pos"] = np.ascontiguousarray(pp[2 * ci:2 * ci + 2].reshape(2, NT, 128).transpose(0, 2, 1))
        in_maps.append(m)
    res = run_bass_kernel_spmd(nc, in_maps, core_ids=list(range(ncores)))
    out = np.stack([np.asarray(r["out"], np.float32).reshape(2, S, DM) for r in res.results], axis=0).reshape(Bsz, S, DM)
    if _dbg:
        return out, [{k: np.asarray(r["dbg_" + k]) for k in dbg_d} for r in res.results]
    return out
```

```python
import math
from contextlib import ExitStack
import numpy as np
import concourse.bass as bass
import concourse.mybir as mybir
from concourse.bass_utils import run_bass_kernel_spmd

F32 = mybir.dt.float32
BF16 = mybir.dt.bfloat16
I32 = mybir.dt.int32
AF = mybir.ActivationFunctionType
ALU = mybir.AluOpType
AX = mybir.AxisListType

import os
_PH = float(os.environ.get("KPH", "9"))
DM = 1024
NE = 32
BLK = 512
EPS = 1e-6
MAGIC = 12582912.0
C1 = 6.28125
C2 = 2.0 * math.pi - 6.28125


class Res:
    __slots__ = ("w", "rs")

    def __init__(self):
        self.w = None
        self.rs = {}


class Eng:
    def __init__(self, h, sid, is_pe=False):
        self.h = h
        self.sid = sid
        self.n = 0
        self.known = {}
        self.is_pe = is_pe
        self.ring = []
        self.rpos = 0


class Kern:
    def __init__(self, nc, es):
        self.nc = nc
        self.sems = []
        self.semvals = []
        self.es = es
        mk = lambda nm: self._newsem(nm)
        self.pe = Eng(nc.tensor, mk("s_pe"), True)
        self.dve = Eng(nc.vector, mk("s_dve"))
        self.act = Eng(nc.scalar, mk("s_act"))
        self.pool = Eng(nc.gpsimd, mk("s_pool"))
        self.sp = Eng(nc.sync, mk("s_sp"))
        for e, n in ((self.sp, 44), (self.act, 12), (self.pool, 36)):
            e.ring = [mk("d%d_%d" % (e.sid, i)) for i in range(n)]
        self.out_evs = []

    def _newsem(self, nm):
        s = self.es.enter_context(self.nc.semaphore(nm))
        self.sems.append(s)
        self.semvals.append(0)
        return len(self.sems) - 1

    def _wait(self, eng, evs):
        best = {}
        for sid, val in evs:
            if val > best.get(sid, 0):
                best[sid] = val
        for sid, val in best.items():
            if eng.is_pe and sid == eng.sid:
                continue
            if eng.known.get(sid, 0) >= val:
                continue
            eng.h.wait_ge(self.sems[sid], val)
            eng.known[sid] = val

    @staticmethod
    def _deps(reads, writes):
        evs = []
        for r in reads:
            if r.w is not None:
                evs.append(r.w)
        for r in writes:
            if r.w is not None:
                evs.append(r.w)
            evs.extend(r.rs.items())
        return evs

    @staticmethod
    def _mark(ev, reads, writes):
        for r in reads:
            if ev[1] > r.rs.get(ev[0], 0):
                r.rs[ev[0]] = ev[1]
        for r in writes:
            r.w = ev
            r.rs = {}

    def I(self, eng, fn, reads=(), writes=()):
        evs = self._deps(reads, writes)
        own = eng.sid
        evs2 = []
        raw = set()
        for r in reads:
            if r.w is not None:
                raw.add(r.w)
        for ev in evs:
            if ev[0] == own and ev not in raw:
                continue
            evs2.append(ev)
        self._wait(eng, evs2)
        ins = fn()
        eng.n += 1
        ins.then_inc(self.sems[eng.sid], 1)
        self._mark((eng.sid, eng.n), reads, writes)

    def dma(self, q, fn, reads=(), writes=(), is_out=False):
        evs = self._deps(reads, writes)
        sid = q.ring[q.rpos % len(q.ring)]
        q.rpos += 1
        prev = self.semvals[sid]
        if prev > 0:
            evs.append((sid, prev))
        self._wait(q, evs)
        ins = fn()
        self.semvals[sid] = prev + 16
        ins.then_inc(self.sems[sid], 16)
        ev = (sid, prev + 16)
        self._mark(ev, reads, writes)
        if is_out:
            self.out_evs.append(ev)
        return ev

    def barrier(self):
        evs = []
        for e in (self.sp, self.act, self.pool):
            evs += [(sid, self.semvals[sid]) for sid in e.ring if self.semvals[sid] > 0]
        for e in (self.pe, self.dve, self.act, self.pool):
            if e.n > 0:
                evs.append((e.sid, e.n))
        for e in (self.pe, self.dve, self.act, self.pool, self.sp):
            self._wait(e, [ev for ev in evs if ev[0] != e.sid])

    def finish(self):
        evs = list(self.out_evs)
        for e in (self.sp, self.act, self.pool):
            evs += [(sid, self.semvals[sid]) for sid in e.ring if self.semvals[sid] > 0]
        for e in (self.pe, self.dve, self.act, self.pool):
            if e.n > 0:
                evs.append((e.sid, e.n))
        self._wait(self.sp, evs)


class _Cut(Exception):
    pass


def build(S, dbg=()):
    ctx = {}
    try:
        return _build_inner(S, dbg, ctx)
    except _Cut:
        ctx["K"].finish()
        return ctx["nc"], ctx["dbg_d"]


def _build_inner(S, dbg, ctx):
    NT = S // 128
    NG = S // 512
    T = 2 * S
    NTT = T // 128
    NB = (T * 4) // BLK + NE
    nc = bass.Bass("TRN2", target_bir_lowering=False)
    es = ExitStack()
    K = Kern(nc, es)
    pe, dve, act, pool, sp = K.pe, K.dve, K.act, K.pool, K.sp
    dbg_d = {}

    def DI(name, shape, dt=F32):
        return nc.dram_tensor(name, list(shape), dt, kind="ExternalInput").ap()

    def DS(name, shape, dt=F32):
        if name in dbg:
            d = nc.dram_tensor("dbg_" + name, list(shape), dt, kind="ExternalOutput").ap()
            dbg_d[name] = d
            return d
        return nc.dram_tensor(name, list(shape), dt, kind="Internal").ap()

    x_d = DI("x", [T, DM])
    cT_d = DI("cT", [128, 8, 2])
    pos_d = DI("pos", [2, 128, NT], I32)
    wada_d = DI("w_ada", [DM, 6 * DM]).rearrange("(k p) f -> p k f", p=128)
    badaT_d = DI("b_adaT", [128, 48])
    gpre1_d = DI("g_pre1", [128, 8])
    gpost1_d = DI("g_post1", [128, 8])
    gpre2_d = DI("g_pre2", [128, 8])
    gpost2_d = DI("g_post2", [128, 8])
    wtm_d = DI("w_tm", [DM, 552]).rearrange("(k p) f -> p k f", p=128)
    wfm_d = DI("w_fm", [DM, 1664]).rearrange("(k p) f -> p k f", p=128)
    convw_d = DI("convw", [128, 8, 4])
    convb_d = DI("convb", [128, 8])
    dtb_d = DI("dtb", [128, 8])
    alog_d = DI("alog", [128, 8])
    dsk_d = DI("dsk", [128, 8])
    ssdn_d = DI("ssdn", [128, 512])
    mlan_d = DI("mlan", [128, 512])
    qan_d = DI("qan", [128, 3])
    kvan_d = DI("kvan", [128, 2])
    wq_d = DI("wq", [384, 768]).rearrange("(k p) f -> p k f", p=128)
    wkv_d = DI("wkv", [256, 1024]).rearrange("(k p) f -> p k f", p=128)
    wout_d = DI("wout", [DM, DM]).rearrange("(k p) f -> p k f", p=128)
    wr_d = DI("wr", [DM, NE]).rearrange("(k p) f -> p k f", p=128)
    br_d = DI("br", [128, NE])
    wgu_d = DI("wgu", [NE * DM, 2 * DM])
    wd_d = DI("wd", [NE * DM, DM])
    bgu_d = DI("bgu", [NE * 128, 16])
    bd_d = DI("bd", [NE * 128, DM])
    identf_d = DI("identf", [128, 128])
    U1_d = DI("U1", [128, 128])
    U2_d = DI("U2", [128, 128])
    Lst_d = DI("Lst", [128, 128])
    m01_d = DI("m01", [128, 128])
    invf_d = DI("invf", [128, 16])
    iotab_d = DI("iotab", [128, NB])
    pk_d = DI("pk", [128, 8])
    rm_d = DI("rm", [128, 2])
    x1s_d = DS("x1s", [T, DM])
    h2s_d = DS("h2s", [T, DM], BF16)
    xg_d = DS("xg", [NB * BLK, DM], BF16)
    yb_d = DS("yb", [NB * BLK, DM])
    out_d = nc.dram_tensor("out", [T, DM], F32, kind="ExternalOutput").ap()
    ctx.update(K=K, es=es, nc=nc, dbg_d=dbg_d)

    def cut(level):
        if _PH < level:
            raise _Cut()

    def SB(name, shape, dt=F32, stack=None):
        t = (stack or es).enter_context(nc.sbuf_tensor("sb_" + name, list(shape), dt))
        return t, Res()

    def PS(name, shape, dt=F32):
        return es.enter_context(nc.psum_tensor("ps_" + name, list(shape), dt))

    P2 = PS("P2", [128, 1024])
    rP2a, rP2b = Res(), Res()
    PBs = [PS("PB%d" % i, [128, 512]) for i in range(5)]
    rPB = [Res() for _ in range(5)]
    PT = PS("PT", [128, 1024], BF16)
    rPT = Res()

    def mm(out, lhsT, rhs, start, stop, rd, wr):
        K.I(pe, lambda: nc.tensor.matmul(out, lhsT, rhs, start=start, stop=stop), reads=rd, writes=wr)

    def tr(out, in_, ident, rd, wr):
        K.I(pe, lambda: nc.tensor.transpose(out, in_, ident), reads=rd, writes=wr)

    def actf(out, in_, func, rd, wr, bias=None, scale=None, accum_out=None):
        kw = {}
        if bias is not None:
            kw["bias"] = bias
        if scale is not None:
            kw["scale"] = scale
        if accum_out is not None:
            kw["accum_out"] = accum_out
        K.I(act, lambda: nc.scalar.activation(out, in_, func, **kw), reads=rd, writes=wr)

    def ts(eng, out, in0, s1, s2, op0, op1, rd, wr):
        h = eng.h
        if op1 is None:
            K.I(eng, lambda: h.tensor_scalar(out, in0, s1, None, op0), reads=rd, writes=wr)
        else:
            K.I(eng, lambda: h.tensor_scalar(out, in0, s1, s2, op0, op1), reads=rd, writes=wr)

    def tt(eng, out, in0, in1, op, rd, wr):
        h = eng.h
        K.I(eng, lambda: h.tensor_tensor(out, in0, in1, op), reads=rd, writes=wr)

    def stt(eng, out, in0, scalar, in1, op0, op1, rd, wr):
        h = eng.h
        K.I(eng, lambda: h.scalar_tensor_tensor(out, in0, scalar, in1, op0, op1), reads=rd, writes=wr)

    def cp(eng, out, in_, rd, wr):
        h = eng.h
        if eng is act:
            K.I(eng, lambda: nc.scalar.activation(out, in_, AF.Copy), reads=rd, writes=wr)
        else:
            K.I(eng, lambda: h.tensor_copy(out, in_), reads=rd, writes=wr)

    def ld(q, out, in_, wr, rd=()):
        K.dma(q, lambda: q.h.dma_start(out=out, in_=in_), reads=rd, writes=wr)

    def dump(name, ap, res, shape, dt=F32):
        if name not in dbg:
            return
        d = nc.dram_tensor("dbg_" + name, list(shape), dt, kind="ExternalOutput").ap()
        dbg_d[name] = d
        K.dma(sp, lambda: nc.sync.dma_start(out=d, in_=ap), reads=[res], writes=[Res()], is_out=True)

    identf, r_identf = SB("identf", [128, 128])
    identb, r_identb = SB("identb", [128, 128], BF16)
    onesf, r_onesf = SB("onesf", [128, 128])
    onesb, r_onesb = SB("onesb", [128, 128], BF16)
    U1, r_U1 = SB("U1", [128, 128])
    U2, r_U2 = SB("U2", [128, 128])
    Lst, r_Lst = SB("Lst", [128, 128])
    m01, r_m01 = SB("m01", [128, 128])
    invf, r_invf = SB("invf", [128, 16])
    ld(sp, identf[:], identf_d, [r_identf])
    ld(sp, U1[:], U1_d, [r_U1])
    ld(sp, U2[:], U2_d, [r_U2])
    ld(sp, Lst[:], Lst_d, [r_Lst])
    ld(sp, m01[:], m01_d, [r_m01])
    ld(sp, invf[:], invf_d, [r_invf])
    rm, r_rm = SB("rm", [128, 2])
    Mc, r_Mc = SB("Mc", [128, 2, 128])
    ld(sp, rm[:], rm_d, [r_rm])
    cp(dve, identb[:], identf[:], [r_identf], [r_identb])
    K.I(dve, lambda: nc.vector.memset(onesf[:], 1.0), writes=[r_onesf])
    K.I(dve, lambda: nc.vector.memset(onesb[:], 1.0), writes=[r_onesb])
    for c in range(2):
        ts(dve, Mc[:, c, :], onesf[:], rm[:, c:c + 1], None, ALU.mult, None, [r_onesf, r_rm], [r_Mc])
    epsb, r_epsb = SB("epsb", [128, 1])
    oneb, r_oneb = SB("oneb", [128, 1])
    K.I(dve, lambda: nc.vector.memset(epsb[:], EPS), writes=[r_epsb])
    K.I(dve, lambda: nc.vector.memset(oneb[:], 1.0), writes=[r_oneb])

    small = {}
    for nm, d, shp in (("gpre1", gpre1_d, [128, 8]), ("gpost1", gpost1_d, [128, 8]), ("gpre2", gpre2_d, [128, 8]),
                       ("gpost2", gpost2_d, [128, 8]), ("convw", convw_d, [128, 8, 4]), ("convb", convb_d, [128, 8]),
                       ("dtb", dtb_d, [128, 8]), ("alog", alog_d, [128, 8]), ("dsk", dsk_d, [128, 8]),
                       ("ssdn", ssdn_d, [128, 512]), ("mlan", mlan_d, [128, 512]), ("qan", qan_d, [128, 3]),
                       ("kvan", kvan_d, [128, 2]), ("br", br_d, [128, NE]), ("badaT", badaT_d, [128, 48]),
                       ("cT", cT_d, [128, 8, 2]), ("wr", wr_d, [128, 8, NE])):
        t, r = SB("c_" + nm, shp)
        ld(sp, t[:], d, [r])
        small[nm] = (t, r)

    aneg, r_aneg = SB("aneg", [128, 8])
    actf(aneg[:], small["alog"][0][:], AF.Exp, [small["alog"][1]], [r_aneg])
    ts(dve, aneg[:], aneg[:], -1.0, None, ALU.mult, None, [r_aneg], [r_aneg])

    modT, r_modT = SB("modT", [128, 48, 2])
    cact, r_cact = SB("cact", [128, 8, 2])
    actf(cact[:], small["cT"][0][:], AF.Silu, [small["cT"][1]], [r_cact])
    with ExitStack() as st0:
        wst = [SB("wadast%d" % i, [128, 8, 512], F32, st0) for i in range(2)]
        for blk in range(12):
            w_t, w_r = wst[blk % 2]
            ld(sp, w_t[:], wada_d[:, :, blk * 512:(blk + 1) * 512], [w_r])
            for j in range(4):
                fc = blk * 4 + j
                pb, rpb = PBs[fc % 2], rPB[fc % 2]
                for k in range(8):
                    mm(pb[:, 0:2], w_t[:, k, j * 128:(j + 1) * 128], cact[:, k, :], k == 0, k == 7,
                       [w_r, r_cact], [rpb])
                ts(dve, modT[:, fc, :], pb[:, 0:2], small["badaT"][0][:, fc:fc + 1], None, ALU.add, None,
                   [rpb, small["badaT"][1]], [r_modT])
    K.barrier()
    a1T, r_a1T = SB("a1T", [128, 2, 8])
    sh1T, r_sh1T = SB("sh1T", [128, 2, 8])
    vtmp, r_vtmp = SB("vtmp", [128, 8])
    btmp = [SB("btmp%d" % i, [128, 128]) for i in range(2)]
    bcn = [0]

    def bcast_rows(vT_ap, vres, dst_ap, dres):
        for half in range(2):
            pb, rpb = PBs[2 + half], rPB[2 + half]
            for kk in range(4):
                k = half * 4 + kk
                bt, rbt = btmp[bcn[0] % 2]
                bcn[0] += 1
                ts(dve, bt[:], onesf[:], vT_ap[:, k:k + 1], None, ALU.mult, None, [r_onesf, vres], [rbt])
                mm(pb[:, kk * 128:(kk + 1) * 128], bt[:], identf[:], True, True, [rbt, r_identf], [rpb])
            cp(act, dst_ap[:, half * 512:(half + 1) * 512], pb[:], [rpb], [dres])

    for b in range(2):
        ts(dve, vtmp[:], modT[:, 8:16, b], 1.0, None, ALU.add, None, [r_modT], [r_vtmp])
        tt(dve, a1T[:, b, :], vtmp[:], small["gpre1"][0][:], ALU.mult, [r_vtmp, small["gpre1"][1]], [r_a1T])
        cp(dve, sh1T[:, b, :], modT[:, 0:8, b], [r_modT], [r_sh1T])

    lg_all, r_lg = SB("lg_all", [128, NTT, NE])
    v8_all, r_v8 = SB("v8_all", [128, NTT, 8])
    gate_all, r_gate = SB("gate_all", [128, NTT, 4])

    castn = [0]

    def cast_any(out, in_, rd, wr):
        e = (dve, pool, act)[castn[0] % 3]
        castn[0] += 1
        cp(e, out, in_, rd, wr)

    def rstd_from(ss_ap, ssres, out_ap, ores, n, eng=dve):
        actf(out_ap, ss_ap, AF.Ln, [ssres], [ores], bias=epsb[:, 0:1], scale=1.0 / n)
        actf(out_ap, out_ap, AF.Exp, [ores], [ores], scale=-0.5)

    class Bag:
        def __init__(self):
            self.d = {}

        def add(self, ev):
            if ev[1] > self.d.get(ev[0], 0):
                self.d[ev[0]] = ev[1]

        def evs(self):
            return list(self.d.items())

    kn_s = DS("kn_s", [2, 128, 4 * S], BF16)
    kr_s = DS("kr_s", [2, 128, S], BF16)
    v_s = DS("v_s", [2, 128, NT * 8 * 65], BF16)
    q_s = DS("q_s", [2 * NT, 128, 2048], BF16)
    mix_s = DS("mix_s", [T, DM], BF16)
    bagA = [Bag(), Bag()]
    bagT = [Bag(), Bag()]

    def st(q, out, in_, rd, bag):
        ev = K.dma(q, lambda: q.h.dma_start(out=out, in_=in_), reads=rd, writes=[])
        bag.add(ev)

    dump("modT", modT[:], r_modT, [128, 48, 2])
    cut(0.2)
    with ExitStack() as p1:
        wtm, r_wtm = SB("wtm", [128, 8, 552], BF16, p1)
        wfm, r_wfm = SB("wfm", [128, 8, 1664], BF16, p1)
        wq, r_wq = SB("wq", [128, 3, 768], BF16, p1)
        wkv, r_wkv = SB("wkv", [128, 2, 1024], BF16, p1)
        with ExitStack() as pst:
            stg = [SB("stg%d" % i, [128, 1664], F32, pst) for i in range(2)]
            sn = 0
            for (dst, rdst, src, nk, ncol) in ((wtm, r_wtm, wtm_d, 8, 552), (wfm, r_wfm, wfm_d, 8, 1664),
                                               (wq, r_wq, wq_d, 3, 768), (wkv, r_wkv, wkv_d, 2, 1024)):
                for k in range(nk):
                    s_t, s_r = stg[sn % 2]
                    sn += 1
                    ld(sp, s_t[:, 0:ncol], src[:, k, :], [s_r])
                    cast_any(dst[:, k, :], s_t[:, 0:ncol], [s_r], [rdst])
        K.barrier()
        cut(0.25)
        Sst, r_Sst = SB("Sst", [128, 8, 64], F32, p1)
        Sbf, r_Sbf = SB("Sbf", [128, 8, 64], BF16, p1)
        cosT, r_cos = SB("cosT", [128, NT, 16], F32, p1)
        sinT, r_sin = SB("sinT", [128, NT, 16], F32, p1)
        posi, r_posi = SB("posi", [128, NT], I32, p1)
        posf, r_posf = SB("posf", [128, NT], F32, p1)
        ang, r_ang = SB("ang", [128, NT, 16], F32, p1)
        ang2, r_ang2 = SB("ang2", [128, NT, 16], F32, p1)
        xt = [SB("xt%d" % i, [128, DM], F32, p1) for i in range(2)]
        junk, r_junk = SB("junk", [128, DM], BF16, p1)
        xn, r_xn = SB("xn", [128, DM], BF16, p1)
        st1 = [SB("st1_%d" % i, [128, 8], F32, p1) for i in range(4)]
        hT2 = [SB("hT%d" % i, [128, 8, 512], BF16, p1) for i in range(2)]
        st0 = [SB("st0_%d" % i, [128, 2], F32, p1) for i in range(4)]
        rw, r_rw = SB("raw", [128, 8, 515], F32, p1)
        halo, r_halo = SB("halo", [128, 8, 3], F32, p1)
        cacc, r_cacc = SB("cacc", [128, 512], F32, p1)
        xact, r_xact = SB("xact", [128, 8, 512], BF16, p1)
        qag, r_qag = SB("qag", [128, 3, 512], F32, p1)
        kvag, r_kvag = SB("kvag", [128, 2, 512], F32, p1)
        sq5, r_sq5 = SB("sq5", [128, 5, 512], BF16, p1)
        rsb, r_rsb = SB("rsb", [128, 2, 512], F32, p1)
        qaTn, r_qaTn = SB("qaTn", [128, 3, 512], BF16, p1)
        kvaTn, r_kvaTn = SB("kvaTn", [128, 2, 512], BF16, p1)
        kng, r_kng = SB("kng", [128, 4, 512], BF16, p1)
        zs, r_zs = SB("zs", [128, 512], F32, p1)
        dtt, r_dtt = SB("dtt", [128, 8], F32, p1)
        dta, r_dta = SB("dta", [128, 8], F32, p1)
        krp, r_krp = SB("krp", [128, 4, 32], BF16, p1)
        K.I(pool, lambda: nc.gpsimd.memset(krp[:], 0.0), writes=[r_krp])
        krT, r_krT = SB("krT", [128, 128], BF16, p1)
        rtmp = [SB("rtmp%d" % i, [128, 8, 16], F32, p1) for i in range(4)]
        qn_tm, r_qn = SB("qn_tm", [128, 512], BF16, p1)
        qr_pad, r_qrp = SB("qr_pad", [128, 4, 2, 64], BF16, p1)
        K.I(pool, lambda: nc.gpsimd.memset(qr_pad[:], 0.0), writes=[r_qrp])
        QT, r_QT = SB("QT", [128, 2048], BF16, p1)
        K.I(pool, lambda: nc.gpsimd.memset(QT[:], 0.0), writes=[r_QT])
        Vt, r_Vt = SB("Vt", [128, 8, 65], BF16, p1)
        K.I(pool, lambda: nc.gpsimd.memset(Vt[:], 1.0), writes=[r_Vt])
        xs_tm, r_xs = SB("xs_tm", [128, 8, 64], BF16, p1)
        B_tm, r_Btm = SB("B_tm", [128, 256], BF16, p1)
        xdt, r_xdt = SB("xdt", [128, 8, 64], BF16, p1)
        xdtd, r_xdtd = SB("xdtd", [128, 2, 8, 64], BF16, p1)
        ecsm, r_ecsm = SB("ecsm", [128, 2, 8], F32, p1)
        dtem, r_dtem = SB("dtem", [128, 2, 8], F32, p1)
        lseg, r_lseg = SB("lseg", [128, 8, 128], F32, p1)
        dec, r_dec = SB("dec", [128, 8, 128], F32, p1)
        cbm, r_cbm = SB("cbm", [128, 2, 128], F32, p1)
        MT, r_MT = SB("MT", [128, 8, 128], BF16, p1)
        ecs, r_ecs = SB("ecs", [128, 8], F32, p1)
        dte, r_dte = SB("dte", [128, 8], F32, p1)
        cdB, r_cdB = SB("cdB", [128, 2, 8], F32, p1)
        yd, r_yd = SB("yd", [128, 8, 64], F32, p1)
        yt, r_yt = SB("yt", [128, 8, 64], F32, p1)
        yt2, r_yt2 = SB("yt2", [128, 8, 64], F32, p1)
        mixs, r_mixs = SB("mixs", [128, 512], BF16, p1)

        pbn = [0]

        def next_pb():
            i = pbn[0] % 2
            pbn[0] += 1
            return PBs[i], rPB[i]

        for b in range(2):
            bag = bagA[b]
            ld(sp, posi[:], pos_d[b], [r_posi])
            cp(dve, posf[:], posi[:], [r_posi], [r_posf])
            tt(dve, ang[:], posf[:].unsqueeze(2).broadcast_to((128, NT, 16)),
               invf[:].unsqueeze(1).broadcast_to((128, NT, 16)), ALU.mult, [r_posf, r_invf], [r_ang])
            ts(dve, ang2[:], ang[:], 1.0 / (2.0 * math.pi), MAGIC, ALU.mult, ALU.add, [r_ang], [r_ang2])
            ts(dve, ang2[:], ang2[:], -MAGIC, None, ALU.add, None, [r_ang2], [r_ang2])
            stt(dve, ang[:], ang2[:], -C1, ang[:], ALU.mult, ALU.add, [r_ang2, r_ang], [r_ang])
            stt(dve, ang[:], ang2[:], -C2, ang[:], ALU.mult, ALU.add, [r_ang2, r_ang], [r_ang])
            ts(dve, ang[:], ang[:], 3.14159, -3.14159, ALU.min, ALU.max, [r_ang], [r_ang])
            actf(sinT[:], ang[:], AF.Sin, [r_ang], [r_sin])
            ts(dve, ang2[:], ang[:], -1.0, None, ALU.mult, None, [r_ang], [r_ang2])
            tt(dve, ang2[:], ang2[:], ang[:], ALU.max, [r_ang2, r_ang], [r_ang2])
            ts(dve, ang2[:], ang2[:], -1.0, math.pi / 2.0, ALU.mult, ALU.add, [r_ang2], [r_ang2])
            actf(cosT[:], ang2[:], AF.Sin, [r_ang2], [r_cos])
            cut(0.30)
            K.I(dve, lambda: nc.vector.memset(Sst[:], 0.0), writes=[r_Sst])
            K.I(pool, lambda: nc.gpsimd.memset(Sbf[:], 0.0), writes=[r_Sbf])
            K.I(pool, lambda: nc.gpsimd.memset(halo[:], 0.0), writes=[r_halo])

            def step1(gi):
                hTW, r_hTW = hT2[gi % 2]
                for i in range(4):
                    ti = gi * 4 + i
                    gt = b * NT + ti
                    x_t, x_r = xt[gt % 2]
                    s1, r_s1 = st0[i]
                    ld(sp, x_t[:], x_d[gt * 128:(gt + 1) * 128, :], [x_r])
                    actf(junk[:], x_t[:], AF.Square, [x_r], [r_junk, r_s1], accum_out=s1[:, 0:1])
                    rstd_from(s1[:, 0:1], r_s1, s1[:, 1:2], r_s1, DM)
                    ts(dve, xn[:], x_t[:], s1[:, 1:2], None, ALU.mult, None, [x_r, r_s1], [r_xn])
                    for k in range(8):
                        tr(PT[:, k * 128:(k + 1) * 128], xn[:, k * 128:(k + 1) * 128], identb[:], [r_xn, r_identb], [rPT])
                    for k in range(8):
                        actf(hTW[:, k, i * 128:(i + 1) * 128], PT[:, k * 128:(k + 1) * 128], AF.Identity,
                             [rPT, r_a1T, r_sh1T], [r_hTW], bias=sh1T[:, b, k:k + 1], scale=a1T[:, b, k:k + 1])

            step1(0)
            for gi in range(NG):
                hT, r_hT = hT2[gi % 2]
                cut(0.32)
                cp(pool, rw[:, :, 0:3], halo[:], [r_halo], [r_rw])
                for mc in range(13):
                    pb, rpb = next_pb()
                    for k in range(8):
                        mm(pb[:], wfm[:, k, mc * 128:(mc + 1) * 128], hT[:, k, :], k == 0, k == 7, [r_wfm, r_hT], [rpb])
                    if mc < 8:
                        cp(act, rw[:, mc, 3:515], pb[:], [rpb], [r_rw])
                    elif mc < 11:
                        c = mc - 8
                        actf(qag[:, c, :], pb[:], AF.Copy, [rpb, small["qan"][1]], [r_qag], scale=small["qan"][0][:, c:c + 1])
                        actf(sq5[:, c, :], pb[:], AF.Square, [rpb], [r_sq5])
                    else:
                        c = mc - 11
                        actf(kvag[:, c, :], pb[:], AF.Copy, [rpb, small["kvan"][1]], [r_kvag], scale=small["kvan"][0][:, c:c + 1])
                        actf(sq5[:, 3 + c, :], pb[:], AF.Square, [rpb], [r_sq5])
                cp(pool, halo[:], rw[:, :, 512:515], [r_rw], [r_halo])
                cut(0.34)
                cw, r_cw = small["convw"]
                cb_, r_cb = small["convb"]
                for mc in range(8):
                    ts(dve, cacc[:], rw[:, mc, 0:512], cw[:, mc, 0:1], cb_[:, mc:mc + 1], ALU.mult, ALU.add,
                       [r_rw, r_cw, r_cb], [r_cacc])
                    for kk in range(1, 4):
                        stt(dve, cacc[:], rw[:, mc, kk:kk + 512], cw[:, mc, kk:kk + 1], cacc[:], ALU.mult, ALU.add,
                            [r_rw, r_cw, r_cacc], [r_cacc])
                    actf(xact[:, mc, :], cacc[:], AF.Silu, [r_cacc], [r_xact])
                cut(0.36)
                for (c0, ncn, n, slot) in ((0, 3, 384, 0), (3, 2, 256, 1)):
                    pb, rpb = PBs[2], rPB[2]
                    for c in range(ncn):
                        mm(pb[:], onesb[:], sq5[:, c0 + c, :], c == 0, c == ncn - 1, [r_onesb, r_sq5], [rpb])
                    rstd_from(pb[:], rpb, rsb[:, slot, :], r_rsb, n)
                for c in range(3):
                    tt(dve, qaTn[:, c, :], qag[:, c, :], rsb[:, 0, :], ALU.mult, [r_qag, r_rsb], [r_qaTn])
                for c in range(2):
                    tt(dve, kvaTn[:, c, :], kvag[:, c, :], rsb[:, 1, :], ALU.mult, [r_kvag, r_rsb], [r_kvaTn])
                cut(0.38)
                for j in range(4):
                    pb, rpb = next_pb()
                    for c in range(2):
                        mm(pb[:], wkv[:, c, j * 128:(j + 1) * 128], kvaTn[:, c, :], c == 0, c == 1, [r_wkv, r_kvaTn], [rpb])
                    cp(act, kng[:, j, :], pb[:], [rpb], [r_kng])
                st(sp, kn_s[b].rearrange("p (j s) -> p j s", j=4)[:, :, gi * 512:(gi + 1) * 512], kng[:], [r_kng], bag)
                cut(0.40)

                if gi + 1 < NG:
                    step1(gi + 1)
                for i in range(4):
                    ti = gi * 4 + i
                    gt = b * NT + ti
                    cs_ = slice(i * 128, (i + 1) * 128)
                    s1, r_s1 = st1[i]
                    for k in range(8):
                        mm(P2[:, 0:512], hT[:, k, cs_], wtm[:, k, 0:512], k == 0, k == 7, [r_hT, r_wtm], [rP2a])
                    for k in range(8):
                        mm(P2[:, 512:552], hT[:, k, cs_], wtm[:, k, 512:552], k == 0, k == 7, [r_hT, r_wtm], [rP2b])
                    actf(zs[:], P2[:, 0:512], AF.Silu, [rP2a], [r_zs])
                    tt(dve, dtt[:], P2[:, 512:520], small["dtb"][0][:], ALU.add, [rP2b, small["dtb"][1]], [r_dtt])
                    actf(dtt[:], dtt[:], AF.Exp, [r_dtt], [r_dtt])
                    actf(dtt[:], dtt[:], AF.Ln, [r_dtt], [r_dtt], bias=oneb[:, 0:1])
                    tt(dve, dta[:], dtt[:], aneg[:], ALU.mult, [r_dtt, r_aneg], [r_dta])
                    cut(0.42)
                    cs16 = cosT[:, ti, :]
                    sn16 = sinT[:, ti, :]
                    k1 = P2[:, 520:536]
                    k2 = P2[:, 536:552]
                    t0_, t1_, t2_, t3_ = [rtmp[q][0][:, 0, :] for q in range(4)]
                    rr = [rtmp[q][1] for q in range(4)]
                    tt(dve, t0_, k1, cs16, ALU.mult, [rP2b, r_cos], [rr[0]])
                    tt(dve, t1_, k2, sn16, ALU.mult, [rP2b, r_sin], [rr[1]])
                    tt(dve, t2_, k2, cs16, ALU.mult, [rP2b, r_cos], [rr[2]])
                    tt(dve, t3_, k1, sn16, ALU.mult, [rP2b, r_sin], [rr[3]])
                    tt(dve, krp[:, 0, 0:16], t0_, t1_, ALU.subtract, [rr[0], rr[1]], [r_krp])
                    tt(dve, krp[:, 0, 16:32], t2_, t3_, ALU.add, [rr[2], rr[3]], [r_krp])
                    cp(dve, krp[:, 2, :], krp[:, 0, :], [r_krp], [r_krp])
                    tr(PT[:, 0:128], krp[:].rearrange("p a c -> p (a c)"), identb[:], [r_krp, r_identb], [rPT])
                    cp(act, krT[:], PT[:, 0:128], [rPT], [r_krT])
                    st(sp, kr_s[b][:, ti * 128:(ti + 1) * 128], krT[:], [r_krT], bag)
                    cut(0.44)
                    for c in range(3):
                        mm(P2[:, 0:512], qaTn[:, c, cs_], wq[:, c, 0:512], c == 0, c == 2, [r_qaTn, r_wq], [rP2a])
                    for c in range(3):
                        mm(P2[:, 512:768], qaTn[:, c, cs_], wq[:, c, 512:768], c == 0, c == 2, [r_qaTn, r_wq], [rP2b])
                    cp(act, qn_tm[:], P2[:, 0:512], [rP2a], [r_qn])
                    qr = P2[:, 512:768].rearrange("p (h c) -> p h c", c=32)
                    q1 = qr[:, :, 0:16]
                    q2 = qr[:, :, 16:32]
                    cb8 = cs16.unsqueeze(1).broadcast_to((128, 8, 16))
                    sb8 = sn16.unsqueeze(1).broadcast_to((128, 8, 16))
                    T0, T1, T2, T3 = [rtmp[q][0][:] for q in range(4)]
                    tt(dve, T0, q1, cb8, ALU.mult, [rP2b, r_cos], [rr[0]])
                    tt(dve, T1, q2, sb8, ALU.mult, [rP2b, r_sin], [rr[1]])
                    tt(dve, T2, q2, cb8, ALU.mult, [rP2b, r_cos], [rr[2]])
                    tt(dve, T3, q1, sb8, ALU.mult, [rP2b, r_sin], [rr[3]])
                    qrv = qr_pad[:].rearrange("p j s c -> p s j c")
                    for s_ in range(2):
                        tt(dve, qrv[:, s_, :, 0:16], rtmp[0][0][:, s_ * 4:(s_ + 1) * 4, :], rtmp[1][0][:, s_ * 4:(s_ + 1) * 4, :],
                           ALU.subtract, [rr[0], rr[1]], [r_qrp])
                        tt(dve, qrv[:, s_, :, 16:32], rtmp[2][0][:, s_ * 4:(s_ + 1) * 4, :], rtmp[3][0][:, s_ * 4:(s_ + 1) * 4, :],
                           ALU.add, [rr[2], rr[3]], [r_qrp])
                    for j in range(4):
                        tr(PT[:, j * 128:(j + 1) * 128], qn_tm[:, j * 128:(j + 1) * 128], identb[:], [r_qn, r_identb], [rPT])
                    for j in range(4):
                        tr(PT[:, 512 + j * 128:512 + (j + 1) * 128], qr_pad[:, j, :, :].rearrange("p s c -> p (s c)"),
                           identb[:], [r_qrp, r_identb], [rPT])
                    cp(act, QT[0:64, 0:512], PT[0:64, 0:512], [rPT], [r_QT])
                    cp(act, QT[64:128, 512:1024], PT[64:128, 0:512], [rPT], [r_QT])
                    cp(act, QT[0:64, 1024:1536], PT[0:64, 512:1024], [rPT], [r_QT])
                    cp(act, QT[64:128, 1536:2048], PT[64:128, 512:1024], [rPT], [r_QT])
                    st(sp, q_s[gt], QT[:], [r_QT], bag)
                    cut(0.46)
                    pb, rpb = PBs[2], rPB[2]
                    for c in range(2):
                        mm(pb[:], kvaTn[:, c, cs_], wkv[:, c, 512:1024], c == 0, c == 1, [r_kvaTn, r_wkv], [rpb])
                    cp(act, Vt[:, :, 0:64], pb[:].rearrange("p (h c) -> p h c", c=64), [rpb], [r_Vt])
                    cut(0.475)
                    st(sp, v_s[b][:, ti * 520:(ti + 1) * 520], Vt[:].rearrange("p h c -> p (h c)"), [r_Vt], bag)
                    cut(0.48)

                    for j in range(4):
                        tr(PT[:, j * 128:(j + 1) * 128], xact[:, j, cs_], identb[:], [r_xact, r_identb], [rPT])
                    for j in range(2):
                        tr(PT[:, 512 + j * 128:512 + (j + 1) * 128], xact[:, 4 + j, cs_], identb[:], [r_xact, r_identb], [rPT])
                    cp(act, xs_tm[:].rearrange("p h c -> p (h c)"), PT[:, 0:512], [rPT], [r_xs])
                    cp(act, B_tm[:], PT[:, 512:768], [rPT], [r_Btm])
                    cut(0.495)
                    dt_b = dtt[:].unsqueeze(2).broadcast_to((128, 8, 64))
                    tt(dve, xdt[:], xs_tm[:], dt_b, ALU.mult, [r_xs, r_dtt], [r_xdt])
                    cut(0.50)
                    tt(dve, lseg[:], U1[:].unsqueeze(1).broadcast_to((128, 8, 128)),
                       dta[:].unsqueeze(2).broadcast_to((128, 8, 128)), ALU.mult, [r_U1, r_dta], [r_lseg])
                    for h in range(8):
                        rp = rP2a if h < 4 else rP2b
                        mm(P2[:, h * 128:(h + 1) * 128], lseg[:, h, :], U2[:], True, True, [r_lseg, r_U2], [rp])
                    actf(dec[:].rearrange("p h l -> p (h l)"), P2[:], AF.Exp, [rP2a, rP2b], [r_dec])
                    cut(0.51)
                    pb3, rpb3 = PBs[3], rPB[3]
                    for g in range(2):
                        mm(pb3[:, g * 128:(g + 1) * 128], xact[:, 4 + g, cs_], xact[:, 6 + g, cs_], True, True, [r_xact], [rpb3])
                    cut(0.516)
                    tt(dve, cbm[:], pb3[:, 0:256].rearrange("p (g l) -> p g l", g=2),
                       m01[:].unsqueeze(1).broadcast_to((128, 2, 128)), ALU.mult, [rpb3, r_m01], [r_cbm])
                    cut(0.518)
                    for h in range(8):
                        tt(dve, MT[:, h, :], dec[:, h, :], cbm[:, h // 4, :], ALU.mult, [r_dec, r_cbm], [r_MT])
                    cut(0.52)
                    pb4, rpb4 = PBs[4], rPB[4]
                    mm(pb4[:, 0:8], U2[:], dta[:], True, True, [r_U2, r_dta], [rpb4])
                    mm(pb4[:, 8:16], U1[:], dta[:], True, True, [r_U1, r_dta], [rpb4])
                    mm(pb4[:, 16:24], Mc[:, 0, :], dta[:], True, True, [r_Mc, r_dta], [rpb4])
                    mm(pb4[:, 24:32], Mc[:, 1, :], dta[:], True, True, [r_Mc, r_dta], [rpb4])
                    actf(ecs[:], pb4[:, 0:8], AF.Exp, [rpb4], [r_ecs])
                    actf(dte[:], pb4[:, 8:16], AF.Exp, [rpb4], [r_dte])
                    actf(cdB[:].rearrange("p c h -> p (c h)"), pb4[:, 16:32], AF.Exp, [rpb4], [r_cdB])
                    for c in range(2):
                        ts(dve, ecsm[:, c, :], ecs[:], rm[:, c:c + 1], None, ALU.mult, None, [r_ecs, r_rm], [r_ecsm])
                        ts(dve, dtem[:, c, :], dte[:], rm[:, c:c + 1], None, ALU.mult, None, [r_dte, r_rm], [r_dtem])
                        tt(dve, xdtd[:, c, :, :], xdt[:], dtem[:, c, :].unsqueeze(2).broadcast_to((128, 8, 64)), ALU.mult,
                           [r_xdt, r_dtem], [r_xdtd])
                    cut(0.53)
                    pb0, rpb0 = PBs[0], rPB[0]
                    for h in range(8):
                        mm(pb0[:, h * 64:(h + 1) * 64], MT[:, h, :], xdt[:, h, :], True, True, [r_MT, r_xdt], [rpb0])
                    cp(act, yd[:].rearrange("p h c -> p (h c)"), pb0[:], [rpb0], [r_yd])
                    cut(0.54)
                    pb2, rpb2 = PBs[2], rPB[2]
                    pbY = (PBs[1], PBs[3])
                    rpbY = (rPB[1], rPB[3])
                    for c in range(2):
                        for g in range(2):
                            mm(pbY[c][:, g * 256:(g + 1) * 256], xact[:, 6 + g, cs_],
                               Sbf[:, g * 4:(g + 1) * 4, :].rearrange("p h c -> p (h c)"), True, True, [r_xact, r_Sbf], [rpbY[c]])
                        for g in range(2):
                            mm(pb2[:, g * 256:(g + 1) * 256], B_tm[:, g * 128:(g + 1) * 128],
                               xdtd[:, c, g * 4:(g + 1) * 4, :].rearrange("p h c -> p (h c)"), True, True, [r_Btm, r_xdtd], [rpb2])
                        tt(dve, Sst[:], Sst[:], cdB[:, c, :].unsqueeze(2).broadcast_to((128, 8, 64)), ALU.mult, [r_Sst, r_cdB], [r_Sst])
                        tt(dve, Sst[:], Sst[:], pb2[:].rearrange("p (h c) -> p h c", c=64), ALU.add, [r_Sst, rpb2], [r_Sst])
                        cp(act, Sbf[:], Sst[:], [r_Sst], [r_Sbf])
                    cut(0.55)
                    tt(dve, yt[:], pbY[0][:].rearrange("p (h c) -> p h c", c=64), ecsm[:, 0, :].unsqueeze(2).broadcast_to((128, 8, 64)),
                       ALU.mult, [rpbY[0], r_ecsm], [r_yt])
                    tt(dve, yt2[:], pbY[1][:].rearrange("p (h c) -> p h c", c=64), ecsm[:, 1, :].unsqueeze(2).broadcast_to((128, 8, 64)),
                       ALU.mult, [rpbY[1], r_ecsm], [r_yt2])
                    tt(dve, yt[:], yt[:], yt2[:], ALU.add, [r_yt, r_yt2], [r_yt])
                    tt(dve, yt[:], yt[:], yd[:], ALU.add, [r_yt, r_yd], [r_yt])
                    tt(dve, yt2[:], xs_tm[:], small["dsk"][0][:].unsqueeze(2).broadcast_to((128, 8, 64)), ALU.mult,
                       [r_xs, small["dsk"][1]], [r_yt2])
                    tt(dve, yt[:], yt[:], yt2[:], ALU.add, [r_yt, r_yt2], [r_yt])
                    tt(dve, yt[:].rearrange("p h c -> p (h c)"), yt[:].rearrange("p h c -> p (h c)"), zs[:], ALU.mult,
                       [r_yt, r_zs], [r_yt])
                    for g in range(2):
                        actf(junk[:, g * 256:(g + 1) * 256], yt[:, g * 4:(g + 1) * 4, :].rearrange("p h c -> p (h c)"), AF.Square,
                             [r_yt], [r_junk, r_s1], accum_out=s1[:, 2 + g:3 + g])
                    rstd_from(s1[:, 2:4], r_s1, s1[:, 2:4], r_s1, 256)
                    for g in range(2):
                        stt(dve, mixs[:, g * 256:(g + 1) * 256], yt[:, g * 4:(g + 1) * 4, :].rearrange("p h c -> p (h c)"),
                            s1[:, 2 + g:3 + g], small["ssdn"][0][:, g * 256:(g + 1) * 256], ALU.mult, ALU.mult,
                            [r_yt, r_s1, small["ssdn"][1]], [r_mixs])
                    st(sp, mix_s[gt * 128:(gt + 1) * 128, 0:512], mixs[:], [r_mixs], bag)
                    cut(0.56)

    K.barrier()
    cut(0.6)
    with ExitStack() as pt_:
        KnT, r_KnT = SB("KnT", [128, 4, S], BF16, pt_)
        KrT, r_KrT = SB("KrT", [128, S], BF16, pt_)
        Vst, r_Vst = SB("Vst", [128, NT, 8, 65], BF16, pt_)
        Qb = [SB("Qb%d" % i, [128, 16, 128], BF16, pt_) for i in range(2)]
        PTs = [SB("PTs%d" % i, [128, 4, 128], BF16, pt_) for i in range(5)]
        rec, r_rec = SB("rec", [128, 8], F32, pt_)
        osb, r_osb = SB("osb", [128, 8, 64], F32, pt_)
        junk2, r_junk2 = SB("junk2", [128, 512], BF16, pt_)
        sA, r_sA = SB("sA", [128, 2], F32, pt_)
        mixm = [SB("mixm%d" % i, [128, 512], BF16, pt_) for i in range(2)]
        sc = 1.0 / math.sqrt(96.0)
        for b in range(2):
            K._wait(sp, bagA[b].evs())
            ld(sp, KnT[:].rearrange("p j s -> p (j s)"), kn_s[b], [r_KnT])
            ld(sp, KrT[:], kr_s[b], [r_KrT])
            ld(sp, Vst[:].rearrange("p t h c -> p (t h c)"), v_s[b], [r_Vst])
            for ti in range(NT):
                gt = b * NT + ti
                q_t, q_r = Qb[ti % 2]
                ld(sp, q_t[:].rearrange("p a q -> p (a q)"), q_s[gt], [q_r])
                nkt = ti + 1
                pbO = (P2[:, 0:512], P2[:, 512:1024])
                rpbO = (rP2a, rP2b)
                groups = [(h, k0, min(4, nkt - k0)) for h in range(8) for k0 in range(0, nkt, 4)]
                DEP = 3

                def emit_qk(gi):
                    h, k0, nk = groups[gi]
                    j = h % 4
                    hs = h // 4
                    pbs, rpbs = PBs[gi % 4], rPB[gi % 4]
                    for kk in range(nk):
                        kt = k0 + kk
                        kc = slice(kt * 128, (kt + 1) * 128)
                        mm(pbs[:, kk * 128:(kk + 1) * 128], KnT[:, j, kc], q_t[:, hs * 4 + j, :], True, False, [r_KnT, q_r], [rpbs])
                        mm(pbs[:, kk * 128:(kk + 1) * 128], KrT[:, kc], q_t[:, 8 + hs * 4 + j, :], False, True, [r_KrT, q_r], [rpbs])

                def emit_pv(gi):
                    h, k0, nk = groups[gi]
                    po, rpo = pbO[h // 4], rpbO[h // 4]
                    ocol = (h % 4) * 65
                    pbs, rpbs = PBs[gi % 4], rPB[gi % 4]
                    pts, rpts = PTs[gi % 5]
                    actf(pts[:, 0:nk, :].rearrange("p a q -> p (a q)"), pbs[:, 0:nk * 128], AF.Exp, [rpbs], [rpts], scale=sc)
                    if k0 + nk == nkt:
                        K.I(dve, lambda: nc.vector.memset(pts[64:128, nk - 1, 0:64], 0.0), writes=[rpts])
                    for kk in range(nk):
                        kt = k0 + kk
                        mm(po[:, ocol:ocol + 65], pts[:, kk, :], Vst[:, kt, h, :], kt == 0, kt == nkt - 1, [rpts, r_Vst], [rpo])

                for gi in range(min(DEP, len(groups))):
                    emit_qk(gi)
                for gi in range(len(groups)):
                    if gi + DEP < len(groups):
                        emit_qk(gi + DEP)
                    emit_pv(gi)
                for hh in range(2):
                    ov = pbO[hh][:, 0:260].rearrange("p (h c) -> p h c", c=65)
                    K.I(dve, lambda: nc.vector.reciprocal(rec[:, hh * 4:(hh + 1) * 4], ov[:, :, 64]), reads=[rpbO[hh]], writes=[r_rec])
                    tt(dve, osb[:, hh * 4:(hh + 1) * 4, :], ov[:, :, 0:64],
                       rec[:, hh * 4:(hh + 1) * 4].unsqueeze(2).broadcast_to((128, 4, 64)), ALU.mult, [rpbO[hh], r_rec], [r_osb])
                if gt == 1:
                    dump("osb", osb[:], r_osb, [128, 8, 64])
                    dump("rec", rec[:], r_rec, [128, 8])
                    dump("pts", PTs[0][0][:], PTs[0][1], [128, 4, 128], BF16)
                    dump("qb", q_t[:], q_r, [128, 16, 128], BF16)
                    dump("KrT", KrT[:], r_KrT, [128, S], BF16)
                    dump("KnT", KnT[:], r_KnT, [128, 4, S], BF16)
                    dump("Vst", Vst[:], r_Vst, [128, NT, 8, 65], BF16)
                actf(junk2[:], osb[:].rearrange("p h c -> p (h c)"), AF.Square, [r_osb], [r_junk2, r_sA], accum_out=sA[:, 0:1])
                rstd_from(sA[:, 0:1], r_sA, sA[:, 1:2], r_sA, 512)
                m_t, m_r = mixm[ti % 2]
                stt(dve, m_t[:], osb[:].rearrange("p h c -> p (h c)"), sA[:, 1:2], small["mlan"][0][:],
                    ALU.mult, ALU.mult, [r_osb, r_sA, small["mlan"][1]], [m_r])
                st(sp, mix_s[gt * 128:(gt + 1) * 128, 512:1024], m_t[:], [m_r], bagT[b])

    K.barrier()
    cut(0.8)
    with ExitStack() as pb_:
        wout, r_wout = SB("wout", [128, 8, 1024], BF16, pb_)
        with ExitStack() as pst:
            stg = [SB("stgo%d" % i, [128, 1024], F32, pst) for i in range(2)]
            for k in range(8):
                s_t, s_r = stg[k % 2]
                ld(sp, s_t[:], wout_d[:, k, :], [s_r])
                cast_any(wout[:, k, :], s_t[:], [s_r], [r_wout])
        K.barrier()
        G1, r_G1 = SB("G1", [128, DM], F32, pb_)
        A2, r_A2 = SB("A2", [128, DM], F32, pb_)
        SH2, r_SH2 = SB("SH2", [128, DM], F32, pb_)
        xt = [SB("xtb%d" % i, [128, DM], F32, pb_) for i in range(2)]
        mixin = [SB("mixin%d" % i, [128, DM], BF16, pb_) for i in range(2)]
        mixT, r_mixT = SB("mixT", [128, 8, 128], BF16, pb_)
        junk, r_junk = SB("junkb", [128, DM], BF16, pb_)
        x1, r_x1 = SB("x1", [128, DM], F32, pb_)
        h2s = [SB("h2_%d" % i, [128, DM], F32, pb_) for i in range(2)]
        h2b, r_h2b = SB("h2b", [128, DM], BF16, pb_)
        h2T, r_h2T = SB("h2T", [128, 8, 128], F32, pb_)
        s1s = [SB("s1b%d" % i, [128, 8], F32, pb_) for i in range(2)]
        nv0, r_nv0 = SB("nv0", [128, 1], F32, pb_)
        e4, r_e4 = SB("e4", [128, 4], F32, pb_)
        pb4, rpb4 = PBs[4], rPB[4]
        bag1 = Bag()
        for b in range(2):
            K._wait(sp, bagT[b].evs() + bagA[b].evs())
            tt(dve, vtmp[:], modT[:, 16:24, b], small["gpost1"][0][:], ALU.mult, [r_modT, small["gpost1"][1]], [r_vtmp])
            bcast_rows(vtmp, r_vtmp, G1, r_G1)
            ts(dve, vtmp[:], modT[:, 32:40, b], 1.0, None, ALU.add, None, [r_modT], [r_vtmp])
            tt(dve, vtmp[:], vtmp[:], small["gpre2"][0][:], ALU.mult, [r_vtmp, small["gpre2"][1]], [r_vtmp])
            bcast_rows(vtmp, r_vtmp, A2, r_A2)
            cp(dve, vtmp[:], modT[:, 24:32, b], [r_modT], [r_vtmp])
            bcast_rows(vtmp, r_vtmp, SH2, r_SH2)
            def stageA(ti):
                gt = b * NT + ti
                x_t, x_r = xt[gt % 2]
                mi, r_mi = mixin[gt % 2]
                h2, r_h2 = h2s[gt % 2]
                s1, r_s1 = s1s[gt % 2]
                ld(sp, x_t[:], x_d[gt * 128:(gt + 1) * 128, :], [x_r])
                ld(sp, mi[:], mix_s[gt * 128:(gt + 1) * 128, :], [r_mi])
                for k in range(8):
                    tr(PT[:, k * 128:(k + 1) * 128], mi[:, k * 128:(k + 1) * 128], identb[:], [r_mi, r_identb], [rPT])
                cp(act, mixT[:].rearrange("p k t -> p (k t)"), PT[:], [rPT], [r_mixT])
                for hf in range(2):
                    rp = rP2a if hf == 0 else rP2b
                    for k in range(8):
                        mm(P2[:, hf * 512:(hf + 1) * 512], mixT[:, k, :], wout[:, k, hf * 512:(hf + 1) * 512], k == 0, k == 7,
                           [r_mixT, r_wout], [rp])
                actf(junk[:], P2[:], AF.Square, [rP2a, rP2b], [r_junk, r_s1], accum_out=s1[:, 0:1])
                rstd_from(s1[:, 0:1], r_s1, s1[:, 1:2], r_s1, DM)
                stt(dve, x1[:], P2[:], s1[:, 1:2], G1[:], ALU.mult, ALU.mult, [rP2a, rP2b, r_s1, r_G1], [r_x1])
                tt(pool, x1[:], x1[:], x_t[:], ALU.add, [r_x1, x_r], [r_x1])
                st(sp, x1s_d[gt * 128:(gt + 1) * 128, :], x1[:], [r_x1], bag1)
                actf(junk[:], x1[:], AF.Square, [r_x1], [r_junk, r_s1], accum_out=s1[:, 2:3])
                rstd_from(s1[:, 2:3], r_s1, s1[:, 3:4], r_s1, DM)
                stt(dve, h2[:], x1[:], s1[:, 3:4], A2[:], ALU.mult, ALU.mult, [r_x1, r_s1, r_A2], [r_h2])
                tt(pool, h2[:], h2[:], SH2[:], ALU.add, [r_h2, r_SH2], [r_h2])
                cp(act, h2b[:], h2[:], [r_h2], [r_h2b])
                st(sp, h2s_d[gt * 128:(gt + 1) * 128, :], h2b[:], [r_h2b], bag1)

            def stageB(ti):
                gt = b * NT + ti
                h2, r_h2 = h2s[gt % 2]
                s1, r_s1 = s1s[gt % 2]
                for k in range(8):
                    pbx, rpx = PBs[k // 4], rPB[k // 4]
                    tr(pbx[:, (k % 4) * 128:(k % 4 + 1) * 128], h2[:, k * 128:(k + 1) * 128], identf[:], [r_h2, r_identf], [rpx])
                for hf in range(2):
                    cp(act, h2T[:, hf * 4:(hf + 1) * 4, :].rearrange("p k t -> p (k t)"), PBs[hf][:], [rPB[hf]], [r_h2T])
                wr_t, r_wr = small["wr"]
                for k in range(8):
                    mm(pb4[:, 0:NE], h2T[:, k, :], wr_t[:, k, :], k == 0, k == 7, [r_h2T, r_wr], [rpb4])
                lgt = lg_all[:, gt, :]
                tt(dve, lgt, pb4[:, 0:NE], small["br"][0][:], ALU.add, [rpb4, small["br"][1]], [r_lg])
                K.I(dve, lambda: nc.vector.max(v8_all[:, gt, :], lgt), reads=[r_lg], writes=[r_v8])
                ts(dve, nv0[:], v8_all[:, gt, 0:1], -1.0, None, ALU.mult, None, [r_v8], [r_nv0])
                actf(e4[:], v8_all[:, gt, 0:4], AF.Exp, [r_v8, r_nv0], [r_e4, r_s1], bias=nv0[:, 0:1], accum_out=s1[:, 4:5])
                K.I(dve, lambda: nc.vector.reciprocal(s1[:, 5:6], s1[:, 4:5]), reads=[r_s1], writes=[r_s1])
                ts(dve, gate_all[:, gt, :], e4[:], s1[:, 5:6], None, ALU.mult, None, [r_e4, r_s1], [r_gate])

            stageA(0)
            for ti in range(NT):
                if ti + 1 < NT:
                    stageA(ti + 1)
                stageB(ti)
    K.barrier()
    dump("lg", lg_all[:], r_lg, [128, NTT, NE])
    dump("v8", v8_all[:], r_v8, [128, NTT, 8])
    cut(2)

    base, r_base = SB("base", [128, NE])
    msk, r_msk = SB("msk", [128, NE])
    pos_all, r_pos = SB("pos_all", [128, NTT, NE])
    K.I(dve, lambda: nc.vector.memset(base[:], 0.0), writes=[r_base])
    pb4, rpb4 = PBs[4], rPB[4]
    for gt in range(NTT):
        ts(dve, msk[:], lg_all[:, gt, :], v8_all[:, gt, 3:4], None, ALU.is_ge, None, [r_lg, r_v8], [r_msk])
        mm(pb4[:, 32:64], Lst[:], msk[:], True, True, [r_Lst, r_msk], [rpb4])
        mm(pb4[:, 64:96], onesf[:], msk[:], True, True, [r_onesf, r_msk], [rpb4])
        tt(dve, pos_all[:, gt, :], pb4[:, 32:64], base[:], ALU.add, [rpb4, r_base], [r_pos])
        tt(dve, base[:], pb4[:, 64:96], base[:], ALU.add, [rpb4, r_base], [r_base])
    nblk, r_nblk = SB("nblk", [128, NE])
    incl, r_incl = SB("incl", [128, NE])
    pstart, r_pstart = SB("pstart", [128, NE])
    iotab, r_iotab = SB("iotab", [128, NB])
    bexp_f, r_bexpf = SB("bexp_f", [128, NB])
    bexp_i, r_bexpi = SB("bexp_i", [128, NB], I32)
    dest_i, r_desti = SB("dest_i", [128, NTT, 4], I32)
    widx_i, r_widx = SB("widx_i", [128, NB, 8], I32)
    bidx_i, r_bidx = SB("bidx_i", [128, NB], I32)
    pk, r_pk = SB("pk", [128, 8])
    c1024, r_c1024 = SB("c1024", [128, 8])
    ld(sp, pk[:], pk_d, [r_pk])
    K.I(dve, lambda: nc.vector.memset(c1024[:], 1024.0), writes=[r_c1024])
    ld(sp, iotab[:], iotab_d, [r_iotab])
    off = -0.5 + 1.0 / 1024.0
    ts(dve, nblk[:], base[:], float(BLK - 1), 1.0 / BLK, ALU.add, ALU.mult, [r_base], [r_nblk])
    ts(dve, nblk[:], nblk[:], off, MAGIC, ALU.add, ALU.add, [r_nblk], [r_nblk])
    ts(dve, nblk[:], nblk[:], -MAGIC, None, ALU.add, None, [r_nblk], [r_nblk])
    K.I(dve, lambda: nc.vector.tensor_tensor_scan(incl[:], onesf[:, 0:NE], nblk[:], 0.0, ALU.mult, ALU.add),
        reads=[r_onesf, r_nblk], writes=[r_incl])
    tt(dve, pstart[:], incl[:], nblk[:], ALU.subtract, [r_incl, r_nblk], [r_pstart])
    ts(dve, pstart[:], pstart[:], float(BLK), None, ALU.mult, None, [r_pstart], [r_pstart])
    with ExitStack() as p2a:
        cmp3, r_cmp3 = SB("cmp3", [128, NB, NE], F32, p2a)
        tt(dve, cmp3[:], incl[:].unsqueeze(1).broadcast_to((128, NB, NE)), iotab[:].unsqueeze(2).broadcast_to((128, NB, NE)),
           ALU.is_le, [r_incl, r_iotab], [r_cmp3])
        K.I(dve, lambda: nc.vector.tensor_reduce(bexp_f[:], cmp3[:], AX.X, ALU.add), reads=[r_cmp3], writes=[r_bexpf])
        ts(dve, bexp_f[:], bexp_f[:], float(NE - 1), None, ALU.min, None, [r_bexpf], [r_bexpf])
        cp(dve, bexp_i[:], bexp_f[:], [r_bexpf], [r_bexpi])
        widx_f, r_widxf = SB("widx_f", [128, NB, 8], F32, p2a)
        tt(dve, widx_f[:], bexp_f[:].unsqueeze(2).broadcast_to((128, NB, 8)), c1024[:].unsqueeze(1).broadcast_to((128, NB, 8)),
           ALU.mult, [r_bexpf, r_c1024], [r_widxf])
        tt(dve, widx_f[:], widx_f[:], pk[:].unsqueeze(1).broadcast_to((128, NB, 8)), ALU.add, [r_widxf, r_pk], [r_widxf])
        cp(dve, widx_i[:], widx_f[:], [r_widxf], [r_widx])
        ts(dve, bexp_f[:], bexp_f[:], 128.0, pk[:, 0:1], ALU.mult, ALU.add, [r_bexpf, r_pk], [r_bexpf])
        cp(dve, bidx_i[:], bexp_f[:], [r_bexpf], [r_bidx])
        destf, r_destf = SB("destf", [128, NE], F32, p2a)
        oh, r_oh = SB("oh", [128, 4, NE], F32, p2a)
        dk, r_dk = SB("dk", [128, 4], F32, p2a)
        hb = [SB("hb%d" % i, [128, DM], BF16, p2a) for i in range(2)]
        r_xg = Res()
        for gt in range(NTT):
            tt(dve, destf[:], pos_all[:, gt, :], pstart[:], ALU.add, [r_pos, r_pstart], [r_destf])
            tt(dve, oh[:], lg_all[:, gt, :].unsqueeze(1).broadcast_to((128, 4, NE)),
               v8_all[:, gt, 0:4].unsqueeze(2).broadcast_to((128, 4, NE)), ALU.is_equal, [r_lg, r_v8], [r_oh])
            tt(dve, oh[:], oh[:], destf[:].unsqueeze(1).broadcast_to((128, 4, NE)), ALU.mult, [r_oh, r_destf], [r_oh])
            K.I(dve, lambda: nc.vector.tensor_reduce(dk[:], oh[:], AX.X, ALU.add), reads=[r_oh], writes=[r_dk])
            cp(dve, dest_i[:, gt, :], dk[:], [r_dk], [r_desti])
            h_t, h_r = hb[gt % 2]
            ld(sp, h_t[:], h2s_d[gt * 128:(gt + 1) * 128, :], [h_r])
            if gt == 0:
                K._wait(sp, bag1.evs())
            for k in range(4):
                K.dma(pool, lambda: nc.gpsimd.indirect_dma_start(
                    out=xg_d, out_offset=bass.IndirectOffsetOnAxis(ap=dest_i[:, gt, k:k + 1], axis=0),
                    in_=h_t[:], in_offset=None), reads=[h_r, r_desti], writes=[])
    K.barrier()
    dump("desti", dest_i[:], r_desti, [128, NTT, 4], I32)
    dump("bexp", bexp_i[:], r_bexpi, [128, NB], I32)

    cut(3)
    scat_evs = [(sid, K.semvals[sid]) for sid in pool.ring if K.semvals[sid] > 0]

    with ExitStack() as p2:
        wgu = [SB("wgu%d" % i, [128, 8, 2 * DM], BF16, p2) for i in range(2)]
        wdn = [SB("wdn%d" % i, [128, 8, DM], BF16, p2) for i in range(2)]
        bgu = [SB("bgus%d" % i, [128, 16], F32, p2) for i in range(2)]
        bdn = [SB("bdns%d" % i, [128, DM], F32, p2) for i in range(2)]
        xgt = [SB("xgt%d" % i, [128, DM], BF16, p2) for i in range(2)]
        xgTs = [SB("xgT%d" % i, [128, 8, BLK], BF16, p2) for i in range(2)]
        actT, r_actT = SB("actT", [128, 8, BLK], BF16, p2)
        gg, r_gg = SB("gg", [128, BLK], F32, p2)
        sg, r_sg = SB("sg", [128, BLK], F32, p2)
        ll, r_ll = SB("ll", [128, BLK], F32, p2)
        yo = [SB("yo%d" % i, [128, DM], F32, p2) for i in range(2)]
        K._wait(sp, scat_evs)
        wgr = [[Res() for _ in range(8)] for _ in range(2)]
        wdr = [[Res() for _ in range(8)] for _ in range(2)]

        def load_weights(blk):
            wg_t = wgu[blk % 2][0]
            wd_t = wdn[blk % 2][0]
            bg_t, bg_r = bgu[blk % 2]
            bd_t, bd_r = bdn[blk % 2]
            for k in range(8):
                K.dma(pool, lambda: nc.gpsimd.indirect_dma_start(
                    out=wg_t[:, k, :], out_offset=None, in_=wgu_d,
                    in_offset=bass.IndirectOffsetOnAxis(ap=widx_i[:, blk, k:k + 1], axis=0)), reads=[r_widx], writes=[wgr[blk % 2][k]])
            for k in range(8):
                K.dma(pool, lambda: nc.gpsimd.indirect_dma_start(
                    out=wd_t[:, k, :], out_offset=None, in_=wd_d,
                    in_offset=bass.IndirectOffsetOnAxis(ap=widx_i[:, blk, k:k + 1], axis=0)), reads=[r_widx], writes=[wdr[blk % 2][k]])
            K.dma(pool, lambda: nc.gpsimd.indirect_dma_start(
                out=bg_t[:], out_offset=None, in_=bgu_d,
                in_offset=bass.IndirectOffsetOnAxis(ap=bidx_i[:, blk:blk + 1], axis=0)), reads=[r_bidx], writes=[bg_r])
            K.dma(pool, lambda: nc.gpsimd.indirect_dma_start(
                out=bd_t[:], out_offset=None, in_=bd_d,
                in_offset=bass.IndirectOffsetOnAxis(ap=bidx_i[:, blk:blk + 1], axis=0)), reads=[r_bidx], writes=[bd_r])

        PT2 = PBs[4][:].bitcast(BF16)
        ptn = [0]

        def prep_tokens(blk):
            xgT_, r_xgT_ = xgTs[blk % 2]
            for st in range(4):
                g_t, g_r = xgt[st % 2]
                r0 = blk * BLK + st * 128
                ld(sp, g_t[:], xg_d[r0:r0 + 128, :], [g_r])
                if ptn[0] % 2 == 0:
                    pt_ap, pt_r = PT[:], rPT
                else:
                    pt_ap, pt_r = PT2, rPB[4]
                ptn[0] += 1
                for k in range(8):
                    tr(pt_ap[:, k * 128:(k + 1) * 128], g_t[:, k * 128:(k + 1) * 128], identb[:], [g_r, r_identb], [pt_r])
                cp(act, xgT_[:, :, st * 128:(st + 1) * 128],
                   pt_ap.rearrange("p (k t) -> p k t", t=128), [pt_r], [r_xgT_])

        load_weights(0)
        prep_tokens(0)
        for blk in range(NB):
            if blk + 1 < NB:
                load_weights(blk + 1)
                prep_tokens(blk + 1)
            wg_t = wgu[blk % 2][0]
            wd_t = wdn[blk % 2][0]
            wg_rs = wgr[blk % 2]
            wd_rs = wdr[blk % 2]
            bg_t, bg_r = bgu[blk % 2]
            bd_t, bd_r = bdn[blk % 2]
            xgT, r_xgT = xgTs[blk % 2]
            for fc in range(8):
                pg, rpg = PBs[(fc % 2) * 2], rPB[(fc % 2) * 2]
                pl, rpl = PBs[(fc % 2) * 2 + 1], rPB[(fc % 2) * 2 + 1]
                for k in range(8):
                    mm(pg[:], wg_t[:, k, fc * 128:(fc + 1) * 128], xgT[:, k, :], k == 0, k == 7, [wg_rs[k], r_xgT], [rpg])
                for k in range(8):
                    mm(pl[:], wg_t[:, k, DM + fc * 128:DM + (fc + 1) * 128], xgT[:, k, :], k == 0, k == 7, [wg_rs[k], r_xgT], [rpl])
                ts(dve, gg[:], pg[:], bg_t[:, fc:fc + 1], 7.0, ALU.add, ALU.min, [rpg, bg_r], [r_gg])
                actf(sg[:], gg[:], AF.Sigmoid, [r_gg], [r_sg], scale=1.702)
                ts(dve, ll[:], pl[:], bg_t[:, 8 + fc:9 + fc], 7.0, ALU.add, ALU.min, [rpl, bg_r], [r_ll])
                ts(dve, ll[:], ll[:], -7.0, 1.0, ALU.max, ALU.add, [r_ll], [r_ll])
                tt(dve, gg[:], gg[:], sg[:], ALU.mult, [r_gg, r_sg], [r_gg])
                tt(dve, actT[:, fc, :], gg[:], ll[:], ALU.mult, [r_gg, r_ll], [r_actT])
            dbanks = ((P2[:, 0:512], rP2a), (P2[:, 512:1024], rP2b), (PBs[2][:], rPB[2]), (PBs[3][:], rPB[3]))
            for st in range(4):
                y_t, y_r = yo[st % 2]
                for hf in range(2):
                    pd, rpd = dbanks[(st % 2) * 2 + hf]
                    for k in range(8):
                        mm(pd, actT[:, k, st * 128:(st + 1) * 128], wd_t[:, k, hf * 512:(hf + 1) * 512], k == 0, k == 7,
                           [r_actT, wd_rs[k]], [rpd])
                    tt(dve, y_t[:, hf * 512:(hf + 1) * 512], pd, bd_t[:, hf * 512:(hf + 1) * 512], ALU.add, [rpd, bd_r], [y_r])
                r0 = blk * BLK + st * 128
                K.dma(act, lambda: nc.scalar.dma_start(out=yb_d[r0:r0 + 128, :], in_=y_t[:]), reads=[y_r], writes=[])
    K.barrier()
    yb_evs = [(sid, K.semvals[sid]) for sid in act.ring if K.semvals[sid] > 0]

    with ExitStack() as p3:
        yg = [SB("yg%d" % i, [128, DM], F32, p3) for i in range(8)]
        fa, r_fa = SB("fa", [128, DM], F32, p3)
        x1r = [SB("x1r%d" % i, [128, DM], F32, p3) for i in range(2)]
        ob = [SB("ob%d" % i, [128, DM], F32, p3) for i in range(2)]
        junk3, r_junk3 = SB("junk3", [128, DM], BF16, p3)
        s3, r_s3 = SB("s3", [128, 2], F32, p3)
        G2, r_G2 = SB("G2", [128, 2, DM], F32, p3)
        for b in range(2):
            tt(dve, vtmp[:], modT[:, 40:48, b], small["gpost2"][0][:], ALU.mult, [r_modT, small["gpost2"][1]], [r_vtmp])
            bcast_rows(vtmp, r_vtmp, G2[:, b, :], r_G2)
        K._wait(pool, yb_evs)
        K._wait(sp, bag1.evs())
        def p3_loads(gt):
            x_t, x_r = x1r[gt % 2]
            ld(sp, x_t[:], x1s_d[gt * 128:(gt + 1) * 128, :], [x_r])
            for k in range(4):
                y_t, y_r = yg[(gt % 2) * 4 + k]
                K.dma(pool, lambda: nc.gpsimd.indirect_dma_start(
                    out=y_t[:], out_offset=None, in_=yb_d,
                    in_offset=bass.IndirectOffsetOnAxis(ap=dest_i[:, gt, k:k + 1], axis=0)), reads=[r_desti], writes=[y_r])

        def p3_compute(gt):
            b = gt // NT
            x_t, x_r = x1r[gt % 2]
            o_t, o_r = ob[gt % 2]
            ygs = [yg[(gt % 2) * 4 + k] for k in range(4)]
            actf(fa[:], ygs[0][0][:], AF.Copy, [ygs[0][1], r_gate], [r_fa], scale=gate_all[:, gt, 0:1])
            for k in range(1, 4):
                stt(dve, fa[:], ygs[k][0][:], gate_all[:, gt, k:k + 1], fa[:], ALU.mult, ALU.add, [ygs[k][1], r_gate, r_fa], [r_fa])
            actf(junk3[:], fa[:], AF.Square, [r_fa], [r_junk3, r_s3], accum_out=s3[:, 0:1])
            actf(s3[:, 1:2], s3[:, 0:1], AF.Ln, [r_s3], [r_s3], scale=1.0 / DM, bias=epsb[:, 0:1])
            actf(s3[:, 1:2], s3[:, 1:2], AF.Exp, [r_s3], [r_s3], scale=-0.5)
            stt(dve, o_t[:], fa[:], s3[:, 1:2], G2[:, b, :], ALU.mult, ALU.mult, [r_fa, r_s3, r_G2], [o_r])
            tt(pool, o_t[:], o_t[:], x_t[:], ALU.add, [o_r, x_r], [o_r])
            K.dma(sp, lambda: nc.sync.dma_start(out=out_d[gt * 128:(gt + 1) * 128, :], in_=o_t[:]), reads=[o_r], writes=[], is_out=True)

        p3_loads(0)
        for gt in range(NTT):
            if gt + 1 < NTT:
                p3_loads(gt + 1)
            p3_compute(gt)
    K.barrier()
    K.finish()
    es.close()
    return nc, dbg_d


def _consts(NB):
    idx = np.arange(128)
    ch = idx // 64
    same = ch[:, None] == ch[None, :]
    U1 = (same & (idx[:, None] > idx[None, :])).astype(np.float32)
    U2 = (same & (idx[:, None] <= idx[None, :])).astype(np.float32)
    m01 = U2.copy()
    Lst = (idx[:, None] < idx[None, :]).astype(np.float32)
    invf = (np.float32(10000.0) ** (-(np.arange(16, dtype=np.float32) * np.float32(2.0) / np.float32(32.0)))).astype(np.float32)
    rm = np.stack([(ch == 0), (ch == 1)], axis=1).astype(np.float32)
    return dict(identf=np.eye(128, dtype=np.float32), U1=U1, U2=U2, m01=m01, Lst=Lst, rm=rm,
                invf=np.tile(invf[None, :], (128, 1)).astype(np.float32),
                iotab=np.tile(np.arange(NB, dtype=np.float32)[None, :], (128, 1)),
                pk=(np.arange(8, dtype=np.float32)[None, :] * 128 + np.arange(128, dtype=np.float32)[:, None]).astype(np.float32))


def _rep(v, n=128):
    return np.ascontiguousarray(np.tile(np.asarray(v, np.float32).reshape(1, -1), (n, 1)))


def _pk(v, k):
    return np.ascontiguousarray(np.asarray(v, np.float32).reshape(k, 128).T)


_CACHE = {}


def kernel(x, c, positions, w_ada, b_ada, pre_mix_norm, w_in, conv_w, conv_b, dt_bias, a_log, d_skip, ssd_norm,
           q_a_norm, w_q_up, kv_a_norm, w_kv_up, mla_norm, w_out, post_mix_norm, pre_ffn_norm, w_router, b_router,
           w_gate_up, b_gate_up, w_down, b_down, post_ffn_norm, _dbg=()):
    x = np.asarray(x, np.float32)
    Bsz, S, _ = x.shape
    ncores = Bsz // 2
    NT = S // 128
    NB = (2 * S * 4) // BLK + NE
    key = (S, tuple(_dbg))
    if key not in _CACHE:
        _CACHE[key] = build(S, _dbg)
    nc, dbg_d = _CACHE[key]
    f = lambda a: np.ascontiguousarray(np.asarray(a, np.float32))
    w_in = f(w_in)[0]
    w_tm = np.ascontiguousarray(np.concatenate([w_in[:, 0:512], w_in[:, 1536:1544], w_in[:, 2184:2216]], axis=1))
    w_fm = np.ascontiguousarray(np.concatenate([w_in[:, 512:1536], w_in[:, 1544:1928], w_in[:, 1928:2184]], axis=1))
    wq = f(w_q_up)[0].reshape(384, 8, 96)
    qn = wq[:, :, 0:64]
    qr = wq[:, :, 64:96]
    qn_pairs = np.stack([np.concatenate([qn[:, j], qn[:, j + 4]], axis=1) for j in range(4)], axis=1)
    wq2 = np.ascontiguousarray(np.concatenate([qn_pairs.reshape(384, 512), qr.reshape(384, 256)], axis=1))
    wkv = f(w_kv_up)[0].reshape(256, 8, 128)
    kn = wkv[:, :, 0:64]
    vv = wkv[:, :, 64:128]
    kn_pairs = np.stack([np.concatenate([kn[:, j], kn[:, j + 4]], axis=1) for j in range(4)], axis=1)
    wkv2 = np.ascontiguousarray(np.concatenate([kn_pairs.reshape(256, 512), vv.reshape(256, 512)], axis=1))
    cw = f(conv_w)[0]
    convw = np.ascontiguousarray(cw.reshape(4, 8, 128).transpose(2, 1, 0))
    shared = dict(
        w_ada=f(w_ada)[0], b_adaT=_pk(f(b_ada)[0], 48), g_pre1=_pk(f(pre_mix_norm)[0], 8), g_post1=_pk(f(post_mix_norm)[0], 8),
        g_pre2=_pk(f(pre_ffn_norm)[0], 8), g_post2=_pk(f(post_ffn_norm)[0], 8), w_tm=w_tm, w_fm=w_fm, convw=convw,
        convb=_pk(f(conv_b)[0], 8), dtb=_rep(f(dt_bias)[0]), alog=_rep(f(a_log)[0]), dsk=_rep(f(d_skip)[0]),
        ssdn=_rep(f(ssd_norm)[0]), mlan=_rep(f(mla_norm)[0]), qan=_pk(f(q_a_norm)[0], 3), kvan=_pk(f(kv_a_norm)[0], 2),
        wq=wq2, wkv=wkv2, wout=f(w_out)[0], wr=f(w_router)[0], br=_rep(f(b_router)[0]),
        wgu=f(w_gate_up)[0].reshape(NE * DM, 2 * DM), wd=f(w_down)[0].reshape(NE * DM, DM),
        bgu=np.ascontiguousarray(f(b_gate_up)[0].reshape(NE, 16, 128).transpose(0, 2, 1)).reshape(NE * 128, 16),
        bd=np.ascontiguousarray(np.broadcast_to(f(b_down)[0][:, None, :], (NE, 128, DM))).reshape(NE * 128, DM),
    )
    shared.update(_consts(NB))
    cc = f(c)
    pp = np.asarray(positions, np.int32)
    in_maps = []
    for ci in range(ncores):
        m = dict(shared)
        m["x"] = np.ascontiguousarray(x[2 * ci:2 * ci + 2].reshape(2 * S, DM))
        m["cT"] = np.ascontiguousarray(cc[2 * ci:2 * ci + 2].reshape(2, 8, 128).transpose(2, 1, 0))
        m["pos"] = np.ascontiguousarray(pp[2 * ci:2 * ci + 2].reshape(2, NT, 128).transpose(0, 2, 1))
        in_maps.append(m)
    res = run_bass_kernel_spmd(nc, in_maps, core_ids=list(range(ncores)))
    out = np.stack([np.asarray(r["out"], np.float32).reshape(2, S, DM) for r in res.results], axis=0).reshape(Bsz, S, DM)
    if _dbg:
        return out, [{k: np.asarray(r["dbg_" + k]) for k in dbg_d} for r in res.results]
    return out
```

```python
import math
from contextlib import ExitStack
import numpy as np
import concourse.bass as bass
import concourse.mybir as mybir
from concourse.bass_utils import run_bass_kernel_spmd

F32 = mybir.dt.float32
BF16 = mybir.dt.bfloat16
I32 = mybir.dt.int32
AF = mybir.ActivationFunctionType
ALU = mybir.AluOpType
AX = mybir.AxisListType

import os
_PH = float(os.environ.get("KPH", "9"))
DM = 1024
NE = 32
BLK = 512
EPS = 1e-6
MAGIC = 12582912.0
C1 = 6.28125
C2 = 2.0 * math.pi - 6.28125


class Res:
    __slots__ = ("w", "rs")

    def __init__(self):
        self.w = None
        self.rs = {}


class Eng:
    def __init__(self, h, sid, is_pe=False):
        self.h = h
        self.sid = sid
        self.n = 0
        self.known = {}
        self.is_pe = is_pe
        self.ring = []
        self.rpos = 0


class Kern:
    def __init__(self, nc, es):
        self.nc = nc
        self.sems = []
        self.semvals = []
        self.es = es
        mk = lambda nm: self._newsem(nm)
        self.pe = Eng(nc.tensor, mk("s_pe"), True)
        self.dve = Eng(nc.vector, mk("s_dve"))
        self.act = Eng(nc.scalar, mk("s_act"))
        self.pool = Eng(nc.gpsimd, mk("s_pool"))
        self.sp = Eng(nc.sync, mk("s_sp"))
        for e, n in ((self.sp, 44), (self.act, 12), (self.pool, 36)):
            e.ring = [mk("d%d_%d" % (e.sid, i)) for i in range(n)]
        self.out_evs = []

    def _newsem(self, nm):
        s = self.es.enter_context(self.nc.semaphore(nm))
        self.sems.append(s)
        self.semvals.append(0)
        return len(self.sems) - 1

    def _wait(self, eng, evs):
        best = {}
        for sid, val in evs:
            if val > best.get(sid, 0):
                best[sid] = val
        for sid, val in best.items():
            if eng.is_pe and sid == eng.sid:
                continue
            if eng.known.get(sid, 0) >= val:
                continue
            eng.h.wait_ge(self.sems[sid], val)
            eng.known[sid] = val

    @staticmethod
    def _deps(reads, writes):
        evs = []
        for r in reads:
            if r.w is not None:
                evs.append(r.w)
        for r in writes:
            if r.w is not None:
                evs.append(r.w)
            evs.extend(r.rs.items())
        return evs

    @staticmethod
    def _mark(ev, reads, writes):
        for r in reads:
            if ev[1] > r.rs.get(ev[0], 0):
                r.rs[ev[0]] = ev[1]
        for r in writes:
            r.w = ev
            r.rs = {}

    def I(self, eng, fn, reads=(), writes=()):
        evs = self._deps(reads, writes)
        own = eng.sid
        evs2 = []
        raw = set()
        for r in reads:
            if r.w is not None:
                raw.add(r.w)
        for ev in evs:
            if ev[0] == own and ev not in raw:
                continue
            evs2.append(ev)
        self._wait(eng, evs2)
        ins = fn()
        eng.n += 1
        ins.then_inc(self.sems[eng.sid], 1)
        self._mark((eng.sid, eng.n), reads, writes)

    def dma(self, q, fn, reads=(), writes=(), is_out=False):
        evs = self._deps(reads, writes)
        sid = q.ring[q.rpos % len(q.ring)]
        q.rpos += 1
        prev = self.semvals[sid]
        if prev > 0:
            evs.append((sid, prev))
        self._wait(q, evs)
        ins = fn()
        self.semvals[sid] = prev + 16
        ins.then_inc(self.sems[sid], 16)
        ev = (sid, prev + 16)
        self._mark(ev, reads, writes)
        if is_out:
            self.out_evs.append(ev)
        return ev

    def barrier(self):
        evs = []
        for e in (self.sp, self.act, self.pool):
            evs += [(sid, self.semvals[sid]) for sid in e.ring if self.semvals[sid] > 0]
        for e in (self.pe, self.dve, self.act, self.pool):
            if e.n > 0:
                evs.append((e.sid, e.n))
        for e in (self.pe, self.dve, self.act, self.pool, self.sp):
            self._wait(e, [ev for ev in evs if ev[0] != e.sid])

    def finish(self):
        evs = list(self.out_evs)
        for e in (self.sp, self.act, self.pool):
            evs += [(sid, self.semvals[sid]) for sid in e.ring if self.semvals[sid] > 0]
        for e in (self.pe, self.dve, self.act, self.pool):
            if e.n > 0:
                evs.append((e.sid, e.n))
        self._wait(self.sp, evs)


class _Cut(Exception):
    pass


def build(S, dbg=()):
    ctx = {}
    try:
        return _build_inner(S, dbg, ctx)
    except _Cut:
        ctx["K"].finish()
        return ctx["nc"], ctx["dbg_d"]


def _build_inner(S, dbg, ctx):
    NT = S // 128
    NG = S // 512
    T = 2 * S
    NTT = T // 128
    NB = (T * 4) // BLK + NE
    nc = bass.Bass("TRN2", target_bir_lowering=False)
    es = ExitStack()
    K = Kern(nc, es)
    pe, dve, act, pool, sp = K.pe, K.dve, K.act, K.pool, K.sp
    dbg_d = {}

    def DI(name, shape, dt=F32):
        return nc.dram_tensor(name, list(shape), dt, kind="ExternalInput").ap()

    def DS(name, shape, dt=F32):
        if name in dbg:
            d = nc.dram_tensor("dbg_" + name, list(shape), dt, kind="ExternalOutput").ap()
            dbg_d[name] = d
            return d
        return nc.dram_tensor(name, list(shape), dt, kind="Internal").ap()

    x_d = DI("x", [T, DM])
    cT_d = DI("cT", [128, 8, 2])
    pos_d = DI("pos", [2, 128, NT], I32)
    wada_d = DI("w_ada", [DM, 6 * DM]).rearrange("(k p) f -> p k f", p=128)
    badaT_d = DI("b_adaT", [128, 48])
    gpre1_d = DI("g_pre1", [128, 8])
    gpost1_d = DI("g_post1", [128, 8])
    gpre2_d = DI("g_pre2", [128, 8])
    gpost2_d = DI("g_post2", [128, 8])
    wtm_d = DI("w_tm", [DM, 552]).rearrange("(k p) f -> p k f", p=128)
    wfm_d = DI("w_fm", [DM, 1664]).rearrange("(k p) f -> p k f", p=128)
    convw_d = DI("convw", [128, 8, 4])
    convb_d = DI("convb", [128, 8])
    dtb_d = DI("dtb", [128, 8])
    alog_d = DI("alog", [128, 8])
    dsk_d = DI("dsk", [128, 8])
    ssdn_d = DI("ssdn", [128, 512])
    mlan_d = DI("mlan", [128, 512])
    qan_d = DI("qan", [128, 3])
    kvan_d = DI("kvan", [128, 2])
    wq_d = DI("wq", [384, 768]).rearrange("(k p) f -> p k f", p=128)
    wkv_d = DI("wkv", [256, 1024]).rearrange("(k p) f -> p k f", p=128)
    wout_d = DI("wout", [DM, DM]).rearrange("(k p) f -> p k f", p=128)
    wr_d = DI("wr", [DM, NE]).rearrange("(k p) f -> p k f", p=128)
    br_d = DI("br", [128, NE])
    wgu_d = DI("wgu", [NE * DM, 2 * DM])
    wd_d = DI("wd", [NE * DM, DM])
    bgu_d = DI("bgu", [NE * 128, 16])
    bd_d = DI("bd", [NE * 128, DM])
    identf_d = DI("identf", [128, 128])
    U1_d = DI("U1", [128, 128])
    U2_d = DI("U2", [128, 128])
    Lst_d = DI("Lst", [128, 128])
    m01_d = DI("m01", [128, 128])
    invf_d = DI("invf", [128, 16])
    iotab_d = DI("iotab", [128, NB])
    pk_d = DI("pk", [128, 8])
    rm_d = DI("rm", [128, 2])
    x1s_d = DS("x1s", [T, DM])
    h2s_d = DS("h2s", [T, DM], BF16)
    xg_d = DS("xg", [NB * BLK, DM], BF16)
    yb_d = DS("yb", [NB * BLK, DM])
    out_d = nc.dram_tensor("out", [T, DM], F32, kind="ExternalOutput").ap()
    ctx.update(K=K, es=es, nc=nc, dbg_d=dbg_d)

    def cut(level):
        if _PH < level:
            raise _Cut()

    def SB(name, shape, dt=F32, stack=None):
        t = (stack or es).enter_context(nc.sbuf_tensor("sb_" + name, list(shape), dt))
        return t, Res()

    def PS(name, shape, dt=F32):
        return es.enter_context(nc.psum_tensor("ps_" + name, list(shape), dt))

    P2 = PS("P2", [128, 1024])
    rP2a, rP2b = Res(), Res()
    PBs = [PS("PB%d" % i, [128, 512]) for i in range(5)]
    rPB = [Res() for _ in range(5)]
    PT = PS("PT", [128, 1024], BF16)
    rPT = Res()

    def mm(out, lhsT, rhs, start, stop, rd, wr):
        K.I(pe, lambda: nc.tensor.matmul(out, lhsT, rhs, start=start, stop=stop), reads=rd, writes=wr)

    def tr(out, in_, ident, rd, wr):
        K.I(pe, lambda: nc.tensor.transpose(out, in_, ident), reads=rd, writes=wr)

    def actf(out, in_, func, rd, wr, bias=None, scale=None, accum_out=None):
        kw = {}
        if bias is not None:
            kw["bias"] = bias
        if scale is not None:
            kw["scale"] = scale
        if accum_out is not None:
            kw["accum_out"] = accum_out
        K.I(act, lambda: nc.scalar.activation(out, in_, func, **kw), reads=rd, writes=wr)

    def ts(eng, out, in0, s1, s2, op0, op1, rd, wr):
        h = eng.h
        if op1 is None:
            K.I(eng, lambda: h.tensor_scalar(out, in0, s1, None, op0), reads=rd, writes=wr)
        else:
            K.I(eng, lambda: h.tensor_scalar(out, in0, s1, s2, op0, op1), reads=rd, writes=wr)

    def tt(eng, out, in0, in1, op, rd, wr):
        h = eng.h
        K.I(eng, lambda: h.tensor_tensor(out, in0, in1, op), reads=rd, writes=wr)

    def stt(eng, out, in0, scalar, in1, op0, op1, rd, wr):
        h = eng.h
        K.I(eng, lambda: h.scalar_tensor_tensor(out, in0, scalar, in1, op0, op1), reads=rd, writes=wr)

    def cp(eng, out, in_, rd, wr):
        h = eng.h
        if eng is act:
            K.I(eng, lambda: nc.scalar.activation(out, in_, AF.Copy), reads=rd, writes=wr)
        else:
            K.I(eng, lambda: h.tensor_copy(out, in_), reads=rd, writes=wr)

    def ld(q, out, in_, wr, rd=()):
        K.dma(q, lambda: q.h.dma_start(out=out, in_=in_), reads=rd, writes=wr)

    def dump(name, ap, res, shape, dt=F32):
        if name not in dbg:
            return
        d = nc.dram_tensor("dbg_" + name, list(shape), dt, kind="ExternalOutput").ap()
        dbg_d[name] = d
        K.dma(sp, lambda: nc.sync.dma_start(out=d, in_=ap), reads=[res], writes=[Res()], is_out=True)

    identf, r_identf = SB("identf", [128, 128])
    identb, r_identb = SB("identb", [128, 128], BF16)
    onesf, r_onesf = SB("onesf", [128, 128])
    onesb, r_onesb = SB("onesb", [128, 128], BF16)
    U1, r_U1 = SB("U1", [128, 128])
    U2, r_U2 = SB("U2", [128, 128])
    Lst, r_Lst = SB("Lst", [128, 128])
    m01, r_m01 = SB("m01", [128, 128])
    invf, r_invf = SB("invf", [128, 16])
    ld(sp, identf[:], identf_d, [r_identf])
    ld(sp, U1[:], U1_d, [r_U1])
    ld(sp, U2[:], U2_d, [r_U2])
    ld(sp, Lst[:], Lst_d, [r_Lst])
    ld(sp, m01[:], m01_d, [r_m01])
    ld(sp, invf[:], invf_d, [r_invf])
    rm, r_rm = SB("rm", [128, 2])
    Mc, r_Mc = SB("Mc", [128, 2, 128])
    ld(sp, rm[:], rm_d, [r_rm])
    cp(dve, identb[:], identf[:], [r_identf], [r_identb])
    K.I(dve, lambda: nc.vector.memset(onesf[:], 1.0), writes=[r_onesf])
    K.I(dve, lambda: nc.vector.memset(onesb[:], 1.0), writes=[r_onesb])
    for c in range(2):
        ts(dve, Mc[:, c, :], onesf[:], rm[:, c:c + 1], None, ALU.mult, None, [r_onesf, r_rm], [r_Mc])
    epsb, r_epsb = SB("epsb", [128, 1])
    oneb, r_oneb = SB("oneb", [128, 1])
    K.I(dve, lambda: nc.vector.memset(epsb[:], EPS), writes=[r_epsb])
    K.I(dve, lambda: nc.vector.memset(oneb[:], 1.0), writes=[r_oneb])

    small = {}
    for nm, d, shp in (("gpre1", gpre1_d, [128, 8]), ("gpost1", gpost1_d, [128, 8]), ("gpre2", gpre2_d, [128, 8]),
                       ("gpost2", gpost2_d, [128, 8]), ("convw", convw_d, [128, 8, 4]), ("convb", convb_d, [128, 8]),
                       ("dtb", dtb_d, [128, 8]), ("alog", alog_d, [128, 8]), ("dsk", dsk_d, [128, 8]),
                       ("ssdn", ssdn_d, [128, 512]), ("mlan", mlan_d, [128, 512]), ("qan", qan_d, [128, 3]),
                       ("kvan", kvan_d, [128, 2]), ("br", br_d, [128, NE]), ("badaT", badaT_d, [128, 48]),
                       ("cT", cT_d, [128, 8, 2]), ("wr", wr_d, [128, 8, NE])):
        t, r = SB("c_" + nm, shp)
        ld(sp, t[:], d, [r])
        small[nm] = (t, r)

    aneg, r_aneg = SB("aneg", [128, 8])
    actf(aneg[:], small["alog"][0][:], AF.Exp, [small["alog"][1]], [r_aneg])
    ts(dve, aneg[:], aneg[:], -1.0, None, ALU.mult, None, [r_aneg], [r_aneg])

    modT, r_modT = SB("modT", [128, 48, 2])
    cact, r_cact = SB("cact", [128, 8, 2])
    actf(cact[:], small["cT"][0][:], AF.Silu, [small["cT"][1]], [r_cact])
    with ExitStack() as st0:
        wst = [SB("wadast%d" % i, [128, 8, 512], F32, st0) for i in range(2)]
        for blk in range(12):
            w_t, w_r = wst[blk % 2]
            ld(sp, w_t[:], wada_d[:, :, blk * 512:(blk + 1) * 512], [w_r])
            for j in range(4):
                fc = blk * 4 + j
                pb, rpb = PBs[fc % 2], rPB[fc % 2]
                for k in range(8):
                    mm(pb[:, 0:2], w_t[:, k, j * 128:(j + 1) * 128], cact[:, k, :], k == 0, k == 7,
                       [w_r, r_cact], [rpb])
                ts(dve, modT[:, fc, :], pb[:, 0:2], small["badaT"][0][:, fc:fc + 1], None, ALU.add, None,
                   [rpb, small["badaT"][1]], [r_modT])
    K.barrier()
    a1T, r_a1T = SB("a1T", [128, 2, 8])
    sh1T, r_sh1T = SB("sh1T", [128, 2, 8])
    vtmp, r_vtmp = SB("vtmp", [128, 8])
    btmp = [SB("btmp%d" % i, [128, 128]) for i in range(2)]
    bcn = [0]

    def bcast_rows(vT_ap, vres, dst_ap, dres):
        for half in range(2):
            pb, rpb = PBs[2 + half], rPB[2 + half]
            for kk in range(4):
                k = half * 4 + kk
                bt, rbt = btmp[bcn[0] % 2]
                bcn[0] += 1
                ts(dve, bt[:], onesf[:], vT_ap[:, k:k + 1], None, ALU.mult, None, [r_onesf, vres], [rbt])
                mm(pb[:, kk * 128:(kk + 1) * 128], bt[:], identf[:], True, True, [rbt, r_identf], [rpb])
            cp(act, dst_ap[:, half * 512:(half + 1) * 512], pb[:], [rpb], [dres])

    for b in range(2):
        ts(dve, vtmp[:], modT[:, 8:16, b], 1.0, None, ALU.add, None, [r_modT], [r_vtmp])
        tt(dve, a1T[:, b, :], vtmp[:], small["gpre1"][0][:], ALU.mult, [r_vtmp, small["gpre1"][1]], [r_a1T])
        cp(dve, sh1T[:, b, :], modT[:, 0:8, b], [r_modT], [r_sh1T])

    lg_all, r_lg = SB("lg_all", [128, NTT, NE])
    v8_all, r_v8 = SB("v8_all", [128, NTT, 8])
    gate_all, r_gate = SB("gate_all", [128, NTT, 4])

    castn = [0]

    def cast_any(out, in_, rd, wr):
        e = (dve, pool, act)[castn[0] % 3]
        castn[0] += 1
        cp(e, out, in_, rd, wr)

    def rstd_from(ss_ap, ssres, out_ap, ores, n, eng=dve):
        actf(out_ap, ss_ap, AF.Ln, [ssres], [ores], bias=epsb[:, 0:1], scale=1.0 / n)
        actf(out_ap, out_ap, AF.Exp, [ores], [ores], scale=-0.5)

    class Bag:
        def __init__(self):
            self.d = {}

        def add(self, ev):
            if ev[1] > self.d.get(ev[0], 0):
                self.d[ev[0]] = ev[1]

        def evs(self):
            return list(self.d.items())

    kn_s = DS("kn_s", [2, 128, 4 * S], BF16)
    kr_s = DS("kr_s", [2, 128, S], BF16)
    v_s = DS("v_s", [2, 128, NT * 8 * 65], BF16)
    q_s = DS("q_s", [2 * NT, 128, 2048], BF16)
    mix_s = DS("mix_s", [T, DM], BF16)
    bagA = [Bag(), Bag()]
    bagT = [Bag(), Bag()]

    def st(q, out, in_, rd, bag):
        ev = K.dma(q, lambda: q.h.dma_start(out=out, in_=in_), reads=rd, writes=[])
        bag.add(ev)

    dump("modT", modT[:], r_modT, [128, 48, 2])
    cut(0.2)
    with ExitStack() as p1:
        wtm, r_wtm = SB("wtm", [128, 8, 552], BF16, p1)
        wfm, r_wfm = SB("wfm", [128, 8, 1664], BF16, p1)
        wq, r_wq = SB("wq", [128, 3, 768], BF16, p1)
        wkv, r_wkv = SB("wkv", [128, 2, 1024], BF16, p1)
        with ExitStack() as pst:
            stg = [SB("stg%d" % i, [128, 1664], F32, pst) for i in range(2)]
            sn = 0
            for (dst, rdst, src, nk, ncol) in ((wtm, r_wtm, wtm_d, 8, 552), (wfm, r_wfm, wfm_d, 8, 1664),
                                               (wq, r_wq, wq_d, 3, 768), (wkv, r_wkv, wkv_d, 2, 1024)):
                for k in range(nk):
                    s_t, s_r = stg[sn % 2]
                    sn += 1
                    ld(sp, s_t[:, 0:ncol], src[:, k, :], [s_r])
                    cast_any(dst[:, k, :], s_t[:, 0:ncol], [s_r], [rdst])
        K.barrier()
        cut(0.25)
        Sst, r_Sst = SB("Sst", [128, 8, 64], F32, p1)
        Sbf, r_Sbf = SB("Sbf", [128, 8, 64], BF16, p1)
        cosT, r_cos = SB("cosT", [128, NT, 16], F32, p1)
        sinT, r_sin = SB("sinT", [128, NT, 16], F32, p1)
        posi, r_posi = SB("posi", [128, NT], I32, p1)
        posf, r_posf = SB("posf", [128, NT], F32, p1)
        ang, r_ang = SB("ang", [128, NT, 16], F32, p1)
        ang2, r_ang2 = SB("ang2", [128, NT, 16], F32, p1)
        xt = [SB("xt%d" % i, [128, DM], F32, p1) for i in range(2)]
        junk, r_junk = SB("junk", [128, DM], BF16, p1)
        xn, r_xn = SB("xn", [128, DM], BF16, p1)
        st1 = [SB("st1_%d" % i, [128, 8], F32, p1) for i in range(4)]
        hT2 = [SB("hT%d" % i, [128, 8, 512], BF16, p1) for i in range(2)]
        st0 = [SB("st0_%d" % i, [128, 2], F32, p1) for i in range(4)]
        rw, r_rw = SB("raw", [128, 8, 515], F32, p1)
        halo, r_halo = SB("halo", [128, 8, 3], F32, p1)
        cacc, r_cacc = SB("cacc", [128, 512], F32, p1)
        xact, r_xact = SB("xact", [128, 8, 512], BF16, p1)
        qag, r_qag = SB("qag", [128, 3, 512], F32, p1)
        kvag, r_kvag = SB("kvag", [128, 2, 512], F32, p1)
        sq5, r_sq5 = SB("sq5", [128, 5, 512], BF16, p1)
        rsb, r_rsb = SB("rsb", [128, 2, 512], F32, p1)
        qaTn, r_qaTn = SB("qaTn", [128, 3, 512], BF16, p1)
        kvaTn, r_kvaTn = SB("kvaTn", [128, 2, 512], BF16, p1)
        kng, r_kng = SB("kng", [128, 4, 512], BF16, p1)
        zs, r_zs = SB("zs", [128, 512], F32, p1)
        dtt, r_dtt = SB("dtt", [128, 8], F32, p1)
        dta, r_dta = SB("dta", [128, 8], F32, p1)
        krp, r_krp = SB("krp", [128, 4, 32], BF16, p1)
        K.I(pool, lambda: nc.gpsimd.memset(krp[:], 0.0), writes=[r_krp])
        krT, r_krT = SB("krT", [128, 128], BF16, p1)
        rtmp = [SB("rtmp%d" % i, [128, 8, 16], F32, p1) for i in range(4)]
        qn_tm, r_qn = SB("qn_tm", [128, 512], BF16, p1)
        qr_pad, r_qrp = SB("qr_pad", [128, 4, 2, 64], BF16, p1)
        K.I(pool, lambda: nc.gpsimd.memset(qr_pad[:], 0.0), writes=[r_qrp])
        QT, r_QT = SB("QT", [128, 2048], BF16, p1)
        K.I(pool, lambda: nc.gpsimd.memset(QT[:], 0.0), writes=[r_QT])
        Vt, r_Vt = SB("Vt", [128, 8, 65], BF16, p1)
        K.I(pool, lambda: nc.gpsimd.memset(Vt[:], 1.0), writes=[r_Vt])
        xs_tm, r_xs = SB("xs_tm", [128, 8, 64], BF16, p1)
        B_tm, r_Btm = SB("B_tm", [128, 256], BF16, p1)
        xdt, r_xdt = SB("xdt", [128, 8, 64], BF16, p1)
        xdtd, r_xdtd = SB("xdtd", [128, 2, 8, 64], BF16, p1)
        ecsm, r_ecsm = SB("ecsm", [128, 2, 8], F32, p1)
        dtem, r_dtem = SB("dtem", [128, 2, 8], F32, p1)
        lseg, r_lseg = SB("lseg", [128, 8, 128], F32, p1)
        dec, r_dec = SB("dec", [128, 8, 128], F32, p1)
        cbm, r_cbm = SB("cbm", [128, 2, 128], F32, p1)
        MT, r_MT = SB("MT", [128, 8, 128], BF16, p1)
        ecs, r_ecs = SB("ecs", [128, 8], F32, p1)
        dte, r_dte = SB("dte", [128, 8], F32, p1)
        cdB, r_cdB = SB("cdB", [128, 2, 8], F32, p1)
        yd, r_yd = SB("yd", [128, 8, 64], F32, p1)
        yt, r_yt = SB("yt", [128, 8, 64], F32, p1)
        yt2, r_yt2 = SB("yt2", [128, 8, 64], F32, p1)
        mixs, r_mixs = SB("mixs", [128, 512], BF16, p1)

        pbn = [0]

        def next_pb():
            i = pbn[0] % 2
            pbn[0] += 1
            return PBs[i], rPB[i]

        for b in range(2):
            bag = bagA[b]
            ld(sp, posi[:], pos_d[b], [r_posi])
            cp(dve, posf[:], posi[:], [r_posi], [r_posf])
            tt(dve, ang[:], posf[:].unsqueeze(2).broadcast_to((128, NT, 16)),
               invf[:].unsqueeze(1).broadcast_to((128, NT, 16)), ALU.mult, [r_posf, r_invf], [r_ang])
            ts(dve, ang2[:], ang[:], 1.0 / (2.0 * math.pi), MAGIC, ALU.mult, ALU.add, [r_ang], [r_ang2])
            ts(dve, ang2[:], ang2[:], -MAGIC, None, ALU.add, None, [r_ang2], [r_ang2])
            stt(dve, ang[:], ang2[:], -C1, ang[:], ALU.mult, ALU.add, [r_ang2, r_ang], [r_ang])
            stt(dve, ang[:], ang2[:], -C2, ang[:], ALU.mult, ALU.add, [r_ang2, r_ang], [r_ang])
            ts(dve, ang[:], ang[:], 3.14159, -3.14159, ALU.min, ALU.max, [r_ang], [r_ang])
            actf(sinT[:], ang[:], AF.Sin, [r_ang], [r_sin])
            ts(dve, ang2[:], ang[:], -1.0, None, ALU.mult, None, [r_ang], [r_ang2])
            tt(dve, ang2[:], ang2[:], ang[:], ALU.max, [r_ang2, r_ang], [r_ang2])
            ts(dve, ang2[:], ang2[:], -1.0, math.pi / 2.0, ALU.mult, ALU.add, [r_ang2], [r_ang2])
            actf(cosT[:], ang2[:], AF.Sin, [r_ang2], [r_cos])
            cut(0.30)
            K.I(dve, lambda: nc.vector.memset(Sst[:], 0.0), writes=[r_Sst])
            K.I(pool, lambda: nc.gpsimd.memset(Sbf[:], 0.0), writes=[r_Sbf])
            K.I(pool, lambda: nc.gpsimd.memset(halo[:], 0.0), writes=[r_halo])

            def step1(gi):
                hTW, r_hTW = hT2[gi % 2]
                for i in range(4):
                    ti = gi * 4 + i
                    gt = b * NT + ti
                    x_t, x_r = xt[gt % 2]
                    s1, r_s1 = st0[i]
                    ld(sp, x_t[:], x_d[gt * 128:(gt + 1) * 128, :], [x_r])
                    actf(junk[:], x_t[:], AF.Square, [x_r], [r_junk, r_s1], accum_out=s1[:, 0:1])
                    rstd_from(s1[:, 0:1], r_s1, s1[:, 1:2], r_s1, DM)
                    ts(dve, xn[:], x_t[:], s1[:, 1:2], None, ALU.mult, None, [x_r, r_s1], [r_xn])
                    for k in range(8):
                        tr(PT[:, k * 128:(k + 1) * 128], xn[:, k * 128:(k + 1) * 128], identb[:], [r_xn, r_identb], [rPT])
                    for k in range(8):
                        actf(hTW[:, k, i * 128:(i + 1) * 128], PT[:, k * 128:(k + 1) * 128], AF.Identity,
                             [rPT, r_a1T, r_sh1T], [r_hTW], bias=sh1T[:, b, k:k + 1], scale=a1T[:, b, k:k + 1])

            step1(0)
            for gi in range(NG):
                hT, r_hT = hT2[gi % 2]
                cut(0.32)
                cp(pool, rw[:, :, 0:3], halo[:], [r_halo], [r_rw])
                for mc in range(13):
                    pb, rpb = next_pb()
                    for k in range(8):
                        mm(pb[:], wfm[:, k, mc * 128:(mc + 1) * 128], hT[:, k, :], k == 0, k == 7, [r_wfm, r_hT], [rpb])
                    if mc < 8:
                        cp(act, rw[:, mc, 3:515], pb[:], [rpb], [r_rw])
                    elif mc < 11:
                        c = mc - 8
                        actf(qag[:, c, :], pb[:], AF.Copy, [rpb, small["qan"][1]], [r_qag], scale=small["qan"][0][:, c:c + 1])
                        actf(sq5[:, c, :], pb[:], AF.Square, [rpb], [r_sq5])
                    else:
                        c = mc - 11
                        actf(kvag[:, c, :], pb[:], AF.Copy, [rpb, small["kvan"][1]], [r_kvag], scale=small["kvan"][0][:, c:c + 1])
                        actf(sq5[:, 3 + c, :], pb[:], AF.Square, [rpb], [r_sq5])
                cp(pool, halo[:], rw[:, :, 512:515], [r_rw], [r_halo])
                cut(0.34)
                cw, r_cw = small["convw"]
                cb_, r_cb = small["convb"]
                for mc in range(8):
                    ts(dve, cacc[:], rw[:, mc, 0:512], cw[:, mc, 0:1], cb_[:, mc:mc + 1], ALU.mult, ALU.add,
                       [r_rw, r_cw, r_cb], [r_cacc])
                    for kk in range(1, 4):
                        stt(dve, cacc[:], rw[:, mc, kk:kk + 512], cw[:, mc, kk:kk + 1], cacc[:], ALU.mult, ALU.add,
                            [r_rw, r_cw, r_cacc], [r_cacc])
                    actf(xact[:, mc, :], cacc[:], AF.Silu, [r_cacc], [r_xact])
                cut(0.36)
                for (c0, ncn, n, slot) in ((0, 3, 384, 0), (3, 2, 256, 1)):
                    pb, rpb = PBs[2], rPB[2]
                    for c in range(ncn):
                        mm(pb[:], onesb[:], sq5[:, c0 + c, :], c == 0, c == ncn - 1, [r_onesb, r_sq5], [rpb])
                    rstd_from(pb[:], rpb, rsb[:, slot, :], r_rsb, n)
                for c in range(3):
                    tt(dve, qaTn[:, c, :], qag[:, c, :], rsb[:, 0, :], ALU.mult, [r_qag, r_rsb], [r_qaTn])
                for c in range(2):
                    tt(dve, kvaTn[:, c, :], kvag[:, c, :], rsb[:, 1, :], ALU.mult, [r_kvag, r_rsb], [r_kvaTn])
                cut(0.38)
                for j in range(4):
                    pb, rpb = next_pb()
                    for c in range(2):
                        mm(pb[:], wkv[:, c, j * 128:(j + 1) * 128], kvaTn[:, c, :], c == 0, c == 1, [r_wkv, r_kvaTn], [rpb])
                    cp(act, kng[:, j, :], pb[:], [rpb], [r_kng])
                st(sp, kn_s[b].rearrange("p (j s) -> p j s", j=4)[:, :, gi * 512:(gi + 1) * 512], kng[:], [r_kng], bag)
                cut(0.40)

                if gi + 1 < NG:
                    step1(gi + 1)
                for i in range(4):
                    ti = gi * 4 + i
                    gt = b * NT + ti
                    cs_ = slice(i * 128, (i + 1) * 128)
                    s1, r_s1 = st1[i]
                    for k in range(8):
                        mm(P2[:, 0:512], hT[:, k, cs_], wtm[:, k, 0:512], k == 0, k == 7, [r_hT, r_wtm], [rP2a])
                    for k in range(8):
                        mm(P2[:, 512:552], hT[:, k, cs_], wtm[:, k, 512:552], k == 0, k == 7, [r_hT, r_wtm], [rP2b])
                    actf(zs[:], P2[:, 0:512], AF.Silu, [rP2a], [r_zs])
                    tt(dve, dtt[:], P2[:, 512:520], small["dtb"][0][:], ALU.add, [rP2b, small["dtb"][1]], [r_dtt])
                    actf(dtt[:], dtt[:], AF.Exp, [r_dtt], [r_dtt])
                    actf(dtt[:], dtt[:], AF.Ln, [r_dtt], [r_dtt], bias=oneb[:, 0:1])
                    tt(dve, dta[:], dtt[:], aneg[:], ALU.mult, [r_dtt, r_aneg], [r_dta])
                    cut(0.42)
                    cs16 = cosT[:, ti, :]
                    sn16 = sinT[:, ti, :]
                    k1 = P2[:, 520:536]
                    k2 = P2[:, 536:552]
                    t0_, t1_, t2_, t3_ = [rtmp[q][0][:, 0, :] for q in range(4)]
                    rr = [rtmp[q][1] for q in range(4)]
                    tt(dve, t0_, k1, cs16, ALU.mult, [rP2b, r_cos], [rr[0]])
                    tt(dve, t1_, k2, sn16, ALU.mult, [rP2b, r_sin], [rr[1]])
                    tt(dve, t2_, k2, cs16, ALU.mult, [rP2b, r_cos], [rr[2]])
                    tt(dve, t3_, k1, sn16, ALU.mult, [rP2b, r_sin], [rr[3]])
                    tt(dve, krp[:, 0, 0:16], t0_, t1_, ALU.subtract, [rr[0], rr[1]], [r_krp])
                    tt(dve, krp[:, 0, 16:32], t2_, t3_, ALU.add, [rr[2], rr[3]], [r_krp])
                    cp(dve, krp[:, 2, :], krp[:, 0, :], [r_krp], [r_krp])
                    tr(PT[:, 0:128], krp[:].rearrange("p a c -> p (a c)"), identb[:], [r_krp, r_identb], [rPT])
                    cp(act, krT[:], PT[:, 0:128], [rPT], [r_krT])
                    st(sp, kr_s[b][:, ti * 128:(ti + 1) * 128], krT[:], [r_krT], bag)
                    cut(0.44)
                    for c in range(3):
                        mm(P2[:, 0:512], qaTn[:, c, cs_], wq[:, c, 0:512], c == 0, c == 2, [r_qaTn, r_wq], [rP2a])
                    for c in range(3):
                        mm(P2[:, 512:768], qaTn[:, c, cs_], wq[:, c, 512:768], c == 0, c == 2, [r_qaTn, r_wq], [rP2b])
                    cp(act, qn_tm[:], P2[:, 0:512], [rP2a], [r_qn])
                    qr = P2[:, 512:768].rearrange("p (h c) -> p h c", c=32)
                    q1 = qr[:, :, 0:16]
                    q2 = qr[:, :, 16:32]
                    cb8 = cs16.unsqueeze(1).broadcast_to((128, 8, 16))
                    sb8 = sn16.unsqueeze(1).broadcast_to((128, 8, 16))
                    T0, T1, T2, T3 = [rtmp[q][0][:] for q in range(4)]
                    tt(dve, T0, q1, cb8, ALU.mult, [rP2b, r_cos], [rr[0]])
                    tt(dve, T1, q2, sb8, ALU.mult, [rP2b, r_sin], [rr[1]])
                    tt(dve, T2, q2, cb8, ALU.mult, [rP2b, r_cos], [rr[2]])
                    tt(dve, T3, q1, sb8, ALU.mult, [rP2b, r_sin], [rr[3]])
                    qrv = qr_pad[:].rearrange("p j s c -> p s j c")
                    for s_ in range(2):
                        tt(dve, qrv[:, s_, :, 0:16], rtmp[0][0][:, s_ * 4:(s_ + 1) * 4, :], rtmp[1][0][:, s_ * 4:(s_ + 1) * 4, :],
                           ALU.subtract, [rr[0], rr[1]], [r_qrp])
                        tt(dve, qrv[:, s_, :, 16:32], rtmp[2][0][:, s_ * 4:(s_ + 1) * 4, :], rtmp[3][0][:, s_ * 4:(s_ + 1) * 4, :],
                           ALU.add, [rr[2], rr[3]], [r_qrp])
                    for j in range(4):
                        tr(PT[:, j * 128:(j + 1) * 128], qn_tm[:, j * 128:(j + 1) * 128], identb[:], [r_qn, r_identb], [rPT])
                    for j in range(4):
                        tr(PT[:, 512 + j * 128:512 + (j + 1) * 128], qr_pad[:, j, :, :].rearrange("p s c -> p (s c)"),
                           identb[:], [r_qrp, r_identb], [rPT])
                    cp(act, QT[0:64, 0:512], PT[0:64, 0:512], [rPT], [r_QT])
                    cp(act, QT[64:128, 512:1024], PT[64:128, 0:512], [rPT], [r_QT])
                    cp(act, QT[0:64, 1024:1536], PT[0:64, 512:1024], [rPT], [r_QT])
                    cp(act, QT[64:128, 1536:2048], PT[64:128, 512:1024], [rPT], [r_QT])
                    st(sp, q_s[gt], QT[:], [r_QT], bag)
                    cut(0.46)
                    pb, rpb = PBs[2], rPB[2]
                    for c in range(2):
                        mm(pb[:], kvaTn[:, c, cs_], wkv[:, c, 512:1024], c == 0, c == 1, [r_kvaTn, r_wkv], [rpb])
                    cp(act, Vt[:, :, 0:64], pb[:].rearrange("p (h c) -> p h c", c=64), [rpb], [r_Vt])
                    cut(0.475)
                    st(sp, v_s[b][:, ti * 520:(ti + 1) * 520], Vt[:].rearrange("p h c -> p (h c)"), [r_Vt], bag)
                    cut(0.48)

                    for j in range(4):
                        tr(PT[:, j * 128:(j + 1) * 128], xact[:, j, cs_], identb[:], [r_xact, r_identb], [rPT])
                    for j in range(2):
                        tr(PT[:, 512 + j * 128:512 + (j + 1) * 128], xact[:, 4 + j, cs_], identb[:], [r_xact, r_identb], [rPT])
                    cp(act, xs_tm[:].rearrange("p h c -> p (h c)"), PT[:, 0:512], [rPT], [r_xs])
                    cp(act, B_tm[:], PT[:, 512:768], [rPT], [r_Btm])
                    cut(0.495)
                    dt_b = dtt[:].unsqueeze(2).broadcast_to((128, 8, 64))
                    tt(dve, xdt[:], xs_tm[:], dt_b, ALU.mult, [r_xs, r_dtt], [r_xdt])
                    cut(0.50)
                    tt(dve, lseg[:], U1[:].unsqueeze(1).broadcast_to((128, 8, 128)),
                       dta[:].unsqueeze(2).broadcast_to((128, 8, 128)), ALU.mult, [r_U1, r_dta], [r_lseg])
                    for h in range(8):
                        rp = rP2a if h < 4 else rP2b
                        mm(P2[:, h * 128:(h + 1) * 128], lseg[:, h, :], U2[:], True, True, [r_lseg, r_U2], [rp])
                    actf(dec[:].rearrange("p h l -> p (h l)"), P2[:], AF.Exp, [rP2a, rP2b], [r_dec])
                    cut(0.51)
                    pb3, rpb3 = PBs[3], rPB[3]
                    for g in range(2):
                        mm(pb3[:, g * 128:(g + 1) * 128], xact[:, 4 + g, cs_], xact[:, 6 + g, cs_], True, True, [r_xact], [rpb3])
                    cut(0.516)
                    tt(dve, cbm[:], pb3[:, 0:256].rearrange("p (g l) -> p g l", g=2),
                       m01[:].unsqueeze(1).broadcast_to((128, 2, 128)), ALU.mult, [rpb3, r_m01], [r_cbm])
                    cut(0.518)
                    for h in range(8):
                        tt(dve, MT[:, h, :], dec[:, h, :], cbm[:, h // 4, :], ALU.mult, [r_dec, r_cbm], [r_MT])
                    cut(0.52)
                    pb4, rpb4 = PBs[4], rPB[4]
                    mm(pb4[:, 0:8], U2[:], dta[:], True, True, [r_U2, r_dta], [rpb4])
                    mm(pb4[:, 8:16], U1[:], dta[:], True, True, [r_U1, r_dta], [rpb4])
                    mm(pb4[:, 16:24], Mc[:, 0, :], dta[:], True, True, [r_Mc, r_dta], [rpb4])
                    mm(pb4[:, 24:32], Mc[:, 1, :], dta[:], True, True, [r_Mc, r_dta], [rpb4])
                    actf(ecs[:], pb4[:, 0:8], AF.Exp, [rpb4], [r_ecs])
                    actf(dte[:], pb4[:, 8:16], AF.Exp, [rpb4], [r_dte])
                    actf(cdB[:].rearrange("p c h -> p (c h)"), pb4[:, 16:32], AF.Exp, [rpb4], [r_cdB])
                    for c in range(2):
                        ts(dve, ecsm[:, c, :], ecs[:], rm[:, c:c + 1], None, ALU.mult, None, [r_ecs, r_rm], [r_ecsm])
                        ts(dve, dtem[:, c, :], dte[:], rm[:, c:c + 1], None, ALU.mult, None, [r_dte, r_rm], [r_dtem])
                        tt(dve, xdtd[:, c, :, :], xdt[:], dtem[:, c, :].unsqueeze(2).broadcast_to((128, 8, 64)), ALU.mult,
                           [r_xdt, r_dtem], [r_xdtd])
                    cut(0.53)
                    pb0, rpb0 = PBs[0], rPB[0]
                    for h in range(8):
                        mm(pb0[:, h * 64:(h + 1) * 64], MT[:, h, :], xdt[:, h, :], True, True, [r_MT, r_xdt], [rpb0])
                    cp(act, yd[:].rearrange("p h c -> p (h c)"), pb0[:], [rpb0], [r_yd])
                    cut(0.54)
                    pb2, rpb2 = PBs[2], rPB[2]
                    pbY = (PBs[1], PBs[3])
                    rpbY = (rPB[1], rPB[3])
                    for c in range(2):
                        for g in range(2):
                            mm(pbY[c][:, g * 256:(g + 1) * 256], xact[:, 6 + g, cs_],
                               Sbf[:, g * 4:(g + 1) * 4, :].rearrange("p h c -> p (h c)"), True, True, [r_xact, r_Sbf], [rpbY[c]])
                        for g in range(2):
                            mm(pb2[:, g * 256:(g + 1) * 256], B_tm[:, g * 128:(g + 1) * 128],
                               xdtd[:, c, g * 4:(g + 1) * 4, :].rearrange("p h c -> p (h c)"), True, True, [r_Btm, r_xdtd], [rpb2])
                        tt(dve, Sst[:], Sst[:], cdB[:, c, :].unsqueeze(2).broadcast_to((128, 8, 64)), ALU.mult, [r_Sst, r_cdB], [r_Sst])
                        tt(dve, Sst[:], Sst[:], pb2[:].rearrange("p (h c) -> p h c", c=64), ALU.add, [r_Sst, rpb2], [r_Sst])
                        cp(act, Sbf[:], Sst[:], [r_Sst], [r_Sbf])
                    cut(0.55)
                    tt(dve, yt[:], pbY[0][:].rearrange("p (h c) -> p h c", c=64), ecsm[:, 0, :].unsqueeze(2).broadcast_to((128, 8, 64)),
                       ALU.mult, [rpbY[0], r_ecsm], [r_yt])
                    tt(dve, yt2[:], pbY[1][:].rearrange("p (h c) -> p h c", c=64), ecsm[:, 1, :].unsqueeze(2).broadcast_to((128, 8, 64)),
                       ALU.mult, [rpbY[1], r_ecsm], [r_yt2])
                    tt(dve, yt[:], yt[:], yt2[:], ALU.add, [r_yt, r_yt2], [r_yt])
                    tt(dve, yt[:], yt[:], yd[:], ALU.add, [r_yt, r_yd], [r_yt])
                    tt(dve, yt2[:], xs_tm[:], small["dsk"][0][:].unsqueeze(2).broadcast_to((128, 8, 64)), ALU.mult,
                       [r_xs, small["dsk"][1]], [r_yt2])
                    tt(dve, yt[:], yt[:], yt2[:], ALU.add, [r_yt, r_yt2], [r_yt])
                    tt(dve, yt[:].rearrange("p h c -> p (h c)"), yt[:].rearrange("p h c -> p (h c)"), zs[:], ALU.mult,
                       [r_yt, r_zs], [r_yt])
                    for g in range(2):
                        actf(junk[:, g * 256:(g + 1) * 256], yt[:, g * 4:(g + 1) * 4, :].rearrange("p h c -> p (h c)"), AF.Square,
                             [r_yt], [r_junk, r_s1], accum_out=s1[:, 2 + g:3 + g])
                    rstd_from(s1[:, 2:4], r_s1, s1[:, 2:4], r_s1, 256)
                    for g in range(2):
                        stt(dve, mixs[:, g * 256:(g + 1) * 256], yt[:, g * 4:(g + 1) * 4, :].rearrange("p h c -> p (h c)"),
                            s1[:, 2 + g:3 + g], small["ssdn"][0][:, g * 256:(g + 1) * 256], ALU.mult, ALU.mult,
                            [r_yt, r_s1, small["ssdn"][1]], [r_mixs])
                    st(sp, mix_s[gt * 128:(gt + 1) * 128, 0:512], mixs[:], [r_mixs], bag)
                    cut(0.56)

    K.barrier()
    cut(0.6)
    with ExitStack() as pt_:
        KnT, r_KnT = SB("KnT", [128, 4, S], BF16, pt_)
        KrT, r_KrT = SB("KrT", [128, S], BF16, pt_)
        Vst, r_Vst = SB("Vst", [128, NT, 8, 65], BF16, pt_)
        Qb = [SB("Qb%d" % i, [128, 16, 128], BF16, pt_) for i in range(2)]
        PTs = [SB("PTs%d" % i, [128, 4, 128], BF16, pt_) for i in range(5)]
        rec, r_rec = SB("rec", [128, 8], F32, pt_)
        osb, r_osb = SB("osb", [128, 8, 64], F32, pt_)
        junk2, r_junk2 = SB("junk2", [128, 512], BF16, pt_)
        sA, r_sA = SB("sA", [128, 2], F32, pt_)
        mixm = [SB("mixm%d" % i, [128, 512], BF16, pt_) for i in range(2)]
        sc = 1.0 / math.sqrt(96.0)
        for b in range(2):
            K._wait(sp, bagA[b].evs())
            ld(sp, KnT[:].rearrange("p j s -> p (j s)"), kn_s[b], [r_KnT])
            ld(sp, KrT[:], kr_s[b], [r_KrT])
            ld(sp, Vst[:].rearrange("p t h c -> p (t h c)"), v_s[b], [r_Vst])
            for ti in range(NT):
                gt = b * NT + ti
                q_t, q_r = Qb[ti % 2]
                ld(sp, q_t[:].rearrange("p a q -> p (a q)"), q_s[gt], [q_r])
                nkt = ti + 1
                pbO = (P2[:, 0:512], P2[:, 512:1024])
                rpbO = (rP2a, rP2b)
                groups = [(h, k0, min(4, nkt - k0)) for h in range(8) for k0 in range(0, nkt, 4)]
                DEP = 3

                def emit_qk(gi):
                    h, k0, nk = groups[gi]
                    j = h % 4
                    hs = h // 4
                    pbs, rpbs = PBs[gi % 4], rPB[gi % 4]
                    for kk in range(nk):
                        kt = k0 + kk
                        kc = slice(kt * 128, (kt + 1) * 128)
                        mm(pbs[:, kk * 128:(kk + 1) * 128], KnT[:, j, kc], q_t[:, hs * 4 + j, :], True, False, [r_KnT, q_r], [rpbs])
                        mm(pbs[:, kk * 128:(kk + 1) * 128], KrT[:, kc], q_t[:, 8 + hs * 4 + j, :], False, True, [r_KrT, q_r], [rpbs])

                def emit_pv(gi):
                    h, k0, nk = groups[gi]
                    po, rpo = pbO[h // 4], rpbO[h // 4]
                    ocol = (h % 4) * 65
                    pbs, rpbs = PBs[gi % 4], rPB[gi % 4]
                    pts, rpts = PTs[gi % 5]
                    actf(pts[:, 0:nk, :].rearrange("p a q -> p (a q)"), pbs[:, 0:nk * 128], AF.Exp, [rpbs], [rpts], scale=sc)
                    if k0 + nk == nkt:
                        K.I(dve, lambda: nc.vector.memset(pts[64:128, nk - 1, 0:64], 0.0), writes=[rpts])
                    for kk in range(nk):
                        kt = k0 + kk
                        mm(po[:, ocol:ocol + 65], pts[:, kk, :], Vst[:, kt, h, :], kt == 0, kt == nkt - 1, [rpts, r_Vst], [rpo])

                for gi in range(min(DEP, len(groups))):
                    emit_qk(gi)
                for gi in range(len(groups)):
                    if gi + DEP < len(groups):
                        emit_qk(gi + DEP)
                    emit_pv(gi)
                for hh in range(2):
                    ov = pbO[hh][:, 0:260].rearrange("p (h c) -> p h c", c=65)
                    K.I(dve, lambda: nc.vector.reciprocal(rec[:, hh * 4:(hh + 1) * 4], ov[:, :, 64]), reads=[rpbO[hh]], writes=[r_rec])
                    tt(dve, osb[:, hh * 4:(hh + 1) * 4, :], ov[:, :, 0:64],
                       rec[:, hh * 4:(hh + 1) * 4].unsqueeze(2).broadcast_to((128, 4, 64)), ALU.mult, [rpbO[hh], r_rec], [r_osb])
                if gt == 1:
                    dump("osb", osb[:], r_osb, [128, 8, 64])
                    dump("rec", rec[:], r_rec, [128, 8])
                    dump("pts", PTs[0][0][:], PTs[0][1], [128, 4, 128], BF16)
                    dump("qb", q_t[:], q_r, [128, 16, 128], BF16)
                    dump("KrT", KrT[:], r_KrT, [128, S], BF16)
                    dump("KnT", KnT[:], r_KnT, [128, 4, S], BF16)
                    dump("Vst", Vst[:], r_Vst, [128, NT, 8, 65], BF16)
                actf(junk2[:], osb[:].rearrange("p h c -> p (h c)"), AF.Square, [r_osb], [r_junk2, r_sA], accum_out=sA[:, 0:1])
                rstd_from(sA[:, 0:1], r_sA, sA[:, 1:2], r_sA, 512)
                m_t, m_r = mixm[ti % 2]
                stt(dve, m_t[:], osb[:].rearrange("p h c -> p (h c)"), sA[:, 1:2], small["mlan"][0][:],
                    ALU.mult, ALU.mult, [r_osb, r_sA, small["mlan"][1]], [m_r])
                st(sp, mix_s[gt * 128:(gt + 1) * 128, 512:1024], m_t[:], [m_r], bagT[b])

    K.barrier()
    cut(0.8)
    with ExitStack() as pb_:
        wout, r_wout = SB("wout", [128, 8, 1024], BF16, pb_)
        with ExitStack() as pst:
            stg = [SB("stgo%d" % i, [128, 1024], F32, pst) for i in range(2)]
            for k in range(8):
                s_t, s_r = stg[k % 2]
                ld(sp, s_t[:], wout_d[:, k, :], [s_r])
                cast_any(wout[:, k, :], s_t[:], [s_r], [r_wout])
        K.barrier()
        G1, r_G1 = SB("G1", [128, DM], F32, pb_)
        A2, r_A2 = SB("A2", [128, DM], F32, pb_)
        SH2, r_SH2 = SB("SH2", [128, DM], F32, pb_)
        xt = [SB("xtb%d" % i, [128, DM], F32, pb_) for i in range(2)]
        mixin = [SB("mixin%d" % i, [128, DM], BF16, pb_) for i in range(2)]
        mixT, r_mixT = SB("mixT", [128, 8, 128], BF16, pb_)
        junk, r_junk = SB("junkb", [128, DM], BF16, pb_)
        x1, r_x1 = SB("x1", [128, DM], F32, pb_)
        h2s = [SB("h2_%d" % i, [128, DM], F32, pb_) for i in range(2)]
        h2b, r_h2b = SB("h2b", [128, DM], BF16, pb_)
        h2T, r_h2T = SB("h2T", [128, 8, 128], F32, pb_)
        s1s = [SB("s1b%d" % i, [128, 8], F32, pb_) for i in range(2)]
        nv0, r_nv0 = SB("nv0", [128, 1], F32, pb_)
        e4, r_e4 = SB("e4", [128, 4], F32, pb_)
        pb4, rpb4 = PBs[4], rPB[4]
        bag1 = Bag()
        for b in range(2):
            K._wait(sp, bagT[b].evs() + bagA[b].evs())
            tt(dve, vtmp[:], modT[:, 16:24, b], small["gpost1"][0][:], ALU.mult, [r_modT, small["gpost1"][1]], [r_vtmp])
            bcast_rows(vtmp, r_vtmp, G1, r_G1)
            ts(dve, vtmp[:], modT[:, 32:40, b], 1.0, None, ALU.add, None, [r_modT], [r_vtmp])
            tt(dve, vtmp[:], vtmp[:], small["gpre2"][0][:], ALU.mult, [r_vtmp, small["gpre2"][1]], [r_vtmp])
            bcast_rows(vtmp, r_vtmp, A2, r_A2)
            cp(dve, vtmp[:], modT[:, 24:32, b], [r_modT], [r_vtmp])
            bcast_rows(vtmp, r_vtmp, SH2, r_SH2)
            def stageA(ti):
                gt = b * NT + ti
                x_t, x_r = xt[gt % 2]
                mi, r_mi = mixin[gt % 2]
                h2, r_h2 = h2s[gt % 2]
                s1, r_s1 = s1s[gt % 2]
                ld(sp, x_t[:], x_d[gt * 128:(gt + 1) * 128, :], [x_r])
                ld(sp, mi[:], mix_s[gt * 128:(gt + 1) * 128, :], [r_mi])
                for k in range(8):
                    tr(PT[:, k * 128:(k + 1) * 128], mi[:, k * 128:(k + 1) * 128], identb[:], [r_mi, r_identb], [rPT])
                cp(act, mixT[:].rearrange("p k t -> p (k t)"), PT[:], [rPT], [r_mixT])
                for hf in range(2):
                    rp = rP2a if hf == 0 else rP2b
                    for k in range(8):
                        mm(P2[:, hf * 512:(hf + 1) * 512], mixT[:, k, :], wout[:, k, hf * 512:(hf + 1) * 512], k == 0, k == 7,
                           [r_mixT, r_wout], [rp])
                actf(junk[:], P2[:], AF.Square, [rP2a, rP2b], [r_junk, r_s1], accum_out=s1[:, 0:1])
                rstd_from(s1[:, 0:1], r_s1, s1[:, 1:2], r_s1, DM)
                stt(dve, x1[:], P2[:], s1[:, 1:2], G1[:], ALU.mult, ALU.mult, [rP2a, rP2b, r_s1, r_G1], [r_x1])
                tt(dve, x1[:], x1[:], x_t[:], ALU.add, [r_x1, x_r], [r_x1])
                st(sp, x1s_d[gt * 128:(gt + 1) * 128, :], x1[:], [r_x1], bag1)
                actf(junk[:], x1[:], AF.Square, [r_x1], [r_junk, r_s1], accum_out=s1[:, 2:3])
                rstd_from(s1[:, 2:3], r_s1, s1[:, 3:4], r_s1, DM)
                stt(dve, h2[:], x1[:], s1[:, 3:4], A2[:], ALU.mult, ALU.mult, [r_x1, r_s1, r_A2], [r_h2])
                tt(dve, h2[:], h2[:], SH2[:], ALU.add, [r_h2, r_SH2], [r_h2])
                cp(act, h2b[:], h2[:], [r_h2], [r_h2b])
                st(sp, h2s_d[gt * 128:(gt + 1) * 128, :], h2b[:], [r_h2b], bag1)

            def stageB(ti):
                gt = b * NT + ti
                h2, r_h2 = h2s[gt % 2]
                s1, r_s1 = s1s[gt % 2]
                for k in range(8):
                    pbx, rpx = PBs[k // 4], rPB[k // 4]
                    tr(pbx[:, (k % 4) * 128:(k % 4 + 1) * 128], h2[:, k * 128:(k + 1) * 128], identf[:], [r_h2, r_identf], [rpx])
                for hf in range(2):
                    cp(act, h2T[:, hf * 4:(hf + 1) * 4, :].rearrange("p k t -> p (k t)"), PBs[hf][:], [rPB[hf]], [r_h2T])
                wr_t, r_wr = small["wr"]
                for k in range(8):
                    mm(pb4[:, 0:NE], h2T[:, k, :], wr_t[:, k, :], k == 0, k == 7, [r_h2T, r_wr], [rpb4])
                lgt = lg_all[:, gt, :]
                tt(dve, lgt, pb4[:, 0:NE], small["br"][0][:], ALU.add, [rpb4, small["br"][1]], [r_lg])
                K.I(dve, lambda: nc.vector.max(v8_all[:, gt, :], lgt), reads=[r_lg], writes=[r_v8])
                ts(dve, nv0[:], v8_all[:, gt, 0:1], -1.0, None, ALU.mult, None, [r_v8], [r_nv0])
                actf(e4[:], v8_all[:, gt, 0:4], AF.Exp, [r_v8, r_nv0], [r_e4, r_s1], bias=nv0[:, 0:1], accum_out=s1[:, 4:5])
                K.I(dve, lambda: nc.vector.reciprocal(s1[:, 5:6], s1[:, 4:5]), reads=[r_s1], writes=[r_s1])
                ts(dve, gate_all[:, gt, :], e4[:], s1[:, 5:6], None, ALU.mult, None, [r_e4, r_s1], [r_gate])

            stageA(0)
            for ti in range(NT):
                if ti + 1 < NT:
                    stageA(ti + 1)
                stageB(ti)
    K.barrier()
    dump("lg", lg_all[:], r_lg, [128, NTT, NE])
    dump("v8", v8_all[:], r_v8, [128, NTT, 8])
    cut(2)

    base, r_base = SB("base", [128, NE])
    msk, r_msk = SB("msk", [128, NE])
    pos_all, r_pos = SB("pos_all", [128, NTT, NE])
    K.I(dve, lambda: nc.vector.memset(base[:], 0.0), writes=[r_base])
    pb4, rpb4 = PBs[4], rPB[4]
    for gt in range(NTT):
        ts(dve, msk[:], lg_all[:, gt, :], v8_all[:, gt, 3:4], None, ALU.is_ge, None, [r_lg, r_v8], [r_msk])
        mm(pb4[:, 32:64], Lst[:], msk[:], True, True, [r_Lst, r_msk], [rpb4])
        mm(pb4[:, 64:96], onesf[:], msk[:], True, True, [r_onesf, r_msk], [rpb4])
        tt(dve, pos_all[:, gt, :], pb4[:, 32:64], base[:], ALU.add, [rpb4, r_base], [r_pos])
        tt(dve, base[:], pb4[:, 64:96], base[:], ALU.add, [rpb4, r_base], [r_base])
    nblk, r_nblk = SB("nblk", [128, NE])
    incl, r_incl = SB("incl", [128, NE])
    pstart, r_pstart = SB("pstart", [128, NE])
    iotab, r_iotab = SB("iotab", [128, NB])
    bexp_f, r_bexpf = SB("bexp_f", [128, NB])
    bexp_i, r_bexpi = SB("bexp_i", [128, NB], I32)
    dest_i, r_desti = SB("dest_i", [128, NTT, 4], I32)
    widx_i, r_widx = SB("widx_i", [128, NB, 8], I32)
    bidx_i, r_bidx = SB("bidx_i", [128, NB], I32)
    pk, r_pk = SB("pk", [128, 8])
    c1024, r_c1024 = SB("c1024", [128, 8])
    ld(sp, pk[:], pk_d, [r_pk])
    K.I(dve, lambda: nc.vector.memset(c1024[:], 1024.0), writes=[r_c1024])
    ld(sp, iotab[:], iotab_d, [r_iotab])
    off = -0.5 + 1.0 / 1024.0
    ts(dve, nblk[:], base[:], float(BLK - 1), 1.0 / BLK, ALU.add, ALU.mult, [r_base], [r_nblk])
    ts(dve, nblk[:], nblk[:], off, MAGIC, ALU.add, ALU.add, [r_nblk], [r_nblk])
    ts(dve, nblk[:], nblk[:], -MAGIC, None, ALU.add, None, [r_nblk], [r_nblk])
    K.I(dve, lambda: nc.vector.tensor_tensor_scan(incl[:], onesf[:, 0:NE], nblk[:], 0.0, ALU.mult, ALU.add),
        reads=[r_onesf, r_nblk], writes=[r_incl])
    tt(dve, pstart[:], incl[:], nblk[:], ALU.subtract, [r_incl, r_nblk], [r_pstart])
    ts(dve, pstart[:], pstart[:], float(BLK), None, ALU.mult, None, [r_pstart], [r_pstart])
    with ExitStack() as p2a:
        cmp3, r_cmp3 = SB("cmp3", [128, NB, NE], F32, p2a)
        tt(dve, cmp3[:], incl[:].unsqueeze(1).broadcast_to((128, NB, NE)), iotab[:].unsqueeze(2).broadcast_to((128, NB, NE)),
           ALU.is_le, [r_incl, r_iotab], [r_cmp3])
        K.I(dve, lambda: nc.vector.tensor_reduce(bexp_f[:], cmp3[:], AX.X, ALU.add), reads=[r_cmp3], writes=[r_bexpf])
        ts(dve, bexp_f[:], bexp_f[:], float(NE - 1), None, ALU.min, None, [r_bexpf], [r_bexpf])
        cp(dve, bexp_i[:], bexp_f[:], [r_bexpf], [r_bexpi])
        widx_f, r_widxf = SB("widx_f", [128, NB, 8], F32, p2a)
        tt(dve, widx_f[:], bexp_f[:].unsqueeze(2).broadcast_to((128, NB, 8)), c1024[:].unsqueeze(1).broadcast_to((128, NB, 8)),
           ALU.mult, [r_bexpf, r_c1024], [r_widxf])
        tt(dve, widx_f[:], widx_f[:], pk[:].unsqueeze(1).broadcast_to((128, NB, 8)), ALU.add, [r_widxf, r_pk], [r_widxf])
        cp(dve, widx_i[:], widx_f[:], [r_widxf], [r_widx])
        ts(dve, bexp_f[:], bexp_f[:], 128.0, pk[:, 0:1], ALU.mult, ALU.add, [r_bexpf, r_pk], [r_bexpf])
        cp(dve, bidx_i[:], bexp_f[:], [r_bexpf], [r_bidx])
        destf, r_destf = SB("destf", [128, NE], F32, p2a)
        oh, r_oh = SB("oh", [128, 4, NE], F32, p2a)
        dk, r_dk = SB("dk", [128, 4], F32, p2a)
        hb = [SB("hb%d" % i, [128, DM], BF16, p2a) for i in range(2)]
        r_xg = Res()
        for gt in range(NTT):
            tt(dve, destf[:], pos_all[:, gt, :], pstart[:], ALU.add, [r_pos, r_pstart], [r_destf])
            tt(dve, oh[:], lg_all[:, gt, :].unsqueeze(1).broadcast_to((128, 4, NE)),
               v8_all[:, gt, 0:4].unsqueeze(2).broadcast_to((128, 4, NE)), ALU.is_equal, [r_lg, r_v8], [r_oh])
            tt(dve, oh[:], oh[:], destf[:].unsqueeze(1).broadcast_to((128, 4, NE)), ALU.mult, [r_oh, r_destf], [r_oh])
            K.I(dve, lambda: nc.vector.tensor_reduce(dk[:], oh[:], AX.X, ALU.add), reads=[r_oh], writes=[r_dk])
            cp(dve, dest_i[:, gt, :], dk[:], [r_dk], [r_desti])
            h_t, h_r = hb[gt % 2]
            ld(sp, h_t[:], h2s_d[gt * 128:(gt + 1) * 128, :], [h_r])
            if gt == 0:
                K._wait(sp, bag1.evs())
            for k in range(4):
                K.dma(pool, lambda: nc.gpsimd.indirect_dma_start(
                    out=xg_d, out_offset=bass.IndirectOffsetOnAxis(ap=dest_i[:, gt, k:k + 1], axis=0),
                    in_=h_t[:], in_offset=None), reads=[h_r, r_desti], writes=[])
    K.barrier()
    dump("desti", dest_i[:], r_desti, [128, NTT, 4], I32)
    dump("bexp", bexp_i[:], r_bexpi, [128, NB], I32)

    cut(3)
    scat_evs = [(sid, K.semvals[sid]) for sid in pool.ring if K.semvals[sid] > 0]

    with ExitStack() as p2:
        wgu = [SB("wgu%d" % i, [128, 8, 2 * DM], BF16, p2) for i in range(2)]
        wdn = [SB("wdn%d" % i, [128, 8, DM], BF16, p2) for i in range(2)]
        bgu = [SB("bgus%d" % i, [128, 16], F32, p2) for i in range(2)]
        bdn = [SB("bdns%d" % i, [128, DM], F32, p2) for i in range(2)]
        xgt = [SB("xgt%d" % i, [128, DM], BF16, p2) for i in range(2)]
        xgTs = [SB("xgT%d" % i, [128, 8, BLK], BF16, p2) for i in range(2)]
        actT, r_actT = SB("actT", [128, 8, BLK], BF16, p2)
        gg, r_gg = SB("gg", [128, BLK], F32, p2)
        sg, r_sg = SB("sg", [128, BLK], F32, p2)
        ll, r_ll = SB("ll", [128, BLK], F32, p2)
        yo = [SB("yo%d" % i, [128, DM], F32, p2) for i in range(2)]
        K._wait(sp, scat_evs)
        wgr = [[Res() for _ in range(8)] for _ in range(2)]
        wdr = [[Res() for _ in range(8)] for _ in range(2)]

        def load_weights(blk):
            wg_t = wgu[blk % 2][0]
            wd_t = wdn[blk % 2][0]
            bg_t, bg_r = bgu[blk % 2]
            bd_t, bd_r = bdn[blk % 2]
            for k in range(8):
                K.dma(pool, lambda: nc.gpsimd.indirect_dma_start(
                    out=wg_t[:, k, :], out_offset=None, in_=wgu_d,
                    in_offset=bass.IndirectOffsetOnAxis(ap=widx_i[:, blk, k:k + 1], axis=0)), reads=[r_widx], writes=[wgr[blk % 2][k]])
            for k in range(8):
                K.dma(pool, lambda: nc.gpsimd.indirect_dma_start(
                    out=wd_t[:, k, :], out_offset=None, in_=wd_d,
                    in_offset=bass.IndirectOffsetOnAxis(ap=widx_i[:, blk, k:k + 1], axis=0)), reads=[r_widx], writes=[wdr[blk % 2][k]])
            K.dma(pool, lambda: nc.gpsimd.indirect_dma_start(
                out=bg_t[:], out_offset=None, in_=bgu_d,
                in_offset=bass.IndirectOffsetOnAxis(ap=bidx_i[:, blk:blk + 1], axis=0)), reads=[r_bidx], writes=[bg_r])
            K.dma(pool, lambda: nc.gpsimd.indirect_dma_start(
                out=bd_t[:], out_offset=None, in_=bd_d,
                in_offset=bass.IndirectOffsetOnAxis(ap=bidx_i[:, blk:blk + 1], axis=0)), reads=[r_bidx], writes=[bd_r])

        PT2 = PBs[4][:].bitcast(BF16)
        ptn = [0]

        def prep_tokens(blk):
            xgT_, r_xgT_ = xgTs[blk % 2]
            for st in range(4):
                g_t, g_r = xgt[st % 2]
                r0 = blk * BLK + st * 128
                ld(sp, g_t[:], xg_d[r0:r0 + 128, :], [g_r])
                if ptn[0] % 2 == 0:
                    pt_ap, pt_r = PT[:], rPT
                else:
                    pt_ap, pt_r = PT2, rPB[4]
                ptn[0] += 1
                for k in range(8):
                    tr(pt_ap[:, k * 128:(k + 1) * 128], g_t[:, k * 128:(k + 1) * 128], identb[:], [g_r, r_identb], [pt_r])
                cp(act, xgT_[:, :, st * 128:(st + 1) * 128],
                   pt_ap.rearrange("p (k t) -> p k t", t=128), [pt_r], [r_xgT_])

        load_weights(0)
        prep_tokens(0)
        for blk in range(NB):
            if blk + 1 < NB:
                load_weights(blk + 1)
                prep_tokens(blk + 1)
            wg_t = wgu[blk % 2][0]
            wd_t = wdn[blk % 2][0]
            wg_rs = wgr[blk % 2]
            wd_rs = wdr[blk % 2]
            bg_t, bg_r = bgu[blk % 2]
            bd_t, bd_r = bdn[blk % 2]
            xgT, r_xgT = xgTs[blk % 2]
            for fc in range(8):
                pg, rpg = PBs[(fc % 2) * 2], rPB[(fc % 2) * 2]
                pl, rpl = PBs[(fc % 2) * 2 + 1], rPB[(fc % 2) * 2 + 1]
                for k in range(8):
                    mm(pg[:], wg_t[:, k, fc * 128:(fc + 1) * 128], xgT[:, k, :], k == 0, k == 7, [wg_rs[k], r_xgT], [rpg])
                for k in range(8):
                    mm(pl[:], wg_t[:, k, DM + fc * 128:DM + (fc + 1) * 128], xgT[:, k, :], k == 0, k == 7, [wg_rs[k], r_xgT], [rpl])
                ts(dve, gg[:], pg[:], bg_t[:, fc:fc + 1], 7.0, ALU.add, ALU.min, [rpg, bg_r], [r_gg])
                actf(sg[:], gg[:], AF.Sigmoid, [r_gg], [r_sg], scale=1.702)
                ts(dve, ll[:], pl[:], bg_t[:, 8 + fc:9 + fc], 7.0, ALU.add, ALU.min, [rpl, bg_r], [r_ll])
                ts(dve, ll[:], ll[:], -7.0, 1.0, ALU.max, ALU.add, [r_ll], [r_ll])
                tt(dve, gg[:], gg[:], sg[:], ALU.mult, [r_gg, r_sg], [r_gg])
                tt(dve, actT[:, fc, :], gg[:], ll[:], ALU.mult, [r_gg, r_ll], [r_actT])
            dbanks = ((P2[:, 0:512], rP2a), (P2[:, 512:1024], rP2b), (PBs[2][:], rPB[2]), (PBs[3][:], rPB[3]))
            for st in range(4):
                y_t, y_r = yo[st % 2]
                for hf in range(2):
                    pd, rpd = dbanks[(st % 2) * 2 + hf]
                    for k in range(8):
                        mm(pd, actT[:, k, st * 128:(st + 1) * 128], wd_t[:, k, hf * 512:(hf + 1) * 512], k == 0, k == 7,
                           [r_actT, wd_rs[k]], [rpd])
                    tt(dve, y_t[:, hf * 512:(hf + 1) * 512], pd, bd_t[:, hf * 512:(hf + 1) * 512], ALU.add, [rpd, bd_r], [y_r])
                r0 = blk * BLK + st * 128
                K.dma(act, lambda: nc.scalar.dma_start(out=yb_d[r0:r0 + 128, :], in_=y_t[:]), reads=[y_r], writes=[])
    K.barrier()
    yb_evs = [(sid, K.semvals[sid]) for sid in act.ring if K.semvals[sid] > 0]

    with ExitStack() as p3:
        yg = [SB("yg%d" % i, [128, DM], F32, p3) for i in range(8)]
        fa, r_fa = SB("fa", [128, DM], F32, p3)
        x1r = [SB("x1r%d" % i, [128, DM], F32, p3) for i in range(2)]
        ob = [SB("ob%d" % i, [128, DM], F32, p3) for i in range(2)]
        junk3, r_junk3 = SB("junk3", [128, DM], BF16, p3)
        s3, r_s3 = SB("s3", [128, 2], F32, p3)
        G2, r_G2 = SB("G2", [128, 2, DM], F32, p3)
        for b in range(2):
            tt(dve, vtmp[:], modT[:, 40:48, b], small["gpost2"][0][:], ALU.mult, [r_modT, small["gpost2"][1]], [r_vtmp])
            bcast_rows(vtmp, r_vtmp, G2[:, b, :], r_G2)
        K._wait(pool, yb_evs)
        K._wait(sp, bag1.evs())
        def p3_loads(gt):
            x_t, x_r = x1r[gt % 2]
            ld(sp, x_t[:], x1s_d[gt * 128:(gt + 1) * 128, :], [x_r])
            for k in range(4):
                y_t, y_r = yg[(gt % 2) * 4 + k]
                K.dma(pool, lambda: nc.gpsimd.indirect_dma_start(
                    out=y_t[:], out_offset=None, in_=yb_d,
                    in_offset=bass.IndirectOffsetOnAxis(ap=dest_i[:, gt, k:k + 1], axis=0)), reads=[r_desti], writes=[y_r])

        def p3_compute(gt):
            b = gt // NT
            x_t, x_r = x1r[gt % 2]
            o_t, o_r = ob[gt % 2]
            ygs = [yg[(gt % 2) * 4 + k] for k in range(4)]
            actf(fa[:], ygs[0][0][:], AF.Copy, [ygs[0][1], r_gate], [r_fa], scale=gate_all[:, gt, 0:1])
            for k in range(1, 4):
                stt(dve, fa[:], ygs[k][0][:], gate_all[:, gt, k:k + 1], fa[:], ALU.mult, ALU.add, [ygs[k][1], r_gate, r_fa], [r_fa])
            actf(junk3[:], fa[:], AF.Square, [r_fa], [r_junk3, r_s3], accum_out=s3[:, 0:1])
            actf(s3[:, 1:2], s3[:, 0:1], AF.Ln, [r_s3], [r_s3], scale=1.0 / DM, bias=epsb[:, 0:1])
            actf(s3[:, 1:2], s3[:, 1:2], AF.Exp, [r_s3], [r_s3], scale=-0.5)
            stt(dve, o_t[:], fa[:], s3[:, 1:2], G2[:, b, :], ALU.mult, ALU.mult, [r_fa, r_s3, r_G2], [o_r])
            tt(pool, o_t[:], o_t[:], x_t[:], ALU.add, [o_r, x_r], [o_r])
            K.dma(sp, lambda: nc.sync.dma_start(out=out_d[gt * 128:(gt + 1) * 128, :], in_=o_t[:]), reads=[o_r], writes=[], is_out=True)

        p3_loads(0)
        for gt in range(NTT):
            if gt + 1 < NTT:
                p3_loads(gt + 1)
            p3_compute(gt)
    K.barrier()
    K.finish()
    es.close()
    return nc, dbg_d


def _consts(NB):
    idx = np.arange(128)
    ch = idx // 64
    same = ch[:, None] == ch[None, :]
    U1 = (same & (idx[:, None] > idx[None, :])).astype(np.float32)
    U2 = (same & (idx[:, None] <= idx[None, :])).astype(np.float32)
    m01 = U2.copy()
    Lst = (idx[:, None] < idx[None, :]).astype(np.float32)
    invf = (np.float32(10000.0) ** (-(np.arange(16, dtype=np.float32) * np.float32(2.0) / np.float32(32.0)))).astype(np.float32)
    rm = np.stack([(ch == 0), (ch == 1)], axis=1).astype(np.float32)
    return dict(identf=np.eye(128, dtype=np.float32), U1=U1, U2=U2, m01=m01, Lst=Lst, rm=rm,
                invf=np.tile(invf[None, :], (128, 1)).astype(np.float32),
                iotab=np.tile(np.arange(NB, dtype=np.float32)[None, :], (128, 1)),
                pk=(np.arange(8, dtype=np.float32)[None, :] * 128 + np.arange(128, dtype=np.float32)[:, None]).astype(np.float32))


def _rep(v, n=128):
    return np.ascontiguousarray(np.tile(np.asarray(v, np.float32).reshape(1, -1), (n, 1)))


def _pk(v, k):
    return np.ascontiguousarray(np.asarray(v, np.float32).reshape(k, 128).T)


_CACHE = {}


def kernel(x, c, positions, w_ada, b_ada, pre_mix_norm, w_in, conv_w, conv_b, dt_bias, a_log, d_skip, ssd_norm,
           q_a_norm, w_q_up, kv_a_norm, w_kv_up, mla_norm, w_out, post_mix_norm, pre_ffn_norm, w_router, b_router,
           w_gate_up, b_gate_up, w_down, b_down, post_ffn_norm, _dbg=()):
    x = np.asarray(x, np.float32)
    Bsz, S, _ = x.shape
    ncores = Bsz // 2
    NT = S // 128
    NB = (2 * S * 4) // BLK + NE
    key = (S, tuple(_dbg))
    if key not in _CACHE:
        _CACHE[key] = build(S, _dbg)
    nc, dbg_d = _CACHE[key]
    f = lambda a: np.ascontiguousarray(np.asarray(a, np.float32))
    w_in = f(w_in)[0]
    w_tm = np.ascontiguousarray(np.concatenate([w_in[:, 0:512], w_in[:, 1536:1544], w_in[:, 2184:2216]], axis=1))
    w_fm = np.ascontiguousarray(np.concatenate([w_in[:, 512:1536], w_in[:, 1544:1928], w_in[:, 1928:2184]], axis=1))
    wq = f(w_q_up)[0].reshape(384, 8, 96)
    qn = wq[:, :, 0:64]
    qr = wq[:, :, 64:96]
    qn_pairs = np.stack([np.concatenate([qn[:, j], qn[:, j + 4]], axis=1) for j in range(4)], axis=1)
    wq2 = np.ascontiguousarray(np.concatenate([qn_pairs.reshape(384, 512), qr.reshape(384, 256)], axis=1))
    wkv = f(w_kv_up)[0].reshape(256, 8, 128)
    kn = wkv[:, :, 0:64]
    vv = wkv[:, :, 64:128]
    kn_pairs = np.stack([np.concatenate([kn[:, j], kn[:, j + 4]], axis=1) for j in range(4)], axis=1)
    wkv2 = np.ascontiguousarray(np.concatenate([kn_pairs.reshape(256, 512), vv.reshape(256, 512)], axis=1))
    cw = f(conv_w)[0]
    convw = np.ascontiguousarray(cw.reshape(4, 8, 128).transpose(2, 1, 0))
    shared = dict(
        w_ada=f(w_ada)[0], b_adaT=_pk(f(b_ada)[0], 48), g_pre1=_pk(f(pre_mix_norm)[0], 8), g_post1=_pk(f(post_mix_norm)[0], 8),
        g_pre2=_pk(f(pre_ffn_norm)[0], 8), g_post2=_pk(f(post_ffn_norm)[0], 8), w_tm=w_tm, w_fm=w_fm, convw=convw,
        convb=_pk(f(conv_b)[0], 8), dtb=_rep(f(dt_bias)[0]), alog=_rep(f(a_log)[0]), dsk=_rep(f(d_skip)[0]),
        ssdn=_rep(f(ssd_norm)[0]), mlan=_rep(f(mla_norm)[0]), qan=_pk(f(q_a_norm)[0], 3), kvan=_pk(f(kv_a_norm)[0], 2),
        wq=wq2, wkv=wkv2, wout=f(w_out)[0], wr=f(w_router)[0], br=_rep(f(b_router)[0]),
        wgu=f(w_gate_up)[0].reshape(NE * DM, 2 * DM), wd=f(w_down)[0].reshape(NE * DM, DM),
        bgu=np.ascontiguousarray(f(b_gate_up)[0].reshape(NE, 16, 128).transpose(0, 2, 1)).reshape(NE * 128, 16),
        bd=np.ascontiguousarray(np.broadcast_to(f(b_down)[0][:, None, :], (NE, 128, DM))).reshape(NE * 128, DM),
    )
    shared.update(_consts(NB))
    cc = f(c)
    pp = np.asarray(positions, np.int32)
    in_maps = []
    for ci in range(ncores):
        m = dict(shared)
        m["x"] = np.ascontiguousarray(x[2 * ci:2 * ci + 2].reshape(2 * S, DM))
        m["cT"] = np.ascontiguousarray(cc[2 * ci:2 * ci + 2].reshape(2, 8, 128).transpose(2, 1, 0))
        m["pos"] = np.ascontiguousarray(pp[2 * ci:2 * ci + 2].reshape(2, NT, 128).transpose(0, 2, 1))
        in_maps.append(m)
    res = run_bass_kernel_spmd(nc, in_maps, core_ids=list(range(ncores)))
    out = np.stack([np.asarray(r["out"], np.float32).reshape(2, S, DM) for r in res.results], axis=0).reshape(Bsz, S, DM)
    if _dbg:
        return out, [{k: np.asarray(r["dbg_" + k]) for k in dbg_d} for r in res.results]
    return out
```

```python
import math
from contextlib import ExitStack
import numpy as np
import concourse.bass as bass
import concourse.mybir as mybir
from concourse.bass_utils import run_bass_kernel_spmd

F32 = mybir.dt.float32
BF16 = mybir.dt.bfloat16
I32 = mybir.dt.int32
AF = mybir.ActivationFunctionType
ALU = mybir.AluOpType
AX = mybir.AxisListType

import os
_PH = float(os.environ.get("KPH", "9"))
DM = 1024
NE = 32
BLK = 512
EPS = 1e-6
MAGIC = 12582912.0
C1 = 6.28125
C2 = 2.0 * math.pi - 6.28125


class Res:
    __slots__ = ("w", "rs")

    def __init__(self):
        self.w = None
        self.rs = {}


class Eng:
    def __init__(self, h, sid, is_pe=False):
        self.h = h
        self.sid = sid
        self.n = 0
        self.known = {}
        self.is_pe = is_pe
        self.ring = []
        self.rpos = 0


class Kern:
    def __init__(self, nc, es):
        self.nc = nc
        self.sems = []
        self.semvals = []
        self.es = es
        mk = lambda nm: self._newsem(nm)
        self.pe = Eng(nc.tensor, mk("s_pe"), True)
        self.dve = Eng(nc.vector, mk("s_dve"))
        self.act = Eng(nc.scalar, mk("s_act"))
        self.pool = Eng(nc.gpsimd, mk("s_pool"))
        self.sp = Eng(nc.sync, mk("s_sp"))
        for e, n in ((self.sp, 44), (self.act, 12), (self.pool, 36)):
            e.ring = [mk("d%d_%d" % (e.sid, i)) for i in range(n)]
        self.out_evs = []

    def _newsem(self, nm):
        s = self.es.enter_context(self.nc.semaphore(nm))
        self.sems.append(s)
        self.semvals.append(0)
        return len(self.sems) - 1

    def _wait(self, eng, evs):
        best = {}
        for sid, val in evs:
            if val > best.get(sid, 0):
                best[sid] = val
        for sid, val in best.items():
            if eng.is_pe and sid == eng.sid:
                continue
            if eng.known.get(sid, 0) >= val:
                continue
            eng.h.wait_ge(self.sems[sid], val)
            eng.known[sid] = val

    @staticmethod
    def _deps(reads, writes):
        evs = []
        for r in reads:
            if r.w is not None:
                evs.append(r.w)
        for r in writes:
            if r.w is not None:
                evs.append(r.w)
            evs.extend(r.rs.items())
        return evs

    @staticmethod
    def _mark(ev, reads, writes):
        for r in reads:
            if ev[1] > r.rs.get(ev[0], 0):
                r.rs[ev[0]] = ev[1]
        for r in writes:
            r.w = ev
            r.rs = {}

    def I(self, eng, fn, reads=(), writes=()):
        evs = self._deps(reads, writes)
        own = eng.sid
        evs2 = []
        raw = set()
        for r in reads:
            if r.w is not None:
                raw.add(r.w)
        for ev in evs:
            if ev[0] == own and ev not in raw:
                continue
            evs2.append(ev)
        self._wait(eng, evs2)
        ins = fn()
        eng.n += 1
        ins.then_inc(self.sems[eng.sid], 1)
        self._mark((eng.sid, eng.n), reads, writes)

    def dma(self, q, fn, reads=(), writes=(), is_out=False):
        evs = self._deps(reads, writes)
        sid = q.ring[q.rpos % len(q.ring)]
        q.rpos += 1
        prev = self.semvals[sid]
        if prev > 0:
            evs.append((sid, prev))
        self._wait(q, evs)
        ins = fn()
        self.semvals[sid] = prev + 16
        ins.then_inc(self.sems[sid], 16)
        ev = (sid, prev + 16)
        self._mark(ev, reads, writes)
        if is_out:
            self.out_evs.append(ev)
        return ev

    def barrier(self):
        evs = []
        for e in (self.sp, self.act, self.pool):
            evs += [(sid, self.semvals[sid]) for sid in e.ring if self.semvals[sid] > 0]
        for e in (self.pe, self.dve, self.act, self.pool):
            if e.n > 0:
                evs.append((e.sid, e.n))
        for e in (self.pe, self.dve, self.act, self.pool, self.sp):
            self._wait(e, [ev for ev in evs if ev[0] != e.sid])

    def finish(self):
        evs = list(self.out_evs)
        for e in (self.sp, self.act, self.pool):
            evs += [(sid, self.semvals[sid]) for sid in e.ring if self.semvals[sid] > 0]
        for e in (self.pe, self.dve, self.act, self.pool):
            if e.n > 0:
                evs.append((e.sid, e.n))
        self._wait(self.sp, evs)


class _Cut(Exception):
    pass


def build(S, dbg=()):
    ctx = {}
    try:
        return _build_inner(S, dbg, ctx)
    except _Cut:
        ctx["K"].finish()
        return ctx["nc"], ctx["dbg_d"]


def _build_inner(S, dbg, ctx):
    NT = S // 128
    NG = S // 512
    T = 2 * S
    NTT = T // 128
    NB = (T * 4) // BLK + NE
    nc = bass.Bass("TRN2", target_bir_lowering=False)
    es = ExitStack()
    K = Kern(nc, es)
    pe, dve, act, pool, sp = K.pe, K.dve, K.act, K.pool, K.sp
    dbg_d = {}

    def DI(name, shape, dt=F32):
        return nc.dram_tensor(name, list(shape), dt, kind="ExternalInput").ap()

    def DS(name, shape, dt=F32):
        if name in dbg:
            d = nc.dram_tensor("dbg_" + name, list(shape), dt, kind="ExternalOutput").ap()
            dbg_d[name] = d
            return d
        return nc.dram_tensor(name, list(shape), dt, kind="Internal").ap()

    x_d = DI("x", [T, DM])
    cT_d = DI("cT", [128, 8, 2])
    pos_d = DI("pos", [2, 128, NT], I32)
    wada_d = DI("w_ada", [DM, 6 * DM]).rearrange("(k p) f -> p k f", p=128)
    badaT_d = DI("b_adaT", [128, 48])
    gpre1_d = DI("g_pre1", [128, 8])
    gpost1_d = DI("g_post1", [128, 8])
    gpre2_d = DI("g_pre2", [128, 8])
    gpost2_d = DI("g_post2", [128, 8])
    wtm_d = DI("w_tm", [DM, 552]).rearrange("(k p) f -> p k f", p=128)
    wfm_d = DI("w_fm", [DM, 1664]).rearrange("(k p) f -> p k f", p=128)
    convw_d = DI("convw", [128, 8, 4])
    convb_d = DI("convb", [128, 8])
    dtb_d = DI("dtb", [128, 8])
    alog_d = DI("alog", [128, 8])
    dsk_d = DI("dsk", [128, 8])
    ssdn_d = DI("ssdn", [128, 512])
    mlan_d = DI("mlan", [128, 512])
    qan_d = DI("qan", [128, 3])
    kvan_d = DI("kvan", [128, 2])
    wq_d = DI("wq", [384, 768]).rearrange("(k p) f -> p k f", p=128)
    wkv_d = DI("wkv", [256, 1024]).rearrange("(k p) f -> p k f", p=128)
    wout_d = DI("wout", [DM, DM]).rearrange("(k p) f -> p k f", p=128)
    wr_d = DI("wr", [DM, NE]).rearrange("(k p) f -> p k f", p=128)
    br_d = DI("br", [128, NE])
    wgu_d = DI("wgu", [NE * DM, 2 * DM])
    wd_d = DI("wd", [NE * DM, DM])
    bgu_d = DI("bgu", [NE * 128, 16])
    bd_d = DI("bd", [NE * 128, DM])
    identf_d = DI("identf", [128, 128])
    U1_d = DI("U1", [128, 128])
    U2_d = DI("U2", [128, 128])
    Lst_d = DI("Lst", [128, 128])
    m01_d = DI("m01", [128, 128])
    invf_d = DI("invf", [128, 16])
    iotab_d = DI("iotab", [128, NB])
    pk_d = DI("pk", [128, 8])
    rm_d = DI("rm", [128, 2])
    x1s_d = DS("x1s", [T, DM])
    h2s_d = DS("h2s", [T, DM], BF16)
    xg_d = DS("xg", [NB * BLK, DM], BF16)
    yb_d = DS("yb", [NB * BLK, DM])
    out_d = nc.dram_tensor("out", [T, DM], F32, kind="ExternalOutput").ap()
    ctx.update(K=K, es=es, nc=nc, dbg_d=dbg_d)

    def cut(level):
        if _PH < level:
            raise _Cut()

    def SB(name, shape, dt=F32, stack=None):
        t = (stack or es).enter_context(nc.sbuf_tensor("sb_" + name, list(shape), dt))
        return t, Res()

    def PS(name, shape, dt=F32):
        return es.enter_context(nc.psum_tensor("ps_" + name, list(shape), dt))

    P2 = PS("P2", [128, 1024])
    rP2a, rP2b = Res(), Res()
    PBs = [PS("PB%d" % i, [128, 512]) for i in range(5)]
    rPB = [Res() for _ in range(5)]
    PT = PS("PT", [128, 1024], BF16)
    rPT = Res()

    def mm(out, lhsT, rhs, start, stop, rd, wr):
        K.I(pe, lambda: nc.tensor.matmul(out, lhsT, rhs, start=start, stop=stop), reads=rd, writes=wr)

    def tr(out, in_, ident, rd, wr):
        K.I(pe, lambda: nc.tensor.transpose(out, in_, ident), reads=rd, writes=wr)

    def actf(out, in_, func, rd, wr, bias=None, scale=None, accum_out=None):
        kw = {}
        if bias is not None:
            kw["bias"] = bias
        if scale is not None:
            kw["scale"] = scale
        if accum_out is not None:
            kw["accum_out"] = accum_out
        K.I(act, lambda: nc.scalar.activation(out, in_, func, **kw), reads=rd, writes=wr)

    def ts(eng, out, in0, s1, s2, op0, op1, rd, wr):
        h = eng.h
        if op1 is None:
            K.I(eng, lambda: h.tensor_scalar(out, in0, s1, None, op0), reads=rd, writes=wr)
        else:
            K.I(eng, lambda: h.tensor_scalar(out, in0, s1, s2, op0, op1), reads=rd, writes=wr)

    def tt(eng, out, in0, in1, op, rd, wr):
        h = eng.h
        K.I(eng, lambda: h.tensor_tensor(out, in0, in1, op), reads=rd, writes=wr)

    def stt(eng, out, in0, scalar, in1, op0, op1, rd, wr):
        h = eng.h
        K.I(eng, lambda: h.scalar_tensor_tensor(out, in0, scalar, in1, op0, op1), reads=rd, writes=wr)

    def cp(eng, out, in_, rd, wr):
        h = eng.h
        if eng is act:
            K.I(eng, lambda: nc.scalar.activation(out, in_, AF.Copy), reads=rd, writes=wr)
        else:
            K.I(eng, lambda: h.tensor_copy(out, in_), reads=rd, writes=wr)

    def ld(q, out, in_, wr, rd=()):
        K.dma(q, lambda: q.h.dma_start(out=out, in_=in_), reads=rd, writes=wr)

    def dump(name, ap, res, shape, dt=F32):
        if name not in dbg:
            return
        d = nc.dram_tensor("dbg_" + name, list(shape), dt, kind="ExternalOutput").ap()
        dbg_d[name] = d
        K.dma(sp, lambda: nc.sync.dma_start(out=d, in_=ap), reads=[res], writes=[Res()], is_out=True)

    identf, r_identf = SB("identf", [128, 128])
    identb, r_identb = SB("identb", [128, 128], BF16)
    onesf, r_onesf = SB("onesf", [128, 128])
    onesb, r_onesb = SB("onesb", [128, 128], BF16)
    U1, r_U1 = SB("U1", [128, 128])
    U2, r_U2 = SB("U2", [128, 128])
    Lst, r_Lst = SB("Lst", [128, 128])
    m01, r_m01 = SB("m01", [128, 128])
    invf, r_invf = SB("invf", [128, 16])
    ld(sp, identf[:], identf_d, [r_identf])
    ld(sp, U1[:], U1_d, [r_U1])
    ld(sp, U2[:], U2_d, [r_U2])
    ld(sp, Lst[:], Lst_d, [r_Lst])
    ld(sp, m01[:], m01_d, [r_m01])
    ld(sp, invf[:], invf_d, [r_invf])
    rm, r_rm = SB("rm", [128, 2])
    Mc, r_Mc = SB("Mc", [128, 2, 128])
    ld(sp, rm[:], rm_d, [r_rm])
    cp(dve, identb[:], identf[:], [r_identf], [r_identb])
    K.I(dve, lambda: nc.vector.memset(onesf[:], 1.0), writes=[r_onesf])
    K.I(dve, lambda: nc.vector.memset(onesb[:], 1.0), writes=[r_onesb])
    for c in range(2):
        ts(dve, Mc[:, c, :], onesf[:], rm[:, c:c + 1], None, ALU.mult, None, [r_onesf, r_rm], [r_Mc])
    epsb, r_epsb = SB("epsb", [128, 1])
    oneb, r_oneb = SB("oneb", [128, 1])
    K.I(dve, lambda: nc.vector.memset(epsb[:], EPS), writes=[r_epsb])
    K.I(dve, lambda: nc.vector.memset(oneb[:], 1.0), writes=[r_oneb])

    small = {}
    for nm, d, shp in (("gpre1", gpre1_d, [128, 8]), ("gpost1", gpost1_d, [128, 8]), ("gpre2", gpre2_d, [128, 8]),
                       ("gpost2", gpost2_d, [128, 8]), ("convw", convw_d, [128, 8, 4]), ("convb", convb_d, [128, 8]),
                       ("dtb", dtb_d, [128, 8]), ("alog", alog_d, [128, 8]), ("dsk", dsk_d, [128, 8]),
                       ("ssdn", ssdn_d, [128, 512]), ("mlan", mlan_d, [128, 512]), ("qan", qan_d, [128, 3]),
                       ("kvan", kvan_d, [128, 2]), ("br", br_d, [128, NE]), ("badaT", badaT_d, [128, 48]),
                       ("cT", cT_d, [128, 8, 2]), ("wr", wr_d, [128, 8, NE])):
        t, r = SB("c_" + nm, shp)
        ld(sp, t[:], d, [r])
        small[nm] = (t, r)

    aneg, r_aneg = SB("aneg", [128, 8])
    actf(aneg[:], small["alog"][0][:], AF.Exp, [small["alog"][1]], [r_aneg])
    ts(dve, aneg[:], aneg[:], -1.0, None, ALU.mult, None, [r_aneg], [r_aneg])

    modT, r_modT = SB("modT", [128, 48, 2])
    cact, r_cact = SB("cact", [128, 8, 2])
    actf(cact[:], small["cT"][0][:], AF.Silu, [small["cT"][1]], [r_cact])
    with ExitStack() as st0:
        wst = [SB("wadast%d" % i, [128, 8, 512], F32, st0) for i in range(2)]
        for blk in range(12):
            w_t, w_r = wst[blk % 2]
            ld(sp, w_t[:], wada_d[:, :, blk * 512:(blk + 1) * 512], [w_r])
            for j in range(4):
                fc = blk * 4 + j
                pb, rpb = PBs[fc % 2], rPB[fc % 2]
                for k in range(8):
                    mm(pb[:, 0:2], w_t[:, k, j * 128:(j + 1) * 128], cact[:, k, :], k == 0, k == 7,
                       [w_r, r_cact], [rpb])
                ts(dve, modT[:, fc, :], pb[:, 0:2], small["badaT"][0][:, fc:fc + 1], None, ALU.add, None,
                   [rpb, small["badaT"][1]], [r_modT])
    K.barrier()
    a1T, r_a1T = SB("a1T", [128, 2, 8])
    sh1T, r_sh1T = SB("sh1T", [128, 2, 8])
    vtmp, r_vtmp = SB("vtmp", [128, 8])
    btmp = [SB("btmp%d" % i, [128, 128]) for i in range(2)]
    bcn = [0]

    def bcast_rows(vT_ap, vres, dst_ap, dres):
        for half in range(2):
            pb, rpb = PBs[2 + half], rPB[2 + half]
            for kk in range(4):
                k = half * 4 + kk
                bt, rbt = btmp[bcn[0] % 2]
                bcn[0] += 1
                ts(dve, bt[:], onesf[:], vT_ap[:, k:k + 1], None, ALU.mult, None, [r_onesf, vres], [rbt])
                mm(pb[:, kk * 128:(kk + 1) * 128], bt[:], identf[:], True, True, [rbt, r_identf], [rpb])
            cp(act, dst_ap[:, half * 512:(half + 1) * 512], pb[:], [rpb], [dres])

    for b in range(2):
        ts(dve, vtmp[:], modT[:, 8:16, b], 1.0, None, ALU.add, None, [r_modT], [r_vtmp])
        tt(dve, a1T[:, b, :], vtmp[:], small["gpre1"][0][:], ALU.mult, [r_vtmp, small["gpre1"][1]], [r_a1T])
        cp(dve, sh1T[:, b, :], modT[:, 0:8, b], [r_modT], [r_sh1T])

    lg_all, r_lg = SB("lg_all", [128, NTT, NE])
    v8_all, r_v8 = SB("v8_all", [128, NTT, 8])
    gate_all, r_gate = SB("gate_all", [128, NTT, 4])

    castn = [0]

    def cast_any(out, in_, rd, wr):
        e = (dve, pool, act)[castn[0] % 3]
        castn[0] += 1
        cp(e, out, in_, rd, wr)

    def rstd_from(ss_ap, ssres, out_ap, ores, n, eng=dve):
        actf(out_ap, ss_ap, AF.Ln, [ssres], [ores], bias=epsb[:, 0:1], scale=1.0 / n)
        actf(out_ap, out_ap, AF.Exp, [ores], [ores], scale=-0.5)

    class Bag:
        def __init__(self):
            self.d = {}

        def add(self, ev):
            if ev[1] > self.d.get(ev[0], 0):
                self.d[ev[0]] = ev[1]

        def evs(self):
            return list(self.d.items())

    kn_s = DS("kn_s", [2, 128, 4 * S], BF16)
    kr_s = DS("kr_s", [2, 128, S], BF16)
    v_s = DS("v_s", [2, 128, NT * 8 * 65], BF16)
    q_s = DS("q_s", [2 * NT, 128, 2048], BF16)
    mix_s = DS("mix_s", [T, DM], BF16)
    bagA = [Bag(), Bag()]
    bagT = [Bag(), Bag()]

    def st(q, out, in_, rd, bag):
        ev = K.dma(q, lambda: q.h.dma_start(out=out, in_=in_), reads=rd, writes=[])
        bag.add(ev)

    dump("modT", modT[:], r_modT, [128, 48, 2])
    cut(0.2)
    with ExitStack() as p1:
        wtm, r_wtm = SB("wtm", [128, 8, 552], BF16, p1)
        wfm, r_wfm = SB("wfm", [128, 8, 1664], BF16, p1)
        wq, r_wq = SB("wq", [128, 3, 768], BF16, p1)
        wkv, r_wkv = SB("wkv", [128, 2, 1024], BF16, p1)
        with ExitStack() as pst:
            stg = [SB("stg%d" % i, [128, 1664], F32, pst) for i in range(2)]
            sn = 0
            for (dst, rdst, src, nk, ncol) in ((wtm, r_wtm, wtm_d, 8, 552), (wfm, r_wfm, wfm_d, 8, 1664),
                                               (wq, r_wq, wq_d, 3, 768), (wkv, r_wkv, wkv_d, 2, 1024)):
                for k in range(nk):
                    s_t, s_r = stg[sn % 2]
                    sn += 1
                    ld(sp, s_t[:, 0:ncol], src[:, k, :], [s_r])
                    cast_any(dst[:, k, :], s_t[:, 0:ncol], [s_r], [rdst])
        K.barrier()
        cut(0.25)
        Sst, r_Sst = SB("Sst", [128, 8, 64], F32, p1)
        Sbf, r_Sbf = SB("Sbf", [128, 8, 64], BF16, p1)
        cosT, r_cos = SB("cosT", [128, NT, 16], F32, p1)
        sinT, r_sin = SB("sinT", [128, NT, 16], F32, p1)
        posi, r_posi = SB("posi", [128, NT], I32, p1)
        posf, r_posf = SB("posf", [128, NT], F32, p1)
        ang, r_ang = SB("ang", [128, NT, 16], F32, p1)
        ang2, r_ang2 = SB("ang2", [128, NT, 16], F32, p1)
        xt = [SB("xt%d" % i, [128, DM], F32, p1) for i in range(2)]
        junk, r_junk = SB("junk", [128, DM], BF16, p1)
        xn, r_xn = SB("xn", [128, DM], BF16, p1)
        st1 = [SB("st1_%d" % i, [128, 8], F32, p1) for i in range(4)]
        hT2 = [SB("hT%d" % i, [128, 8, 512], BF16, p1) for i in range(2)]
        st0 = [SB("st0_%d" % i, [128, 2], F32, p1) for i in range(4)]
        rw, r_rw = SB("raw", [128, 8, 515], F32, p1)
        halo, r_halo = SB("halo", [128, 8, 3], F32, p1)
        cacc, r_cacc = SB("cacc", [128, 512], F32, p1)
        xact, r_xact = SB("xact", [128, 8, 512], BF16, p1)
        qag, r_qag = SB("qag", [128, 3, 512], F32, p1)
        kvag, r_kvag = SB("kvag", [128, 2, 512], F32, p1)
        sq5, r_sq5 = SB("sq5", [128, 5, 512], BF16, p1)
        rsb, r_rsb = SB("rsb", [128, 2, 512], F32, p1)
        qaTn, r_qaTn = SB("qaTn", [128, 3, 512], BF16, p1)
        kvaTn, r_kvaTn = SB("kvaTn", [128, 2, 512], BF16, p1)
        kng, r_kng = SB("kng", [128, 4, 512], BF16, p1)
        zs4 = [SB("zs%d" % i, [128, 512], F32, p1) for i in range(4)]
        dtt, r_dtt = SB("dtt", [128, 8], F32, p1)
        dta, r_dta = SB("dta", [128, 8], F32, p1)
        krp, r_krp = SB("krp", [128, 4, 32], BF16, p1)
        K.I(pool, lambda: nc.gpsimd.memset(krp[:], 0.0), writes=[r_krp])
        krT, r_krT = SB("krT", [128, 128], BF16, p1)
        rtmp = [SB("rtmp%d" % i, [128, 8, 16], F32, p1) for i in range(4)]
        qn_tm, r_qn = SB("qn_tm", [128, 512], BF16, p1)
        qr_pad, r_qrp = SB("qr_pad", [128, 4, 2, 64], BF16, p1)
        K.I(pool, lambda: nc.gpsimd.memset(qr_pad[:], 0.0), writes=[r_qrp])
        QT, r_QT = SB("QT", [128, 2048], BF16, p1)
        K.I(pool, lambda: nc.gpsimd.memset(QT[:], 0.0), writes=[r_QT])
        Vt, r_Vt = SB("Vt", [128, 8, 65], BF16, p1)
        K.I(pool, lambda: nc.gpsimd.memset(Vt[:], 1.0), writes=[r_Vt])
        xs_tm, r_xs = SB("xs_tm", [128, 8, 64], BF16, p1)
        B_tm, r_Btm = SB("B_tm", [128, 256], BF16, p1)
        xdt, r_xdt = SB("xdt", [128, 8, 64], BF16, p1)
        xdtd, r_xdtd = SB("xdtd", [128, 2, 8, 64], BF16, p1)
        ecsm, r_ecsm = SB("ecsm", [128, 2, 8], F32, p1)
        dtem, r_dtem = SB("dtem", [128, 2, 8], F32, p1)
        lseg, r_lseg = SB("lseg", [128, 8, 128], F32, p1)
        dec, r_dec = SB("dec", [128, 8, 128], F32, p1)
        cbm, r_cbm = SB("cbm", [128, 2, 128], F32, p1)
        MT, r_MT = SB("MT", [128, 8, 128], BF16, p1)
        ecs, r_ecs = SB("ecs", [128, 8], F32, p1)
        dte, r_dte = SB("dte", [128, 8], F32, p1)
        cdB, r_cdB = SB("cdB", [128, 2, 8], F32, p1)
        yd, r_yd = SB("yd", [128, 8, 64], F32, p1)
        yt, r_yt = SB("yt", [128, 8, 64], F32, p1)
        yt2, r_yt2 = SB("yt2", [128, 8, 64], F32, p1)
        mixs, r_mixs = SB("mixs", [128, 512], BF16, p1)

        pbn = [0]

        def next_pb():
            i = pbn[0] % 2
            pbn[0] += 1
            return PBs[i], rPB[i]

        for b in range(2):
            bag = bagA[b]
            ld(sp, posi[:], pos_d[b], [r_posi])
            cp(dve, posf[:], posi[:], [r_posi], [r_posf])
            tt(dve, ang[:], posf[:].unsqueeze(2).broadcast_to((128, NT, 16)),
               invf[:].unsqueeze(1).broadcast_to((128, NT, 16)), ALU.mult, [r_posf, r_invf], [r_ang])
            ts(dve, ang2[:], ang[:], 1.0 / (2.0 * math.pi), MAGIC, ALU.mult, ALU.add, [r_ang], [r_ang2])
            ts(dve, ang2[:], ang2[:], -MAGIC, None, ALU.add, None, [r_ang2], [r_ang2])
            stt(dve, ang[:], ang2[:], -C1, ang[:], ALU.mult, ALU.add, [r_ang2, r_ang], [r_ang])
            stt(dve, ang[:], ang2[:], -C2, ang[:], ALU.mult, ALU.add, [r_ang2, r_ang], [r_ang])
            ts(dve, ang[:], ang[:], 3.14159, -3.14159, ALU.min, ALU.max, [r_ang], [r_ang])
            actf(sinT[:], ang[:], AF.Sin, [r_ang], [r_sin])
            ts(dve, ang2[:], ang[:], -1.0, None, ALU.mult, None, [r_ang], [r_ang2])
            tt(dve, ang2[:], ang2[:], ang[:], ALU.max, [r_ang2, r_ang], [r_ang2])
            ts(dve, ang2[:], ang2[:], -1.0, math.pi / 2.0, ALU.mult, ALU.add, [r_ang2], [r_ang2])
            actf(cosT[:], ang2[:], AF.Sin, [r_ang2], [r_cos])
            cut(0.30)
            K.I(dve, lambda: nc.vector.memset(Sst[:], 0.0), writes=[r_Sst])
            K.I(pool, lambda: nc.gpsimd.memset(Sbf[:], 0.0), writes=[r_Sbf])
            K.I(pool, lambda: nc.gpsimd.memset(halo[:], 0.0), writes=[r_halo])

            def step1(gi):
                hTW, r_hTW = hT2[gi % 2]
                for i in range(4):
                    ti = gi * 4 + i
                    gt = b * NT + ti
                    x_t, x_r = xt[gt % 2]
                    s1, r_s1 = st0[i]
                    ld(sp, x_t[:], x_d[gt * 128:(gt + 1) * 128, :], [x_r])
                    actf(junk[:], x_t[:], AF.Square, [x_r], [r_junk, r_s1], accum_out=s1[:, 0:1])
                    rstd_from(s1[:, 0:1], r_s1, s1[:, 1:2], r_s1, DM)
                    ts(dve, xn[:], x_t[:], s1[:, 1:2], None, ALU.mult, None, [x_r, r_s1], [r_xn])
                    for k in range(8):
                        tr(PT[:, k * 128:(k + 1) * 128], xn[:, k * 128:(k + 1) * 128], identb[:], [r_xn, r_identb], [rPT])
                    for k in range(8):
                        actf(hTW[:, k, i * 128:(i + 1) * 128], PT[:, k * 128:(k + 1) * 128], AF.Identity,
                             [rPT, r_a1T, r_sh1T], [r_hTW], bias=sh1T[:, b, k:k + 1], scale=a1T[:, b, k:k + 1])

            step1(0)
            for gi in range(NG):
                hT, r_hT = hT2[gi % 2]
                cut(0.32)
                cp(pool, rw[:, :, 0:3], halo[:], [r_halo], [r_rw])
                for mc in range(13):
                    pb, rpb = next_pb()
                    for k in range(8):
                        mm(pb[:], wfm[:, k, mc * 128:(mc + 1) * 128], hT[:, k, :], k == 0, k == 7, [r_wfm, r_hT], [rpb])
                    if mc < 8:
                        cp(act, rw[:, mc, 3:515], pb[:], [rpb], [r_rw])
                    elif mc < 11:
                        c = mc - 8
                        actf(qag[:, c, :], pb[:], AF.Copy, [rpb, small["qan"][1]], [r_qag], scale=small["qan"][0][:, c:c + 1])
                        actf(sq5[:, c, :], pb[:], AF.Square, [rpb], [r_sq5])
                    else:
                        c = mc - 11
                        actf(kvag[:, c, :], pb[:], AF.Copy, [rpb, small["kvan"][1]], [r_kvag], scale=small["kvan"][0][:, c:c + 1])
                        actf(sq5[:, 3 + c, :], pb[:], AF.Square, [rpb], [r_sq5])
                cp(pool, halo[:], rw[:, :, 512:515], [r_rw], [r_halo])
                cut(0.34)
                cw, r_cw = small["convw"]
                cb_, r_cb = small["convb"]
                for mc in range(8):
                    ts(dve, cacc[:], rw[:, mc, 0:512], cw[:, mc, 0:1], cb_[:, mc:mc + 1], ALU.mult, ALU.add,
                       [r_rw, r_cw, r_cb], [r_cacc])
                    for kk in range(1, 4):
                        stt(dve, cacc[:], rw[:, mc, kk:kk + 512], cw[:, mc, kk:kk + 1], cacc[:], ALU.mult, ALU.add,
                            [r_rw, r_cw, r_cacc], [r_cacc])
                    actf(xact[:, mc, :], cacc[:], AF.Silu, [r_cacc], [r_xact])
                for i4 in range(4):
                    pb, rpb = next_pb()
                    for k in range(8):
                        mm(pb[:], hT[:, k, i4 * 128:(i4 + 1) * 128], wtm[:, k, 0:512], k == 0, k == 7, [r_hT, r_wtm], [rpb])
                    actf(zs4[i4][0][:], pb[:], AF.Silu, [rpb], [zs4[i4][1]])
                cut(0.36)
                for (c0, ncn, n, slot) in ((0, 3, 384, 0), (3, 2, 256, 1)):
                    pb, rpb = PBs[2], rPB[2]
                    for c in range(ncn):
                        mm(pb[:], onesb[:], sq5[:, c0 + c, :], c == 0, c == ncn - 1, [r_onesb, r_sq5], [rpb])
                    rstd_from(pb[:], rpb, rsb[:, slot, :], r_rsb, n)
                for c in range(3):
                    tt(dve, qaTn[:, c, :], qag[:, c, :], rsb[:, 0, :], ALU.mult, [r_qag, r_rsb], [r_qaTn])
                for c in range(2):
                    tt(dve, kvaTn[:, c, :], kvag[:, c, :], rsb[:, 1, :], ALU.mult, [r_kvag, r_rsb], [r_kvaTn])
                cut(0.38)
                for j in range(4):
                    pb, rpb = next_pb()
                    for c in range(2):
                        mm(pb[:], wkv[:, c, j * 128:(j + 1) * 128], kvaTn[:, c, :], c == 0, c == 1, [r_wkv, r_kvaTn], [rpb])
                    cp(act, kng[:, j, :], pb[:], [rpb], [r_kng])
                st(sp, kn_s[b].rearrange("p (j s) -> p j s", j=4)[:, :, gi * 512:(gi + 1) * 512], kng[:], [r_kng], bag)
                cut(0.40)

                if gi + 1 < NG:
                    step1(gi + 1)
                for i in range(4):
                    ti = gi * 4 + i
                    gt = b * NT + ti
                    cs_ = slice(i * 128, (i + 1) * 128)
                    s1, r_s1 = st1[i]
                    for k in range(8):
                        mm(P2[:, 512:552], hT[:, k, cs_], wtm[:, k, 512:552], k == 0, k == 7, [r_hT, r_wtm], [rP2b])
                    zs, r_zs = zs4[i]
                    tt(dve, dtt[:], P2[:, 512:520], small["dtb"][0][:], ALU.add, [rP2b, small["dtb"][1]], [r_dtt])
                    actf(dtt[:], dtt[:], AF.Exp, [r_dtt], [r_dtt])
                    actf(dtt[:], dtt[:], AF.Ln, [r_dtt], [r_dtt], bias=oneb[:, 0:1])
                    tt(dve, dta[:], dtt[:], aneg[:], ALU.mult, [r_dtt, r_aneg], [r_dta])
                    cut(0.42)
                    cs16 = cosT[:, ti, :]
                    sn16 = sinT[:, ti, :]
                    k1 = P2[:, 520:536]
                    k2 = P2[:, 536:552]
                    t0_, t1_, t2_, t3_ = [rtmp[q][0][:, 0, :] for q in range(4)]
                    rr = [rtmp[q][1] for q in range(4)]
                    tt(dve, t0_, k1, cs16, ALU.mult, [rP2b, r_cos], [rr[0]])
                    tt(dve, t1_, k2, sn16, ALU.mult, [rP2b, r_sin], [rr[1]])
                    tt(dve, t2_, k2, cs16, ALU.mult, [rP2b, r_cos], [rr[2]])
                    tt(dve, t3_, k1, sn16, ALU.mult, [rP2b, r_sin], [rr[3]])
                    tt(dve, krp[:, 0, 0:16], t0_, t1_, ALU.subtract, [rr[0], rr[1]], [r_krp])
                    tt(dve, krp[:, 0, 16:32], t2_, t3_, ALU.add, [rr[2], rr[3]], [r_krp])
                    cp(dve, krp[:, 2, :], krp[:, 0, :], [r_krp], [r_krp])
                    tr(PT[:, 0:128], krp[:].rearrange("p a c -> p (a c)"), identb[:], [r_krp, r_identb], [rPT])
                    cp(act, krT[:], PT[:, 0:128], [rPT], [r_krT])
                    st(sp, kr_s[b][:, ti * 128:(ti + 1) * 128], krT[:], [r_krT], bag)
                    cut(0.44)
                    for c in range(3):
                        mm(P2[:, 0:512], qaTn[:, c, cs_], wq[:, c, 0:512], c == 0, c == 2, [r_qaTn, r_wq], [rP2a])
                    for c in range(3):
                        mm(P2[:, 512:768], qaTn[:, c, cs_], wq[:, c, 512:768], c == 0, c == 2, [r_qaTn, r_wq], [rP2b])
                    cp(act, qn_tm[:], P2[:, 0:512], [rP2a], [r_qn])
                    qr = P2[:, 512:768].rearrange("p (h c) -> p h c", c=32)
                    q1 = qr[:, :, 0:16]
                    q2 = qr[:, :, 16:32]
                    cb8 = cs16.unsqueeze(1).broadcast_to((128, 8, 16))
                    sb8 = sn16.unsqueeze(1).broadcast_to((128, 8, 16))
                    T0, T1, T2, T3 = [rtmp[q][0][:] for q in range(4)]
                    tt(dve, T0, q1, cb8, ALU.mult, [rP2b, r_cos], [rr[0]])
                    tt(dve, T1, q2, sb8, ALU.mult, [rP2b, r_sin], [rr[1]])
                    tt(dve, T2, q2, cb8, ALU.mult, [rP2b, r_cos], [rr[2]])
                    tt(dve, T3, q1, sb8, ALU.mult, [rP2b, r_sin], [rr[3]])
                    qrv = qr_pad[:].rearrange("p j s c -> p s j c")
                    for s_ in range(2):
                        tt(dve, qrv[:, s_, :, 0:16], rtmp[0][0][:, s_ * 4:(s_ + 1) * 4, :], rtmp[1][0][:, s_ * 4:(s_ + 1) * 4, :],
                           ALU.subtract, [rr[0], rr[1]], [r_qrp])
                        tt(dve, qrv[:, s_, :, 16:32], rtmp[2][0][:, s_ * 4:(s_ + 1) * 4, :], rtmp[3][0][:, s_ * 4:(s_ + 1) * 4, :],
                           ALU.add, [rr[2], rr[3]], [r_qrp])
                    for j in range(4):
                        tr(PT[:, j * 128:(j + 1) * 128], qn_tm[:, j * 128:(j + 1) * 128], identb[:], [r_qn, r_identb], [rPT])
                    for j in range(4):
                        tr(PT[:, 512 + j * 128:512 + (j + 1) * 128], qr_pad[:, j, :, :].rearrange("p s c -> p (s c)"),
                           identb[:], [r_qrp, r_identb], [rPT])
                    cp(act, QT[0:64, 0:512], PT[0:64, 0:512], [rPT], [r_QT])
                    cp(act, QT[64:128, 512:1024], PT[64:128, 0:512], [rPT], [r_QT])
                    cp(act, QT[0:64, 1024:1536], PT[0:64, 512:1024], [rPT], [r_QT])
                    cp(act, QT[64:128, 1536:2048], PT[64:128, 512:1024], [rPT], [r_QT])
                    st(sp, q_s[gt], QT[:], [r_QT], bag)
                    cut(0.46)
                    pb, rpb = PBs[2], rPB[2]
                    for c in range(2):
                        mm(pb[:], kvaTn[:, c, cs_], wkv[:, c, 512:1024], c == 0, c == 1, [r_kvaTn, r_wkv], [rpb])
                    cp(act, Vt[:, :, 0:64], pb[:].rearrange("p (h c) -> p h c", c=64), [rpb], [r_Vt])
                    cut(0.475)
                    st(sp, v_s[b][:, ti * 520:(ti + 1) * 520], Vt[:].rearrange("p h c -> p (h c)"), [r_Vt], bag)
                    cut(0.48)

                    for j in range(4):
                        tr(PT[:, j * 128:(j + 1) * 128], xact[:, j, cs_], identb[:], [r_xact, r_identb], [rPT])
                    for j in range(2):
                        tr(PT[:, 512 + j * 128:512 + (j + 1) * 128], xact[:, 4 + j, cs_], identb[:], [r_xact, r_identb], [rPT])
                    cp(act, xs_tm[:].rearrange("p h c -> p (h c)"), PT[:, 0:512], [rPT], [r_xs])
                    cp(act, B_tm[:], PT[:, 512:768], [rPT], [r_Btm])
                    cut(0.495)
                    dt_b = dtt[:].unsqueeze(2).broadcast_to((128, 8, 64))
                    tt(dve, xdt[:], xs_tm[:], dt_b, ALU.mult, [r_xs, r_dtt], [r_xdt])
                    cut(0.50)
                    tt(dve, lseg[:], U1[:].unsqueeze(1).broadcast_to((128, 8, 128)),
                       dta[:].unsqueeze(2).broadcast_to((128, 8, 128)), ALU.mult, [r_U1, r_dta], [r_lseg])
                    for h in range(8):
                        rp = rP2a if h < 4 else rP2b
                        mm(P2[:, h * 128:(h + 1) * 128], lseg[:, h, :], U2[:], True, True, [r_lseg, r_U2], [rp])
                    actf(dec[:].rearrange("p h l -> p (h l)"), P2[:], AF.Exp, [rP2a, rP2b], [r_dec])
                    cut(0.51)
                    pb3, rpb3 = PBs[3], rPB[3]
                    for g in range(2):
                        mm(pb3[:, g * 128:(g + 1) * 128], xact[:, 4 + g, cs_], xact[:, 6 + g, cs_], True, True, [r_xact], [rpb3])
                    cut(0.516)
                    tt(dve, cbm[:], pb3[:, 0:256].rearrange("p (g l) -> p g l", g=2),
                       m01[:].unsqueeze(1).broadcast_to((128, 2, 128)), ALU.mult, [rpb3, r_m01], [r_cbm])
                    cut(0.518)
                    for h in range(8):
                        tt(dve, MT[:, h, :], dec[:, h, :], cbm[:, h // 4, :], ALU.mult, [r_dec, r_cbm], [r_MT])
                    cut(0.52)
                    pb4, rpb4 = PBs[4], rPB[4]
                    mm(pb4[:, 0:8], U2[:], dta[:], True, True, [r_U2, r_dta], [rpb4])
                    mm(pb4[:, 8:16], U1[:], dta[:], True, True, [r_U1, r_dta], [rpb4])
                    mm(pb4[:, 16:24], Mc[:, 0, :], dta[:], True, True, [r_Mc, r_dta], [rpb4])
                    mm(pb4[:, 24:32], Mc[:, 1, :], dta[:], True, True, [r_Mc, r_dta], [rpb4])
                    actf(ecs[:], pb4[:, 0:8], AF.Exp, [rpb4], [r_ecs])
                    actf(dte[:], pb4[:, 8:16], AF.Exp, [rpb4], [r_dte])
                    actf(cdB[:].rearrange("p c h -> p (c h)"), pb4[:, 16:32], AF.Exp, [rpb4], [r_cdB])
                    for c in range(2):
                        ts(dve, ecsm[:, c, :], ecs[:], rm[:, c:c + 1], None, ALU.mult, None, [r_ecs, r_rm], [r_ecsm])
                        ts(dve, dtem[:, c, :], dte[:], rm[:, c:c + 1], None, ALU.mult, None, [r_dte, r_rm], [r_dtem])
                        tt(dve, xdtd[:, c, :, :], xdt[:], dtem[:, c, :].unsqueeze(2).broadcast_to((128, 8, 64)), ALU.mult,
                           [r_xdt, r_dtem], [r_xdtd])
                    cut(0.53)
                    pb0, rpb0 = PBs[0], rPB[0]
                    for h in range(8):
                        mm(pb0[:, h * 64:(h + 1) * 64], MT[:, h, :], xdt[:, h, :], True, True, [r_MT, r_xdt], [rpb0])
                    cp(act, yd[:].rearrange("p h c -> p (h c)"), pb0[:], [rpb0], [r_yd])
                    cut(0.54)
                    pb2, rpb2 = PBs[2], rPB[2]
                    pbY = (PBs[1], PBs[3])
                    rpbY = (rPB[1], rPB[3])
                    for c in range(2):
                        for g in range(2):
                            mm(pbY[c][:, g * 256:(g + 1) * 256], xact[:, 6 + g, cs_],
                               Sbf[:, g * 4:(g + 1) * 4, :].rearrange("p h c -> p (h c)"), True, True, [r_xact, r_Sbf], [rpbY[c]])
                        for g in range(2):
                            mm(pb2[:, g * 256:(g + 1) * 256], B_tm[:, g * 128:(g + 1) * 128],
                               xdtd[:, c, g * 4:(g + 1) * 4, :].rearrange("p h c -> p (h c)"), True, True, [r_Btm, r_xdtd], [rpb2])
                        tt(dve, Sst[:], Sst[:], cdB[:, c, :].unsqueeze(2).broadcast_to((128, 8, 64)), ALU.mult, [r_Sst, r_cdB], [r_Sst])
                        tt(dve, Sst[:], Sst[:], pb2[:].rearrange("p (h c) -> p h c", c=64), ALU.add, [r_Sst, rpb2], [r_Sst])
                        cp(act, Sbf[:], Sst[:], [r_Sst], [r_Sbf])
                    cut(0.55)
                    tt(dve, yt[:], pbY[0][:].rearrange("p (h c) -> p h c", c=64), ecsm[:, 0, :].unsqueeze(2).broadcast_to((128, 8, 64)),
                       ALU.mult, [rpbY[0], r_ecsm], [r_yt])
                    tt(dve, yt2[:], pbY[1][:].rearrange("p (h c) -> p h c", c=64), ecsm[:, 1, :].unsqueeze(2).broadcast_to((128, 8, 64)),
                       ALU.mult, [rpbY[1], r_ecsm], [r_yt2])
                    tt(dve, yt[:], yt[:], yt2[:], ALU.add, [r_yt, r_yt2], [r_yt])
                    tt(dve, yt[:], yt[:], yd[:], ALU.add, [r_yt, r_yd], [r_yt])
                    tt(dve, yt2[:], xs_tm[:], small["dsk"][0][:].unsqueeze(2).broadcast_to((128, 8, 64)), ALU.mult,
                       [r_xs, small["dsk"][1]], [r_yt2])
                    tt(dve, yt[:], yt[:], yt2[:], ALU.add, [r_yt, r_yt2], [r_yt])
                    tt(dve, yt[:].rearrange("p h c -> p (h c)"), yt[:].rearrange("p h c -> p (h c)"), zs[:], ALU.mult,
                       [r_yt, r_zs], [r_yt])
                    for g in range(2):
                        actf(junk[:, g * 256:(g + 1) * 256], yt[:, g * 4:(g + 1) * 4, :].rearrange("p h c -> p (h c)"), AF.Square,
                             [r_yt], [r_junk, r_s1], accum_out=s1[:, 2 + g:3 + g])
                    rstd_from(s1[:, 2:4], r_s1, s1[:, 2:4], r_s1, 256)
                    for g in range(2):
                        stt(dve, mixs[:, g * 256:(g + 1) * 256], yt[:, g * 4:(g + 1) * 4, :].rearrange("p h c -> p (h c)"),
                            s1[:, 2 + g:3 + g], small["ssdn"][0][:, g * 256:(g + 1) * 256], ALU.mult, ALU.mult,
                            [r_yt, r_s1, small["ssdn"][1]], [r_mixs])
                    st(sp, mix_s[gt * 128:(gt + 1) * 128, 0:512], mixs[:], [r_mixs], bag)
                    cut(0.56)

    K.barrier()
    cut(0.6)
    with ExitStack() as pt_:
        KnT, r_KnT = SB("KnT", [128, 4, S], BF16, pt_)
        KrT, r_KrT = SB("KrT", [128, S], BF16, pt_)
        Vst, r_Vst = SB("Vst", [128, NT, 8, 65], BF16, pt_)
        Qb = [SB("Qb%d" % i, [128, 16, 128], BF16, pt_) for i in range(2)]
        PTs = [SB("PTs%d" % i, [128, 4, 128], BF16, pt_) for i in range(5)]
        rec, r_rec = SB("rec", [128, 8], F32, pt_)
        osb, r_osb = SB("osb", [128, 8, 64], F32, pt_)
        junk2, r_junk2 = SB("junk2", [128, 512], BF16, pt_)
        sA, r_sA = SB("sA", [128, 2], F32, pt_)
        mixm = [SB("mixm%d" % i, [128, 512], BF16, pt_) for i in range(2)]
        sc = 1.0 / math.sqrt(96.0)
        for b in range(2):
            K._wait(sp, bagA[b].evs())
            ld(sp, KnT[:].rearrange("p j s -> p (j s)"), kn_s[b], [r_KnT])
            ld(sp, KrT[:], kr_s[b], [r_KrT])
            ld(sp, Vst[:].rearrange("p t h c -> p (t h c)"), v_s[b], [r_Vst])
            for ti in range(NT):
                gt = b * NT + ti
                q_t, q_r = Qb[ti % 2]
                ld(sp, q_t[:].rearrange("p a q -> p (a q)"), q_s[gt], [q_r])
                nkt = ti + 1
                pbO = (P2[:, 0:512], P2[:, 512:1024])
                rpbO = (rP2a, rP2b)
                groups = [(h, k0, min(4, nkt - k0)) for h in range(8) for k0 in range(0, nkt, 4)]
                DEP = 3

                def emit_qk(gi):
                    h, k0, nk = groups[gi]
                    j = h % 4
                    hs = h // 4
                    pbs, rpbs = PBs[gi % 4], rPB[gi % 4]
                    for kk in range(nk):
                        kt = k0 + kk
                        kc = slice(kt * 128, (kt + 1) * 128)
                        mm(pbs[:, kk * 128:(kk + 1) * 128], KnT[:, j, kc], q_t[:, hs * 4 + j, :], True, False, [r_KnT, q_r], [rpbs])
                        mm(pbs[:, kk * 128:(kk + 1) * 128], KrT[:, kc], q_t[:, 8 + hs * 4 + j, :], False, True, [r_KrT, q_r], [rpbs])

                def emit_pv(gi):
                    h, k0, nk = groups[gi]
                    po, rpo = pbO[h // 4], rpbO[h // 4]
                    ocol = (h % 4) * 65
                    pbs, rpbs = PBs[gi % 4], rPB[gi % 4]
                    pts, rpts = PTs[gi % 5]
                    actf(pts[:, 0:nk, :].rearrange("p a q -> p (a q)"), pbs[:, 0:nk * 128], AF.Exp, [rpbs], [rpts], scale=sc)
                    if k0 + nk == nkt:
                        K.I(dve, lambda: nc.vector.memset(pts[64:128, nk - 1, 0:64], 0.0), writes=[rpts])
                    for kk in range(nk):
                        kt = k0 + kk
                        mm(po[:, ocol:ocol + 65], pts[:, kk, :], Vst[:, kt, h, :], kt == 0, kt == nkt - 1, [rpts, r_Vst], [rpo])

                for gi in range(min(DEP, len(groups))):
                    emit_qk(gi)
                for gi in range(len(groups)):
                    if gi + DEP < len(groups):
                        emit_qk(gi + DEP)
                    emit_pv(gi)
                for hh in range(2):
                    ov = pbO[hh][:, 0:260].rearrange("p (h c) -> p h c", c=65)
                    K.I(dve, lambda: nc.vector.reciprocal(rec[:, hh * 4:(hh + 1) * 4], ov[:, :, 64]), reads=[rpbO[hh]], writes=[r_rec])
                    tt(dve, osb[:, hh * 4:(hh + 1) * 4, :], ov[:, :, 0:64],
                       rec[:, hh * 4:(hh + 1) * 4].unsqueeze(2).broadcast_to((128, 4, 64)), ALU.mult, [rpbO[hh], r_rec], [r_osb])
                if gt == 1:
                    dump("osb", osb[:], r_osb, [128, 8, 64])
                    dump("rec", rec[:], r_rec, [128, 8])
                    dump("pts", PTs[0][0][:], PTs[0][1], [128, 4, 128], BF16)
                    dump("qb", q_t[:], q_r, [128, 16, 128], BF16)
                    dump("KrT", KrT[:], r_KrT, [128, S], BF16)
                    dump("KnT", KnT[:], r_KnT, [128, 4, S], BF16)
                    dump("Vst", Vst[:], r_Vst, [128, NT, 8, 65], BF16)
                actf(junk2[:], osb[:].rearrange("p h c -> p (h c)"), AF.Square, [r_osb], [r_junk2, r_sA], accum_out=sA[:, 0:1])
                rstd_from(sA[:, 0:1], r_sA, sA[:, 1:2], r_sA, 512)
                m_t, m_r = mixm[ti % 2]
                stt(dve, m_t[:], osb[:].rearrange("p h c -> p (h c)"), sA[:, 1:2], small["mlan"][0][:],
                    ALU.mult, ALU.mult, [r_osb, r_sA, small["mlan"][1]], [m_r])
                st(sp, mix_s[gt * 128:(gt + 1) * 128, 512:1024], m_t[:], [m_r], bagT[b])

    K.barrier()
    cut(0.8)
    with ExitStack() as pb_:
        wout, r_wout = SB("wout", [128, 8, 1024], BF16, pb_)
        with ExitStack() as pst:
            stg = [SB("stgo%d" % i, [128, 1024], F32, pst) for i in range(2)]
            for k in range(8):
                s_t, s_r = stg[k % 2]
                ld(sp, s_t[:], wout_d[:, k, :], [s_r])
                cast_any(wout[:, k, :], s_t[:], [s_r], [r_wout])
        K.barrier()
        G1, r_G1 = SB("G1", [128, DM], F32, pb_)
        A2, r_A2 = SB("A2", [128, DM], F32, pb_)
        SH2, r_SH2 = SB("SH2", [128, DM], F32, pb_)
        xt = [SB("xtb%d" % i, [128, DM], F32, pb_) for i in range(2)]
        mixin = [SB("mixin%d" % i, [128, DM], BF16, pb_) for i in range(2)]
        mixT, r_mixT = SB("mixT", [128, 8, 128], BF16, pb_)
        junk, r_junk = SB("junkb", [128, DM], BF16, pb_)
        x1, r_x1 = SB("x1", [128, DM], F32, pb_)
        h2s = [SB("h2_%d" % i, [128, DM], F32, pb_) for i in range(2)]
        h2b, r_h2b = SB("h2b", [128, DM], BF16, pb_)
        h2T, r_h2T = SB("h2T", [128, 8, 128], F32, pb_)
        s1s = [SB("s1b%d" % i, [128, 8], F32, pb_) for i in range(2)]
        nv0, r_nv0 = SB("nv0", [128, 1], F32, pb_)
        e4, r_e4 = SB("e4", [128, 4], F32, pb_)
        pb4, rpb4 = PBs[4], rPB[4]
        bag1 = Bag()
        for b in range(2):
            K._wait(sp, bagT[b].evs() + bagA[b].evs())
            tt(dve, vtmp[:], modT[:, 16:24, b], small["gpost1"][0][:], ALU.mult, [r_modT, small["gpost1"][1]], [r_vtmp])
            bcast_rows(vtmp, r_vtmp, G1, r_G1)
            ts(dve, vtmp[:], modT[:, 32:40, b], 1.0, None, ALU.add, None, [r_modT], [r_vtmp])
            tt(dve, vtmp[:], vtmp[:], small["gpre2"][0][:], ALU.mult, [r_vtmp, small["gpre2"][1]], [r_vtmp])
            bcast_rows(vtmp, r_vtmp, A2, r_A2)
            cp(dve, vtmp[:], modT[:, 24:32, b], [r_modT], [r_vtmp])
            bcast_rows(vtmp, r_vtmp, SH2, r_SH2)
            def stageA(ti):
                gt = b * NT + ti
                x_t, x_r = xt[gt % 2]
                mi, r_mi = mixin[gt % 2]
                h2, r_h2 = h2s[gt % 2]
                s1, r_s1 = s1s[gt % 2]
                ld(sp, x_t[:], x_d[gt * 128:(gt + 1) * 128, :], [x_r])
                ld(sp, mi[:], mix_s[gt * 128:(gt + 1) * 128, :], [r_mi])
                for k in range(8):
                    tr(PT[:, k * 128:(k + 1) * 128], mi[:, k * 128:(k + 1) * 128], identb[:], [r_mi, r_identb], [rPT])
                cp(act, mixT[:].rearrange("p k t -> p (k t)"), PT[:], [rPT], [r_mixT])
                for hf in range(2):
                    rp = rP2a if hf == 0 else rP2b
                    for k in range(8):
                        mm(P2[:, hf * 512:(hf + 1) * 512], mixT[:, k, :], wout[:, k, hf * 512:(hf + 1) * 512], k == 0, k == 7,
                           [r_mixT, r_wout], [rp])
                actf(junk[:], P2[:], AF.Square, [rP2a, rP2b], [r_junk, r_s1], accum_out=s1[:, 0:1])
                rstd_from(s1[:, 0:1], r_s1, s1[:, 1:2], r_s1, DM)
                stt(dve, x1[:], P2[:], s1[:, 1:2], G1[:], ALU.mult, ALU.mult, [rP2a, rP2b, r_s1, r_G1], [r_x1])
                tt(dve, x1[:], x1[:], x_t[:], ALU.add, [r_x1, x_r], [r_x1])
                st(sp, x1s_d[gt * 128:(gt + 1) * 128, :], x1[:], [r_x1], bag1)
                actf(junk[:], x1[:], AF.Square, [r_x1], [r_junk, r_s1], accum_out=s1[:, 2:3])
                rstd_from(s1[:, 2:3], r_s1, s1[:, 3:4], r_s1, DM)
                stt(dve, h2[:], x1[:], s1[:, 3:4], A2[:], ALU.mult, ALU.mult, [r_x1, r_s1, r_A2], [r_h2])
                tt(dve, h2[:], h2[:], SH2[:], ALU.add, [r_h2, r_SH2], [r_h2])
                cp(act, h2b[:], h2[:], [r_h2], [r_h2b])
                st(sp, h2s_d[gt * 128:(gt + 1) * 128, :], h2b[:], [r_h2b], bag1)

            def stageB(ti):
                gt = b * NT + ti
                h2, r_h2 = h2s[gt % 2]
                s1, r_s1 = s1s[gt % 2]
                for k in range(8):
                    pbx, rpx = PBs[k // 4], rPB[k // 4]
                    tr(pbx[:, (k % 4) * 128:(k % 4 + 1) * 128], h2[:, k * 128:(k + 1) * 128], identf[:], [r_h2, r_identf], [rpx])
                for hf in range(2):
                    cp(act, h2T[:, hf * 4:(hf + 1) * 4, :].rearrange("p k t -> p (k t)"), PBs[hf][:], [rPB[hf]], [r_h2T])
                wr_t, r_wr = small["wr"]
                for k in range(8):
                    mm(pb4[:, 0:NE], h2T[:, k, :], wr_t[:, k, :], k == 0, k == 7, [r_h2T, r_wr], [rpb4])
                lgt = lg_all[:, gt, :]
                tt(dve, lgt, pb4[:, 0:NE], small["br"][0][:], ALU.add, [rpb4, small["br"][1]], [r_lg])
                K.I(dve, lambda: nc.vector.max(v8_all[:, gt, :], lgt), reads=[r_lg], writes=[r_v8])
                ts(dve, nv0[:], v8_all[:, gt, 0:1], -1.0, None, ALU.mult, None, [r_v8], [r_nv0])
                actf(e4[:], v8_all[:, gt, 0:4], AF.Exp, [r_v8, r_nv0], [r_e4, r_s1], bias=nv0[:, 0:1], accum_out=s1[:, 4:5])
                K.I(dve, lambda: nc.vector.reciprocal(s1[:, 5:6], s1[:, 4:5]), reads=[r_s1], writes=[r_s1])
                ts(dve, gate_all[:, gt, :], e4[:], s1[:, 5:6], None, ALU.mult, None, [r_e4, r_s1], [r_gate])

            stageA(0)
            for ti in range(NT):
                if ti + 1 < NT:
                    stageA(ti + 1)
                stageB(ti)
    K.barrier()
    dump("lg", lg_all[:], r_lg, [128, NTT, NE])
    dump("v8", v8_all[:], r_v8, [128, NTT, 8])
    cut(2)

    base, r_base = SB("base", [128, NE])
    msk, r_msk = SB("msk", [128, NE])
    pos_all, r_pos = SB("pos_all", [128, NTT, NE])
    K.I(dve, lambda: nc.vector.memset(base[:], 0.0), writes=[r_base])
    pb4, rpb4 = PBs[4], rPB[4]
    for gt in range(NTT):
        ts(dve, msk[:], lg_all[:, gt, :], v8_all[:, gt, 3:4], None, ALU.is_ge, None, [r_lg, r_v8], [r_msk])
        mm(pb4[:, 32:64], Lst[:], msk[:], True, True, [r_Lst, r_msk], [rpb4])
        mm(pb4[:, 64:96], onesf[:], msk[:], True, True, [r_onesf, r_msk], [rpb4])
        tt(dve, pos_all[:, gt, :], pb4[:, 32:64], base[:], ALU.add, [rpb4, r_base], [r_pos])
        tt(dve, base[:], pb4[:, 64:96], base[:], ALU.add, [rpb4, r_base], [r_base])
    nblk, r_nblk = SB("nblk", [128, NE])
    incl, r_incl = SB("incl", [128, NE])
    pstart, r_pstart = SB("pstart", [128, NE])
    iotab, r_iotab = SB("iotab", [128, NB])
    bexp_f, r_bexpf = SB("bexp_f", [128, NB])
    bexp_i, r_bexpi = SB("bexp_i", [128, NB], I32)
    dest_i, r_desti = SB("dest_i", [128, NTT, 4], I32)
    widx_i, r_widx = SB("widx_i", [128, NB, 8], I32)
    bidx_i, r_bidx = SB("bidx_i", [128, NB], I32)
    pk, r_pk = SB("pk", [128, 8])
    c1024, r_c1024 = SB("c1024", [128, 8])
    ld(sp, pk[:], pk_d, [r_pk])
    K.I(dve, lambda: nc.vector.memset(c1024[:], 1024.0), writes=[r_c1024])
    ld(sp, iotab[:], iotab_d, [r_iotab])
    off = -0.5 + 1.0 / 1024.0
    ts(dve, nblk[:], base[:], float(BLK - 1), 1.0 / BLK, ALU.add, ALU.mult, [r_base], [r_nblk])
    ts(dve, nblk[:], nblk[:], off, MAGIC, ALU.add, ALU.add, [r_nblk], [r_nblk])
    ts(dve, nblk[:], nblk[:], -MAGIC, None, ALU.add, None, [r_nblk], [r_nblk])
    K.I(dve, lambda: nc.vector.tensor_tensor_scan(incl[:], onesf[:, 0:NE], nblk[:], 0.0, ALU.mult, ALU.add),
        reads=[r_onesf, r_nblk], writes=[r_incl])
    tt(dve, pstart[:], incl[:], nblk[:], ALU.subtract, [r_incl, r_nblk], [r_pstart])
    ts(dve, pstart[:], pstart[:], float(BLK), None, ALU.mult, None, [r_pstart], [r_pstart])
    with ExitStack() as p2a:
        cmp3, r_cmp3 = SB("cmp3", [128, NB, NE], F32, p2a)
        tt(dve, cmp3[:], incl[:].unsqueeze(1).broadcast_to((128, NB, NE)), iotab[:].unsqueeze(2).broadcast_to((128, NB, NE)),
           ALU.is_le, [r_incl, r_iotab], [r_cmp3])
        K.I(dve, lambda: nc.vector.tensor_reduce(bexp_f[:], cmp3[:], AX.X, ALU.add), reads=[r_cmp3], writes=[r_bexpf])
        ts(dve, bexp_f[:], bexp_f[:], float(NE - 1), None, ALU.min, None, [r_bexpf], [r_bexpf])
        cp(dve, bexp_i[:], bexp_f[:], [r_bexpf], [r_bexpi])
        widx_f, r_widxf = SB("widx_f", [128, NB, 8], F32, p2a)
        tt(dve, widx_f[:], bexp_f[:].unsqueeze(2).broadcast_to((128, NB, 8)), c1024[:].unsqueeze(1).broadcast_to((128, NB, 8)),
           ALU.mult, [r_bexpf, r_c1024], [r_widxf])
        tt(dve, widx_f[:], widx_f[:], pk[:].unsqueeze(1).broadcast_to((128, NB, 8)), ALU.add, [r_widxf, r_pk], [r_widxf])
        cp(dve, widx_i[:], widx_f[:], [r_widxf], [r_widx])
        ts(dve, bexp_f[:], bexp_f[:], 128.0, pk[:, 0:1], ALU.mult, ALU.add, [r_bexpf, r_pk], [r_bexpf])
        cp(dve, bidx_i[:], bexp_f[:], [r_bexpf], [r_bidx])
        destf, r_destf = SB("destf", [128, NE], F32, p2a)
        oh, r_oh = SB("oh", [128, 4, NE], F32, p2a)
        dk, r_dk = SB("dk", [128, 4], F32, p2a)
        hb = [SB("hb%d" % i, [128, DM], BF16, p2a) for i in range(2)]
        r_xg = Res()
        for gt in range(NTT):
            tt(dve, destf[:], pos_all[:, gt, :], pstart[:], ALU.add, [r_pos, r_pstart], [r_destf])
            tt(dve, oh[:], lg_all[:, gt, :].unsqueeze(1).broadcast_to((128, 4, NE)),
               v8_all[:, gt, 0:4].unsqueeze(2).broadcast_to((128, 4, NE)), ALU.is_equal, [r_lg, r_v8], [r_oh])
            tt(dve, oh[:], oh[:], destf[:].unsqueeze(1).broadcast_to((128, 4, NE)), ALU.mult, [r_oh, r_destf], [r_oh])
            K.I(dve, lambda: nc.vector.tensor_reduce(dk[:], oh[:], AX.X, ALU.add), reads=[r_oh], writes=[r_dk])
            cp(dve, dest_i[:, gt, :], dk[:], [r_dk], [r_desti])
            h_t, h_r = hb[gt % 2]
            ld(sp, h_t[:], h2s_d[gt * 128:(gt + 1) * 128, :], [h_r])
            if gt == 0:
                K._wait(sp, bag1.evs())
            for k in range(4):
                K.dma(pool, lambda: nc.gpsimd.indirect_dma_start(
                    out=xg_d, out_offset=bass.IndirectOffsetOnAxis(ap=dest_i[:, gt, k:k + 1], axis=0),
                    in_=h_t[:], in_offset=None), reads=[h_r, r_desti], writes=[])
    K.barrier()
    dump("desti", dest_i[:], r_desti, [128, NTT, 4], I32)
    dump("bexp", bexp_i[:], r_bexpi, [128, NB], I32)

    cut(3)
    scat_evs = [(sid, K.semvals[sid]) for sid in pool.ring if K.semvals[sid] > 0]

    with ExitStack() as p2:
        wgu = [SB("wgu%d" % i, [128, 8, 2 * DM], BF16, p2) for i in range(2)]
        wdn = [SB("wdn%d" % i, [128, 8, DM], BF16, p2) for i in range(2)]
        bgu = [SB("bgus%d" % i, [128, 16], F32, p2) for i in range(2)]
        bdn = [SB("bdns%d" % i, [128, DM], F32, p2) for i in range(2)]
        xgt = [SB("xgt%d" % i, [128, DM], BF16, p2) for i in range(2)]
        xgTs = [SB("xgT%d" % i, [128, 8, BLK], BF16, p2) for i in range(2)]
        actT, r_actT = SB("actT", [128, 8, BLK], BF16, p2)
        gg, r_gg = SB("gg", [128, BLK], F32, p2)
        sg, r_sg = SB("sg", [128, BLK], F32, p2)
        ll, r_ll = SB("ll", [128, BLK], F32, p2)
        yo = [SB("yo%d" % i, [128, DM], F32, p2) for i in range(2)]
        K._wait(sp, scat_evs)
        wgr = [[Res() for _ in range(8)] for _ in range(2)]
        wdr = [[Res() for _ in range(8)] for _ in range(2)]

        def load_weights(blk):
            wg_t = wgu[blk % 2][0]
            wd_t = wdn[blk % 2][0]
            bg_t, bg_r = bgu[blk % 2]
            bd_t, bd_r = bdn[blk % 2]
            for k in range(8):
                K.dma(pool, lambda: nc.gpsimd.indirect_dma_start(
                    out=wg_t[:, k, :], out_offset=None, in_=wgu_d,
                    in_offset=bass.IndirectOffsetOnAxis(ap=widx_i[:, blk, k:k + 1], axis=0)), reads=[r_widx], writes=[wgr[blk % 2][k]])
            for k in range(8):
                K.dma(pool, lambda: nc.gpsimd.indirect_dma_start(
                    out=wd_t[:, k, :], out_offset=None, in_=wd_d,
                    in_offset=bass.IndirectOffsetOnAxis(ap=widx_i[:, blk, k:k + 1], axis=0)), reads=[r_widx], writes=[wdr[blk % 2][k]])
            K.dma(pool, lambda: nc.gpsimd.indirect_dma_start(
                out=bg_t[:], out_offset=None, in_=bgu_d,
                in_offset=bass.IndirectOffsetOnAxis(ap=bidx_i[:, blk:blk + 1], axis=0)), reads=[r_bidx], writes=[bg_r])
            K.dma(pool, lambda: nc.gpsimd.indirect_dma_start(
                out=bd_t[:], out_offset=None, in_=bd_d,
                in_offset=bass.IndirectOffsetOnAxis(ap=bidx_i[:, blk:blk + 1], axis=0)), reads=[r_bidx], writes=[bd_r])

        PT2 = PBs[4][:].bitcast(BF16)
        ptn = [0]

        def prep_tokens(blk):
            xgT_, r_xgT_ = xgTs[blk % 2]
            for st in range(4):
                g_t, g_r = xgt[st % 2]
                r0 = blk * BLK + st * 128
                ld(sp, g_t[:], xg_d[r0:r0 + 128, :], [g_r])
                if ptn[0] % 2 == 0:
                    pt_ap, pt_r = PT[:], rPT
                else:
                    pt_ap, pt_r = PT2, rPB[4]
                ptn[0] += 1
                for k in range(8):
                    tr(pt_ap[:, k * 128:(k + 1) * 128], g_t[:, k * 128:(k + 1) * 128], identb[:], [g_r, r_identb], [pt_r])
                cp(act, xgT_[:, :, st * 128:(st + 1) * 128],
                   pt_ap.rearrange("p (k t) -> p k t", t=128), [pt_r], [r_xgT_])

        load_weights(0)
        prep_tokens(0)
        for blk in range(NB):
            if blk + 1 < NB:
                load_weights(blk + 1)
                prep_tokens(blk + 1)
            wg_t = wgu[blk % 2][0]
            wd_t = wdn[blk % 2][0]
            wg_rs = wgr[blk % 2]
            wd_rs = wdr[blk % 2]
            bg_t, bg_r = bgu[blk % 2]
            bd_t, bd_r = bdn[blk % 2]
            xgT, r_xgT = xgTs[blk % 2]
            for fc in range(8):
                pg, rpg = PBs[(fc % 2) * 2], rPB[(fc % 2) * 2]
                pl, rpl = PBs[(fc % 2) * 2 + 1], rPB[(fc % 2) * 2 + 1]
                for k in range(8):
                    mm(pg[:], wg_t[:, k, fc * 128:(fc + 1) * 128], xgT[:, k, :], k == 0, k == 7, [wg_rs[k], r_xgT], [rpg])
                for k in range(8):
                    mm(pl[:], wg_t[:, k, DM + fc * 128:DM + (fc + 1) * 128], xgT[:, k, :], k == 0, k == 7, [wg_rs[k], r_xgT], [rpl])
                ts(dve, gg[:], pg[:], bg_t[:, fc:fc + 1], 7.0, ALU.add, ALU.min, [rpg, bg_r], [r_gg])
                actf(sg[:], gg[:], AF.Sigmoid, [r_gg], [r_sg], scale=1.702)
                ts(dve, ll[:], pl[:], bg_t[:, 8 + fc:9 + fc], 7.0, ALU.add, ALU.min, [rpl, bg_r], [r_ll])
                ts(dve, ll[:], ll[:], -7.0, 1.0, ALU.max, ALU.add, [r_ll], [r_ll])
                tt(dve, gg[:], gg[:], sg[:], ALU.mult, [r_gg, r_sg], [r_gg])
                tt(dve, actT[:, fc, :], gg[:], ll[:], ALU.mult, [r_gg, r_ll], [r_actT])
            dbanks = ((P2[:, 0:512], rP2a), (P2[:, 512:1024], rP2b), (PBs[2][:], rPB[2]), (PBs[3][:], rPB[3]))
            for st in range(4):
                y_t, y_r = yo[st % 2]
                for hf in range(2):
                    pd, rpd = dbanks[(st % 2) * 2 + hf]
                    for k in range(8):
                        mm(pd, actT[:, k, st * 128:(st + 1) * 128], wd_t[:, k, hf * 512:(hf + 1) * 512], k == 0, k == 7,
                           [r_actT, wd_rs[k]], [rpd])
                    tt(dve, y_t[:, hf * 512:(hf + 1) * 512], pd, bd_t[:, hf * 512:(hf + 1) * 512], ALU.add, [rpd, bd_r], [y_r])
                r0 = blk * BLK + st * 128
                K.dma(act, lambda: nc.scalar.dma_start(out=yb_d[r0:r0 + 128, :], in_=y_t[:]), reads=[y_r], writes=[])
    K.barrier()
    yb_evs = [(sid, K.semvals[sid]) for sid in act.ring if K.semvals[sid] > 0]

    with ExitStack() as p3:
        yg = [SB("yg%d" % i, [128, DM], F32, p3) for i in range(8)]
        fa, r_fa = SB("fa", [128, DM], F32, p3)
        x1r = [SB("x1r%d" % i, [128, DM], F32, p3) for i in range(2)]
        ob = [SB("ob%d" % i, [128, DM], F32, p3) for i in range(2)]
        junk3, r_junk3 = SB("junk3", [128, DM], BF16, p3)
        s3, r_s3 = SB("s3", [128, 2], F32, p3)
        G2, r_G2 = SB("G2", [128, 2, DM], F32, p3)
        for b in range(2):
            tt(dve, vtmp[:], modT[:, 40:48, b], small["gpost2"][0][:], ALU.mult, [r_modT, small["gpost2"][1]], [r_vtmp])
            bcast_rows(vtmp, r_vtmp, G2[:, b, :], r_G2)
        K._wait(pool, yb_evs)
        K._wait(sp, bag1.evs())
        def p3_loads(gt):
            x_t, x_r = x1r[gt % 2]
            ld(sp, x_t[:], x1s_d[gt * 128:(gt + 1) * 128, :], [x_r])
            for k in range(4):
                y_t, y_r = yg[(gt % 2) * 4 + k]
                K.dma(pool, lambda: nc.gpsimd.indirect_dma_start(
                    out=y_t[:], out_offset=None, in_=yb_d,
                    in_offset=bass.IndirectOffsetOnAxis(ap=dest_i[:, gt, k:k + 1], axis=0)), reads=[r_desti], writes=[y_r])

        def p3_compute(gt):
            b = gt // NT
            x_t, x_r = x1r[gt % 2]
            o_t, o_r = ob[gt % 2]
            ygs = [yg[(gt % 2) * 4 + k] for k in range(4)]
            actf(fa[:], ygs[0][0][:], AF.Copy, [ygs[0][1], r_gate], [r_fa], scale=gate_all[:, gt, 0:1])
            for k in range(1, 4):
                stt(dve, fa[:], ygs[k][0][:], gate_all[:, gt, k:k + 1], fa[:], ALU.mult, ALU.add, [ygs[k][1], r_gate, r_fa], [r_fa])
            actf(junk3[:], fa[:], AF.Square, [r_fa], [r_junk3, r_s3], accum_out=s3[:, 0:1])
            actf(s3[:, 1:2], s3[:, 0:1], AF.Ln, [r_s3], [r_s3], scale=1.0 / DM, bias=epsb[:, 0:1])
            actf(s3[:, 1:2], s3[:, 1:2], AF.Exp, [r_s3], [r_s3], scale=-0.5)
            stt(dve, o_t[:], fa[:], s3[:, 1:2], G2[:, b, :], ALU.mult, ALU.mult, [r_fa, r_s3, r_G2], [o_r])
            tt(pool, o_t[:], o_t[:], x_t[:], ALU.add, [o_r, x_r], [o_r])
            K.dma(sp, lambda: nc.sync.dma_start(out=out_d[gt * 128:(gt + 1) * 128, :], in_=o_t[:]), reads=[o_r], writes=[], is_out=True)

        p3_loads(0)
        for gt in range(NTT):
            if gt + 1 < NTT:
                p3_loads(gt + 1)
            p3_compute(gt)
    K.barrier()
    K.finish()
    es.close()
    return nc, dbg_d


def _consts(NB):
    idx = np.arange(128)
    ch = idx // 64
    same = ch[:, None] == ch[None, :]
    U1 = (same & (idx[:, None] > idx[None, :])).astype(np.float32)
    U2 = (same & (idx[:, None] <= idx[None, :])).astype(np.float32)
    m01 = U2.copy()
    Lst = (idx[:, None] < idx[None, :]).astype(np.float32)
    invf = (np.float32(10000.0) ** (-(np.arange(16, dtype=np.float32) * np.float32(2.0) / np.float32(32.0)))).astype(np.float32)
    rm = np.stack([(ch == 0), (ch == 1)], axis=1).astype(np.float32)
    return dict(identf=np.eye(128, dtype=np.float32), U1=U1, U2=U2, m01=m01, Lst=Lst, rm=rm,
                invf=np.tile(invf[None, :], (128, 1)).astype(np.float32),
                iotab=np.tile(np.arange(NB, dtype=np.float32)[None, :], (128, 1)),
                pk=(np.arange(8, dtype=np.float32)[None, :] * 128 + np.arange(128, dtype=np.float32)[:, None]).astype(np.float32))


def _rep(v, n=128):
    return np.ascontiguousarray(np.tile(np.asarray(v, np.float32).reshape(1, -1), (n, 1)))


def _pk(v, k):
    return np.ascontiguousarray(np.asarray(v, np.float32).reshape(k, 128).T)


_CACHE = {}


def kernel(x, c, positions, w_ada, b_ada, pre_mix_norm, w_in, conv_w, conv_b, dt_bias, a_log, d_skip, ssd_norm,
           q_a_norm, w_q_up, kv_a_norm, w_kv_up, mla_norm, w_out, post_mix_norm, pre_ffn_norm, w_router, b_router,
           w_gate_up, b_gate_up, w_down, b_down, post_ffn_norm, _dbg=()):
    x = np.asarray(x, np.float32)
    Bsz, S, _ = x.shape
    ncores = Bsz // 2
    NT = S // 128
    NB = (2 * S * 4) // BLK + NE
    key = (S, tuple(_dbg))
    if key not in _CACHE:
        _CACHE[key] = build(S, _dbg)
    nc, dbg_d = _CACHE[key]
    f = lambda a: np.ascontiguousarray(np.asarray(a, np.float32))
    w_in = f(w_in)[0]
    w_tm = np.ascontiguousarray(np.concatenate([w_in[:, 0:512], w_in[:, 1536:1544], w_in[:, 2184:2216]], axis=1))
    w_fm = np.ascontiguousarray(np.concatenate([w_in[:, 512:1536], w_in[:, 1544:1928], w_in[:, 1928:2184]], axis=1))
    wq = f(w_q_up)[0].reshape(384, 8, 96)
    qn = wq[:, :, 0:64]
    qr = wq[:, :, 64:96]
    qn_pairs = np.stack([np.concatenate([qn[:, j], qn[:, j + 4]], axis=1) for j in range(4)], axis=1)
    wq2 = np.ascontiguousarray(np.concatenate([qn_pairs.reshape(384, 512), qr.reshape(384, 256)], axis=1))
    wkv = f(w_kv_up)[0].reshape(256, 8, 128)
    kn = wkv[:, :, 0:64]
    vv = wkv[:, :, 64:128]
    kn_pairs = np.stack([np.concatenate([kn[:, j], kn[:, j + 4]], axis=1) for j in range(4)], axis=1)
    wkv2 = np.ascontiguousarray(np.concatenate([kn_pairs.reshape(256, 512), vv.reshape(256, 512)], axis=1))
    cw = f(conv_w)[0]
    convw = np.ascontiguousarray(cw.reshape(4, 8, 128).transpose(2, 1, 0))
    shared = dict(
        w_ada=f(w_ada)[0], b_adaT=_pk(f(b_ada)[0], 48), g_pre1=_pk(f(pre_mix_norm)[0], 8), g_post1=_pk(f(post_mix_norm)[0], 8),
        g_pre2=_pk(f(pre_ffn_norm)[0], 8), g_post2=_pk(f(post_ffn_norm)[0], 8), w_tm=w_tm, w_fm=w_fm, convw=convw,
        convb=_pk(f(conv_b)[0], 8), dtb=_rep(f(dt_bias)[0]), alog=_rep(f(a_log)[0]), dsk=_rep(f(d_skip)[0]),
        ssdn=_rep(f(ssd_norm)[0]), mlan=_rep(f(mla_norm)[0]), qan=_pk(f(q_a_norm)[0], 3), kvan=_pk(f(kv_a_norm)[0], 2),
        wq=wq2, wkv=wkv2, wout=f(w_out)[0], wr=f(w_router)[0], br=_rep(f(b_router)[0]),
        wgu=f(w_gate_up)[0].reshape(NE * DM, 2 * DM), wd=f(w_down)[0].reshape(NE * DM, DM),
        bgu=np.ascontiguousarray(f(b_gate_up)[0].reshape(NE, 16, 128).transpose(0, 2, 1)).reshape(NE * 128, 16),
        bd=np.ascontiguousarray(np.broadcast_to(f(b_down)[0][:, None, :], (NE, 128, DM))).reshape(NE * 128, DM),
    )
    shared.update(_consts(NB))
    cc = f(c)
    pp = np.asarray(positions, np.int32)
    in_maps = []
    for ci in range(ncores):
        m = dict(shared)
        m["x"] = np.ascontiguousarray(x[2 * ci:2 * ci + 2].reshape(2 * S, DM))
        m["cT"] = np.ascontiguousarray(cc[2 * ci:2 * ci + 2].reshape(2, 8, 128).transpose(2, 1, 0))
        m["pos"] = np.ascontiguousarray(pp[2 * ci:2 * ci + 2].reshape(2, NT, 128).transpose(0, 2, 1))
        in_maps.append(m)
    res = run_bass_kernel_spmd(nc, in_maps, core_ids=list(range(ncores)))
    out = np.stack([np.asarray(r["out"], np.float32).reshape(2, S, DM) for r in res.results], axis=0).reshape(Bsz, S, DM)
    if _dbg:
        return out, [{k: np.asarray(r["dbg_" + k]) for k in dbg_d} for r in res.results]
    return out
```

```python
import math
from contextlib import ExitStack
import numpy as np
import concourse.bass as bass
import concourse.mybir as mybir
from concourse.bass_utils import run_bass_kernel_spmd

F32 = mybir.dt.float32
BF16 = mybir.dt.bfloat16
I32 = mybir.dt.int32
AF = mybir.ActivationFunctionType
ALU = mybir.AluOpType
AX = mybir.AxisListType

import os
_PH = float(os.environ.get("KPH", "9"))
DM = 1024
NE = 32
BLK = 512
EPS = 1e-6
MAGIC = 12582912.0
C1 = 6.28125
C2 = 2.0 * math.pi - 6.28125


class Res:
    __slots__ = ("w", "rs")

    def __init__(self):
        self.w = None
        self.rs = {}


class Eng:
    def __init__(self, h, sid, is_pe=False):
        self.h = h
        self.sid = sid
        self.n = 0
        self.known = {}
        self.is_pe = is_pe
        self.ring = []
        self.rpos = 0


class Kern:
    def __init__(self, nc, es):
        self.nc = nc
        self.sems = []
        self.semvals = []
        self.es = es
        mk = lambda nm: self._newsem(nm)
        self.pe = Eng(nc.tensor, mk("s_pe"), True)
        self.dve = Eng(nc.vector, mk("s_dve"))
        self.act = Eng(nc.scalar, mk("s_act"))
        self.pool = Eng(nc.gpsimd, mk("s_pool"))
        self.sp = Eng(nc.sync, mk("s_sp"))
        for e, n in ((self.sp, 44), (self.act, 12), (self.pool, 36)):
            e.ring = [mk("d%d_%d" % (e.sid, i)) for i in range(n)]
        self.out_evs = []

    def _newsem(self, nm):
        s = self.es.enter_context(self.nc.semaphore(nm))
        self.sems.append(s)
        self.semvals.append(0)
        return len(self.sems) - 1

    def _wait(self, eng, evs):
        best = {}
        for sid, val in evs:
            if val > best.get(sid, 0):
                best[sid] = val
        for sid, val in best.items():
            if eng.is_pe and sid == eng.sid:
                continue
            if eng.known.get(sid, 0) >= val:
                continue
            eng.h.wait_ge(self.sems[sid], val)
            eng.known[sid] = val

    @staticmethod
    def _deps(reads, writes):
        evs = []
        for r in reads:
            if r.w is not None:
                evs.append(r.w)
        for r in writes:
            if r.w is not None:
                evs.append(r.w)
            evs.extend(r.rs.items())
        return evs

    @staticmethod
    def _mark(ev, reads, writes):
        for r in reads:
            if ev[1] > r.rs.get(ev[0], 0):
                r.rs[ev[0]] = ev[1]
        for r in writes:
            r.w = ev
            r.rs = {}

    def I(self, eng, fn, reads=(), writes=()):
        evs = self._deps(reads, writes)
        own = eng.sid
        evs2 = []
        raw = set()
        for r in reads:
            if r.w is not None:
                raw.add(r.w)
        for ev in evs:
            if ev[0] == own and ev not in raw:
                continue
            evs2.append(ev)
        self._wait(eng, evs2)
        ins = fn()
        eng.n += 1
        ins.then_inc(self.sems[eng.sid], 1)
        self._mark((eng.sid, eng.n), reads, writes)

    def dma(self, q, fn, reads=(), writes=(), is_out=False):
        evs = self._deps(reads, writes)
        sid = q.ring[q.rpos % len(q.ring)]
        q.rpos += 1
        prev = self.semvals[sid]
        if prev > 0:
            evs.append((sid, prev))
        self._wait(q, evs)
        ins = fn()
        self.semvals[sid] = prev + 16
        ins.then_inc(self.sems[sid], 16)
        ev = (sid, prev + 16)
        self._mark(ev, reads, writes)
        if is_out:
            self.out_evs.append(ev)
        return ev

    def barrier(self):
        evs = []
        for e in (self.sp, self.act, self.pool):
            evs += [(sid, self.semvals[sid]) for sid in e.ring if self.semvals[sid] > 0]
        for e in (self.pe, self.dve, self.act, self.pool):
            if e.n > 0:
                evs.append((e.sid, e.n))
        for e in (self.pe, self.dve, self.act, self.pool, self.sp):
            self._wait(e, [ev for ev in evs if ev[0] != e.sid])

    def finish(self):
        evs = list(self.out_evs)
        for e in (self.sp, self.act, self.pool):
            evs += [(sid, self.semvals[sid]) for sid in e.ring if self.semvals[sid] > 0]
        for e in (self.pe, self.dve, self.act, self.pool):
            if e.n > 0:
                evs.append((e.sid, e.n))
        self._wait(self.sp, evs)


class _Cut(Exception):
    pass


def build(S, dbg=()):
    ctx = {}
    try:
        return _build_inner(S, dbg, ctx)
    except _Cut:
        ctx["K"].finish()
        return ctx["nc"], ctx["dbg_d"]


def _build_inner(S, dbg, ctx):
    NT = S // 128
    NG = S // 512
    T = 2 * S
    NTT = T // 128
    NB = (T * 4) // BLK + NE
    nc = bass.Bass("TRN2", target_bir_lowering=False)
    es = ExitStack()
    K = Kern(nc, es)
    pe, dve, act, pool, sp = K.pe, K.dve, K.act, K.pool, K.sp
    dbg_d = {}

    def DI(name, shape, dt=F32):
        return nc.dram_tensor(name, list(shape), dt, kind="ExternalInput").ap()

    def DS(name, shape, dt=F32):
        if name in dbg:
            d = nc.dram_tensor("dbg_" + name, list(shape), dt, kind="ExternalOutput").ap()
            dbg_d[name] = d
            return d
        return nc.dram_tensor(name, list(shape), dt, kind="Internal").ap()

    x_d = DI("x", [T, DM])
    cT_d = DI("cT", [128, 8, 2])
    pos_d = DI("pos", [2, 128, NT], I32)
    wada_d = DI("w_ada", [DM, 6 * DM]).rearrange("(k p) f -> p k f", p=128)
    badaT_d = DI("b_adaT", [128, 48])
    gpre1_d = DI("g_pre1", [128, 8])
    gpost1_d = DI("g_post1", [128, 8])
    gpre2_d = DI("g_pre2", [128, 8])
    gpost2_d = DI("g_post2", [128, 8])
    wtm_d = DI("w_tm", [DM, 552]).rearrange("(k p) f -> p k f", p=128)
    wfm_d = DI("w_fm", [DM, 1664]).rearrange("(k p) f -> p k f", p=128)
    convw_d = DI("convw", [128, 8, 4])
    convb_d = DI("convb", [128, 8])
    dtb_d = DI("dtb", [128, 8])
    alog_d = DI("alog", [128, 8])
    dsk_d = DI("dsk", [128, 8])
    ssdn_d = DI("ssdn", [128, 512])
    mlan_d = DI("mlan", [128, 512])
    qan_d = DI("qan", [128, 3])
    kvan_d = DI("kvan", [128, 2])
    wq_d = DI("wq", [384, 768]).rearrange("(k p) f -> p k f", p=128)
    wkv_d = DI("wkv", [256, 1024]).rearrange("(k p) f -> p k f", p=128)
    wout_d = DI("wout", [DM, DM]).rearrange("(k p) f -> p k f", p=128)
    wr_d = DI("wr", [DM, NE]).rearrange("(k p) f -> p k f", p=128)
    br_d = DI("br", [128, NE])
    wgu_d = DI("wgu", [NE * DM, 2 * DM])
    wd_d = DI("wd", [NE * DM, DM])
    bgu_d = DI("bgu", [NE * 128, 16])
    bd_d = DI("bd", [NE * 128, DM])
    identf_d = DI("identf", [128, 128])
    U1_d = DI("U1", [128, 128])
    U2_d = DI("U2", [128, 128])
    Lst_d = DI("Lst", [128, 128])
    m01_d = DI("m01", [128, 128])
    invf_d = DI("invf", [128, 16])
    iotab_d = DI("iotab", [128, NB])
    pk_d = DI("pk", [128, 8])
    rm_d = DI("rm", [128, 2])
    x1s_d = DS("x1s", [T, DM])
    h2s_d = DS("h2s", [T, DM], BF16)
    xg_d = DS("xg", [NB * BLK, DM], BF16)
    yb_d = DS("yb", [NB * BLK, DM])
    out_d = nc.dram_tensor("out", [T, DM], F32, kind="ExternalOutput").ap()
    ctx.update(K=K, es=es, nc=nc, dbg_d=dbg_d)

    def cut(level):
        if _PH < level:
            raise _Cut()

    def SB(name, shape, dt=F32, stack=None):
        t = (stack or es).enter_context(nc.sbuf_tensor("sb_" + name, list(shape), dt))
        return t, Res()

    def PS(name, shape, dt=F32):
        return es.enter_context(nc.psum_tensor("ps_" + name, list(shape), dt))

    P2 = PS("P2", [128, 1024])
    rP2a, rP2b = Res(), Res()
    PBs = [PS("PB%d" % i, [128, 512]) for i in range(5)]
    rPB = [Res() for _ in range(5)]
    PT = PS("PT", [128, 1024], BF16)
    rPT = Res()

    def mm(out, lhsT, rhs, start, stop, rd, wr):
        K.I(pe, lambda: nc.tensor.matmul(out, lhsT, rhs, start=start, stop=stop), reads=rd, writes=wr)

    def tr(out, in_, ident, rd, wr):
        K.I(pe, lambda: nc.tensor.transpose(out, in_, ident), reads=rd, writes=wr)

    def actf(out, in_, func, rd, wr, bias=None, scale=None, accum_out=None):
        kw = {}
        if bias is not None:
            kw["bias"] = bias
        if scale is not None:
            kw["scale"] = scale
        if accum_out is not None:
            kw["accum_out"] = accum_out
        K.I(act, lambda: nc.scalar.activation(out, in_, func, **kw), reads=rd, writes=wr)

    def ts(eng, out, in0, s1, s2, op0, op1, rd, wr):
        h = eng.h
        if op1 is None:
            K.I(eng, lambda: h.tensor_scalar(out, in0, s1, None, op0), reads=rd, writes=wr)
        else:
            K.I(eng, lambda: h.tensor_scalar(out, in0, s1, s2, op0, op1), reads=rd, writes=wr)

    def tt(eng, out, in0, in1, op, rd, wr):
        h = eng.h
        K.I(eng, lambda: h.tensor_tensor(out, in0, in1, op), reads=rd, writes=wr)

    def stt(eng, out, in0, scalar, in1, op0, op1, rd, wr):
        h = eng.h
        K.I(eng, lambda: h.scalar_tensor_tensor(out, in0, scalar, in1, op0, op1), reads=rd, writes=wr)

    def cp(eng, out, in_, rd, wr):
        h = eng.h
        if eng is act:
            K.I(eng, lambda: nc.scalar.activation(out, in_, AF.Copy), reads=rd, writes=wr)
        else:
            K.I(eng, lambda: h.tensor_copy(out, in_), reads=rd, writes=wr)

    def ld(q, out, in_, wr, rd=()):
        K.dma(q, lambda: q.h.dma_start(out=out, in_=in_), reads=rd, writes=wr)

    def dump(name, ap, res, shape, dt=F32):
        if name not in dbg:
            return
        d = nc.dram_tensor("dbg_" + name, list(shape), dt, kind="ExternalOutput").ap()
        dbg_d[name] = d
        K.dma(sp, lambda: nc.sync.dma_start(out=d, in_=ap), reads=[res], writes=[Res()], is_out=True)

    identf, r_identf = SB("identf", [128, 128])
    identb, r_identb = SB("identb", [128, 128], BF16)
    onesf, r_onesf = SB("onesf", [128, 128])
    onesb, r_onesb = SB("onesb", [128, 128], BF16)
    U1, r_U1 = SB("U1", [128, 128])
    U2, r_U2 = SB("U2", [128, 128])
    Lst, r_Lst = SB("Lst", [128, 128])
    m01, r_m01 = SB("m01", [128, 128])
    invf, r_invf = SB("invf", [128, 16])
    ld(sp, identf[:], identf_d, [r_identf])
    ld(sp, U1[:], U1_d, [r_U1])
    ld(sp, U2[:], U2_d, [r_U2])
    ld(sp, Lst[:], Lst_d, [r_Lst])
    ld(sp, m01[:], m01_d, [r_m01])
    ld(sp, invf[:], invf_d, [r_invf])
    rm, r_rm = SB("rm", [128, 2])
    Mc, r_Mc = SB("Mc", [128, 2, 128])
    ld(sp, rm[:], rm_d, [r_rm])
    cp(dve, identb[:], identf[:], [r_identf], [r_identb])
    K.I(dve, lambda: nc.vector.memset(onesf[:], 1.0), writes=[r_onesf])
    K.I(dve, lambda: nc.vector.memset(onesb[:], 1.0), writes=[r_onesb])
    for c in range(2):
        ts(dve, Mc[:, c, :], onesf[:], rm[:, c:c + 1], None, ALU.mult, None, [r_onesf, r_rm], [r_Mc])
    epsb, r_epsb = SB("epsb", [128, 1])
    oneb, r_oneb = SB("oneb", [128, 1])
    K.I(dve, lambda: nc.vector.memset(epsb[:], EPS), writes=[r_epsb])
    K.I(dve, lambda: nc.vector.memset(oneb[:], 1.0), writes=[r_oneb])

    small = {}
    for nm, d, shp in (("gpre1", gpre1_d, [128, 8]), ("gpost1", gpost1_d, [128, 8]), ("gpre2", gpre2_d, [128, 8]),
                       ("gpost2", gpost2_d, [128, 8]), ("convw", convw_d, [128, 8, 4]), ("convb", convb_d, [128, 8]),
                       ("dtb", dtb_d, [128, 8]), ("alog", alog_d, [128, 8]), ("dsk", dsk_d, [128, 8]),
                       ("ssdn", ssdn_d, [128, 512]), ("mlan", mlan_d, [128, 512]), ("qan", qan_d, [128, 3]),
                       ("kvan", kvan_d, [128, 2]), ("br", br_d, [128, NE]), ("badaT", badaT_d, [128, 48]),
                       ("cT", cT_d, [128, 8, 2]), ("wr", wr_d, [128, 8, NE])):
        t, r = SB("c_" + nm, shp)
        ld(sp, t[:], d, [r])
        small[nm] = (t, r)

    aneg, r_aneg = SB("aneg", [128, 8])
    actf(aneg[:], small["alog"][0][:], AF.Exp, [small["alog"][1]], [r_aneg])
    ts(dve, aneg[:], aneg[:], -1.0, None, ALU.mult, None, [r_aneg], [r_aneg])

    modT, r_modT = SB("modT", [128, 48, 2])
    cact, r_cact = SB("cact", [128, 8, 2])
    actf(cact[:], small["cT"][0][:], AF.Silu, [small["cT"][1]], [r_cact])
    with ExitStack() as st0:
        wst = [SB("wadast%d" % i, [128, 8, 512], F32, st0) for i in range(2)]
        for blk in range(12):
            w_t, w_r = wst[blk % 2]
            ld(sp, w_t[:], wada_d[:, :, blk * 512:(blk + 1) * 512], [w_r])
            for j in range(4):
                fc = blk * 4 + j
                pb, rpb = PBs[fc % 2], rPB[fc % 2]
                for k in range(8):
                    mm(pb[:, 0:2], w_t[:, k, j * 128:(j + 1) * 128], cact[:, k, :], k == 0, k == 7,
                       [w_r, r_cact], [rpb])
                ts(dve, modT[:, fc, :], pb[:, 0:2], small["badaT"][0][:, fc:fc + 1], None, ALU.add, None,
                   [rpb, small["badaT"][1]], [r_modT])
    K.barrier()
    a1T, r_a1T = SB("a1T", [128, 2, 8])
    sh1T, r_sh1T = SB("sh1T", [128, 2, 8])
    vtmp, r_vtmp = SB("vtmp", [128, 8])
    btmp = [SB("btmp%d" % i, [128, 128]) for i in range(2)]
    bcn = [0]

    def bcast_rows(vT_ap, vres, dst_ap, dres):
        for half in range(2):
            pb, rpb = PBs[2 + half], rPB[2 + half]
            for kk in range(4):
                k = half * 4 + kk
                bt, rbt = btmp[bcn[0] % 2]
                bcn[0] += 1
                ts(dve, bt[:], onesf[:], vT_ap[:, k:k + 1], None, ALU.mult, None, [r_onesf, vres], [rbt])
                mm(pb[:, kk * 128:(kk + 1) * 128], bt[:], identf[:], True, True, [rbt, r_identf], [rpb])
            cp(act, dst_ap[:, half * 512:(half + 1) * 512], pb[:], [rpb], [dres])

    for b in range(2):
        ts(dve, vtmp[:], modT[:, 8:16, b], 1.0, None, ALU.add, None, [r_modT], [r_vtmp])
        tt(dve, a1T[:, b, :], vtmp[:], small["gpre1"][0][:], ALU.mult, [r_vtmp, small["gpre1"][1]], [r_a1T])
        cp(dve, sh1T[:, b, :], modT[:, 0:8, b], [r_modT], [r_sh1T])

    lg_all, r_lg = SB("lg_all", [128, NTT, NE])
    v8_all, r_v8 = SB("v8_all", [128, NTT, 8])
    gate_all, r_gate = SB("gate_all", [128, NTT, 4])

    castn = [0]

    def cast_any(out, in_, rd, wr):
        e = (dve, pool, act)[castn[0] % 3]
        castn[0] += 1
        cp(e, out, in_, rd, wr)

    def rstd_from(ss_ap, ssres, out_ap, ores, n, eng=dve):
        actf(out_ap, ss_ap, AF.Ln, [ssres], [ores], bias=epsb[:, 0:1], scale=1.0 / n)
        actf(out_ap, out_ap, AF.Exp, [ores], [ores], scale=-0.5)

    class Bag:
        def __init__(self):
            self.d = {}

        def add(self, ev):
            if ev[1] > self.d.get(ev[0], 0):
                self.d[ev[0]] = ev[1]

        def evs(self):
            return list(self.d.items())

    kn_s = DS("kn_s", [2, 128, 4 * S], BF16)
    kr_s = DS("kr_s", [2, 128, S], BF16)
    v_s = DS("v_s", [2, 128, NT * 8 * 65], BF16)
    q_s = DS("q_s", [2 * NT, 128, 2048], BF16)
    mix_s = DS("mix_s", [T, DM], BF16)
    bagA = [Bag(), Bag()]
    bagT = [Bag(), Bag()]

    def st(q, out, in_, rd, bag):
        ev = K.dma(q, lambda: q.h.dma_start(out=out, in_=in_), reads=rd, writes=[])
        bag.add(ev)

    dump("modT", modT[:], r_modT, [128, 48, 2])
    cut(0.2)
    with ExitStack() as p1:
        wtm, r_wtm = SB("wtm", [128, 8, 552], BF16, p1)
        wfm, r_wfm = SB("wfm", [128, 8, 1664], BF16, p1)
        wq, r_wq = SB("wq", [128, 3, 768], BF16, p1)
        wkv, r_wkv = SB("wkv", [128, 2, 1024], BF16, p1)
        with ExitStack() as pst:
            stg = [SB("stg%d" % i, [128, 1664], F32, pst) for i in range(2)]
            sn = 0
            for (dst, rdst, src, nk, ncol) in ((wtm, r_wtm, wtm_d, 8, 552), (wfm, r_wfm, wfm_d, 8, 1664),
                                               (wq, r_wq, wq_d, 3, 768), (wkv, r_wkv, wkv_d, 2, 1024)):
                for k in range(nk):
                    s_t, s_r = stg[sn % 2]
                    sn += 1
                    ld(sp, s_t[:, 0:ncol], src[:, k, :], [s_r])
                    cast_any(dst[:, k, :], s_t[:, 0:ncol], [s_r], [rdst])
        K.barrier()
        cut(0.25)
        Sst, r_Sst = SB("Sst", [128, 8, 64], F32, p1)
        Sbf, r_Sbf = SB("Sbf", [128, 8, 64], BF16, p1)
        cosT, r_cos = SB("cosT", [128, NT, 16], F32, p1)
        sinT, r_sin = SB("sinT", [128, NT, 16], F32, p1)
        posi, r_posi = SB("posi", [128, NT], I32, p1)
        posf, r_posf = SB("posf", [128, NT], F32, p1)
        ang, r_ang = SB("ang", [128, NT, 16], F32, p1)
        ang2, r_ang2 = SB("ang2", [128, NT, 16], F32, p1)
        xt = [SB("xt%d" % i, [128, DM], F32, p1) for i in range(2)]
        junk, r_junk = SB("junk", [128, DM], BF16, p1)
        xn, r_xn = SB("xn", [128, DM], BF16, p1)
        st1 = [SB("st1_%d" % i, [128, 8], F32, p1) for i in range(4)]
        hT2 = [SB("hT%d" % i, [128, 8, 512], BF16, p1) for i in range(2)]
        st0 = [SB("st0_%d" % i, [128, 2], F32, p1) for i in range(4)]
        rw, r_rw = SB("raw", [128, 8, 515], F32, p1)
        halo, r_halo = SB("halo", [128, 8, 3], F32, p1)
        cacc, r_cacc = SB("cacc", [128, 512], F32, p1)
        xact, r_xact = SB("xact", [128, 8, 512], BF16, p1)
        qag, r_qag = SB("qag", [128, 3, 512], F32, p1)
        kvag, r_kvag = SB("kvag", [128, 2, 512], F32, p1)
        sq5, r_sq5 = SB("sq5", [128, 5, 512], BF16, p1)
        rsb, r_rsb = SB("rsb", [128, 2, 512], F32, p1)
        qaTn, r_qaTn = SB("qaTn", [128, 3, 512], BF16, p1)
        kvaTn, r_kvaTn = SB("kvaTn", [128, 2, 512], BF16, p1)
        kng, r_kng = SB("kng", [128, 4, 512], BF16, p1)
        zs4 = [SB("zs%d" % i, [128, 512], F32, p1) for i in range(4)]
        dtt, r_dtt = SB("dtt", [128, 8], F32, p1)
        dta, r_dta = SB("dta", [128, 8], F32, p1)
        krp, r_krp = SB("krp", [128, 4, 32], BF16, p1)
        K.I(pool, lambda: nc.gpsimd.memset(krp[:], 0.0), writes=[r_krp])
        krT, r_krT = SB("krT", [128, 128], BF16, p1)
        rtmp = [SB("rtmp%d" % i, [128, 8, 16], F32, p1) for i in range(4)]
        qn_tm, r_qn = SB("qn_tm", [128, 512], BF16, p1)
        qr_pad, r_qrp = SB("qr_pad", [128, 4, 2, 64], BF16, p1)
        K.I(pool, lambda: nc.gpsimd.memset(qr_pad[:], 0.0), writes=[r_qrp])
        QT, r_QT = SB("QT", [128, 2048], BF16, p1)
        K.I(pool, lambda: nc.gpsimd.memset(QT[:], 0.0), writes=[r_QT])
        Vt, r_Vt = SB("Vt", [128, 8, 65], BF16, p1)
        K.I(pool, lambda: nc.gpsimd.memset(Vt[:], 1.0), writes=[r_Vt])
        xs_tm, r_xs = SB("xs_tm", [128, 8, 64], BF16, p1)
        B_tm, r_Btm = SB("B_tm", [128, 256], BF16, p1)
        xdt, r_xdt = SB("xdt", [128, 8, 64], BF16, p1)
        xdtd, r_xdtd = SB("xdtd", [128, 2, 8, 64], BF16, p1)
        ecsm, r_ecsm = SB("ecsm", [128, 2, 8], F32, p1)
        dtem, r_dtem = SB("dtem", [128, 2, 8], F32, p1)
        lseg, r_lseg = SB("lseg", [128, 8, 128], F32, p1)
        dec, r_dec = SB("dec", [128, 8, 128], F32, p1)
        cbm, r_cbm = SB("cbm", [128, 2, 128], F32, p1)
        MT, r_MT = SB("MT", [128, 8, 128], BF16, p1)
        ecs, r_ecs = SB("ecs", [128, 8], F32, p1)
        dte, r_dte = SB("dte", [128, 8], F32, p1)
        cdB, r_cdB = SB("cdB", [128, 2, 8], F32, p1)
        yd, r_yd = SB("yd", [128, 8, 64], F32, p1)
        yt, r_yt = SB("yt", [128, 8, 64], F32, p1)
        yt2, r_yt2 = SB("yt2", [128, 8, 64], F32, p1)
        mixs, r_mixs = SB("mixs", [128, 512], BF16, p1)

        pbn = [0]

        def next_pb():
            i = (0, 1, 3, 4)[pbn[0] % 4]
            pbn[0] += 1
            return PBs[i], rPB[i]

        for b in range(2):
            bag = bagA[b]
            ld(sp, posi[:], pos_d[b], [r_posi])
            cp(dve, posf[:], posi[:], [r_posi], [r_posf])
            tt(dve, ang[:], posf[:].unsqueeze(2).broadcast_to((128, NT, 16)),
               invf[:].unsqueeze(1).broadcast_to((128, NT, 16)), ALU.mult, [r_posf, r_invf], [r_ang])
            ts(dve, ang2[:], ang[:], 1.0 / (2.0 * math.pi), MAGIC, ALU.mult, ALU.add, [r_ang], [r_ang2])
            ts(dve, ang2[:], ang2[:], -MAGIC, None, ALU.add, None, [r_ang2], [r_ang2])
            stt(dve, ang[:], ang2[:], -C1, ang[:], ALU.mult, ALU.add, [r_ang2, r_ang], [r_ang])
            stt(dve, ang[:], ang2[:], -C2, ang[:], ALU.mult, ALU.add, [r_ang2, r_ang], [r_ang])
            ts(dve, ang[:], ang[:], 3.14159, -3.14159, ALU.min, ALU.max, [r_ang], [r_ang])
            actf(sinT[:], ang[:], AF.Sin, [r_ang], [r_sin])
            ts(dve, ang2[:], ang[:], -1.0, None, ALU.mult, None, [r_ang], [r_ang2])
            tt(dve, ang2[:], ang2[:], ang[:], ALU.max, [r_ang2, r_ang], [r_ang2])
            ts(dve, ang2[:], ang2[:], -1.0, math.pi / 2.0, ALU.mult, ALU.add, [r_ang2], [r_ang2])
            actf(cosT[:], ang2[:], AF.Sin, [r_ang2], [r_cos])
            cut(0.30)
            K.I(dve, lambda: nc.vector.memset(Sst[:], 0.0), writes=[r_Sst])
            K.I(pool, lambda: nc.gpsimd.memset(Sbf[:], 0.0), writes=[r_Sbf])
            K.I(pool, lambda: nc.gpsimd.memset(halo[:], 0.0), writes=[r_halo])

            def step1(gi):
                hTW, r_hTW = hT2[gi % 2]
                for i in range(4):
                    ti = gi * 4 + i
                    gt = b * NT + ti
                    x_t, x_r = xt[gt % 2]
                    s1, r_s1 = st0[i]
                    ld(sp, x_t[:], x_d[gt * 128:(gt + 1) * 128, :], [x_r])
                    actf(junk[:], x_t[:], AF.Square, [x_r], [r_junk, r_s1], accum_out=s1[:, 0:1])
                    rstd_from(s1[:, 0:1], r_s1, s1[:, 1:2], r_s1, DM)
                    ts(dve, xn[:], x_t[:], s1[:, 1:2], None, ALU.mult, None, [x_r, r_s1], [r_xn])
                    for k in range(8):
                        tr(PT[:, k * 128:(k + 1) * 128], xn[:, k * 128:(k + 1) * 128], identb[:], [r_xn, r_identb], [rPT])
                    for k in range(8):
                        actf(hTW[:, k, i * 128:(i + 1) * 128], PT[:, k * 128:(k + 1) * 128], AF.Identity,
                             [rPT, r_a1T, r_sh1T], [r_hTW], bias=sh1T[:, b, k:k + 1], scale=a1T[:, b, k:k + 1])

            step1(0)
            for gi in range(NG):
                hT, r_hT = hT2[gi % 2]
                cut(0.32)
                cp(pool, rw[:, :, 0:3], halo[:], [r_halo], [r_rw])
                for mc in range(13):
                    pb, rpb = next_pb()
                    for k in range(8):
                        mm(pb[:], wfm[:, k, mc * 128:(mc + 1) * 128], hT[:, k, :], k == 0, k == 7, [r_wfm, r_hT], [rpb])
                    if mc < 8:
                        cp(act, rw[:, mc, 3:515], pb[:], [rpb], [r_rw])
                    elif mc < 11:
                        c = mc - 8
                        actf(qag[:, c, :], pb[:], AF.Copy, [rpb, small["qan"][1]], [r_qag], scale=small["qan"][0][:, c:c + 1])
                        actf(sq5[:, c, :], pb[:], AF.Square, [rpb], [r_sq5])
                    else:
                        c = mc - 11
                        actf(kvag[:, c, :], pb[:], AF.Copy, [rpb, small["kvan"][1]], [r_kvag], scale=small["kvan"][0][:, c:c + 1])
                        actf(sq5[:, 3 + c, :], pb[:], AF.Square, [rpb], [r_sq5])
                cp(pool, halo[:], rw[:, :, 512:515], [r_rw], [r_halo])
                cut(0.34)
                cw, r_cw = small["convw"]
                cb_, r_cb = small["convb"]
                for mc in range(8):
                    ts(dve, cacc[:], rw[:, mc, 0:512], cw[:, mc, 0:1], cb_[:, mc:mc + 1], ALU.mult, ALU.add,
                       [r_rw, r_cw, r_cb], [r_cacc])
                    for kk in range(1, 4):
                        stt(dve, cacc[:], rw[:, mc, kk:kk + 512], cw[:, mc, kk:kk + 1], cacc[:], ALU.mult, ALU.add,
                            [r_rw, r_cw, r_cacc], [r_cacc])
                    actf(xact[:, mc, :], cacc[:], AF.Silu, [r_cacc], [r_xact])
                for i4 in range(4):
                    pb, rpb = next_pb()
                    for k in range(8):
                        mm(pb[:], hT[:, k, i4 * 128:(i4 + 1) * 128], wtm[:, k, 0:512], k == 0, k == 7, [r_hT, r_wtm], [rpb])
                    actf(zs4[i4][0][:], pb[:], AF.Silu, [rpb], [zs4[i4][1]])
                cut(0.36)
                for (c0, ncn, n, slot) in ((0, 3, 384, 0), (3, 2, 256, 1)):
                    pb, rpb = PBs[2], rPB[2]
                    for c in range(ncn):
                        mm(pb[:], onesb[:], sq5[:, c0 + c, :], c == 0, c == ncn - 1, [r_onesb, r_sq5], [rpb])
                    rstd_from(pb[:], rpb, rsb[:, slot, :], r_rsb, n)
                for c in range(3):
                    tt(dve, qaTn[:, c, :], qag[:, c, :], rsb[:, 0, :], ALU.mult, [r_qag, r_rsb], [r_qaTn])
                for c in range(2):
                    tt(dve, kvaTn[:, c, :], kvag[:, c, :], rsb[:, 1, :], ALU.mult, [r_kvag, r_rsb], [r_kvaTn])
                cut(0.38)
                for j in range(4):
                    pb, rpb = next_pb()
                    for c in range(2):
                        mm(pb[:], wkv[:, c, j * 128:(j + 1) * 128], kvaTn[:, c, :], c == 0, c == 1, [r_wkv, r_kvaTn], [rpb])
                    cp(act, kng[:, j, :], pb[:], [rpb], [r_kng])
                st(sp, kn_s[b].rearrange("p (j s) -> p j s", j=4)[:, :, gi * 512:(gi + 1) * 512], kng[:], [r_kng], bag)
                cut(0.40)

                if gi + 1 < NG:
                    step1(gi + 1)
                for i in range(4):
                    ti = gi * 4 + i
                    gt = b * NT + ti
                    cs_ = slice(i * 128, (i + 1) * 128)
                    s1, r_s1 = st1[i]
                    for k in range(8):
                        mm(P2[:, 512:552], hT[:, k, cs_], wtm[:, k, 512:552], k == 0, k == 7, [r_hT, r_wtm], [rP2b])
                    zs, r_zs = zs4[i]
                    tt(dve, dtt[:], P2[:, 512:520], small["dtb"][0][:], ALU.add, [rP2b, small["dtb"][1]], [r_dtt])
                    actf(dtt[:], dtt[:], AF.Exp, [r_dtt], [r_dtt])
                    actf(dtt[:], dtt[:], AF.Ln, [r_dtt], [r_dtt], bias=oneb[:, 0:1])
                    tt(dve, dta[:], dtt[:], aneg[:], ALU.mult, [r_dtt, r_aneg], [r_dta])
                    cut(0.42)
                    cs16 = cosT[:, ti, :]
                    sn16 = sinT[:, ti, :]
                    k1 = P2[:, 520:536]
                    k2 = P2[:, 536:552]
                    t0_, t1_, t2_, t3_ = [rtmp[q][0][:, 0, :] for q in range(4)]
                    rr = [rtmp[q][1] for q in range(4)]
                    tt(dve, t0_, k1, cs16, ALU.mult, [rP2b, r_cos], [rr[0]])
                    tt(dve, t1_, k2, sn16, ALU.mult, [rP2b, r_sin], [rr[1]])
                    tt(dve, t2_, k2, cs16, ALU.mult, [rP2b, r_cos], [rr[2]])
                    tt(dve, t3_, k1, sn16, ALU.mult, [rP2b, r_sin], [rr[3]])
                    tt(dve, krp[:, 0, 0:16], t0_, t1_, ALU.subtract, [rr[0], rr[1]], [r_krp])
                    tt(dve, krp[:, 0, 16:32], t2_, t3_, ALU.add, [rr[2], rr[3]], [r_krp])
                    cp(dve, krp[:, 2, :], krp[:, 0, :], [r_krp], [r_krp])
                    tr(PT[:, 0:128], krp[:].rearrange("p a c -> p (a c)"), identb[:], [r_krp, r_identb], [rPT])
                    cp(act, krT[:], PT[:, 0:128], [rPT], [r_krT])
                    st(sp, kr_s[b][:, ti * 128:(ti + 1) * 128], krT[:], [r_krT], bag)
                    cut(0.44)
                    for c in range(3):
                        mm(P2[:, 0:512], qaTn[:, c, cs_], wq[:, c, 0:512], c == 0, c == 2, [r_qaTn, r_wq], [rP2a])
                    for c in range(3):
                        mm(P2[:, 512:768], qaTn[:, c, cs_], wq[:, c, 512:768], c == 0, c == 2, [r_qaTn, r_wq], [rP2b])
                    cp(act, qn_tm[:], P2[:, 0:512], [rP2a], [r_qn])
                    qr = P2[:, 512:768].rearrange("p (h c) -> p h c", c=32)
                    q1 = qr[:, :, 0:16]
                    q2 = qr[:, :, 16:32]
                    cb8 = cs16.unsqueeze(1).broadcast_to((128, 8, 16))
                    sb8 = sn16.unsqueeze(1).broadcast_to((128, 8, 16))
                    T0, T1, T2, T3 = [rtmp[q][0][:] for q in range(4)]
                    tt(dve, T0, q1, cb8, ALU.mult, [rP2b, r_cos], [rr[0]])
                    tt(dve, T1, q2, sb8, ALU.mult, [rP2b, r_sin], [rr[1]])
                    tt(dve, T2, q2, cb8, ALU.mult, [rP2b, r_cos], [rr[2]])
                    tt(dve, T3, q1, sb8, ALU.mult, [rP2b, r_sin], [rr[3]])
                    qrv = qr_pad[:].rearrange("p j s c -> p s j c")
                    for s_ in range(2):
                        tt(dve, qrv[:, s_, :, 0:16], rtmp[0][0][:, s_ * 4:(s_ + 1) * 4, :], rtmp[1][0][:, s_ * 4:(s_ + 1) * 4, :],
                           ALU.subtract, [rr[0], rr[1]], [r_qrp])
                        tt(dve, qrv[:, s_, :, 16:32], rtmp[2][0][:, s_ * 4:(s_ + 1) * 4, :], rtmp[3][0][:, s_ * 4:(s_ + 1) * 4, :],
                           ALU.add, [rr[2], rr[3]], [r_qrp])
                    for j in range(4):
                        tr(PT[:, j * 128:(j + 1) * 128], qn_tm[:, j * 128:(j + 1) * 128], identb[:], [r_qn, r_identb], [rPT])
                    for j in range(4):
                        tr(PT[:, 512 + j * 128:512 + (j + 1) * 128], qr_pad[:, j, :, :].rearrange("p s c -> p (s c)"),
                           identb[:], [r_qrp, r_identb], [rPT])
                    cp(act, QT[0:64, 0:512], PT[0:64, 0:512], [rPT], [r_QT])
                    cp(act, QT[64:128, 512:1024], PT[64:128, 0:512], [rPT], [r_QT])
                    cp(act, QT[0:64, 1024:1536], PT[0:64, 512:1024], [rPT], [r_QT])
                    cp(act, QT[64:128, 1536:2048], PT[64:128, 512:1024], [rPT], [r_QT])
                    st(sp, q_s[gt], QT[:], [r_QT], bag)
                    cut(0.46)
                    pb, rpb = PBs[2], rPB[2]
                    for c in range(2):
                        mm(pb[:], kvaTn[:, c, cs_], wkv[:, c, 512:1024], c == 0, c == 1, [r_kvaTn, r_wkv], [rpb])
                    cp(act, Vt[:, :, 0:64], pb[:].rearrange("p (h c) -> p h c", c=64), [rpb], [r_Vt])
                    cut(0.475)
                    st(sp, v_s[b][:, ti * 520:(ti + 1) * 520], Vt[:].rearrange("p h c -> p (h c)"), [r_Vt], bag)
                    cut(0.48)

                    for j in range(4):
                        tr(PT[:, j * 128:(j + 1) * 128], xact[:, j, cs_], identb[:], [r_xact, r_identb], [rPT])
                    for j in range(2):
                        tr(PT[:, 512 + j * 128:512 + (j + 1) * 128], xact[:, 4 + j, cs_], identb[:], [r_xact, r_identb], [rPT])
                    cp(act, xs_tm[:].rearrange("p h c -> p (h c)"), PT[:, 0:512], [rPT], [r_xs])
                    cp(act, B_tm[:], PT[:, 512:768], [rPT], [r_Btm])
                    cut(0.495)
                    dt_b = dtt[:].unsqueeze(2).broadcast_to((128, 8, 64))
                    tt(dve, xdt[:], xs_tm[:], dt_b, ALU.mult, [r_xs, r_dtt], [r_xdt])
                    cut(0.50)
                    tt(dve, lseg[:], U1[:].unsqueeze(1).broadcast_to((128, 8, 128)),
                       dta[:].unsqueeze(2).broadcast_to((128, 8, 128)), ALU.mult, [r_U1, r_dta], [r_lseg])
                    for h in range(8):
                        rp = rP2a if h < 4 else rP2b
                        mm(P2[:, h * 128:(h + 1) * 128], lseg[:, h, :], U2[:], True, True, [r_lseg, r_U2], [rp])
                    actf(dec[:].rearrange("p h l -> p (h l)"), P2[:], AF.Exp, [rP2a, rP2b], [r_dec])
                    cut(0.51)
                    pb3, rpb3 = PBs[3], rPB[3]
                    for g in range(2):
                        mm(pb3[:, g * 128:(g + 1) * 128], xact[:, 4 + g, cs_], xact[:, 6 + g, cs_], True, True, [r_xact], [rpb3])
                    cut(0.516)
                    tt(dve, cbm[:], pb3[:, 0:256].rearrange("p (g l) -> p g l", g=2),
                       m01[:].unsqueeze(1).broadcast_to((128, 2, 128)), ALU.mult, [rpb3, r_m01], [r_cbm])
                    cut(0.518)
                    for h in range(8):
                        tt(dve, MT[:, h, :], dec[:, h, :], cbm[:, h // 4, :], ALU.mult, [r_dec, r_cbm], [r_MT])
                    cut(0.52)
                    pb4, rpb4 = PBs[4], rPB[4]
                    mm(pb4[:, 0:8], U2[:], dta[:], True, True, [r_U2, r_dta], [rpb4])
                    mm(pb4[:, 8:16], U1[:], dta[:], True, True, [r_U1, r_dta], [rpb4])
                    mm(pb4[:, 16:24], Mc[:, 0, :], dta[:], True, True, [r_Mc, r_dta], [rpb4])
                    mm(pb4[:, 24:32], Mc[:, 1, :], dta[:], True, True, [r_Mc, r_dta], [rpb4])
                    actf(ecs[:], pb4[:, 0:8], AF.Exp, [rpb4], [r_ecs])
                    actf(dte[:], pb4[:, 8:16], AF.Exp, [rpb4], [r_dte])
                    actf(cdB[:].rearrange("p c h -> p (c h)"), pb4[:, 16:32], AF.Exp, [rpb4], [r_cdB])
                    for c in range(2):
                        ts(dve, ecsm[:, c, :], ecs[:], rm[:, c:c + 1], None, ALU.mult, None, [r_ecs, r_rm], [r_ecsm])
                        ts(dve, dtem[:, c, :], dte[:], rm[:, c:c + 1], None, ALU.mult, None, [r_dte, r_rm], [r_dtem])
                        tt(dve, xdtd[:, c, :, :], xdt[:], dtem[:, c, :].unsqueeze(2).broadcast_to((128, 8, 64)), ALU.mult,
                           [r_xdt, r_dtem], [r_xdtd])
                    cut(0.53)
                    pb0, rpb0 = PBs[0], rPB[0]
                    for h in range(8):
                        mm(pb0[:, h * 64:(h + 1) * 64], MT[:, h, :], xdt[:, h, :], True, True, [r_MT, r_xdt], [rpb0])
                    cp(act, yd[:].rearrange("p h c -> p (h c)"), pb0[:], [rpb0], [r_yd])
                    cut(0.54)
                    pb2, rpb2 = PBs[2], rPB[2]
                    pbY = (PBs[1], PBs[3])
                    rpbY = (rPB[1], rPB[3])
                    for c in range(2):
                        for g in range(2):
                            mm(pbY[c][:, g * 256:(g + 1) * 256], xact[:, 6 + g, cs_],
                               Sbf[:, g * 4:(g + 1) * 4, :].rearrange("p h c -> p (h c)"), True, True, [r_xact, r_Sbf], [rpbY[c]])
                        for g in range(2):
                            mm(pb2[:, g * 256:(g + 1) * 256], B_tm[:, g * 128:(g + 1) * 128],
                               xdtd[:, c, g * 4:(g + 1) * 4, :].rearrange("p h c -> p (h c)"), True, True, [r_Btm, r_xdtd], [rpb2])
                        tt(dve, Sst[:], Sst[:], cdB[:, c, :].unsqueeze(2).broadcast_to((128, 8, 64)), ALU.mult, [r_Sst, r_cdB], [r_Sst])
                        tt(dve, Sst[:], Sst[:], pb2[:].rearrange("p (h c) -> p h c", c=64), ALU.add, [r_Sst, rpb2], [r_Sst])
                        cp(act, Sbf[:], Sst[:], [r_Sst], [r_Sbf])
                    cut(0.55)
                    tt(dve, yt[:], pbY[0][:].rearrange("p (h c) -> p h c", c=64), ecsm[:, 0, :].unsqueeze(2).broadcast_to((128, 8, 64)),
                       ALU.mult, [rpbY[0], r_ecsm], [r_yt])
                    tt(dve, yt2[:], pbY[1][:].rearrange("p (h c) -> p h c", c=64), ecsm[:, 1, :].unsqueeze(2).broadcast_to((128, 8, 64)),
                       ALU.mult, [rpbY[1], r_ecsm], [r_yt2])
                    tt(dve, yt[:], yt[:], yt2[:], ALU.add, [r_yt, r_yt2], [r_yt])
                    tt(dve, yt[:], yt[:], yd[:], ALU.add, [r_yt, r_yd], [r_yt])
                    tt(dve, yt2[:], xs_tm[:], small["dsk"][0][:].unsqueeze(2).broadcast_to((128, 8, 64)), ALU.mult,
                       [r_xs, small["dsk"][1]], [r_yt2])
                    tt(dve, yt[:], yt[:], yt2[:], ALU.add, [r_yt, r_yt2], [r_yt])
                    tt(dve, yt[:].rearrange("p h c -> p (h c)"), yt[:].rearrange("p h c -> p (h c)"), zs[:], ALU.mult,
                       [r_yt, r_zs], [r_yt])
                    for g in range(2):
                        actf(junk[:, g * 256:(g + 1) * 256], yt[:, g * 4:(g + 1) * 4, :].rearrange("p h c -> p (h c)"), AF.Square,
                             [r_yt], [r_junk, r_s1], accum_out=s1[:, 2 + g:3 + g])
                    rstd_from(s1[:, 2:4], r_s1, s1[:, 2:4], r_s1, 256)
                    for g in range(2):
                        stt(dve, mixs[:, g * 256:(g + 1) * 256], yt[:, g * 4:(g + 1) * 4, :].rearrange("p h c -> p (h c)"),
                            s1[:, 2 + g:3 + g], small["ssdn"][0][:, g * 256:(g + 1) * 256], ALU.mult, ALU.mult,
                            [r_yt, r_s1, small["ssdn"][1]], [r_mixs])
                    st(sp, mix_s[gt * 128:(gt + 1) * 128, 0:512], mixs[:], [r_mixs], bag)
                    cut(0.56)

    K.barrier()
    cut(0.6)
    with ExitStack() as pt_:
        KnT, r_KnT = SB("KnT", [128, 4, S], BF16, pt_)
        KrT, r_KrT = SB("KrT", [128, S], BF16, pt_)
        Vst, r_Vst = SB("Vst", [128, NT, 8, 65], BF16, pt_)
        Qb = [SB("Qb%d" % i, [128, 16, 128], BF16, pt_) for i in range(2)]
        PTs = [SB("PTs%d" % i, [128, 4, 128], BF16, pt_) for i in range(5)]
        rec, r_rec = SB("rec", [128, 8], F32, pt_)
        osb, r_osb = SB("osb", [128, 8, 64], F32, pt_)
        junk2, r_junk2 = SB("junk2", [128, 512], BF16, pt_)
        sA, r_sA = SB("sA", [128, 2], F32, pt_)
        mixm = [SB("mixm%d" % i, [128, 512], BF16, pt_) for i in range(2)]
        sc = 1.0 / math.sqrt(96.0)
        for b in range(2):
            K._wait(sp, bagA[b].evs())
            ld(sp, KnT[:].rearrange("p j s -> p (j s)"), kn_s[b], [r_KnT])
            ld(sp, KrT[:], kr_s[b], [r_KrT])
            ld(sp, Vst[:].rearrange("p t h c -> p (t h c)"), v_s[b], [r_Vst])
            for ti in range(NT):
                gt = b * NT + ti
                q_t, q_r = Qb[ti % 2]
                ld(sp, q_t[:].rearrange("p a q -> p (a q)"), q_s[gt], [q_r])
                nkt = ti + 1
                pbO = (P2[:, 0:512], P2[:, 512:1024])
                rpbO = (rP2a, rP2b)
                groups = [(h, k0, min(4, nkt - k0)) for h in range(8) for k0 in range(0, nkt, 4)]
                DEP = 3

                def emit_qk(gi):
                    h, k0, nk = groups[gi]
                    j = h % 4
                    hs = h // 4
                    pbs, rpbs = PBs[gi % 4], rPB[gi % 4]
                    for kk in range(nk):
                        kt = k0 + kk
                        kc = slice(kt * 128, (kt + 1) * 128)
                        mm(pbs[:, kk * 128:(kk + 1) * 128], KnT[:, j, kc], q_t[:, hs * 4 + j, :], True, False, [r_KnT, q_r], [rpbs])
                        mm(pbs[:, kk * 128:(kk + 1) * 128], KrT[:, kc], q_t[:, 8 + hs * 4 + j, :], False, True, [r_KrT, q_r], [rpbs])

                def emit_pv(gi):
                    h, k0, nk = groups[gi]
                    po, rpo = pbO[h // 4], rpbO[h // 4]
                    ocol = (h % 4) * 65
                    pbs, rpbs = PBs[gi % 4], rPB[gi % 4]
                    pts, rpts = PTs[gi % 5]
                    actf(pts[:, 0:nk, :].rearrange("p a q -> p (a q)"), pbs[:, 0:nk * 128], AF.Exp, [rpbs], [rpts], scale=sc)
                    if k0 + nk == nkt:
                        K.I(dve, lambda: nc.vector.memset(pts[64:128, nk - 1, 0:64], 0.0), writes=[rpts])
                    for kk in range(nk):
                        kt = k0 + kk
                        mm(po[:, ocol:ocol + 65], pts[:, kk, :], Vst[:, kt, h, :], kt == 0, kt == nkt - 1, [rpts, r_Vst], [rpo])

                for gi in range(min(DEP, len(groups))):
                    emit_qk(gi)
                for gi in range(len(groups)):
                    if gi + DEP < len(groups):
                        emit_qk(gi + DEP)
                    emit_pv(gi)
                for hh in range(2):
                    ov = pbO[hh][:, 0:260].rearrange("p (h c) -> p h c", c=65)
                    K.I(dve, lambda: nc.vector.reciprocal(rec[:, hh * 4:(hh + 1) * 4], ov[:, :, 64]), reads=[rpbO[hh]], writes=[r_rec])
                    tt(dve, osb[:, hh * 4:(hh + 1) * 4, :], ov[:, :, 0:64],
                       rec[:, hh * 4:(hh + 1) * 4].unsqueeze(2).broadcast_to((128, 4, 64)), ALU.mult, [rpbO[hh], r_rec], [r_osb])
                if gt == 1:
                    dump("osb", osb[:], r_osb, [128, 8, 64])
                    dump("rec", rec[:], r_rec, [128, 8])
                    dump("pts", PTs[0][0][:], PTs[0][1], [128, 4, 128], BF16)
                    dump("qb", q_t[:], q_r, [128, 16, 128], BF16)
                    dump("KrT", KrT[:], r_KrT, [128, S], BF16)
                    dump("KnT", KnT[:], r_KnT, [128, 4, S], BF16)
                    dump("Vst", Vst[:], r_Vst, [128, NT, 8, 65], BF16)
                actf(junk2[:], osb[:].rearrange("p h c -> p (h c)"), AF.Square, [r_osb], [r_junk2, r_sA], accum_out=sA[:, 0:1])
                rstd_from(sA[:, 0:1], r_sA, sA[:, 1:2], r_sA, 512)
                m_t, m_r = mixm[ti % 2]
                stt(dve, m_t[:], osb[:].rearrange("p h c -> p (h c)"), sA[:, 1:2], small["mlan"][0][:],
                    ALU.mult, ALU.mult, [r_osb, r_sA, small["mlan"][1]], [m_r])
                st(sp, mix_s[gt * 128:(gt + 1) * 128, 512:1024], m_t[:], [m_r], bagT[b])

    K.barrier()
    cut(0.8)
    with ExitStack() as pb_:
        wout, r_wout = SB("wout", [128, 8, 1024], BF16, pb_)
        with ExitStack() as pst:
            stg = [SB("stgo%d" % i, [128, 1024], F32, pst) for i in range(2)]
            for k in range(8):
                s_t, s_r = stg[k % 2]
                ld(sp, s_t[:], wout_d[:, k, :], [s_r])
                cast_any(wout[:, k, :], s_t[:], [s_r], [r_wout])
        K.barrier()
        G1, r_G1 = SB("G1", [128, DM], F32, pb_)
        A2, r_A2 = SB("A2", [128, DM], F32, pb_)
        SH2, r_SH2 = SB("SH2", [128, DM], F32, pb_)
        xt = [SB("xtb%d" % i, [128, DM], F32, pb_) for i in range(2)]
        mixin = [SB("mixin%d" % i, [128, DM], BF16, pb_) for i in range(2)]
        mixT, r_mixT = SB("mixT", [128, 8, 128], BF16, pb_)
        junk, r_junk = SB("junkb", [128, DM], BF16, pb_)
        x1, r_x1 = SB("x1", [128, DM], F32, pb_)
        h2s = [SB("h2_%d" % i, [128, DM], F32, pb_) for i in range(2)]
        h2b, r_h2b = SB("h2b", [128, DM], BF16, pb_)
        h2T, r_h2T = SB("h2T", [128, 8, 128], F32, pb_)
        s1s = [SB("s1b%d" % i, [128, 8], F32, pb_) for i in range(2)]
        nv0, r_nv0 = SB("nv0", [128, 1], F32, pb_)
        e4, r_e4 = SB("e4", [128, 4], F32, pb_)
        pb4, rpb4 = PBs[4], rPB[4]
        bag1 = Bag()
        for b in range(2):
            K._wait(sp, bagT[b].evs() + bagA[b].evs())
            tt(dve, vtmp[:], modT[:, 16:24, b], small["gpost1"][0][:], ALU.mult, [r_modT, small["gpost1"][1]], [r_vtmp])
            bcast_rows(vtmp, r_vtmp, G1, r_G1)
            ts(dve, vtmp[:], modT[:, 32:40, b], 1.0, None, ALU.add, None, [r_modT], [r_vtmp])
            tt(dve, vtmp[:], vtmp[:], small["gpre2"][0][:], ALU.mult, [r_vtmp, small["gpre2"][1]], [r_vtmp])
            bcast_rows(vtmp, r_vtmp, A2, r_A2)
            cp(dve, vtmp[:], modT[:, 24:32, b], [r_modT], [r_vtmp])
            bcast_rows(vtmp, r_vtmp, SH2, r_SH2)
            def stageA(ti):
                gt = b * NT + ti
                x_t, x_r = xt[gt % 2]
                mi, r_mi = mixin[gt % 2]
                h2, r_h2 = h2s[gt % 2]
                s1, r_s1 = s1s[gt % 2]
                ld(sp, x_t[:], x_d[gt * 128:(gt + 1) * 128, :], [x_r])
                ld(sp, mi[:], mix_s[gt * 128:(gt + 1) * 128, :], [r_mi])
                for k in range(8):
                    tr(PT[:, k * 128:(k + 1) * 128], mi[:, k * 128:(k + 1) * 128], identb[:], [r_mi, r_identb], [rPT])
                cp(act, mixT[:].rearrange("p k t -> p (k t)"), PT[:], [rPT], [r_mixT])
                for hf in range(2):
                    rp = rP2a if hf == 0 else rP2b
                    for k in range(8):
                        mm(P2[:, hf * 512:(hf + 1) * 512], mixT[:, k, :], wout[:, k, hf * 512:(hf + 1) * 512], k == 0, k == 7,
                           [r_mixT, r_wout], [rp])
                actf(junk[:], P2[:], AF.Square, [rP2a, rP2b], [r_junk, r_s1], accum_out=s1[:, 0:1])
                rstd_from(s1[:, 0:1], r_s1, s1[:, 1:2], r_s1, DM)
                stt(dve, x1[:], P2[:], s1[:, 1:2], G1[:], ALU.mult, ALU.mult, [rP2a, rP2b, r_s1, r_G1], [r_x1])
                tt(dve, x1[:], x1[:], x_t[:], ALU.add, [r_x1, x_r], [r_x1])
                st(sp, x1s_d[gt * 128:(gt + 1) * 128, :], x1[:], [r_x1], bag1)
                actf(junk[:], x1[:], AF.Square, [r_x1], [r_junk, r_s1], accum_out=s1[:, 2:3])
                rstd_from(s1[:, 2:3], r_s1, s1[:, 3:4], r_s1, DM)
                stt(dve, h2[:], x1[:], s1[:, 3:4], A2[:], ALU.mult, ALU.mult, [r_x1, r_s1, r_A2], [r_h2])
                tt(dve, h2[:], h2[:], SH2[:], ALU.add, [r_h2, r_SH2], [r_h2])
                cp(act, h2b[:], h2[:], [r_h2], [r_h2b])
                st(sp, h2s_d[gt * 128:(gt + 1) * 128, :], h2b[:], [r_h2b], bag1)

            def stageB(ti):
                gt = b * NT + ti
                h2, r_h2 = h2s[gt % 2]
                s1, r_s1 = s1s[gt % 2]
                for k in range(8):
                    pbx, rpx = PBs[k // 4], rPB[k // 4]
                    tr(pbx[:, (k % 4) * 128:(k % 4 + 1) * 128], h2[:, k * 128:(k + 1) * 128], identf[:], [r_h2, r_identf], [rpx])
                for hf in range(2):
                    cp(act, h2T[:, hf * 4:(hf + 1) * 4, :].rearrange("p k t -> p (k t)"), PBs[hf][:], [rPB[hf]], [r_h2T])
                wr_t, r_wr = small["wr"]
                for k in range(8):
                    mm(pb4[:, 0:NE], h2T[:, k, :], wr_t[:, k, :], k == 0, k == 7, [r_h2T, r_wr], [rpb4])
                lgt = lg_all[:, gt, :]
                tt(dve, lgt, pb4[:, 0:NE], small["br"][0][:], ALU.add, [rpb4, small["br"][1]], [r_lg])
                K.I(dve, lambda: nc.vector.max(v8_all[:, gt, :], lgt), reads=[r_lg], writes=[r_v8])
                ts(dve, nv0[:], v8_all[:, gt, 0:1], -1.0, None, ALU.mult, None, [r_v8], [r_nv0])
                actf(e4[:], v8_all[:, gt, 0:4], AF.Exp, [r_v8, r_nv0], [r_e4, r_s1], bias=nv0[:, 0:1], accum_out=s1[:, 4:5])
                K.I(dve, lambda: nc.vector.reciprocal(s1[:, 5:6], s1[:, 4:5]), reads=[r_s1], writes=[r_s1])
                ts(dve, gate_all[:, gt, :], e4[:], s1[:, 5:6], None, ALU.mult, None, [r_e4, r_s1], [r_gate])

            stageA(0)
            for ti in range(NT):
                if ti + 1 < NT:
                    stageA(ti + 1)
                stageB(ti)
    K.barrier()
    dump("lg", lg_all[:], r_lg, [128, NTT, NE])
    dump("v8", v8_all[:], r_v8, [128, NTT, 8])
    cut(2)

    base, r_base = SB("base", [128, NE])
    msk, r_msk = SB("msk", [128, NE])
    pos_all, r_pos = SB("pos_all", [128, NTT, NE])
    K.I(dve, lambda: nc.vector.memset(base[:], 0.0), writes=[r_base])
    pb4, rpb4 = PBs[4], rPB[4]
    for gt in range(NTT):
        ts(dve, msk[:], lg_all[:, gt, :], v8_all[:, gt, 3:4], None, ALU.is_ge, None, [r_lg, r_v8], [r_msk])
        mm(pb4[:, 32:64], Lst[:], msk[:], True, True, [r_Lst, r_msk], [rpb4])
        mm(pb4[:, 64:96], onesf[:], msk[:], True, True, [r_onesf, r_msk], [rpb4])
        tt(dve, pos_all[:, gt, :], pb4[:, 32:64], base[:], ALU.add, [rpb4, r_base], [r_pos])
        tt(dve, base[:], pb4[:, 64:96], base[:], ALU.add, [rpb4, r_base], [r_base])
    nblk, r_nblk = SB("nblk", [128, NE])
    incl, r_incl = SB("incl", [128, NE])
    pstart, r_pstart = SB("pstart", [128, NE])
    iotab, r_iotab = SB("iotab", [128, NB])
    bexp_f, r_bexpf = SB("bexp_f", [128, NB])
    bexp_i, r_bexpi = SB("bexp_i", [128, NB], I32)
    dest_i, r_desti = SB("dest_i", [128, NTT, 4], I32)
    widx_i, r_widx = SB("widx_i", [128, NB, 8], I32)
    bidx_i, r_bidx = SB("bidx_i", [128, NB], I32)
    pk, r_pk = SB("pk", [128, 8])
    c1024, r_c1024 = SB("c1024", [128, 8])
    ld(sp, pk[:], pk_d, [r_pk])
    K.I(dve, lambda: nc.vector.memset(c1024[:], 1024.0), writes=[r_c1024])
    ld(sp, iotab[:], iotab_d, [r_iotab])
    off = -0.5 + 1.0 / 1024.0
    ts(dve, nblk[:], base[:], float(BLK - 1), 1.0 / BLK, ALU.add, ALU.mult, [r_base], [r_nblk])
    ts(dve, nblk[:], nblk[:], off, MAGIC, ALU.add, ALU.add, [r_nblk], [r_nblk])
    ts(dve, nblk[:], nblk[:], -MAGIC, None, ALU.add, None, [r_nblk], [r_nblk])
    K.I(dve, lambda: nc.vector.tensor_tensor_scan(incl[:], onesf[:, 0:NE], nblk[:], 0.0, ALU.mult, ALU.add),
        reads=[r_onesf, r_nblk], writes=[r_incl])
    tt(dve, pstart[:], incl[:], nblk[:], ALU.subtract, [r_incl, r_nblk], [r_pstart])
    ts(dve, pstart[:], pstart[:], float(BLK), None, ALU.mult, None, [r_pstart], [r_pstart])
    with ExitStack() as p2a:
        cmp3, r_cmp3 = SB("cmp3", [128, NB, NE], F32, p2a)
        tt(dve, cmp3[:], incl[:].unsqueeze(1).broadcast_to((128, NB, NE)), iotab[:].unsqueeze(2).broadcast_to((128, NB, NE)),
           ALU.is_le, [r_incl, r_iotab], [r_cmp3])
        K.I(dve, lambda: nc.vector.tensor_reduce(bexp_f[:], cmp3[:], AX.X, ALU.add), reads=[r_cmp3], writes=[r_bexpf])
        ts(dve, bexp_f[:], bexp_f[:], float(NE - 1), None, ALU.min, None, [r_bexpf], [r_bexpf])
        cp(dve, bexp_i[:], bexp_f[:], [r_bexpf], [r_bexpi])
        widx_f, r_widxf = SB("widx_f", [128, NB, 8], F32, p2a)
        tt(dve, widx_f[:], bexp_f[:].unsqueeze(2).broadcast_to((128, NB, 8)), c1024[:].unsqueeze(1).broadcast_to((128, NB, 8)),
           ALU.mult, [r_bexpf, r_c1024], [r_widxf])
        tt(dve, widx_f[:], widx_f[:], pk[:].unsqueeze(1).broadcast_to((128, NB, 8)), ALU.add, [r_widxf, r_pk], [r_widxf])
        cp(dve, widx_i[:], widx_f[:], [r_widxf], [r_widx])
        ts(dve, bexp_f[:], bexp_f[:], 128.0, pk[:, 0:1], ALU.mult, ALU.add, [r_bexpf, r_pk], [r_bexpf])
        cp(dve, bidx_i[:], bexp_f[:], [r_bexpf], [r_bidx])
        destf, r_destf = SB("destf", [128, NE], F32, p2a)
        oh, r_oh = SB("oh", [128, 4, NE], F32, p2a)
        dk, r_dk = SB("dk", [128, 4], F32, p2a)
        hb = [SB("hb%d" % i, [128, DM], BF16, p2a) for i in range(2)]
        r_xg = Res()
        for gt in range(NTT):
            tt(dve, destf[:], pos_all[:, gt, :], pstart[:], ALU.add, [r_pos, r_pstart], [r_destf])
            tt(dve, oh[:], lg_all[:, gt, :].unsqueeze(1).broadcast_to((128, 4, NE)),
               v8_all[:, gt, 0:4].unsqueeze(2).broadcast_to((128, 4, NE)), ALU.is_equal, [r_lg, r_v8], [r_oh])
            tt(dve, oh[:], oh[:], destf[:].unsqueeze(1).broadcast_to((128, 4, NE)), ALU.mult, [r_oh, r_destf], [r_oh])
            K.I(dve, lambda: nc.vector.tensor_reduce(dk[:], oh[:], AX.X, ALU.add), reads=[r_oh], writes=[r_dk])
            cp(dve, dest_i[:, gt, :], dk[:], [r_dk], [r_desti])
            h_t, h_r = hb[gt % 2]
            ld(sp, h_t[:], h2s_d[gt * 128:(gt + 1) * 128, :], [h_r])
            if gt == 0:
                K._wait(sp, bag1.evs())
            for k in range(4):
                K.dma(pool, lambda: nc.gpsimd.indirect_dma_start(
                    out=xg_d, out_offset=bass.IndirectOffsetOnAxis(ap=dest_i[:, gt, k:k + 1], axis=0),
                    in_=h_t[:], in_offset=None), reads=[h_r, r_desti], writes=[])
    K.barrier()
    dump("desti", dest_i[:], r_desti, [128, NTT, 4], I32)
    dump("bexp", bexp_i[:], r_bexpi, [128, NB], I32)

    cut(3)
    scat_evs = [(sid, K.semvals[sid]) for sid in pool.ring if K.semvals[sid] > 0]

    with ExitStack() as p2:
        wgu = [SB("wgu%d" % i, [128, 8, 2 * DM], BF16, p2) for i in range(2)]
        wdn = [SB("wdn%d" % i, [128, 8, DM], BF16, p2) for i in range(2)]
        bgu = [SB("bgus%d" % i, [128, 16], F32, p2) for i in range(2)]
        bdn = [SB("bdns%d" % i, [128, DM], F32, p2) for i in range(2)]
        xgt = [SB("xgt%d" % i, [128, DM], BF16, p2) for i in range(2)]
        xgTs = [SB("xgT%d" % i, [128, 8, BLK], BF16, p2) for i in range(2)]
        actT, r_actT = SB("actT", [128, 8, BLK], BF16, p2)
        gg, r_gg = SB("gg", [128, BLK], F32, p2)
        sg, r_sg = SB("sg", [128, BLK], F32, p2)
        ll, r_ll = SB("ll", [128, BLK], F32, p2)
        yo = [SB("yo%d" % i, [128, DM], F32, p2) for i in range(2)]
        K._wait(sp, scat_evs)
        wgr = [[Res() for _ in range(8)] for _ in range(2)]
        wdr = [[Res() for _ in range(8)] for _ in range(2)]

        def load_weights(blk):
            wg_t = wgu[blk % 2][0]
            wd_t = wdn[blk % 2][0]
            bg_t, bg_r = bgu[blk % 2]
            bd_t, bd_r = bdn[blk % 2]
            for k in range(8):
                K.dma(pool, lambda: nc.gpsimd.indirect_dma_start(
                    out=wg_t[:, k, :], out_offset=None, in_=wgu_d,
                    in_offset=bass.IndirectOffsetOnAxis(ap=widx_i[:, blk, k:k + 1], axis=0)), reads=[r_widx], writes=[wgr[blk % 2][k]])
            for k in range(8):
                K.dma(pool, lambda: nc.gpsimd.indirect_dma_start(
                    out=wd_t[:, k, :], out_offset=None, in_=wd_d,
                    in_offset=bass.IndirectOffsetOnAxis(ap=widx_i[:, blk, k:k + 1], axis=0)), reads=[r_widx], writes=[wdr[blk % 2][k]])
            K.dma(pool, lambda: nc.gpsimd.indirect_dma_start(
                out=bg_t[:], out_offset=None, in_=bgu_d,
                in_offset=bass.IndirectOffsetOnAxis(ap=bidx_i[:, blk:blk + 1], axis=0)), reads=[r_bidx], writes=[bg_r])
            K.dma(pool, lambda: nc.gpsimd.indirect_dma_start(
                out=bd_t[:], out_offset=None, in_=bd_d,
                in_offset=bass.IndirectOffsetOnAxis(ap=bidx_i[:, blk:blk + 1], axis=0)), reads=[r_bidx], writes=[bd_r])

        PT2 = PBs[4][:].bitcast(BF16)
        ptn = [0]

        def prep_tokens(blk):
            xgT_, r_xgT_ = xgTs[blk % 2]
            for st in range(4):
                g_t, g_r = xgt[st % 2]
                r0 = blk * BLK + st * 128
                ld(sp, g_t[:], xg_d[r0:r0 + 128, :], [g_r])
                if ptn[0] % 2 == 0:
                    pt_ap, pt_r = PT[:], rPT
                else:
                    pt_ap, pt_r = PT2, rPB[4]
                ptn[0] += 1
                for k in range(8):
                    tr(pt_ap[:, k * 128:(k + 1) * 128], g_t[:, k * 128:(k + 1) * 128], identb[:], [g_r, r_identb], [pt_r])
                cp(act, xgT_[:, :, st * 128:(st + 1) * 128],
                   pt_ap.rearrange("p (k t) -> p k t", t=128), [pt_r], [r_xgT_])

        load_weights(0)
        prep_tokens(0)
        for blk in range(NB):
            if blk + 1 < NB:
                load_weights(blk + 1)
                prep_tokens(blk + 1)
            wg_t = wgu[blk % 2][0]
            wd_t = wdn[blk % 2][0]
            wg_rs = wgr[blk % 2]
            wd_rs = wdr[blk % 2]
            bg_t, bg_r = bgu[blk % 2]
            bd_t, bd_r = bdn[blk % 2]
            xgT, r_xgT = xgTs[blk % 2]
            for fc in range(8):
                pg, rpg = PBs[(fc % 2) * 2], rPB[(fc % 2) * 2]
                pl, rpl = PBs[(fc % 2) * 2 + 1], rPB[(fc % 2) * 2 + 1]
                for k in range(8):
                    mm(pg[:], wg_t[:, k, fc * 128:(fc + 1) * 128], xgT[:, k, :], k == 0, k == 7, [wg_rs[k], r_xgT], [rpg])
                for k in range(8):
                    mm(pl[:], wg_t[:, k, DM + fc * 128:DM + (fc + 1) * 128], xgT[:, k, :], k == 0, k == 7, [wg_rs[k], r_xgT], [rpl])
                ts(dve, gg[:], pg[:], bg_t[:, fc:fc + 1], 7.0, ALU.add, ALU.min, [rpg, bg_r], [r_gg])
                actf(sg[:], gg[:], AF.Sigmoid, [r_gg], [r_sg], scale=1.702)
                ts(dve, ll[:], pl[:], bg_t[:, 8 + fc:9 + fc], 7.0, ALU.add, ALU.min, [rpl, bg_r], [r_ll])
                ts(dve, ll[:], ll[:], -7.0, 1.0, ALU.max, ALU.add, [r_ll], [r_ll])
                tt(dve, gg[:], gg[:], sg[:], ALU.mult, [r_gg, r_sg], [r_gg])
                tt(dve, actT[:, fc, :], gg[:], ll[:], ALU.mult, [r_gg, r_ll], [r_actT])
            dbanks = ((P2[:, 0:512], rP2a), (P2[:, 512:1024], rP2b), (PBs[2][:], rPB[2]), (PBs[3][:], rPB[3]))
            for st in range(4):
                y_t, y_r = yo[st % 2]
                for hf in range(2):
                    pd, rpd = dbanks[(st % 2) * 2 + hf]
                    for k in range(8):
                        mm(pd, actT[:, k, st * 128:(st + 1) * 128], wd_t[:, k, hf * 512:(hf + 1) * 512], k == 0, k == 7,
                           [r_actT, wd_rs[k]], [rpd])
                    tt(dve, y_t[:, hf * 512:(hf + 1) * 512], pd, bd_t[:, hf * 512:(hf + 1) * 512], ALU.add, [rpd, bd_r], [y_r])
                r0 = blk * BLK + st * 128
                K.dma(act, lambda: nc.scalar.dma_start(out=yb_d[r0:r0 + 128, :], in_=y_t[:]), reads=[y_r], writes=[])
    K.barrier()
    yb_evs = [(sid, K.semvals[sid]) for sid in act.ring if K.semvals[sid] > 0]

    with ExitStack() as p3:
        yg = [SB("yg%d" % i, [128, DM], F32, p3) for i in range(8)]
        fa, r_fa = SB("fa", [128, DM], F32, p3)
        x1r = [SB("x1r%d" % i, [128, DM], F32, p3) for i in range(2)]
        ob = [SB("ob%d" % i, [128, DM], F32, p3) for i in range(2)]
        junk3, r_junk3 = SB("junk3", [128, DM], BF16, p3)
        s3, r_s3 = SB("s3", [128, 2], F32, p3)
        G2, r_G2 = SB("G2", [128, 2, DM], F32, p3)
        for b in range(2):
            tt(dve, vtmp[:], modT[:, 40:48, b], small["gpost2"][0][:], ALU.mult, [r_modT, small["gpost2"][1]], [r_vtmp])
            bcast_rows(vtmp, r_vtmp, G2[:, b, :], r_G2)
        K._wait(pool, yb_evs)
        K._wait(sp, bag1.evs())
        def p3_loads(gt):
            x_t, x_r = x1r[gt % 2]
            ld(sp, x_t[:], x1s_d[gt * 128:(gt + 1) * 128, :], [x_r])
            for k in range(4):
                y_t, y_r = yg[(gt % 2) * 4 + k]
                K.dma(pool, lambda: nc.gpsimd.indirect_dma_start(
                    out=y_t[:], out_offset=None, in_=yb_d,
                    in_offset=bass.IndirectOffsetOnAxis(ap=dest_i[:, gt, k:k + 1], axis=0)), reads=[r_desti], writes=[y_r])

        def p3_compute(gt):
            b = gt // NT
            x_t, x_r = x1r[gt % 2]
            o_t, o_r = ob[gt % 2]
            ygs = [yg[(gt % 2) * 4 + k] for k in range(4)]
            actf(fa[:], ygs[0][0][:], AF.Copy, [ygs[0][1], r_gate], [r_fa], scale=gate_all[:, gt, 0:1])
            for k in range(1, 4):
                stt(dve, fa[:], ygs[k][0][:], gate_all[:, gt, k:k + 1], fa[:], ALU.mult, ALU.add, [ygs[k][1], r_gate, r_fa], [r_fa])
            actf(junk3[:], fa[:], AF.Square, [r_fa], [r_junk3, r_s3], accum_out=s3[:, 0:1])
            actf(s3[:, 1:2], s3[:, 0:1], AF.Ln, [r_s3], [r_s3], scale=1.0 / DM, bias=epsb[:, 0:1])
            actf(s3[:, 1:2], s3[:, 1:2], AF.Exp, [r_s3], [r_s3], scale=-0.5)
            stt(dve, o_t[:], fa[:], s3[:, 1:2], G2[:, b, :], ALU.mult, ALU.mult, [r_fa, r_s3, r_G2], [o_r])
            tt(pool, o_t[:], o_t[:], x_t[:], ALU.add, [o_r, x_r], [o_r])
            K.dma(sp, lambda: nc.sync.dma_start(out=out_d[gt * 128:(gt + 1) * 128, :], in_=o_t[:]), reads=[o_r], writes=[], is_out=True)

        p3_loads(0)
        for gt in range(NTT):
            if gt + 1 < NTT:
                p3_loads(gt + 1)
            p3_compute(gt)
    K.barrier()
    K.finish()
    es.close()
    return nc, dbg_d


def _consts(NB):
    idx = np.arange(128)
    ch = idx // 64
    same = ch[:, None] == ch[None, :]
    U1 = (same & (idx[:, None] > idx[None, :])).astype(np.float32)
    U2 = (same & (idx[:, None] <= idx[None, :])).astype(np.float32)
    m01 = U2.copy()
    Lst = (idx[:, None] < idx[None, :]).astype(np.float32)
    invf = (np.float32(10000.0) ** (-(np.arange(16, dtype=np.float32) * np.float32(2.0) / np.float32(32.0)))).astype(np.float32)
    rm = np.stack([(ch == 0), (ch == 1)], axis=1).astype(np.float32)
    return dict(identf=np.eye(128, dtype=np.float32), U1=U1, U2=U2, m01=m01, Lst=Lst, rm=rm,
                invf=np.tile(invf[None, :], (128, 1)).astype(np.float32),
                iotab=np.tile(np.arange(NB, dtype=np.float32)[None, :], (128, 1)),
                pk=(np.arange(8, dtype=np.float32)[None, :] * 128 + np.arange(128, dtype=np.float32)[:, None]).astype(np.float32))


def _rep(v, n=128):
    return np.ascontiguousarray(np.tile(np.asarray(v, np.float32).reshape(1, -1), (n, 1)))


def _pk(v, k):
    return np.ascontiguousarray(np.asarray(v, np.float32).reshape(k, 128).T)


_CACHE = {}


def kernel(x, c, positions, w_ada, b_ada, pre_mix_norm, w_in, conv_w, conv_b, dt_bias, a_log, d_skip, ssd_norm,
           q_a_norm, w_q_up, kv_a_norm, w_kv_up, mla_norm, w_out, post_mix_norm, pre_ffn_norm, w_router, b_router,
           w_gate_up, b_gate_up, w_down, b_down, post_ffn_norm, _dbg=()):
    x = np.asarray(x, np.float32)
    Bsz, S, _ = x.shape
    ncores = Bsz // 2
    NT = S // 128
    NB = (2 * S * 4) // BLK + NE
    key = (S, tuple(_dbg))
    if key not in _CACHE:
        _CACHE[key] = build(S, _dbg)
    nc, dbg_d = _CACHE[key]
    f = lambda a: np.ascontiguousarray(np.asarray(a, np.float32))
    w_in = f(w_in)[0]
    w_tm = np.ascontiguousarray(np.concatenate([w_in[:, 0:512], w_in[:, 1536:1544], w_in[:, 2184:2216]], axis=1))
    w_fm = np.ascontiguousarray(np.concatenate([w_in[:, 512:1536], w_in[:, 1544:1928], w_in[:, 1928:2184]], axis=1))
    wq = f(w_q_up)[0].reshape(384, 8, 96)
    qn = wq[:, :, 0:64]
    qr = wq[:, :, 64:96]
    qn_pairs = np.stack([np.concatenate([qn[:, j], qn[:, j + 4]], axis=1) for j in range(4)], axis=1)
    wq2 = np.ascontiguousarray(np.concatenate([qn_pairs.reshape(384, 512), qr.reshape(384, 256)], axis=1))
    wkv = f(w_kv_up)[0].reshape(256, 8, 128)
    kn = wkv[:, :, 0:64]
    vv = wkv[:, :, 64:128]
    kn_pairs = np.stack([np.concatenate([kn[:, j], kn[:, j + 4]], axis=1) for j in range(4)], axis=1)
    wkv2 = np.ascontiguousarray(np.concatenate([kn_pairs.reshape(256, 512), vv.reshape(256, 512)], axis=1))
    cw = f(conv_w)[0]
    convw = np.ascontiguousarray(cw.reshape(4, 8, 128).transpose(2, 1, 0))
    shared = dict(
        w_ada=f(w_ada)[0], b_adaT=_pk(f(b_ada)[0], 48), g_pre1=_pk(f(pre_mix_norm)[0], 8), g_post1=_pk(f(post_mix_norm)[0], 8),
        g_pre2=_pk(f(pre_ffn_norm)[0], 8), g_post2=_pk(f(post_ffn_norm)[0], 8), w_tm=w_tm, w_fm=w_fm, convw=convw,
        convb=_pk(f(conv_b)[0], 8), dtb=_rep(f(dt_bias)[0]), alog=_rep(f(a_log)[0]), dsk=_rep(f(d_skip)[0]),
        ssdn=_rep(f(ssd_norm)[0]), mlan=_rep(f(mla_norm)[0]), qan=_pk(f(q_a_norm)[0], 3), kvan=_pk(f(kv_a_norm)[0], 2),
        wq=wq2, wkv=wkv2, wout=f(w_out)[0], wr=f(w_router)[0], br=_rep(f(b_router)[0]),
        wgu=f(w_gate_up)[0].reshape(NE * DM, 2 * DM), wd=f(w_down)[0].reshape(NE * DM, DM),
        bgu=np.ascontiguousarray(f(b_gate_up)[0].reshape(NE, 16, 128).transpose(0, 2, 1)).reshape(NE * 128, 16),
        bd=np.ascontiguousarray(np.broadcast_to(f(b_down)[0][:, None, :], (NE, 128, DM))).reshape(NE * 128, DM),
    )
    shared.update(_consts(NB))
    cc = f(c)
    pp = np.asarray(positions, np.int32)
    in_maps = []
    for ci in range(ncores):
        m = dict(shared)
        m["x"] = np.ascontiguousarray(x[2 * ci:2 * ci + 2].reshape(2 * S, DM))
        m["cT"] = np.ascontiguousarray(cc[2 * ci:2 * ci + 2].reshape(2, 8, 128).transpose(2, 1, 0))
        m["pos"] = np.ascontiguousarray(pp[2 * ci:2 * ci + 2].reshape(2, NT, 128).transpose(0, 2, 1))
        in_maps.append(m)
    res = run_bass_kernel_spmd(nc, in_maps, core_ids=list(range(ncores)))
    out = np.stack([np.asarray(r["out"], np.float32).reshape(2, S, DM) for r in res.results], axis=0).reshape(Bsz, S, DM)
    if _dbg:
        return out, [{k: np.asarray(r["dbg_" + k]) for k in dbg_d} for r in res.results]
    return out
```

```python
import math
from contextlib import ExitStack
import numpy as np
import concourse.bass as bass
import concourse.mybir as mybir
from concourse.bass_utils import run_bass_kernel_spmd

F32 = mybir.dt.float32
BF16 = mybir.dt.bfloat16
I32 = mybir.dt.int32
AF = mybir.ActivationFunctionType
ALU = mybir.AluOpType
AX = mybir.AxisListType

import os
_PH = float(os.environ.get("KPH", "9"))
DM = 1024
NE = 32
BLK = 512
EPS = 1e-6
MAGIC = 12582912.0
C1 = 6.28125
C2 = 2.0 * math.pi - 6.28125


class Res:
    __slots__ = ("w", "rs")

    def __init__(self):
        self.w = None
        self.rs = {}


class Eng:
    def __init__(self, h, sid, is_pe=False):
        self.h = h
        self.sid = sid
        self.n = 0
        self.known = {}
        self.is_pe = is_pe
        self.ring = []
        self.rpos = 0


class Kern:
    def __init__(self, nc, es):
        self.nc = nc
        self.sems = []
        self.semvals = []
        self.es = es
        mk = lambda nm: self._newsem(nm)
        self.pe = Eng(nc.tensor, mk("s_pe"), True)
        self.dve = Eng(nc.vector, mk("s_dve"))
        self.act = Eng(nc.scalar, mk("s_act"))
        self.pool = Eng(nc.gpsimd, mk("s_pool"))
        self.sp = Eng(nc.sync, mk("s_sp"))
        for e, n in ((self.sp, 44), (self.act, 12), (self.pool, 36)):
            e.ring = [mk("d%d_%d" % (e.sid, i)) for i in range(n)]
        self.out_evs = []

    def _newsem(self, nm):
        s = self.es.enter_context(self.nc.semaphore(nm))
        self.sems.append(s)
        self.semvals.append(0)
        return len(self.sems) - 1

    def _wait(self, eng, evs):
        best = {}
        for sid, val in evs:
            if val > best.get(sid, 0):
                best[sid] = val
        for sid, val in best.items():
            if eng.is_pe and sid == eng.sid:
                continue
            if eng.known.get(sid, 0) >= val:
                continue
            eng.h.wait_ge(self.sems[sid], val)
            eng.known[sid] = val

    @staticmethod
    def _deps(reads, writes):
        evs = []
        for r in reads:
            if r.w is not None:
                evs.append(r.w)
        for r in writes:
            if r.w is not None:
                evs.append(r.w)
            evs.extend(r.rs.items())
        return evs

    @staticmethod
    def _mark(ev, reads, writes):
        for r in reads:
            if ev[1] > r.rs.get(ev[0], 0):
                r.rs[ev[0]] = ev[1]
        for r in writes:
            r.w = ev
            r.rs = {}

    def I(self, eng, fn, reads=(), writes=()):
        evs = self._deps(reads, writes)
        own = eng.sid
        evs2 = []
        raw = set()
        for r in reads:
            if r.w is not None:
                raw.add(r.w)
        for ev in evs:
            if ev[0] == own and ev not in raw:
                continue
            evs2.append(ev)
        self._wait(eng, evs2)
        ins = fn()
        eng.n += 1
        ins.then_inc(self.sems[eng.sid], 1)
        self._mark((eng.sid, eng.n), reads, writes)

    def dma(self, q, fn, reads=(), writes=(), is_out=False):
        evs = self._deps(reads, writes)
        sid = q.ring[q.rpos % len(q.ring)]
        q.rpos += 1
        prev = self.semvals[sid]
        if prev > 0:
            evs.append((sid, prev))
        self._wait(q, evs)
        ins = fn()
        self.semvals[sid] = prev + 16
        ins.then_inc(self.sems[sid], 16)
        ev = (sid, prev + 16)
        self._mark(ev, reads, writes)
        if is_out:
            self.out_evs.append(ev)
        return ev

    def barrier(self):
        evs = []
        for e in (self.sp, self.act, self.pool):
            evs += [(sid, self.semvals[sid]) for sid in e.ring if self.semvals[sid] > 0]
        for e in (self.pe, self.dve, self.act, self.pool):
            if e.n > 0:
                evs.append((e.sid, e.n))
        for e in (self.pe, self.dve, self.act, self.pool, self.sp):
            self._wait(e, [ev for ev in evs if ev[0] != e.sid])

    def finish(self):
        evs = list(self.out_evs)
        for e in (self.sp, self.act, self.pool):
            evs += [(sid, self.semvals[sid]) for sid in e.ring if self.semvals[sid] > 0]
        for e in (self.pe, self.dve, self.act, self.pool):
            if e.n > 0:
                evs.append((e.sid, e.n))
        self._wait(self.sp, evs)


class _Cut(Exception):
    pass


def build(S, dbg=()):
    ctx = {}
    try:
        return _build_inner(S, dbg, ctx)
    except _Cut:
        ctx["K"].finish()
        return ctx["nc"], ctx["dbg_d"]


def _build_inner(S, dbg, ctx):
    NT = S // 128
    NG = S // 512
    T = 2 * S
    NTT = T // 128
    NB = (T * 4) // BLK + NE
    nc = bass.Bass("TRN2", target_bir_lowering=False)
    es = ExitStack()
    K = Kern(nc, es)
    pe, dve, act, pool, sp = K.pe, K.dve, K.act, K.pool, K.sp
    dbg_d = {}

    def DI(name, shape, dt=F32):
        return nc.dram_tensor(name, list(shape), dt, kind="ExternalInput").ap()

    def DS(name, shape, dt=F32):
        if name in dbg:
            d = nc.dram_tensor("dbg_" + name, list(shape), dt, kind="ExternalOutput").ap()
            dbg_d[name] = d
            return d
        return nc.dram_tensor(name, list(shape), dt, kind="Internal").ap()

    x_d = DI("x", [T, DM])
    cT_d = DI("cT", [128, 8, 2])
    pos_d = DI("pos", [2, 128, NT], I32)
    wada_d = DI("w_ada", [DM, 6 * DM]).rearrange("(k p) f -> p k f", p=128)
    badaT_d = DI("b_adaT", [128, 48])
    gpre1_d = DI("g_pre1", [128, 8])
    gpost1_d = DI("g_post1", [128, 8])
    gpre2_d = DI("g_pre2", [128, 8])
    gpost2_d = DI("g_post2", [128, 8])
    wtm_d = DI("w_tm", [DM, 552]).rearrange("(k p) f -> p k f", p=128)
    wfm_d = DI("w_fm", [DM, 1664]).rearrange("(k p) f -> p k f", p=128)
    convw_d = DI("convw", [128, 8, 4])
    convb_d = DI("convb", [128, 8])
    dtb_d = DI("dtb", [128, 8])
    alog_d = DI("alog", [128, 8])
    dsk_d = DI("dsk", [128, 8])
    ssdn_d = DI("ssdn", [128, 512])
    mlan_d = DI("mlan", [128, 512])
    qan_d = DI("qan", [128, 3])
    kvan_d = DI("kvan", [128, 2])
    wq_d = DI("wq", [384, 768]).rearrange("(k p) f -> p k f", p=128)
    wkv_d = DI("wkv", [256, 1024]).rearrange("(k p) f -> p k f", p=128)
    wout_d = DI("wout", [DM, DM]).rearrange("(k p) f -> p k f", p=128)
    wr_d = DI("wr", [DM, NE]).rearrange("(k p) f -> p k f", p=128)
    br_d = DI("br", [128, NE])
    wgu_d = DI("wgu", [NE * DM, 2 * DM])
    wd_d = DI("wd", [NE * DM, DM])
    bgu_d = DI("bgu", [NE * 128, 16])
    bd_d = DI("bd", [NE * 128, DM])
    identf_d = DI("identf", [128, 128])
    U1_d = DI("U1", [128, 128])
    U2_d = DI("U2", [128, 128])
    Lst_d = DI("Lst", [128, 128])
    m01_d = DI("m01", [128, 128])
    invf_d = DI("invf", [128, 16])
    iotab_d = DI("iotab", [128, NB])
    pk_d = DI("pk", [128, 8])
    rm_d = DI("rm", [128, 2])
    x1s_d = DS("x1s", [T, DM])
    h2s_d = DS("h2s", [T, DM], BF16)
    xg_d = DS("xg", [NB * BLK, DM], BF16)
    yb_d = DS("yb", [NB * BLK, DM])
    out_d = nc.dram_tensor("out", [T, DM], F32, kind="ExternalOutput").ap()
    ctx.update(K=K, es=es, nc=nc, dbg_d=dbg_d)

    def cut(level):
        if _PH < level:
            raise _Cut()

    def SB(name, shape, dt=F32, stack=None):
        t = (stack or es).enter_context(nc.sbuf_tensor("sb_" + name, list(shape), dt))
        return t, Res()

    def PS(name, shape, dt=F32):
        return es.enter_context(nc.psum_tensor("ps_" + name, list(shape), dt))

    P2 = PS("P2", [128, 1024])
    rP2a, rP2b = Res(), Res()
    PBs = [PS("PB%d" % i, [128, 512]) for i in range(5)]
    rPB = [Res() for _ in range(5)]
    PT = PS("PT", [128, 1024], BF16)
    rPT = Res()

    def mm(out, lhsT, rhs, start, stop, rd, wr):
        K.I(pe, lambda: nc.tensor.matmul(out, lhsT, rhs, start=start, stop=stop), reads=rd, writes=wr)

    def tr(out, in_, ident, rd, wr):
        K.I(pe, lambda: nc.tensor.transpose(out, in_, ident), reads=rd, writes=wr)

    def actf(out, in_, func, rd, wr, bias=None, scale=None, accum_out=None):
        kw = {}
        if bias is not None:
            kw["bias"] = bias
        if scale is not None:
            kw["scale"] = scale
        if accum_out is not None:
            kw["accum_out"] = accum_out
        K.I(act, lambda: nc.scalar.activation(out, in_, func, **kw), reads=rd, writes=wr)

    def ts(eng, out, in0, s1, s2, op0, op1, rd, wr):
        h = eng.h
        if op1 is None:
            K.I(eng, lambda: h.tensor_scalar(out, in0, s1, None, op0), reads=rd, writes=wr)
        else:
            K.I(eng, lambda: h.tensor_scalar(out, in0, s1, s2, op0, op1), reads=rd, writes=wr)

    def tt(eng, out, in0, in1, op, rd, wr):
        h = eng.h
        K.I(eng, lambda: h.tensor_tensor(out, in0, in1, op), reads=rd, writes=wr)

    def stt(eng, out, in0, scalar, in1, op0, op1, rd, wr):
        h = eng.h
        K.I(eng, lambda: h.scalar_tensor_tensor(out, in0, scalar, in1, op0, op1), reads=rd, writes=wr)

    def cp(eng, out, in_, rd, wr):
        h = eng.h
        if eng is act:
            K.I(eng, lambda: nc.scalar.activation(out, in_, AF.Copy), reads=rd, writes=wr)
        else:
            K.I(eng, lambda: h.tensor_copy(out, in_), reads=rd, writes=wr)

    def ld(q, out, in_, wr, rd=()):
        K.dma(q, lambda: q.h.dma_start(out=out, in_=in_), reads=rd, writes=wr)

    def dump(name, ap, res, shape, dt=F32):
        if name not in dbg:
            return
        d = nc.dram_tensor("dbg_" + name, list(shape), dt, kind="ExternalOutput").ap()
        dbg_d[name] = d
        K.dma(sp, lambda: nc.sync.dma_start(out=d, in_=ap), reads=[res], writes=[Res()], is_out=True)

    identf, r_identf = SB("identf", [128, 128])
    identb, r_identb = SB("identb", [128, 128], BF16)
    onesf, r_onesf = SB("onesf", [128, 128])
    onesb, r_onesb = SB("onesb", [128, 128], BF16)
    U1, r_U1 = SB("U1", [128, 128])
    U2, r_U2 = SB("U2", [128, 128])
    Lst, r_Lst = SB("Lst", [128, 128])
    m01, r_m01 = SB("m01", [128, 128])
    invf, r_invf = SB("invf", [128, 16])
    ld(sp, identf[:], identf_d, [r_identf])
    ld(sp, U1[:], U1_d, [r_U1])
    ld(sp, U2[:], U2_d, [r_U2])
    ld(sp, Lst[:], Lst_d, [r_Lst])
    ld(sp, m01[:], m01_d, [r_m01])
    ld(sp, invf[:], invf_d, [r_invf])
    rm, r_rm = SB("rm", [128, 2])
    Mc, r_Mc = SB("Mc", [128, 2, 128])
    ld(sp, rm[:], rm_d, [r_rm])
    cp(dve, identb[:], identf[:], [r_identf], [r_identb])
    K.I(dve, lambda: nc.vector.memset(onesf[:], 1.0), writes=[r_onesf])
    K.I(dve, lambda: nc.vector.memset(onesb[:], 1.0), writes=[r_onesb])
    for c in range(2):
        ts(dve, Mc[:, c, :], onesf[:], rm[:, c:c + 1], None, ALU.mult, None, [r_onesf, r_rm], [r_Mc])
    epsb, r_epsb = SB("epsb", [128, 1])
    oneb, r_oneb = SB("oneb", [128, 1])
    K.I(dve, lambda: nc.vector.memset(epsb[:], EPS), writes=[r_epsb])
    K.I(dve, lambda: nc.vector.memset(oneb[:], 1.0), writes=[r_oneb])

    small = {}
    for nm, d, shp in (("gpre1", gpre1_d, [128, 8]), ("gpost1", gpost1_d, [128, 8]), ("gpre2", gpre2_d, [128, 8]),
                       ("gpost2", gpost2_d, [128, 8]), ("convw", convw_d, [128, 8, 4]), ("convb", convb_d, [128, 8]),
                       ("dtb", dtb_d, [128, 8]), ("alog", alog_d, [128, 8]), ("dsk", dsk_d, [128, 8]),
                       ("ssdn", ssdn_d, [128, 512]), ("mlan", mlan_d, [128, 512]), ("qan", qan_d, [128, 3]),
                       ("kvan", kvan_d, [128, 2]), ("br", br_d, [128, NE]), ("badaT", badaT_d, [128, 48]),
                       ("cT", cT_d, [128, 8, 2]), ("wr", wr_d, [128, 8, NE])):
        t, r = SB("c_" + nm, shp)
        ld(sp, t[:], d, [r])
        small[nm] = (t, r)

    aneg, r_aneg = SB("aneg", [128, 8])
    actf(aneg[:], small["alog"][0][:], AF.Exp, [small["alog"][1]], [r_aneg])
    ts(dve, aneg[:], aneg[:], -1.0, None, ALU.mult, None, [r_aneg], [r_aneg])

    modT, r_modT = SB("modT", [128, 48, 2])
    cact, r_cact = SB("cact", [128, 8, 2])
    actf(cact[:], small["cT"][0][:], AF.Silu, [small["cT"][1]], [r_cact])
    with ExitStack() as st0:
        wst = [SB("wadast%d" % i, [128, 8, 512], F32, st0) for i in range(2)]
        for blk in range(12):
            w_t, w_r = wst[blk % 2]
            ld(sp, w_t[:], wada_d[:, :, blk * 512:(blk + 1) * 512], [w_r])
            for j in range(4):
                fc = blk * 4 + j
                pb, rpb = PBs[fc % 2], rPB[fc % 2]
                for k in range(8):
                    mm(pb[:, 0:2], w_t[:, k, j * 128:(j + 1) * 128], cact[:, k, :], k == 0, k == 7,
                       [w_r, r_cact], [rpb])
                ts(dve, modT[:, fc, :], pb[:, 0:2], small["badaT"][0][:, fc:fc + 1], None, ALU.add, None,
                   [rpb, small["badaT"][1]], [r_modT])
    K.barrier()
    a1T, r_a1T = SB("a1T", [128, 2, 8])
    sh1T, r_sh1T = SB("sh1T", [128, 2, 8])
    vtmp, r_vtmp = SB("vtmp", [128, 8])
    btmp = [SB("btmp%d" % i, [128, 128]) for i in range(2)]
    bcn = [0]

    def bcast_rows(vT_ap, vres, dst_ap, dres):
        for half in range(2):
            pb, rpb = PBs[2 + half], rPB[2 + half]
            for kk in range(4):
                k = half * 4 + kk
                bt, rbt = btmp[bcn[0] % 2]
                bcn[0] += 1
                ts(dve, bt[:], onesf[:], vT_ap[:, k:k + 1], None, ALU.mult, None, [r_onesf, vres], [rbt])
                mm(pb[:, kk * 128:(kk + 1) * 128], bt[:], identf[:], True, True, [rbt, r_identf], [rpb])
            cp(act, dst_ap[:, half * 512:(half + 1) * 512], pb[:], [rpb], [dres])

    for b in range(2):
        ts(dve, vtmp[:], modT[:, 8:16, b], 1.0, None, ALU.add, None, [r_modT], [r_vtmp])
        tt(dve, a1T[:, b, :], vtmp[:], small["gpre1"][0][:], ALU.mult, [r_vtmp, small["gpre1"][1]], [r_a1T])
        cp(dve, sh1T[:, b, :], modT[:, 0:8, b], [r_modT], [r_sh1T])

    lg_all, r_lg = SB("lg_all", [128, NTT, NE])
    v8_all, r_v8 = SB("v8_all", [128, NTT, 8])
    gate_all, r_gate = SB("gate_all", [128, NTT, 4])

    castn = [0]

    def cast_any(out, in_, rd, wr):
        e = (dve, pool, act)[castn[0] % 3]
        castn[0] += 1
        cp(e, out, in_, rd, wr)

    def rstd_from(ss_ap, ssres, out_ap, ores, n, eng=dve):
        actf(out_ap, ss_ap, AF.Ln, [ssres], [ores], bias=epsb[:, 0:1], scale=1.0 / n)
        actf(out_ap, out_ap, AF.Exp, [ores], [ores], scale=-0.5)

    class Bag:
        def __init__(self):
            self.d = {}

        def add(self, ev):
            if ev[1] > self.d.get(ev[0], 0):
                self.d[ev[0]] = ev[1]

        def evs(self):
            return list(self.d.items())

    kn_s = DS("kn_s", [2, 128, 4 * S], BF16)
    kr_s = DS("kr_s", [2, 128, S], BF16)
    v_s = DS("v_s", [2, 128, NT * 8 * 65], BF16)
    q_s = DS("q_s", [2 * NT, 128, 2048], BF16)
    mix_s = DS("mix_s", [T, DM], BF16)
    bagA = [Bag(), Bag()]
    bagT = [Bag(), Bag()]

    def st(q, out, in_, rd, bag):
        ev = K.dma(q, lambda: q.h.dma_start(out=out, in_=in_), reads=rd, writes=[])
        bag.add(ev)

    dump("modT", modT[:], r_modT, [128, 48, 2])
    cut(0.2)
    with ExitStack() as p1:
        wtm, r_wtm = SB("wtm", [128, 8, 552], BF16, p1)
        wfm, r_wfm = SB("wfm", [128, 8, 1664], BF16, p1)
        wq, r_wq = SB("wq", [128, 3, 768], BF16, p1)
        wkv, r_wkv = SB("wkv", [128, 2, 1024], BF16, p1)
        with ExitStack() as pst:
            stg = [SB("stg%d" % i, [128, 1664], F32, pst) for i in range(2)]
            sn = 0
            for (dst, rdst, src, nk, ncol) in ((wtm, r_wtm, wtm_d, 8, 552), (wfm, r_wfm, wfm_d, 8, 1664),
                                               (wq, r_wq, wq_d, 3, 768), (wkv, r_wkv, wkv_d, 2, 1024)):
                for k in range(nk):
                    s_t, s_r = stg[sn % 2]
                    sn += 1
                    ld(sp, s_t[:, 0:ncol], src[:, k, :], [s_r])
                    cast_any(dst[:, k, :], s_t[:, 0:ncol], [s_r], [rdst])
        K.barrier()
        cut(0.25)
        Sst, r_Sst = SB("Sst", [128, 8, 64], F32, p1)
        Sbf, r_Sbf = SB("Sbf", [128, 8, 64], BF16, p1)
        cosT, r_cos = SB("cosT", [128, NT, 16], F32, p1)
        sinT, r_sin = SB("sinT", [128, NT, 16], F32, p1)
        posi, r_posi = SB("posi", [128, NT], I32, p1)
        posf, r_posf = SB("posf", [128, NT], F32, p1)
        ang, r_ang = SB("ang", [128, NT, 16], F32, p1)
        ang2, r_ang2 = SB("ang2", [128, NT, 16], F32, p1)
        xt = [SB("xt%d" % i, [128, DM], F32, p1) for i in range(2)]
        junk, r_junk = SB("junk", [128, DM], BF16, p1)
        xn, r_xn = SB("xn", [128, DM], BF16, p1)
        st1 = [SB("st1_%d" % i, [128, 8], F32, p1) for i in range(4)]
        hT2 = [SB("hT%d" % i, [128, 8, 512], BF16, p1) for i in range(2)]
        st0 = [SB("st0_%d" % i, [128, 2], F32, p1) for i in range(4)]
        rw, r_rw = SB("raw", [128, 8, 515], F32, p1)
        halo, r_halo = SB("halo", [128, 8, 3], F32, p1)
        cacc, r_cacc = SB("cacc", [128, 512], F32, p1)
        xact, r_xact = SB("xact", [128, 8, 512], BF16, p1)
        qag, r_qag = SB("qag", [128, 3, 512], F32, p1)
        kvag, r_kvag = SB("kvag", [128, 2, 512], F32, p1)
        sq5, r_sq5 = SB("sq5", [128, 5, 512], BF16, p1)
        rsb, r_rsb = SB("rsb", [128, 2, 512], F32, p1)
        qaTn, r_qaTn = SB("qaTn", [128, 3, 512], BF16, p1)
        kvaTn, r_kvaTn = SB("kvaTn", [128, 2, 512], BF16, p1)
        kng, r_kng = SB("kng", [128, 4, 512], BF16, p1)
        zs4 = [SB("zs%d" % i, [128, 512], F32, p1) for i in range(4)]
        dtt, r_dtt = SB("dtt", [128, 8], F32, p1)
        dta, r_dta = SB("dta", [128, 8], F32, p1)
        krp, r_krp = SB("krp", [128, 4, 32], BF16, p1)
        K.I(pool, lambda: nc.gpsimd.memset(krp[:], 0.0), writes=[r_krp])
        krT, r_krT = SB("krT", [128, 128], BF16, p1)
        rtmp = [SB("rtmp%d" % i, [128, 8, 16], F32, p1) for i in range(4)]
        qn_tm, r_qn = SB("qn_tm", [128, 512], BF16, p1)
        qr_pad, r_qrp = SB("qr_pad", [128, 4, 2, 64], BF16, p1)
        K.I(pool, lambda: nc.gpsimd.memset(qr_pad[:], 0.0), writes=[r_qrp])
        QT, r_QT = SB("QT", [128, 2048], BF16, p1)
        K.I(pool, lambda: nc.gpsimd.memset(QT[:], 0.0), writes=[r_QT])
        Vt, r_Vt = SB("Vt", [128, 8, 65], BF16, p1)
        K.I(pool, lambda: nc.gpsimd.memset(Vt[:], 1.0), writes=[r_Vt])
        xs_tm, r_xs = SB("xs_tm", [128, 8, 64], BF16, p1)
        B_tm, r_Btm = SB("B_tm", [128, 256], BF16, p1)
        xdt, r_xdt = SB("xdt", [128, 8, 64], BF16, p1)
        xdtd, r_xdtd = SB("xdtd", [128, 2, 8, 64], BF16, p1)
        ecsm, r_ecsm = SB("ecsm", [128, 2, 8], F32, p1)
        dtem, r_dtem = SB("dtem", [128, 2, 8], F32, p1)
        lseg, r_lseg = SB("lseg", [128, 8, 128], F32, p1)
        dec, r_dec = SB("dec", [128, 8, 128], F32, p1)
        cbm, r_cbm = SB("cbm", [128, 2, 128], F32, p1)
        MT, r_MT = SB("MT", [128, 8, 128], BF16, p1)
        ecs, r_ecs = SB("ecs", [128, 8], F32, p1)
        dte, r_dte = SB("dte", [128, 8], F32, p1)
        cdB, r_cdB = SB("cdB", [128, 2, 8], F32, p1)
        yd, r_yd = SB("yd", [128, 8, 64], F32, p1)
        yt, r_yt = SB("yt", [128, 8, 64], F32, p1)
        yt2, r_yt2 = SB("yt2", [128, 8, 64], F32, p1)
        mixs, r_mixs = SB("mixs", [128, 512], BF16, p1)

        pbn = [0]

        def next_pb():
            i = (0, 1, 3, 4)[pbn[0] % 4]
            pbn[0] += 1
            return PBs[i], rPB[i]

        for b in range(2):
            bag = bagA[b]
            ld(sp, posi[:], pos_d[b], [r_posi])
            cp(dve, posf[:], posi[:], [r_posi], [r_posf])
            tt(dve, ang[:], posf[:].unsqueeze(2).broadcast_to((128, NT, 16)),
               invf[:].unsqueeze(1).broadcast_to((128, NT, 16)), ALU.mult, [r_posf, r_invf], [r_ang])
            ts(dve, ang2[:], ang[:], 1.0 / (2.0 * math.pi), MAGIC, ALU.mult, ALU.add, [r_ang], [r_ang2])
            ts(dve, ang2[:], ang2[:], -MAGIC, None, ALU.add, None, [r_ang2], [r_ang2])
            stt(dve, ang[:], ang2[:], -C1, ang[:], ALU.mult, ALU.add, [r_ang2, r_ang], [r_ang])
            stt(dve, ang[:], ang2[:], -C2, ang[:], ALU.mult, ALU.add, [r_ang2, r_ang], [r_ang])
            ts(dve, ang[:], ang[:], 3.14159, -3.14159, ALU.min, ALU.max, [r_ang], [r_ang])
            actf(sinT[:], ang[:], AF.Sin, [r_ang], [r_sin])
            ts(dve, ang2[:], ang[:], -1.0, None, ALU.mult, None, [r_ang], [r_ang2])
            tt(dve, ang2[:], ang2[:], ang[:], ALU.max, [r_ang2, r_ang], [r_ang2])
            ts(dve, ang2[:], ang2[:], -1.0, math.pi / 2.0, ALU.mult, ALU.add, [r_ang2], [r_ang2])
            actf(cosT[:], ang2[:], AF.Sin, [r_ang2], [r_cos])
            cut(0.30)
            K.I(dve, lambda: nc.vector.memset(Sst[:], 0.0), writes=[r_Sst])
            K.I(pool, lambda: nc.gpsimd.memset(Sbf[:], 0.0), writes=[r_Sbf])
            K.I(pool, lambda: nc.gpsimd.memset(halo[:], 0.0), writes=[r_halo])

            def step1(gi):
                hTW, r_hTW = hT2[gi % 2]
                for i in range(4):
                    ti = gi * 4 + i
                    gt = b * NT + ti
                    x_t, x_r = xt[gt % 2]
                    s1, r_s1 = st0[i]
                    ld(sp, x_t[:], x_d[gt * 128:(gt + 1) * 128, :], [x_r])
                    actf(junk[:], x_t[:], AF.Square, [x_r], [r_junk, r_s1], accum_out=s1[:, 0:1])
                    rstd_from(s1[:, 0:1], r_s1, s1[:, 1:2], r_s1, DM)
                    ts(dve, xn[:], x_t[:], s1[:, 1:2], None, ALU.mult, None, [x_r, r_s1], [r_xn])
                    for k in range(8):
                        tr(PT[:, k * 128:(k + 1) * 128], xn[:, k * 128:(k + 1) * 128], identb[:], [r_xn, r_identb], [rPT])
                    for k in range(8):
                        actf(hTW[:, k, i * 128:(i + 1) * 128], PT[:, k * 128:(k + 1) * 128], AF.Identity,
                             [rPT, r_a1T, r_sh1T], [r_hTW], bias=sh1T[:, b, k:k + 1], scale=a1T[:, b, k:k + 1])

            step1(0)
            for gi in range(NG):
                hT, r_hT = hT2[gi % 2]
                cut(0.32)
                cp(pool, rw[:, :, 0:3], halo[:], [r_halo], [r_rw])
                for mc in range(13):
                    pb, rpb = next_pb()
                    for k in range(8):
                        mm(pb[:], wfm[:, k, mc * 128:(mc + 1) * 128], hT[:, k, :], k == 0, k == 7, [r_wfm, r_hT], [rpb])
                    if mc < 8:
                        cp(act, rw[:, mc, 3:515], pb[:], [rpb], [r_rw])
                    elif mc < 11:
                        c = mc - 8
                        actf(qag[:, c, :], pb[:], AF.Copy, [rpb, small["qan"][1]], [r_qag], scale=small["qan"][0][:, c:c + 1])
                        actf(sq5[:, c, :], pb[:], AF.Square, [rpb], [r_sq5])
                    else:
                        c = mc - 11
                        actf(kvag[:, c, :], pb[:], AF.Copy, [rpb, small["kvan"][1]], [r_kvag], scale=small["kvan"][0][:, c:c + 1])
                        actf(sq5[:, 3 + c, :], pb[:], AF.Square, [rpb], [r_sq5])
                cp(pool, halo[:], rw[:, :, 512:515], [r_rw], [r_halo])
                cut(0.34)
                cw, r_cw = small["convw"]
                cb_, r_cb = small["convb"]
                for mc in range(8):
                    ts(dve, cacc[:], rw[:, mc, 0:512], cw[:, mc, 0:1], cb_[:, mc:mc + 1], ALU.mult, ALU.add,
                       [r_rw, r_cw, r_cb], [r_cacc])
                    for kk in range(1, 4):
                        stt(dve, cacc[:], rw[:, mc, kk:kk + 512], cw[:, mc, kk:kk + 1], cacc[:], ALU.mult, ALU.add,
                            [r_rw, r_cw, r_cacc], [r_cacc])
                    actf(xact[:, mc, :], cacc[:], AF.Silu, [r_cacc], [r_xact])
                for i4 in range(4):
                    pb, rpb = next_pb()
                    for k in range(8):
                        mm(pb[:], hT[:, k, i4 * 128:(i4 + 1) * 128], wtm[:, k, 0:512], k == 0, k == 7, [r_hT, r_wtm], [rpb])
                    actf(zs4[i4][0][:], pb[:], AF.Silu, [rpb], [zs4[i4][1]])
                cut(0.36)
                for (c0, ncn, n, slot) in ((0, 3, 384, 0), (3, 2, 256, 1)):
                    pb, rpb = PBs[2], rPB[2]
                    for c in range(ncn):
                        mm(pb[:], onesb[:], sq5[:, c0 + c, :], c == 0, c == ncn - 1, [r_onesb, r_sq5], [rpb])
                    rstd_from(pb[:], rpb, rsb[:, slot, :], r_rsb, n)
                for c in range(3):
                    tt(dve, qaTn[:, c, :], qag[:, c, :], rsb[:, 0, :], ALU.mult, [r_qag, r_rsb], [r_qaTn])
                for c in range(2):
                    tt(dve, kvaTn[:, c, :], kvag[:, c, :], rsb[:, 1, :], ALU.mult, [r_kvag, r_rsb], [r_kvaTn])
                cut(0.38)
                for j in range(4):
                    pb, rpb = next_pb()
                    for c in range(2):
                        mm(pb[:], wkv[:, c, j * 128:(j + 1) * 128], kvaTn[:, c, :], c == 0, c == 1, [r_wkv, r_kvaTn], [rpb])
                    cp(act, kng[:, j, :], pb[:], [rpb], [r_kng])
                st(sp, kn_s[b].rearrange("p (j s) -> p j s", j=4)[:, :, gi * 512:(gi + 1) * 512], kng[:], [r_kng], bag)
                cut(0.40)

                if gi + 1 < NG:
                    step1(gi + 1)
                for i in range(4):
                    ti = gi * 4 + i
                    gt = b * NT + ti
                    cs_ = slice(i * 128, (i + 1) * 128)
                    s1, r_s1 = st1[i]
                    for k in range(8):
                        mm(P2[:, 512:552], hT[:, k, cs_], wtm[:, k, 512:552], k == 0, k == 7, [r_hT, r_wtm], [rP2b])
                    zs, r_zs = zs4[i]
                    tt(dve, dtt[:], P2[:, 512:520], small["dtb"][0][:], ALU.add, [rP2b, small["dtb"][1]], [r_dtt])
                    actf(dtt[:], dtt[:], AF.Exp, [r_dtt], [r_dtt])
                    actf(dtt[:], dtt[:], AF.Ln, [r_dtt], [r_dtt], bias=oneb[:, 0:1])
                    tt(dve, dta[:], dtt[:], aneg[:], ALU.mult, [r_dtt, r_aneg], [r_dta])
                    cut(0.42)
                    cs16 = cosT[:, ti, :]
                    sn16 = sinT[:, ti, :]
                    k1 = P2[:, 520:536]
                    k2 = P2[:, 536:552]
                    t0_, t1_, t2_, t3_ = [rtmp[q][0][:, 0, :] for q in range(4)]
                    rr = [rtmp[q][1] for q in range(4)]
                    tt(dve, t0_, k1, cs16, ALU.mult, [rP2b, r_cos], [rr[0]])
                    tt(dve, t1_, k2, sn16, ALU.mult, [rP2b, r_sin], [rr[1]])
                    tt(dve, t2_, k2, cs16, ALU.mult, [rP2b, r_cos], [rr[2]])
                    tt(dve, t3_, k1, sn16, ALU.mult, [rP2b, r_sin], [rr[3]])
                    tt(dve, krp[:, 0, 0:16], t0_, t1_, ALU.subtract, [rr[0], rr[1]], [r_krp])
                    tt(dve, krp[:, 0, 16:32], t2_, t3_, ALU.add, [rr[2], rr[3]], [r_krp])
                    cp(dve, krp[:, 2, :], krp[:, 0, :], [r_krp], [r_krp])
                    tr(PT[:, 0:128], krp[:].rearrange("p a c -> p (a c)"), identb[:], [r_krp, r_identb], [rPT])
                    cp(act, krT[:], PT[:, 0:128], [rPT], [r_krT])
                    st(sp, kr_s[b][:, ti * 128:(ti + 1) * 128], krT[:], [r_krT], bag)
                    cut(0.44)
                    for c in range(3):
                        mm(P2[:, 0:512], qaTn[:, c, cs_], wq[:, c, 0:512], c == 0, c == 2, [r_qaTn, r_wq], [rP2a])
                    for c in range(3):
                        mm(P2[:, 512:768], qaTn[:, c, cs_], wq[:, c, 512:768], c == 0, c == 2, [r_qaTn, r_wq], [rP2b])
                    cp(act, qn_tm[:], P2[:, 0:512], [rP2a], [r_qn])
                    qr = P2[:, 512:768].rearrange("p (h c) -> p h c", c=32)
                    q1 = qr[:, :, 0:16]
                    q2 = qr[:, :, 16:32]
                    cb8 = cs16.unsqueeze(1).broadcast_to((128, 8, 16))
                    sb8 = sn16.unsqueeze(1).broadcast_to((128, 8, 16))
                    T0, T1, T2, T3 = [rtmp[q][0][:] for q in range(4)]
                    tt(dve, T0, q1, cb8, ALU.mult, [rP2b, r_cos], [rr[0]])
                    tt(dve, T1, q2, sb8, ALU.mult, [rP2b, r_sin], [rr[1]])
                    tt(dve, T2, q2, cb8, ALU.mult, [rP2b, r_cos], [rr[2]])
                    tt(dve, T3, q1, sb8, ALU.mult, [rP2b, r_sin], [rr[3]])
                    qrv = qr_pad[:].rearrange("p j s c -> p s j c")
                    for s_ in range(2):
                        tt(dve, qrv[:, s_, :, 0:16], rtmp[0][0][:, s_ * 4:(s_ + 1) * 4, :], rtmp[1][0][:, s_ * 4:(s_ + 1) * 4, :],
                           ALU.subtract, [rr[0], rr[1]], [r_qrp])
                        tt(dve, qrv[:, s_, :, 16:32], rtmp[2][0][:, s_ * 4:(s_ + 1) * 4, :], rtmp[3][0][:, s_ * 4:(s_ + 1) * 4, :],
                           ALU.add, [rr[2], rr[3]], [r_qrp])
                    for j in range(4):
                        tr(PT[:, j * 128:(j + 1) * 128], qn_tm[:, j * 128:(j + 1) * 128], identb[:], [r_qn, r_identb], [rPT])
                    for j in range(4):
                        tr(PT[:, 512 + j * 128:512 + (j + 1) * 128], qr_pad[:, j, :, :].rearrange("p s c -> p (s c)"),
                           identb[:], [r_qrp, r_identb], [rPT])
                    cp(act, QT[0:64, 0:512], PT[0:64, 0:512], [rPT], [r_QT])
                    cp(act, QT[64:128, 512:1024], PT[64:128, 0:512], [rPT], [r_QT])
                    cp(act, QT[0:64, 1024:1536], PT[0:64, 512:1024], [rPT], [r_QT])
                    cp(act, QT[64:128, 1536:2048], PT[64:128, 512:1024], [rPT], [r_QT])
                    st(sp, q_s[gt], QT[:], [r_QT], bag)
                    cut(0.46)
                    pb, rpb = PBs[2], rPB[2]
                    for c in range(2):
                        mm(pb[:], kvaTn[:, c, cs_], wkv[:, c, 512:1024], c == 0, c == 1, [r_kvaTn, r_wkv], [rpb])
                    cp(act, Vt[:, :, 0:64], pb[:].rearrange("p (h c) -> p h c", c=64), [rpb], [r_Vt])
                    cut(0.475)
                    st(sp, v_s[b][:, ti * 520:(ti + 1) * 520], Vt[:].rearrange("p h c -> p (h c)"), [r_Vt], bag)
                    cut(0.48)

                    for j in range(4):
                        tr(PT[:, j * 128:(j + 1) * 128], xact[:, j, cs_], identb[:], [r_xact, r_identb], [rPT])
                    for j in range(2):
                        tr(PT[:, 512 + j * 128:512 + (j + 1) * 128], xact[:, 4 + j, cs_], identb[:], [r_xact, r_identb], [rPT])
                    cp(act, xs_tm[:].rearrange("p h c -> p (h c)"), PT[:, 0:512], [rPT], [r_xs])
                    cp(act, B_tm[:], PT[:, 512:768], [rPT], [r_Btm])
                    cut(0.495)
                    dt_b = dtt[:].unsqueeze(2).broadcast_to((128, 8, 64))
                    tt(dve, xdt[:], xs_tm[:], dt_b, ALU.mult, [r_xs, r_dtt], [r_xdt])
                    cut(0.50)
                    tt(dve, lseg[:], U1[:].unsqueeze(1).broadcast_to((128, 8, 128)),
                       dta[:].unsqueeze(2).broadcast_to((128, 8, 128)), ALU.mult, [r_U1, r_dta], [r_lseg])
                    for h in range(8):
                        rp = rP2a if h < 4 else rP2b
                        mm(P2[:, h * 128:(h + 1) * 128], lseg[:, h, :], U2[:], True, True, [r_lseg, r_U2], [rp])
                    actf(dec[:].rearrange("p h l -> p (h l)"), P2[:], AF.Exp, [rP2a, rP2b], [r_dec])
                    cut(0.51)
                    pb3, rpb3 = PBs[3], rPB[3]
                    for g in range(2):
                        mm(pb3[:, g * 128:(g + 1) * 128], xact[:, 4 + g, cs_], xact[:, 6 + g, cs_], True, True, [r_xact], [rpb3])
                    cut(0.516)
                    tt(dve, cbm[:], pb3[:, 0:256].rearrange("p (g l) -> p g l", g=2),
                       m01[:].unsqueeze(1).broadcast_to((128, 2, 128)), ALU.mult, [rpb3, r_m01], [r_cbm])
                    cut(0.518)
                    for h in range(8):
                        tt(dve, MT[:, h, :], dec[:, h, :], cbm[:, h // 4, :], ALU.mult, [r_dec, r_cbm], [r_MT])
                    cut(0.52)
                    pb4, rpb4 = PBs[4], rPB[4]
                    mm(pb4[:, 0:8], U2[:], dta[:], True, True, [r_U2, r_dta], [rpb4])
                    mm(pb4[:, 8:16], U1[:], dta[:], True, True, [r_U1, r_dta], [rpb4])
                    mm(pb4[:, 16:24], Mc[:, 0, :], dta[:], True, True, [r_Mc, r_dta], [rpb4])
                    mm(pb4[:, 24:32], Mc[:, 1, :], dta[:], True, True, [r_Mc, r_dta], [rpb4])
                    actf(ecs[:], pb4[:, 0:8], AF.Exp, [rpb4], [r_ecs])
                    actf(dte[:], pb4[:, 8:16], AF.Exp, [rpb4], [r_dte])
                    actf(cdB[:].rearrange("p c h -> p (c h)"), pb4[:, 16:32], AF.Exp, [rpb4], [r_cdB])
                    for c in range(2):
                        ts(dve, ecsm[:, c, :], ecs[:], rm[:, c:c + 1], None, ALU.mult, None, [r_ecs, r_rm], [r_ecsm])
                        ts(dve, dtem[:, c, :], dte[:], rm[:, c:c + 1], None, ALU.mult, None, [r_dte, r_rm], [r_dtem])
                        tt(dve, xdtd[:, c, :, :], xdt[:], dtem[:, c, :].unsqueeze(2).broadcast_to((128, 8, 64)), ALU.mult,
                           [r_xdt, r_dtem], [r_xdtd])
                    cut(0.53)
                    pb0, rpb0 = PBs[0], rPB[0]
                    for h in range(8):
                        mm(pb0[:, h * 64:(h + 1) * 64], MT[:, h, :], xdt[:, h, :], True, True, [r_MT, r_xdt], [rpb0])
                    cp(act, yd[:].rearrange("p h c -> p (h c)"), pb0[:], [rpb0], [r_yd])
                    cut(0.54)
                    pb2, rpb2 = PBs[2], rPB[2]
                    pbY = (PBs[1], PBs[3])
                    rpbY = (rPB[1], rPB[3])
                    for c in range(2):
                        for g in range(2):
                            mm(pbY[c][:, g * 256:(g + 1) * 256], xact[:, 6 + g, cs_],
                               Sbf[:, g * 4:(g + 1) * 4, :].rearrange("p h c -> p (h c)"), True, True, [r_xact, r_Sbf], [rpbY[c]])
                        for g in range(2):
                            mm(pb2[:, g * 256:(g + 1) * 256], B_tm[:, g * 128:(g + 1) * 128],
                               xdtd[:, c, g * 4:(g + 1) * 4, :].rearrange("p h c -> p (h c)"), True, True, [r_Btm, r_xdtd], [rpb2])
                        tt(dve, Sst[:], Sst[:], cdB[:, c, :].unsqueeze(2).broadcast_to((128, 8, 64)), ALU.mult, [r_Sst, r_cdB], [r_Sst])
                        tt(dve, Sst[:], Sst[:], pb2[:].rearrange("p (h c) -> p h c", c=64), ALU.add, [r_Sst, rpb2], [r_Sst])
                        cp(act, Sbf[:], Sst[:], [r_Sst], [r_Sbf])
                    cut(0.55)
                    tt(dve, yt[:], pbY[0][:].rearrange("p (h c) -> p h c", c=64), ecsm[:, 0, :].unsqueeze(2).broadcast_to((128, 8, 64)),
                       ALU.mult, [rpbY[0], r_ecsm], [r_yt])
                    tt(dve, yt2[:], pbY[1][:].rearrange("p (h c) -> p h c", c=64), ecsm[:, 1, :].unsqueeze(2).broadcast_to((128, 8, 64)),
                       ALU.mult, [rpbY[1], r_ecsm], [r_yt2])
                    tt(dve, yt[:], yt[:], yt2[:], ALU.add, [r_yt, r_yt2], [r_yt])
                    tt(dve, yt[:], yt[:], yd[:], ALU.add, [r_yt, r_yd], [r_yt])
                    tt(dve, yt2[:], xs_tm[:], small["dsk"][0][:].unsqueeze(2).broadcast_to((128, 8, 64)), ALU.mult,
                       [r_xs, small["dsk"][1]], [r_yt2])
                    tt(dve, yt[:], yt[:], yt2[:], ALU.add, [r_yt, r_yt2], [r_yt])
                    tt(dve, yt[:].rearrange("p h c -> p (h c)"), yt[:].rearrange("p h c -> p (h c)"), zs[:], ALU.mult,
                       [r_yt, r_zs], [r_yt])
                    for g in range(2):
                        actf(junk[:, g * 256:(g + 1) * 256], yt[:, g * 4:(g + 1) * 4, :].rearrange("p h c -> p (h c)"), AF.Square,
                             [r_yt], [r_junk, r_s1], accum_out=s1[:, 2 + g:3 + g])
                    rstd_from(s1[:, 2:4], r_s1, s1[:, 2:4], r_s1, 256)
                    for g in range(2):
                        stt(dve, mixs[:, g * 256:(g + 1) * 256], yt[:, g * 4:(g + 1) * 4, :].rearrange("p h c -> p (h c)"),
                            s1[:, 2 + g:3 + g], small["ssdn"][0][:, g * 256:(g + 1) * 256], ALU.mult, ALU.mult,
                            [r_yt, r_s1, small["ssdn"][1]], [r_mixs])
                    st(sp, mix_s[gt * 128:(gt + 1) * 128, 0:512], mixs[:], [r_mixs], bag)
                    cut(0.56)

    K.barrier()
    cut(0.6)
    with ExitStack() as pt_:
        KnT, r_KnT = SB("KnT", [128, 4, S], BF16, pt_)
        KrT, r_KrT = SB("KrT", [128, S], BF16, pt_)
        Vst, r_Vst = SB("Vst", [128, NT, 8, 65], BF16, pt_)
        Qb = [SB("Qb%d" % i, [128, 16, 128], BF16, pt_) for i in range(2)]
        PTs = [SB("PTs%d" % i, [128, 4, 128], BF16, pt_) for i in range(6)]
        rec, r_rec = SB("rec", [128, 8], F32, pt_)
        osb, r_osb = SB("osb", [128, 8, 64], F32, pt_)
        junk2, r_junk2 = SB("junk2", [128, 512], BF16, pt_)
        sA, r_sA = SB("sA", [128, 2], F32, pt_)
        mixm = [SB("mixm%d" % i, [128, 512], BF16, pt_) for i in range(2)]
        sc = 1.0 / math.sqrt(96.0)
        for b in range(2):
            K._wait(sp, bagA[b].evs())
            ld(sp, KnT[:].rearrange("p j s -> p (j s)"), kn_s[b], [r_KnT])
            ld(sp, KrT[:], kr_s[b], [r_KrT])
            ld(sp, Vst[:].rearrange("p t h c -> p (t h c)"), v_s[b], [r_Vst])
            for ti in range(NT):
                gt = b * NT + ti
                q_t, q_r = Qb[ti % 2]
                ld(sp, q_t[:].rearrange("p a q -> p (a q)"), q_s[gt], [q_r])
                nkt = ti + 1
                pbO = (P2[:, 0:512], P2[:, 512:1024])
                rpbO = (rP2a, rP2b)
                groups = [(h, k0, min(4, nkt - k0)) for h in range(8) for k0 in range(0, nkt, 4)]
                DEP = 4

                def emit_qk(gi):
                    h, k0, nk = groups[gi]
                    j = h % 4
                    hs = h // 4
                    pbs, rpbs = PBs[gi % 5], rPB[gi % 5]
                    for kk in range(nk):
                        kt = k0 + kk
                        kc = slice(kt * 128, (kt + 1) * 128)
                        mm(pbs[:, kk * 128:(kk + 1) * 128], KnT[:, j, kc], q_t[:, hs * 4 + j, :], True, False, [r_KnT, q_r], [rpbs])
                        mm(pbs[:, kk * 128:(kk + 1) * 128], KrT[:, kc], q_t[:, 8 + hs * 4 + j, :], False, True, [r_KrT, q_r], [rpbs])

                def emit_pv(gi):
                    h, k0, nk = groups[gi]
                    po, rpo = pbO[h // 4], rpbO[h // 4]
                    ocol = (h % 4) * 65
                    pbs, rpbs = PBs[gi % 5], rPB[gi % 5]
                    pts, rpts = PTs[gi % 6]
                    actf(pts[:, 0:nk, :].rearrange("p a q -> p (a q)"), pbs[:, 0:nk * 128], AF.Exp, [rpbs], [rpts], scale=sc)
                    if k0 + nk == nkt:
                        K.I(dve, lambda: nc.vector.memset(pts[64:128, nk - 1, 0:64], 0.0), writes=[rpts])
                    for kk in range(nk):
                        kt = k0 + kk
                        mm(po[:, ocol:ocol + 65], pts[:, kk, :], Vst[:, kt, h, :], kt == 0, kt == nkt - 1, [rpts, r_Vst], [rpo])

                for gi in range(min(DEP, len(groups))):
                    emit_qk(gi)
                for gi in range(len(groups)):
                    if gi + DEP < len(groups):
                        emit_qk(gi + DEP)
                    emit_pv(gi)
                for hh in range(2):
                    ov = pbO[hh][:, 0:260].rearrange("p (h c) -> p h c", c=65)
                    K.I(dve, lambda: nc.vector.reciprocal(rec[:, hh * 4:(hh + 1) * 4], ov[:, :, 64]), reads=[rpbO[hh]], writes=[r_rec])
                    tt(dve, osb[:, hh * 4:(hh + 1) * 4, :], ov[:, :, 0:64],
                       rec[:, hh * 4:(hh + 1) * 4].unsqueeze(2).broadcast_to((128, 4, 64)), ALU.mult, [rpbO[hh], r_rec], [r_osb])
                if gt == 1:
                    dump("osb", osb[:], r_osb, [128, 8, 64])
                    dump("rec", rec[:], r_rec, [128, 8])
                    dump("pts", PTs[0][0][:], PTs[0][1], [128, 4, 128], BF16)
                    dump("qb", q_t[:], q_r, [128, 16, 128], BF16)
                    dump("KrT", KrT[:], r_KrT, [128, S], BF16)
                    dump("KnT", KnT[:], r_KnT, [128, 4, S], BF16)
                    dump("Vst", Vst[:], r_Vst, [128, NT, 8, 65], BF16)
                actf(junk2[:], osb[:].rearrange("p h c -> p (h c)"), AF.Square, [r_osb], [r_junk2, r_sA], accum_out=sA[:, 0:1])
                rstd_from(sA[:, 0:1], r_sA, sA[:, 1:2], r_sA, 512)
                m_t, m_r = mixm[ti % 2]
                stt(dve, m_t[:], osb[:].rearrange("p h c -> p (h c)"), sA[:, 1:2], small["mlan"][0][:],
                    ALU.mult, ALU.mult, [r_osb, r_sA, small["mlan"][1]], [m_r])
                st(sp, mix_s[gt * 128:(gt + 1) * 128, 512:1024], m_t[:], [m_r], bagT[b])

    K.barrier()
    cut(0.8)
    with ExitStack() as pb_:
        wout, r_wout = SB("wout", [128, 8, 1024], BF16, pb_)
        with ExitStack() as pst:
            stg = [SB("stgo%d" % i, [128, 1024], F32, pst) for i in range(2)]
            for k in range(8):
                s_t, s_r = stg[k % 2]
                ld(sp, s_t[:], wout_d[:, k, :], [s_r])
                cast_any(wout[:, k, :], s_t[:], [s_r], [r_wout])
        K.barrier()
        G1, r_G1 = SB("G1", [128, DM], F32, pb_)
        A2, r_A2 = SB("A2", [128, DM], F32, pb_)
        SH2, r_SH2 = SB("SH2", [128, DM], F32, pb_)
        xt = [SB("xtb%d" % i, [128, DM], F32, pb_) for i in range(2)]
        mixin = [SB("mixin%d" % i, [128, DM], BF16, pb_) for i in range(2)]
        mixT, r_mixT = SB("mixT", [128, 8, 128], BF16, pb_)
        junk, r_junk = SB("junkb", [128, DM], BF16, pb_)
        x1, r_x1 = SB("x1", [128, DM], F32, pb_)
        h2s = [SB("h2_%d" % i, [128, DM], F32, pb_) for i in range(2)]
        h2b, r_h2b = SB("h2b", [128, DM], BF16, pb_)
        h2T, r_h2T = SB("h2T", [128, 8, 128], F32, pb_)
        s1s = [SB("s1b%d" % i, [128, 8], F32, pb_) for i in range(2)]
        nv0, r_nv0 = SB("nv0", [128, 1], F32, pb_)
        e4, r_e4 = SB("e4", [128, 4], F32, pb_)
        pb4, rpb4 = PBs[4], rPB[4]
        bag1 = Bag()
        for b in range(2):
            K._wait(sp, bagT[b].evs() + bagA[b].evs())
            tt(dve, vtmp[:], modT[:, 16:24, b], small["gpost1"][0][:], ALU.mult, [r_modT, small["gpost1"][1]], [r_vtmp])
            bcast_rows(vtmp, r_vtmp, G1, r_G1)
            ts(dve, vtmp[:], modT[:, 32:40, b], 1.0, None, ALU.add, None, [r_modT], [r_vtmp])
            tt(dve, vtmp[:], vtmp[:], small["gpre2"][0][:], ALU.mult, [r_vtmp, small["gpre2"][1]], [r_vtmp])
            bcast_rows(vtmp, r_vtmp, A2, r_A2)
            cp(dve, vtmp[:], modT[:, 24:32, b], [r_modT], [r_vtmp])
            bcast_rows(vtmp, r_vtmp, SH2, r_SH2)
            def stageA(ti):
                gt = b * NT + ti
                x_t, x_r = xt[gt % 2]
                mi, r_mi = mixin[gt % 2]
                h2, r_h2 = h2s[gt % 2]
                s1, r_s1 = s1s[gt % 2]
                ld(sp, x_t[:], x_d[gt * 128:(gt + 1) * 128, :], [x_r])
                ld(sp, mi[:], mix_s[gt * 128:(gt + 1) * 128, :], [r_mi])
                for k in range(8):
                    tr(PT[:, k * 128:(k + 1) * 128], mi[:, k * 128:(k + 1) * 128], identb[:], [r_mi, r_identb], [rPT])
                cp(act, mixT[:].rearrange("p k t -> p (k t)"), PT[:], [rPT], [r_mixT])
                for hf in range(2):
                    rp = rP2a if hf == 0 else rP2b
                    for k in range(8):
                        mm(P2[:, hf * 512:(hf + 1) * 512], mixT[:, k, :], wout[:, k, hf * 512:(hf + 1) * 512], k == 0, k == 7,
                           [r_mixT, r_wout], [rp])
                actf(junk[:], P2[:], AF.Square, [rP2a, rP2b], [r_junk, r_s1], accum_out=s1[:, 0:1])
                rstd_from(s1[:, 0:1], r_s1, s1[:, 1:2], r_s1, DM)
                stt(dve, x1[:], P2[:], s1[:, 1:2], G1[:], ALU.mult, ALU.mult, [rP2a, rP2b, r_s1, r_G1], [r_x1])
                tt(dve, x1[:], x1[:], x_t[:], ALU.add, [r_x1, x_r], [r_x1])
                st(sp, x1s_d[gt * 128:(gt + 1) * 128, :], x1[:], [r_x1], bag1)
                actf(junk[:], x1[:], AF.Square, [r_x1], [r_junk, r_s1], accum_out=s1[:, 2:3])
                rstd_from(s1[:, 2:3], r_s1, s1[:, 3:4], r_s1, DM)
                stt(dve, h2[:], x1[:], s1[:, 3:4], A2[:], ALU.mult, ALU.mult, [r_x1, r_s1, r_A2], [r_h2])
                tt(dve, h2[:], h2[:], SH2[:], ALU.add, [r_h2, r_SH2], [r_h2])
                cp(act, h2b[:], h2[:], [r_h2], [r_h2b])
                st(sp, h2s_d[gt * 128:(gt + 1) * 128, :], h2b[:], [r_h2b], bag1)

            def stageB(ti):
                gt = b * NT + ti
                h2, r_h2 = h2s[gt % 2]
                s1, r_s1 = s1s[gt % 2]
                for k in range(8):
                    pbx, rpx = PBs[k // 4], rPB[k // 4]
                    tr(pbx[:, (k % 4) * 128:(k % 4 + 1) * 128], h2[:, k * 128:(k + 1) * 128], identf[:], [r_h2, r_identf], [rpx])
                for hf in range(2):
                    cp(act, h2T[:, hf * 4:(hf + 1) * 4, :].rearrange("p k t -> p (k t)"), PBs[hf][:], [rPB[hf]], [r_h2T])
                wr_t, r_wr = small["wr"]
                for k in range(8):
                    mm(pb4[:, 0:NE], h2T[:, k, :], wr_t[:, k, :], k == 0, k == 7, [r_h2T, r_wr], [rpb4])
                lgt = lg_all[:, gt, :]
                tt(dve, lgt, pb4[:, 0:NE], small["br"][0][:], ALU.add, [rpb4, small["br"][1]], [r_lg])
                K.I(dve, lambda: nc.vector.max(v8_all[:, gt, :], lgt), reads=[r_lg], writes=[r_v8])
                ts(dve, nv0[:], v8_all[:, gt, 0:1], -1.0, None, ALU.mult, None, [r_v8], [r_nv0])
                actf(e4[:], v8_all[:, gt, 0:4], AF.Exp, [r_v8, r_nv0], [r_e4, r_s1], bias=nv0[:, 0:1], accum_out=s1[:, 4:5])
                K.I(dve, lambda: nc.vector.reciprocal(s1[:, 5:6], s1[:, 4:5]), reads=[r_s1], writes=[r_s1])
                ts(dve, gate_all[:, gt, :], e4[:], s1[:, 5:6], None, ALU.mult, None, [r_e4, r_s1], [r_gate])

            stageA(0)
            for ti in range(NT):
                if ti + 1 < NT:
                    stageA(ti + 1)
                stageB(ti)
    K.barrier()
    dump("lg", lg_all[:], r_lg, [128, NTT, NE])
    dump("v8", v8_all[:], r_v8, [128, NTT, 8])
    cut(2)

    base, r_base = SB("base", [128, NE])
    msk, r_msk = SB("msk", [128, NE])
    pos_all, r_pos = SB("pos_all", [128, NTT, NE])
    K.I(dve, lambda: nc.vector.memset(base[:], 0.0), writes=[r_base])
    pb4, rpb4 = PBs[4], rPB[4]
    for gt in range(NTT):
        ts(dve, msk[:], lg_all[:, gt, :], v8_all[:, gt, 3:4], None, ALU.is_ge, None, [r_lg, r_v8], [r_msk])
        mm(pb4[:, 32:64], Lst[:], msk[:], True, True, [r_Lst, r_msk], [rpb4])
        mm(pb4[:, 64:96], onesf[:], msk[:], True, True, [r_onesf, r_msk], [rpb4])
        tt(dve, pos_all[:, gt, :], pb4[:, 32:64], base[:], ALU.add, [rpb4, r_base], [r_pos])
        tt(dve, base[:], pb4[:, 64:96], base[:], ALU.add, [rpb4, r_base], [r_base])
    nblk, r_nblk = SB("nblk", [128, NE])
    incl, r_incl = SB("incl", [128, NE])
    pstart, r_pstart = SB("pstart", [128, NE])
    iotab, r_iotab = SB("iotab", [128, NB])
    bexp_f, r_bexpf = SB("bexp_f", [128, NB])
    bexp_i, r_bexpi = SB("bexp_i", [128, NB], I32)
    dest_i, r_desti = SB("dest_i", [128, NTT, 4], I32)
    widx_i, r_widx = SB("widx_i", [128, NB, 8], I32)
    bidx_i, r_bidx = SB("bidx_i", [128, NB], I32)
    pk, r_pk = SB("pk", [128, 8])
    c1024, r_c1024 = SB("c1024", [128, 8])
    ld(sp, pk[:], pk_d, [r_pk])
    K.I(dve, lambda: nc.vector.memset(c1024[:], 1024.0), writes=[r_c1024])
    ld(sp, iotab[:], iotab_d, [r_iotab])
    off = -0.5 + 1.0 / 1024.0
    ts(dve, nblk[:], base[:], float(BLK - 1), 1.0 / BLK, ALU.add, ALU.mult, [r_base], [r_nblk])
    ts(dve, nblk[:], nblk[:], off, MAGIC, ALU.add, ALU.add, [r_nblk], [r_nblk])
    ts(dve, nblk[:], nblk[:], -MAGIC, None, ALU.add, None, [r_nblk], [r_nblk])
    K.I(dve, lambda: nc.vector.tensor_tensor_scan(incl[:], onesf[:, 0:NE], nblk[:], 0.0, ALU.mult, ALU.add),
        reads=[r_onesf, r_nblk], writes=[r_incl])
    tt(dve, pstart[:], incl[:], nblk[:], ALU.subtract, [r_incl, r_nblk], [r_pstart])
    ts(dve, pstart[:], pstart[:], float(BLK), None, ALU.mult, None, [r_pstart], [r_pstart])
    with ExitStack() as p2a:
        cmp3, r_cmp3 = SB("cmp3", [128, NB, NE], F32, p2a)
        tt(dve, cmp3[:], incl[:].unsqueeze(1).broadcast_to((128, NB, NE)), iotab[:].unsqueeze(2).broadcast_to((128, NB, NE)),
           ALU.is_le, [r_incl, r_iotab], [r_cmp3])
        K.I(dve, lambda: nc.vector.tensor_reduce(bexp_f[:], cmp3[:], AX.X, ALU.add), reads=[r_cmp3], writes=[r_bexpf])
        ts(dve, bexp_f[:], bexp_f[:], float(NE - 1), None, ALU.min, None, [r_bexpf], [r_bexpf])
        cp(dve, bexp_i[:], bexp_f[:], [r_bexpf], [r_bexpi])
        widx_f, r_widxf = SB("widx_f", [128, NB, 8], F32, p2a)
        tt(dve, widx_f[:], bexp_f[:].unsqueeze(2).broadcast_to((128, NB, 8)), c1024[:].unsqueeze(1).broadcast_to((128, NB, 8)),
           ALU.mult, [r_bexpf, r_c1024], [r_widxf])
        tt(dve, widx_f[:], widx_f[:], pk[:].unsqueeze(1).broadcast_to((128, NB, 8)), ALU.add, [r_widxf, r_pk], [r_widxf])
        cp(dve, widx_i[:], widx_f[:], [r_widxf], [r_widx])
        ts(dve, bexp_f[:], bexp_f[:], 128.0, pk[:, 0:1], ALU.mult, ALU.add, [r_bexpf, r_pk], [r_bexpf])
        cp(dve, bidx_i[:], bexp_f[:], [r_bexpf], [r_bidx])
        destf, r_destf = SB("destf", [128, NE], F32, p2a)
        oh, r_oh = SB("oh", [128, 4, NE], F32, p2a)
        dk, r_dk = SB("dk", [128, 4], F32, p2a)
        hb = [SB("hb%d" % i, [128, DM], BF16, p2a) for i in range(2)]
        r_xg = Res()
        for gt in range(NTT):
            tt(dve, destf[:], pos_all[:, gt, :], pstart[:], ALU.add, [r_pos, r_pstart], [r_destf])
            tt(dve, oh[:], lg_all[:, gt, :].unsqueeze(1).broadcast_to((128, 4, NE)),
               v8_all[:, gt, 0:4].unsqueeze(2).broadcast_to((128, 4, NE)), ALU.is_equal, [r_lg, r_v8], [r_oh])
            tt(dve, oh[:], oh[:], destf[:].unsqueeze(1).broadcast_to((128, 4, NE)), ALU.mult, [r_oh, r_destf], [r_oh])
            K.I(dve, lambda: nc.vector.tensor_reduce(dk[:], oh[:], AX.X, ALU.add), reads=[r_oh], writes=[r_dk])
            cp(dve, dest_i[:, gt, :], dk[:], [r_dk], [r_desti])
            h_t, h_r = hb[gt % 2]
            ld(sp, h_t[:], h2s_d[gt * 128:(gt + 1) * 128, :], [h_r])
            if gt == 0:
                K._wait(sp, bag1.evs())
            for k in range(4):
                K.dma(pool, lambda: nc.gpsimd.indirect_dma_start(
                    out=xg_d, out_offset=bass.IndirectOffsetOnAxis(ap=dest_i[:, gt, k:k + 1], axis=0),
                    in_=h_t[:], in_offset=None), reads=[h_r, r_desti], writes=[])
    K.barrier()
    dump("desti", dest_i[:], r_desti, [128, NTT, 4], I32)
    dump("bexp", bexp_i[:], r_bexpi, [128, NB], I32)

    cut(3)
    scat_evs = [(sid, K.semvals[sid]) for sid in pool.ring if K.semvals[sid] > 0]

    with ExitStack() as p2:
        wgu = [SB("wgu%d" % i, [128, 8, 2 * DM], BF16, p2) for i in range(2)]
        wdn = [SB("wdn%d" % i, [128, 8, DM], BF16, p2) for i in range(2)]
        bgu = [SB("bgus%d" % i, [128, 16], F32, p2) for i in range(2)]
        bdn = [SB("bdns%d" % i, [128, DM], F32, p2) for i in range(2)]
        xgt = [SB("xgt%d" % i, [128, DM], BF16, p2) for i in range(2)]
        xgTs = [SB("xgT%d" % i, [128, 8, BLK], BF16, p2) for i in range(2)]
        actT, r_actT = SB("actT", [128, 8, BLK], BF16, p2)
        gg, r_gg = SB("gg", [128, BLK], F32, p2)
        sg, r_sg = SB("sg", [128, BLK], F32, p2)
        ll, r_ll = SB("ll", [128, BLK], F32, p2)
        yo = [SB("yo%d" % i, [128, DM], F32, p2) for i in range(2)]
        K._wait(sp, scat_evs)
        wgr = [[Res() for _ in range(8)] for _ in range(2)]
        wdr = [[Res() for _ in range(8)] for _ in range(2)]

        def load_weights(blk):
            wg_t = wgu[blk % 2][0]
            wd_t = wdn[blk % 2][0]
            bg_t, bg_r = bgu[blk % 2]
            bd_t, bd_r = bdn[blk % 2]
            for k in range(8):
                K.dma(pool, lambda: nc.gpsimd.indirect_dma_start(
                    out=wg_t[:, k, :], out_offset=None, in_=wgu_d,
                    in_offset=bass.IndirectOffsetOnAxis(ap=widx_i[:, blk, k:k + 1], axis=0)), reads=[r_widx], writes=[wgr[blk % 2][k]])
            for k in range(8):
                K.dma(pool, lambda: nc.gpsimd.indirect_dma_start(
                    out=wd_t[:, k, :], out_offset=None, in_=wd_d,
                    in_offset=bass.IndirectOffsetOnAxis(ap=widx_i[:, blk, k:k + 1], axis=0)), reads=[r_widx], writes=[wdr[blk % 2][k]])
            K.dma(pool, lambda: nc.gpsimd.indirect_dma_start(
                out=bg_t[:], out_offset=None, in_=bgu_d,
                in_offset=bass.IndirectOffsetOnAxis(ap=bidx_i[:, blk:blk + 1], axis=0)), reads=[r_bidx], writes=[bg_r])
            K.dma(pool, lambda: nc.gpsimd.indirect_dma_start(
                out=bd_t[:], out_offset=None, in_=bd_d,
                in_offset=bass.IndirectOffsetOnAxis(ap=bidx_i[:, blk:blk + 1], axis=0)), reads=[r_bidx], writes=[bd_r])

        PT2 = PBs[4][:].bitcast(BF16)
        ptn = [0]

        def prep_tokens(blk):
            xgT_, r_xgT_ = xgTs[blk % 2]
            for st in range(4):
                g_t, g_r = xgt[st % 2]
                r0 = blk * BLK + st * 128
                ld(sp, g_t[:], xg_d[r0:r0 + 128, :], [g_r])
                if ptn[0] % 2 == 0:
                    pt_ap, pt_r = PT[:], rPT
                else:
                    pt_ap, pt_r = PT2, rPB[4]
                ptn[0] += 1
                for k in range(8):
                    tr(pt_ap[:, k * 128:(k + 1) * 128], g_t[:, k * 128:(k + 1) * 128], identb[:], [g_r, r_identb], [pt_r])
                cp(act, xgT_[:, :, st * 128:(st + 1) * 128],
                   pt_ap.rearrange("p (k t) -> p k t", t=128), [pt_r], [r_xgT_])

        load_weights(0)
        prep_tokens(0)
        for blk in range(NB):
            if blk + 1 < NB:
                load_weights(blk + 1)
                prep_tokens(blk + 1)
            wg_t = wgu[blk % 2][0]
            wd_t = wdn[blk % 2][0]
            wg_rs = wgr[blk % 2]
            wd_rs = wdr[blk % 2]
            bg_t, bg_r = bgu[blk % 2]
            bd_t, bd_r = bdn[blk % 2]
            xgT, r_xgT = xgTs[blk % 2]
            for fc in range(8):
                pg, rpg = PBs[(fc % 2) * 2], rPB[(fc % 2) * 2]
                pl, rpl = PBs[(fc % 2) * 2 + 1], rPB[(fc % 2) * 2 + 1]
                for k in range(8):
                    mm(pg[:], wg_t[:, k, fc * 128:(fc + 1) * 128], xgT[:, k, :], k == 0, k == 7, [wg_rs[k], r_xgT], [rpg])
                for k in range(8):
                    mm(pl[:], wg_t[:, k, DM + fc * 128:DM + (fc + 1) * 128], xgT[:, k, :], k == 0, k == 7, [wg_rs[k], r_xgT], [rpl])
                ts(dve, gg[:], pg[:], bg_t[:, fc:fc + 1], 7.0, ALU.add, ALU.min, [rpg, bg_r], [r_gg])
                actf(sg[:], gg[:], AF.Sigmoid, [r_gg], [r_sg], scale=1.702)
                ts(dve, ll[:], pl[:], bg_t[:, 8 + fc:9 + fc], 7.0, ALU.add, ALU.min, [rpl, bg_r], [r_ll])
                ts(dve, ll[:], ll[:], -7.0, 1.0, ALU.max, ALU.add, [r_ll], [r_ll])
                tt(dve, gg[:], gg[:], sg[:], ALU.mult, [r_gg, r_sg], [r_gg])
                tt(dve, actT[:, fc, :], gg[:], ll[:], ALU.mult, [r_gg, r_ll], [r_actT])
            dbanks = ((P2[:, 0:512], rP2a), (P2[:, 512:1024], rP2b), (PBs[2][:], rPB[2]), (PBs[3][:], rPB[3]))
            for st in range(4):
                y_t, y_r = yo[st % 2]
                for hf in range(2):
                    pd, rpd = dbanks[(st % 2) * 2 + hf]
                    for k in range(8):
                        mm(pd, actT[:, k, st * 128:(st + 1) * 128], wd_t[:, k, hf * 512:(hf + 1) * 512], k == 0, k == 7,
                           [r_actT, wd_rs[k]], [rpd])
                    tt(dve, y_t[:, hf * 512:(hf + 1) * 512], pd, bd_t[:, hf * 512:(hf + 1) * 512], ALU.add, [rpd, bd_r], [y_r])
                r0 = blk * BLK + st * 128
                K.dma(act, lambda: nc.scalar.dma_start(out=yb_d[r0:r0 + 128, :], in_=y_t[:]), reads=[y_r], writes=[])
    K.barrier()
    yb_evs = [(sid, K.semvals[sid]) for sid in act.ring if K.semvals[sid] > 0]

    with ExitStack() as p3:
        yg = [SB("yg%d" % i, [128, DM], F32, p3) for i in range(8)]
        fa, r_fa = SB("fa", [128, DM], F32, p3)
        x1r = [SB("x1r%d" % i, [128, DM], F32, p3) for i in range(2)]
        ob = [SB("ob%d" % i, [128, DM], F32, p3) for i in range(2)]
        junk3, r_junk3 = SB("junk3", [128, DM], BF16, p3)
        s3, r_s3 = SB("s3", [128, 2], F32, p3)
        G2, r_G2 = SB("G2", [128, 2, DM], F32, p3)
        for b in range(2):
            tt(dve, vtmp[:], modT[:, 40:48, b], small["gpost2"][0][:], ALU.mult, [r_modT, small["gpost2"][1]], [r_vtmp])
            bcast_rows(vtmp, r_vtmp, G2[:, b, :], r_G2)
        K._wait(pool, yb_evs)
        K._wait(sp, bag1.evs())
        def p3_loads(gt):
            x_t, x_r = x1r[gt % 2]
            ld(sp, x_t[:], x1s_d[gt * 128:(gt + 1) * 128, :], [x_r])
            for k in range(4):
                y_t, y_r = yg[(gt % 2) * 4 + k]
                K.dma(pool, lambda: nc.gpsimd.indirect_dma_start(
                    out=y_t[:], out_offset=None, in_=yb_d,
                    in_offset=bass.IndirectOffsetOnAxis(ap=dest_i[:, gt, k:k + 1], axis=0)), reads=[r_desti], writes=[y_r])

        def p3_compute(gt):
            b = gt // NT
            x_t, x_r = x1r[gt % 2]
            o_t, o_r = ob[gt % 2]
            ygs = [yg[(gt % 2) * 4 + k] for k in range(4)]
            actf(fa[:], ygs[0][0][:], AF.Copy, [ygs[0][1], r_gate], [r_fa], scale=gate_all[:, gt, 0:1])
            for k in range(1, 4):
                stt(dve, fa[:], ygs[k][0][:], gate_all[:, gt, k:k + 1], fa[:], ALU.mult, ALU.add, [ygs[k][1], r_gate, r_fa], [r_fa])
            actf(junk3[:], fa[:], AF.Square, [r_fa], [r_junk3, r_s3], accum_out=s3[:, 0:1])
            actf(s3[:, 1:2], s3[:, 0:1], AF.Ln, [r_s3], [r_s3], scale=1.0 / DM, bias=epsb[:, 0:1])
            actf(s3[:, 1:2], s3[:, 1:2], AF.Exp, [r_s3], [r_s3], scale=-0.5)
            stt(dve, o_t[:], fa[:], s3[:, 1:2], G2[:, b, :], ALU.mult, ALU.mult, [r_fa, r_s3, r_G2], [o_r])
            tt(pool, o_t[:], o_t[:], x_t[:], ALU.add, [o_r, x_r], [o_r])
            K.dma(sp, lambda: nc.sync.dma_start(out=out_d[gt * 128:(gt + 1) * 128, :], in_=o_t[:]), reads=[o_r], writes=[], is_out=True)

        p3_loads(0)
        for gt in range(NTT):
            if gt + 1 < NTT:
                p3_loads(gt + 1)
            p3_compute(gt)
    K.barrier()
    K.finish()
    es.close()
    return nc, dbg_d


def _consts(NB):
    idx = np.arange(128)
    ch = idx // 64
    same = ch[:, None] == ch[None, :]
    U1 = (same & (idx[:, None] > idx[None, :])).astype(np.float32)
    U2 = (same & (idx[:, None] <= idx[None, :])).astype(np.float32)
    m01 = U2.copy()
    Lst = (idx[:, None] < idx[None, :]).astype(np.float32)
    invf = (np.float32(10000.0) ** (-(np.arange(16, dtype=np.float32) * np.float32(2.0) / np.float32(32.0)))).astype(np.float32)
    rm = np.stack([(ch == 0), (ch == 1)], axis=1).astype(np.float32)
    return dict(identf=np.eye(128, dtype=np.float32), U1=U1, U2=U2, m01=m01, Lst=Lst, rm=rm,
                invf=np.tile(invf[None, :], (128, 1)).astype(np.float32),
                iotab=np.tile(np.arange(NB, dtype=np.float32)[None, :], (128, 1)),
                pk=(np.arange(8, dtype=np.float32)[None, :] * 128 + np.arange(128, dtype=np.float32)[:, None]).astype(np.float32))


def _rep(v, n=128):
    return np.ascontiguousarray(np.tile(np.asarray(v, np.float32).reshape(1, -1), (n, 1)))


def _pk(v, k):
    return np.ascontiguousarray(np.asarray(v, np.float32).reshape(k, 128).T)


_CACHE = {}


def kernel(x, c, positions, w_ada, b_ada, pre_mix_norm, w_in, conv_w, conv_b, dt_bias, a_log, d_skip, ssd_norm,
           q_a_norm, w_q_up, kv_a_norm, w_kv_up, mla_norm, w_out, post_mix_norm, pre_ffn_norm, w_router, b_router,
           w_gate_up, b_gate_up, w_down, b_down, post_ffn_norm, _dbg=()):
    x = np.asarray(x, np.float32)
    Bsz, S, _ = x.shape
    ncores = Bsz // 2
    NT = S // 128
    NB = (2 * S * 4) // BLK + NE
    key = (S, tuple(_dbg))
    if key not in _CACHE:
        _CACHE[key] = build(S, _dbg)
    nc, dbg_d = _CACHE[key]
    f = lambda a: np.ascontiguousarray(np.asarray(a, np.float32))
    w_in = f(w_in)[0]
    w_tm = np.ascontiguousarray(np.concatenate([w_in[:, 0:512], w_in[:, 1536:1544], w_in[:, 2184:2216]], axis=1))
    w_fm = np.ascontiguousarray(np.concatenate([w_in[:, 512:1536], w_in[:, 1544:1928], w_in[:, 1928:2184]], axis=1))
    wq = f(w_q_up)[0].reshape(384, 8, 96)
    qn = wq[:, :, 0:64]
    qr = wq[:, :, 64:96]
    qn_pairs = np.stack([np.concatenate([qn[:, j], qn[:, j + 4]], axis=1) for j in range(4)], axis=1)
    wq2 = np.ascontiguousarray(np.concatenate([qn_pairs.reshape(384, 512), qr.reshape(384, 256)], axis=1))
    wkv = f(w_kv_up)[0].reshape(256, 8, 128)
    kn = wkv[:, :, 0:64]
    vv = wkv[:, :, 64:128]
    kn_pairs = np.stack([np.concatenate([kn[:, j], kn[:, j + 4]], axis=1) for j in range(4)], axis=1)
    wkv2 = np.ascontiguousarray(np.concatenate([kn_pairs.reshape(256, 512), vv.reshape(256, 512)], axis=1))
    cw = f(conv_w)[0]
    convw = np.ascontiguousarray(cw.reshape(4, 8, 128).transpose(2, 1, 0))
    shared = dict(
        w_ada=f(w_ada)[0], b_adaT=_pk(f(b_ada)[0], 48), g_pre1=_pk(f(pre_mix_norm)[0], 8), g_post1=_pk(f(post_mix_norm)[0], 8),
        g_pre2=_pk(f(pre_ffn_norm)[0], 8), g_post2=_pk(f(post_ffn_norm)[0], 8), w_tm=w_tm, w_fm=w_fm, convw=convw,
        convb=_pk(f(conv_b)[0], 8), dtb=_rep(f(dt_bias)[0]), alog=_rep(f(a_log)[0]), dsk=_rep(f(d_skip)[0]),
        ssdn=_rep(f(ssd_norm)[0]), mlan=_rep(f(mla_norm)[0]), qan=_pk(f(q_a_norm)[0], 3), kvan=_pk(f(kv_a_norm)[0], 2),
        wq=wq2, wkv=wkv2, wout=f(w_out)[0], wr=f(w_router)[0], br=_rep(f(b_router)[0]),
        wgu=f(w_gate_up)[0].reshape(NE * DM, 2 * DM), wd=f(w_down)[0].reshape(NE * DM, DM),
        bgu=np.ascontiguousarray(f(b_gate_up)[0].reshape(NE, 16, 128).transpose(0, 2, 1)).reshape(NE * 128, 16),
        bd=np.ascontiguousarray(np.broadcast_to(f(b_down)[0][:, None, :], (NE, 128, DM))).reshape(NE * 128, DM),
    )
    shared.update(_consts(NB))
    cc = f(c)
    pp = np.asarray(positions, np.int32)
    in_maps = []
    for ci in range(ncores):
        m = dict(shared)
        m["x"] = np.ascontiguousarray(x[2 * ci:2 * ci + 2].reshape(2 * S, DM))
        m["cT"] = np.ascontiguousarray(cc[2 * ci:2 * ci + 2].reshape(2, 8, 128).transpose(2, 1, 0))
        m["pos"] = np.ascontiguousarray(pp[2 * ci:2 * ci + 2].reshape(2, NT, 128).transpose(0, 2, 1))
        in_maps.append(m)
    res = run_bass_kernel_spmd(nc, in_maps, core_ids=list(range(ncores)))
    out = np.stack([np.asarray(r["out"], np.float32).reshape(2, S, DM) for r in res.results], axis=0).reshape(Bsz, S, DM)
    if _dbg:
        return out, [{k: np.asarray(r["dbg_" + k]) for k in dbg_d} for r in res.results]
    return out
```
